# Optimizing a Trainium2 kernel written in Bass

```python
import math
import jax
import jax.numpy as jnp
from jax import lax
import numpy as np

D_MODEL = 1024
BATCH = 4
SEQ = 4096
DEPTH = 2

CTX_LEN = 256
GRID_W = 64
NORM_EPS = 1e-6
N_EVEN = (DEPTH + 1) // 2
N_ODD = DEPTH // 2

HEAD_DIM = 64
A_Q_HEADS = 8
A_KV_HEADS = 2
A_GROUP = A_Q_HEADS // A_KV_HEADS
A_WINDOW = 128
A_BLOCK = 128
ROPE_BASE = 10000.0
A_Q_W = A_Q_HEADS * HEAD_DIM
A_KV_W = A_KV_HEADS * HEAD_DIM

B_HEADS = 8
B_HEAD_DIM = 64
B_WIDTH = B_HEADS * B_HEAD_DIM
B_DECAY_LORA = 64
B_ICLR_LORA = 64
B_GATE_LORA = 128
B_GN_EPS = 64e-5
B_COLS = 3 * B_WIDTH + 2 * B_DECAY_LORA + 2 * B_ICLR_LORA + B_GATE_LORA
EVEN_IN = A_Q_W + 2 * A_KV_W + B_COLS
EVEN_MIX = A_Q_W + B_WIDTH

C_HEADS = 8
C_HEAD_DIM = 128
C_WIDTH = C_HEADS * C_HEAD_DIM
C_CONV = 5
C_CHUNK = 64
ODD_IN = 4 * C_WIDTH + 4 * C_HEADS

N_GROUPS = 4
EXPERTS_PER_GROUP = 8
N_EXPERTS = N_GROUPS * EXPERTS_PER_GROUP
TOP_K = 2
D_EXPERT = 512
MOE_BLOCK = 128

kernel_name = 'hybrid_swa_rwkv7_gdn_hmoe_prefix_dit'


def rms_norm(x, g, eps=NORM_EPS):
    xf = x.astype(jnp.float32)
    y = xf * lax.rsqrt(jnp.mean(xf * xf, axis=-1, keepdims=True) + eps)
    return (y * g.astype(jnp.float32)).astype(x.dtype)


def l2_normalize(x, eps=1e-6):
    xf = x.astype(jnp.float32)
    return xf * lax.rsqrt(jnp.sum(xf * xf, axis=-1, keepdims=True) + eps)


def split_heads(t, n_heads):
    return t.reshape(t.shape[:-1] + (n_heads, t.shape[-1] // n_heads))


def modulate(h, shift, scale):
    return h * (1 + scale[:, None, :]) + shift[:, None, :]


def bi_token_shift(y):
    yp = jnp.pad(y, ((0, 0), (1, 1), (0, 0)))
    return 0.5 * (yp[:, :-2] + yp[:, 2:])


def axial_rope_tables(rows):
    row = jnp.repeat(jnp.arange(rows, dtype=jnp.float32), GRID_W)
    col = jnp.tile(jnp.arange(GRID_W, dtype=jnp.float32), rows)
    axis_dim = HEAD_DIM // 2
    inv_freq = ROPE_BASE ** (-jnp.arange(0, axis_dim, 2, dtype=jnp.float32) / axis_dim)
    ang_row = row[:, None] * inv_freq[None, :]
    ang_col = col[:, None] * inv_freq[None, :]
    return (jnp.cos(ang_row), jnp.sin(ang_row), jnp.cos(ang_col), jnp.sin(ang_col))


def _rotate(xs, cos, sin):
    m = xs.shape[-1] // 2
    x1, x2 = xs[..., :m], xs[..., m:]
    cs = cos[:, None, :].astype(xs.dtype)
    sn = sin[:, None, :].astype(xs.dtype)
    return jnp.concatenate([x1 * cs - x2 * sn, x2 * cs + x1 * sn], axis=-1)


def apply_axial_rope(x, rope):
    cos_r, sin_r, cos_c, sin_c = rope
    h = HEAD_DIM // 2
    return jnp.concatenate([_rotate(x[..., :h], cos_r, sin_r), _rotate(x[..., h:], cos_c, sin_c)], axis=-1)


def window_attention(q, k, v, k_ctx, v_ctx, sink):
    B, L = q.shape[:2]
    nb = L // A_BLOCK
    qb = q.reshape(B, nb, A_BLOCK, A_KV_HEADS, A_GROUP, HEAD_DIM)

    def band(t):
        tp = jnp.pad(t, ((0, 0), (A_BLOCK, A_BLOCK), (0, 0), (0, 0)))
        tp = tp.reshape(B, nb + 2, A_BLOCK, A_KV_HEADS, HEAD_DIM)
        return jnp.concatenate([tp[:, :-2], tp[:, 1:-1], tp[:, 2:]], axis=2)

    kb, vb = band(k), band(v)
    scale = HEAD_DIM ** -0.5
    s_win = jnp.einsum('bnqhgd,bnkhd->bnhgqk', qb, kb).astype(jnp.float32) * scale
    s_ctx = jnp.einsum('bnqhgd,bchd->bnhgqc', qb, k_ctx).astype(jnp.float32) * scale
    q_pos = jnp.arange(nb)[:, None] * A_BLOCK + jnp.arange(A_BLOCK)[None, :]
    k_pos = (jnp.arange(nb)[:, None] - 1) * A_BLOCK + jnp.arange(3 * A_BLOCK)[None, :]
    allowed = (jnp.abs(q_pos[:, :, None] - k_pos[:, None, :]) <= A_WINDOW) & ((k_pos >= 0) & (k_pos < L))[:, None, :]
    s_win = jnp.where(allowed[None, :, None, None], s_win, -jnp.inf)
    sink_col = jnp.broadcast_to(sink.astype(jnp.float32).reshape(1, 1, A_KV_HEADS, A_GROUP, 1, 1), s_win.shape[:-1] + (1,))
    p = jax.nn.softmax(jnp.concatenate([s_win, s_ctx, sink_col], axis=-1), axis=-1)
    nw = 3 * A_BLOCK
    p_win = p[..., :nw].astype(v.dtype)
    p_ctx = p[..., nw:-1].astype(v.dtype)
    o = jnp.einsum('bnhgqk,bnkhd->bnqhgd', p_win, vb) + jnp.einsum('bnhgqc,bchd->bnqhgd', p_ctx, v_ctx)
    return o.reshape(B, L, A_Q_W)


def context_attention(q, k, v, sink):
    B, C = q.shape[:2]
    qg = q.reshape(B, C, A_KV_HEADS, A_GROUP, HEAD_DIM)
    s = jnp.einsum('bqhgd,bkhd->bhgqk', qg, k).astype(jnp.float32) * HEAD_DIM ** -0.5
    sink_col = jnp.broadcast_to(sink.astype(jnp.float32).reshape(1, A_KV_HEADS, A_GROUP, 1, 1), s.shape[:-1] + (1,))
    p = jax.nn.softmax(jnp.concatenate([s, sink_col], axis=-1), axis=-1)[..., :-1]
    o = jnp.einsum('bhgqk,bkhd->bqhgd', p.astype(v.dtype), v)
    return o.reshape(B, C, A_Q_W)


def rwkv7_scan(r, decay, k, v, kk, a, s0, reverse):
    def step(S, inp):
        r_t, w_t, k_t, v_t, kk_t, a_t = inp
        s_kk = jnp.einsum('bhvk,bhk->bhv', S, kk_t)
        S = S * w_t[:, :, None, :] - s_kk[..., None] * (kk_t * a_t)[:, :, None, :] + v_t[..., None] * k_t[:, :, None, :]
        return S, jnp.einsum('bhvk,bhk->bhv', S, r_t)

    xs = tuple(jnp.moveaxis(t, 1, 0) for t in (r, decay, k, v, kk, a))
    s_last, out = lax.scan(step, s0, xs, reverse=reverse)
    return jnp.moveaxis(out, 0, 1), s_last


def rwkv7_prepare(y, p):
    y = (y + p['mu'] * (bi_token_shift(y) - y)).astype(jnp.float32)
    cuts = [B_WIDTH, 2 * B_WIDTH, 3 * B_WIDTH, 3 * B_WIDTH + 2 * B_DECAY_LORA,
            3 * B_WIDTH + 2 * B_DECAY_LORA + 2 * B_ICLR_LORA]
    r, k, v, wd, ad, gd = jnp.split(y, cuts, axis=-1)
    r, k, v = split_heads(r, B_HEADS), split_heads(k, B_HEADS), split_heads(v, B_HEADS)
    kk = l2_normalize(k * p['k_k'])
    wd = wd.reshape(wd.shape[:-1] + (2, B_DECAY_LORA))
    ad = ad.reshape(ad.shape[:-1] + (2, B_ICLR_LORA))
    log_w = -jax.nn.softplus(-(p['dec0'] + jnp.einsum('bldr,drc->bldc', jnp.tanh(wd), p['dec_up']))) - 0.5
    decay = split_heads(jnp.exp(-jnp.exp(log_w)), B_HEADS)
    a = split_heads(jax.nn.sigmoid(p['iclr0'] + jnp.einsum('bldr,drc->bldc', ad, p['iclr_up'])), B_HEADS)
    k_dir = k[:, :, None] * (1 + (a - 1) * p['k_a'])
    gate = jax.nn.sigmoid(gd) @ p['gate_up']
    return r, k, v, kk, decay, a, k_dir, gate


def rwkv7_readout(o, r, k, v, gate, p):
    mu = jnp.mean(o, axis=-1, keepdims=True)
    var = jnp.mean(jnp.square(o - mu), axis=-1, keepdims=True)
    o = (o - mu) * lax.rsqrt(var + B_GN_EPS) * p['gn_w'] + p['gn_b']
    bonus = jnp.sum(r * k * p['r_k'], axis=-1, keepdims=True) * v
    return (o + bonus).reshape(o.shape[:2] + (B_WIDTH,)) * gate


def rwkv7_mix(y_lat, y_ctx, p, need_ctx):
    rl, kl, vl, kkl, decl, al, kdl, gl = rwkv7_prepare(y_lat, p)
    rc, kc, vc, kkc, decc, ac, kdc, gc = rwkv7_prepare(y_ctx, p)
    s0 = jnp.zeros((y_lat.shape[0], B_HEADS, B_HEAD_DIM, B_HEAD_DIM), jnp.float32)
    out_l, out_c = [], []
    for d in range(2):
        rev = d == 1
        oc, s_ctx = rwkv7_scan(rc, decc[:, :, d], kdc[:, :, d], vc, kkc, ac[:, :, d], s0, rev)
        ol, _ = rwkv7_scan(rl, decl[:, :, d], kdl[:, :, d], vl, kkl, al[:, :, d], s_ctx, rev)
        out_c.append(oc)
        out_l.append(ol)
    lat = rwkv7_readout(out_l[0] + out_l[1], rl, kl, vl, gl, p)
    if not need_ctx:
        return lat, None
    return lat, rwkv7_readout(out_c[0] + out_c[1], rc, kc, vc, gc, p)


def even_layer_mixer(h_lat, h_ctx, rope, p, need_ctx):
    y_lat = h_lat @ p['w_in']
    y_ctx = h_ctx @ p['w_in']
    cuts = [A_Q_W, A_Q_W + A_KV_W, A_Q_W + 2 * A_KV_W]
    ql, kl, vl, bl = jnp.split(y_lat, cuts, axis=-1)
    qc, kc, vc, bc = jnp.split(y_ctx, cuts, axis=-1)
    ql = apply_axial_rope(rms_norm(split_heads(ql, A_Q_HEADS), p['q_norm']), rope)
    kl = apply_axial_rope(rms_norm(split_heads(kl, A_KV_HEADS), p['k_norm']), rope)
    vl = split_heads(vl, A_KV_HEADS)
    kc = rms_norm(split_heads(kc, A_KV_HEADS), p['k_norm'])
    vc = split_heads(vc, A_KV_HEADS)
    att_l = window_attention(ql, kl, vl, kc, vc, p['sink'])
    rw_l, rw_c = rwkv7_mix(bl, bc, p, need_ctx)
    out_l = jnp.concatenate([att_l, rw_l.astype(att_l.dtype)], axis=-1) @ p['w_out']
    if not need_ctx:
        return out_l, None
    qc = rms_norm(split_heads(qc, A_Q_HEADS), p['q_norm'])
    att_c = context_attention(qc, kc, vc, p['sink'])
    out_c = jnp.concatenate([att_c, rw_c.astype(att_c.dtype)], axis=-1) @ p['w_out']
    return out_l, out_c


def short_conv(x, w):
    pad = C_CONV // 2
    return lax.conv_general_dilated(x, w[:, None, :].astype(x.dtype), window_strides=(1,),
                                    padding=[(pad, pad)], dimension_numbers=('NWC', 'WIO', 'NWC'),
                                    feature_group_count=x.shape[-1])


def gated_delta_chunked(q, k, v, g, beta, s0, need_out):
    B, L, H, dk = k.shape
    dv = v.shape[-1]
    n = L // C_CHUNK

    def chunks(t):
        return jnp.moveaxis(t.reshape((B, n, C_CHUNK) + t.shape[2:]), 3, 1)

    kc, vc = chunks(k), chunks(v)
    gc = jnp.cumsum(chunks(g), axis=-1)
    bc = chunks(beta)[..., None]
    incl = jnp.tril(jnp.ones((C_CHUNK, C_CHUNK), bool))
    strict = jnp.tril(jnp.ones((C_CHUNK, C_CHUNK), bool), -1)
    diff = gc[..., :, None] - gc[..., None, :]
    decay = jnp.where(incl, jnp.exp(jnp.where(incl, diff, 0.0)), 0.0)
    kb = kc * bc
    lower = jnp.where(strict, jnp.einsum('bhnid,bhnjd->bhnij', kb, kc) * decay, 0.0)
    rhs = jnp.concatenate([vc * bc, kb * jnp.exp(gc)[..., None]], axis=-1)
    sol = lax.linalg.triangular_solve(lower + jnp.eye(C_CHUNK, dtype=jnp.float32), rhs,
                                      left_side=True, lower=True, unit_diagonal=True)
    u, w = sol[..., :dv], sol[..., dv:]
    k_tail = kc * jnp.exp(gc[..., -1:] - gc)[..., None]
    g_tail = jnp.exp(gc[..., -1])[..., None, None]

    def to_scan(t):
        return jnp.moveaxis(t, 2, 0)

    def update(S, u_i, w_i, kt_i, gt_i):
        v_new = u_i - jnp.einsum('bhck,bhkv->bhcv', w_i, S)
        return S * gt_i + jnp.einsum('bhck,bhcv->bhkv', kt_i, v_new), v_new

    if not need_out:
        def step_state(S, inp):
            S_new, _ = update(S, *inp)
            return S_new, None
        s_last, _ = lax.scan(step_state, s0, tuple(map(to_scan, (u, w, k_tail, g_tail))))
        return None, s_last

    qc = chunks(q)
    q_dec = qc * jnp.exp(gc)[..., None]
    intra = jnp.einsum('bhnid,bhnjd->bhnij', qc, kc) * decay

    def step(S, inp):
        u_i, w_i, kt_i, gt_i, qd_i, in_i = inp
        S_new, v_new = update(S, u_i, w_i, kt_i, gt_i)
        o = jnp.einsum('bhck,bhkv->bhcv', qd_i, S) + jnp.einsum('bhij,bhjv->bhiv', in_i, v_new)
        return S_new, o

    s_last, o = lax.scan(step, s0, tuple(map(to_scan, (u, w, k_tail, g_tail, q_dec, intra))))
    o = jnp.moveaxis(jnp.moveaxis(o, 0, 2), 1, 3).reshape(B, L, H, dv)
    return o, s_last


def deltanet_prepare(y, p):
    qkv, z, alpha, beta = jnp.split(y, [3 * C_WIDTH, 4 * C_WIDTH, 4 * C_WIDTH + 2 * C_HEADS], axis=-1)
    qkv = jax.nn.silu(short_conv(qkv, p['conv']))
    q, k, v = jnp.split(qkv, 3, axis=-1)
    q = l2_normalize(split_heads(q, C_HEADS)) * C_HEAD_DIM ** -0.5
    k = l2_normalize(split_heads(k, C_HEADS))
    v = split_heads(v, C_HEADS).astype(jnp.float32)
    alpha = alpha.astype(jnp.float32).reshape(alpha.shape[:-1] + (2, C_HEADS))
    g = -jnp.exp(p['A_log']) * jax.nn.softplus(alpha + p['dt_bias'])
    beta = jax.nn.sigmoid(beta.astype(jnp.float32).reshape(beta.shape[:-1] + (2, C_HEADS)))
    return q, k, v, z, g, beta


def deltanet_readout(o, z, p):
    o = rms_norm(o, p['out_norm']) * jax.nn.silu(split_heads(z, C_HEADS).astype(jnp.float32))
    return o.reshape(o.shape[:2] + (C_WIDTH,)).astype(z.dtype) @ p['w_out']


def odd_layer_mixer(h_lat, h_ctx, p, need_ctx):
    ql, kl, vl, zl, gl, bl = deltanet_prepare(h_lat @ p['w_in'], p)
    qc, kc, vc, zc, gc, bc = deltanet_prepare(h_ctx @ p['w_in'], p)
    s0 = jnp.zeros((h_lat.shape[0], C_HEADS, C_HEAD_DIM, C_HEAD_DIM), jnp.float32)

    def flip(t):
        return t[:, ::-1]

    out_l, out_c = [], []
    for d in range(2):
        orient = (lambda t: t) if d == 0 else flip
        oc, s_ctx = gated_delta_chunked(orient(qc), orient(kc), orient(vc), orient(gc[:, :, d]),
                                        orient(bc[:, :, d]), s0, need_ctx)
        ol, _ = gated_delta_chunked(orient(ql), orient(kl), orient(vl), orient(gl[:, :, d]),
                                    orient(bl[:, :, d]), s_ctx, True)
        out_l.append(orient(ol))
        out_c.append(orient(oc) if need_ctx else None)
    lat = deltanet_readout(out_l[0] + out_l[1], zl, p)
    if not need_ctx:
        return lat, None
    return lat, deltanet_readout(out_c[0] + out_c[1], zc, p)


def routed_expert_ffn(x, expert_ids, gates, w_gate_up, w_down):
    T, D = x.shape
    n_slots = expert_ids.shape[0]
    tok = jnp.arange(n_slots, dtype=jnp.int32) // TOP_K
    order = jnp.argsort(expert_ids)
    e_sorted = expert_ids[order]
    counts = jnp.bincount(expert_ids, length=N_EXPERTS)
    padded = (counts + MOE_BLOCK - 1) // MOE_BLOCK * MOE_BLOCK
    starts = jnp.cumsum(counts) - counts
    p_ends = jnp.cumsum(padded)
    p_starts = p_ends - padded
    dest = p_starts[e_sorted] + jnp.arange(n_slots, dtype=jnp.int32) - starts[e_sorted]
    n_blocks = -(-n_slots // MOE_BLOCK) + N_EXPERTS
    n_rows = n_blocks * MOE_BLOCK
    row_tok = jnp.zeros((n_rows,), jnp.int32).at[dest].set(tok[order])
    row_gate = jnp.zeros((n_rows,), gates.dtype).at[dest].set(gates[order])
    blk_expert = jnp.minimum(jnp.searchsorted(p_ends, jnp.arange(n_blocks, dtype=jnp.int32) * MOE_BLOCK, side='right'),
                             N_EXPERTS - 1)
    x_rows = x[row_tok].reshape(n_blocks, MOE_BLOCK, D)

    def expert_block(args):
        xb, e = args
        gu = xb @ w_gate_up[e]
        return (jax.nn.silu(gu[:, :D_EXPERT]) * gu[:, D_EXPERT:]) @ w_down[e]

    y_rows = lax.map(expert_block, (x_rows, blk_expert)).reshape(n_rows, D)
    return jax.ops.segment_sum(y_rows * row_gate[:, None], row_tok, num_segments=T)


def hier_moe(x, w_grp, b_grp, w_exp, b_exp, w_gate_up, w_down):
    T = x.shape[0]
    xf = x.astype(jnp.float32)
    grp_logits = xf @ w_grp.astype(jnp.float32) + b_grp.astype(jnp.float32)
    grp_prob = jax.nn.softmax(grp_logits, axis=-1)
    _, grp = lax.top_k(grp_logits, 1)
    p_grp = jnp.take_along_axis(grp_prob, grp, axis=-1)
    exp_logits = (xf @ w_exp.astype(jnp.float32) + b_exp.astype(jnp.float32)).reshape(T, N_GROUPS, EXPERTS_PER_GROUP)
    idx = jnp.broadcast_to(grp[:, :, None], (T, 1, EXPERTS_PER_GROUP))
    in_grp = jnp.take_along_axis(exp_logits, idx, axis=1)[:, 0]
    top_vals, top_idx = lax.top_k(in_grp, TOP_K)
    gates = p_grp * jax.nn.softmax(top_vals, axis=-1)
    experts = grp * EXPERTS_PER_GROUP + top_idx
    return routed_expert_ffn(x, experts.reshape(-1), gates.reshape(-1).astype(x.dtype), w_gate_up, w_down)


def setup_inputs(seed: int = 0):
    key = jax.random.key(seed)
    keys = list(jax.random.split(key, 48))

    def nrm(shape, scale):
        return scale * jax.random.normal(keys.pop(), shape, jnp.float32)

    def uni(shape, lo, hi):
        return jax.random.uniform(keys.pop(), shape, jnp.float32, lo, hi)

    D, NE, NO = D_MODEL, N_EVEN, N_ODD
    dt = jnp.exp(uni((NO, 2, C_HEADS), math.log(1e-3), math.log(1e-1)))
    return {
        'x': nrm((BATCH, SEQ, D), 1.0),
        'c': nrm((BATCH, D), 1.0),
        'ctx': nrm((BATCH, CTX_LEN, D), 1.0),
        'c_ctx': nrm((D,), 1.0),
        'ada_w': nrm((DEPTH, D, 6 * D), 0.5 * D ** -0.5),
        'ada_b': nrm((DEPTH, 6 * D), 0.02),
        'norm_mix': 1.0 + nrm((DEPTH, D), 0.02),
        'norm_ffn': 1.0 + nrm((DEPTH, D), 0.02),
        'ev_w_in': nrm((NE, D, EVEN_IN), D ** -0.5),
        'ev_q_norm': 1.0 + nrm((NE, HEAD_DIM), 0.02),
        'ev_k_norm': 1.0 + nrm((NE, HEAD_DIM), 0.02),
        'ev_sink': nrm((NE, A_Q_HEADS), 1.0),
        'ev_mu': uni((NE, B_COLS), 0.0, 1.0),
        'ev_dec0': uni((NE, 2, B_WIDTH), -6.0, 0.0),
        'ev_dec_up': nrm((NE, 2, B_DECAY_LORA, B_WIDTH), 0.5 * B_DECAY_LORA ** -0.5),
        'ev_iclr0': nrm((NE, 2, B_WIDTH), 0.5),
        'ev_iclr_up': nrm((NE, 2, B_ICLR_LORA, B_WIDTH), 0.5 * B_ICLR_LORA ** -0.5),
        'ev_gate_up': nrm((NE, B_GATE_LORA, B_WIDTH), B_GATE_LORA ** -0.5),
        'ev_k_k': 0.85 + nrm((NE, B_HEADS, B_HEAD_DIM), 0.05),
        'ev_k_a': 1.0 + nrm((NE, B_HEADS, B_HEAD_DIM), 0.05),
        'ev_r_k': nrm((NE, B_HEADS, B_HEAD_DIM), 0.1),
        'ev_gn_w': 1.0 + nrm((NE, B_HEADS, B_HEAD_DIM), 0.02),
        'ev_gn_b': nrm((NE, B_HEADS, B_HEAD_DIM), 0.02),
        'ev_w_out': nrm((NE, EVEN_MIX, D), EVEN_MIX ** -0.5),
        'od_w_in': nrm((NO, D, ODD_IN), D ** -0.5),
        'od_conv': nrm((NO, C_CONV, 3 * C_WIDTH), C_CONV ** -0.5),
        'od_A_log': jnp.log(uni((NO, 2, C_HEADS), 1.0, 16.0)),
        'od_dt_bias': dt + jnp.log(-jnp.expm1(-dt)),
        'od_out_norm': 1.0 + nrm((NO, C_HEAD_DIM), 0.02),
        'od_w_out': nrm((NO, C_WIDTH, D), C_WIDTH ** -0.5),
        'moe_w_grp': nrm((DEPTH, D, N_GROUPS), D ** -0.5),
        'moe_b_grp': nrm((DEPTH, N_GROUPS), 0.01),
        'moe_w_exp': nrm((DEPTH, D, N_EXPERTS), D ** -0.5),
        'moe_b_exp': nrm((DEPTH, N_EXPERTS), 0.01),
        'moe_w_gate_up': nrm((DEPTH, N_EXPERTS, D, 2 * D_EXPERT), D ** -0.5),
        'moe_w_down': nrm((DEPTH, N_EXPERTS, D_EXPERT, D), D_EXPERT ** -0.5),
    }


def reference(x, c, ctx, c_ctx, ada_w, ada_b, norm_mix, norm_ffn,
              ev_w_in, ev_q_norm, ev_k_norm, ev_sink, ev_mu, ev_dec0, ev_dec_up, ev_iclr0, ev_iclr_up,
              ev_gate_up, ev_k_k, ev_k_a, ev_r_k, ev_gn_w, ev_gn_b, ev_w_out,
              od_w_in, od_conv, od_A_log, od_dt_bias, od_out_norm, od_w_out,
              moe_w_grp, moe_b_grp, moe_w_exp, moe_b_exp, moe_w_gate_up, moe_w_down):
    n_lat = x.shape[1]
    rows = n_lat // GRID_W
    rope = axial_rope_tables(rows)
    x_lat, x_ctx = x, ctx
    for i in range(DEPTH):
        last = i == DEPTH - 1
        mod_lat = jnp.split(jax.nn.silu(c) @ ada_w[i] + ada_b[i], 6, axis=-1)
        mod_ctx = jnp.split(jax.nn.silu(c_ctx)[None, :] @ ada_w[i] + ada_b[i], 6, axis=-1)
        h_lat = modulate(rms_norm(x_lat, norm_mix[i]), mod_lat[0], mod_lat[1])
        h_ctx = modulate(rms_norm(x_ctx, norm_mix[i]), mod_ctx[0], mod_ctx[1])
        if i % 2 == 0:
            j = i // 2
            p = {'w_in': ev_w_in[j], 'q_norm': ev_q_norm[j], 'k_norm': ev_k_norm[j], 'sink': ev_sink[j],
                 'mu': ev_mu[j], 'dec0': ev_dec0[j], 'dec_up': ev_dec_up[j], 'iclr0': ev_iclr0[j],
                 'iclr_up': ev_iclr_up[j], 'gate_up': ev_gate_up[j], 'k_k': ev_k_k[j], 'k_a': ev_k_a[j],
                 'r_k': ev_r_k[j], 'gn_w': ev_gn_w[j], 'gn_b': ev_gn_b[j], 'w_out': ev_w_out[j]}
            m_lat, m_ctx = even_layer_mixer(h_lat, h_ctx, rope, p, not last)
        else:
            j = i // 2
            p = {'w_in': od_w_in[j], 'conv': od_conv[j], 'A_log': od_A_log[j], 'dt_bias': od_dt_bias[j],
                 'out_norm': od_out_norm[j], 'w_out': od_w_out[j]}
            m_lat, m_ctx = odd_layer_mixer(h_lat, h_ctx, p, not last)
        x_lat = x_lat + mod_lat[2][:, None, :] * m_lat
        f_lat = modulate(rms_norm(x_lat, norm_ffn[i]), mod_lat[3], mod_lat[4])
        moe_p = (moe_w_grp[i], moe_b_grp[i], moe_w_exp[i], moe_b_exp[i], moe_w_gate_up[i], moe_w_down[i])
        if last:
            y_lat = hier_moe(f_lat.reshape(-1, D_MODEL), *moe_p).reshape(x_lat.shape)
        else:
            x_ctx = x_ctx + mod_ctx[2][:, None, :] * m_ctx
            f_ctx = modulate(rms_norm(x_ctx, norm_ffn[i]), mod_ctx[3], mod_ctx[4])
            n_tok_lat = f_lat.shape[0] * f_lat.shape[1]
            y = hier_moe(jnp.concatenate([f_lat.reshape(-1, D_MODEL), f_ctx.reshape(-1, D_MODEL)], axis=0), *moe_p)
            y_lat = y[:n_tok_lat].reshape(x_lat.shape)
            x_ctx = x_ctx + mod_ctx[5][:, None, :] * y[n_tok_lat:].reshape(x_ctx.shape)
        x_lat = x_lat + mod_lat[5][:, None, :] * y_lat
    return x_lat
```

```python
import numpy as np
import concourse.bass as bass
import concourse.mybir as mybir
from concourse.bass_utils import run_bass_kernel_spmd
from contextlib import ExitStack

F32 = mybir.dt.float32
BF16 = mybir.dt.bfloat16
I32 = mybir.dt.int32
U32 = mybir.dt.uint32
AF = mybir.ActivationFunctionType
ALU = mybir.AluOpType
AX = mybir.AxisListType

NDMA = 72
NPOOLSEM = 32
D = 1024
NCORES = 8


class Sched:
    def __init__(self, nc):
        self.nc = nc
        self.eng = {'pe': nc.tensor, 'act': nc.scalar, 'dve': nc.vector, 'pool': nc.gpsimd, 'sp': nc.sync}
        self.sem = {e: nc.alloc_semaphore(f"s_{e}") for e in self.eng}
        self.cnt = {e: 0 for e in self.eng}
        self.known = {e: {} for e in self.eng}
        self.snap = {e: [None] for e in self.eng}
        self.dsem = [nc.alloc_semaphore(f"d_{i}") for i in range(NDMA)]
        self.dcnt = [0] * NDMA
        self.dsnap = [[None] for _ in range(NDMA)]
        self.drr = 0
        self.prr = 0
        self.bufs = {}
        self.nwaits = 0
        self.nins = 0
        self._uid = 0

    def _sem_of(self, tok):
        if tok[0] == 'e':
            return self.sem[tok[1]], tok[2]
        return self.dsem[tok[1]], 16 * tok[2]

    def _snap_of(self, tok):
        if tok[0] == 'e':
            return self.snap[tok[1]][tok[2]]
        return self.dsnap[tok[1]][tok[2]]

    def _wait(self, e, tok):
        key = (tok[0], tok[1])
        kn = self.known[e]
        if kn.get(key, 0) >= tok[2]:
            return
        s, v = self._sem_of(tok)
        self.eng[e].wait_ge(s, v)
        self.nwaits += 1
        kn[key] = tok[2]
        sn = self._snap_of(tok)
        if sn:
            for k2, v2 in sn.items():
                if kn.get(k2, 0) < v2:
                    kn[k2] = v2

    def _deps(self, e, reads, writes):
        toks = []
        for r in reads:
            b = self.bufs.get(r)
            if b and b['w']:
                toks.append(b['w'])
        for w in writes:
            b = self.bufs.get(w)
            if b:
                if b['w']:
                    toks.append(b['w'])
                for t in b['r'].values():
                    toks.append(t)
        if e == 'pe':
            toks = [t for t in toks if not (t[0] == 'e' and t[1] == 'pe')]
        return toks

    def _record(self, tok, reads, writes):
        for r in reads:
            b = self.bufs.setdefault(r, {'w': None, 'r': {}})
            b['r'][(tok[0], tok[1])] = tok
        for w in writes:
            self.bufs[w] = {'w': tok, 'r': {}}

    def op(self, e, fn, reads=(), writes=()):
        for t in self._deps(e, reads, writes):
            self._wait(e, t)
        ins = fn(self.eng[e])
        self.cnt[e] += 1
        self.nins += 1
        ins.then_inc(self.sem[e], 1)
        tok = ('e', e, self.cnt[e])
        self.snap[e].append(dict(self.known[e]))
        self._record(tok, reads, writes)
        return tok

    def dma(self, q, out, in_, reads=(), writes=(), indirect=None, **kw):
        if q == 'pool':
            j = NDMA - NPOOLSEM + self.prr
            self.prr = (self.prr + 1) % NPOOLSEM
        else:
            j = self.drr
            self.drr = (self.drr + 1) % (NDMA - NPOOLSEM)
        if self.dcnt[j] > 0:
            self._wait(q, ('d', j, self.dcnt[j]))
        for t in self._deps(q, reads, writes):
            self._wait(q, t)
        if indirect is None:
            ins = self.eng[q].dma_start(out=out, in_=in_, **kw)
        else:
            ins = self.eng[q].indirect_dma_start(out=out, in_=in_, **indirect)
        self.dcnt[j] += 1
        self.nins += 1
        ins.then_inc(self.dsem[j], 16)
        tok = ('d', j, self.dcnt[j])
        self.dsnap[j].append(dict(self.known[q]))
        self._record(tok, reads, writes)
        return tok

    def join(self):
        toks = [('e', e2, self.cnt[e2]) for e2 in self.eng if self.cnt[e2] > 0]
        toks += [('d', j, self.dcnt[j]) for j in range(NDMA) if self.dcnt[j] > 0]
        for e in self.eng:
            for t in toks:
                self._wait(e, t)

    def finish(self, e='sp'):
        for e2 in self.eng:
            if self.cnt[e2] > 0:
                self._wait(e, ('e', e2, self.cnt[e2]))
        for j in range(NDMA):
            if self.dcnt[j] > 0:
                self._wait(e, ('d', j, self.dcnt[j]))


class Ctx:
    def __init__(self):
        self.nc = bass.Bass("TRN2", target_bir_lowering=False)
        self.S = Sched(self.nc)
        self.n = 0
        self.stack = []
        self.bgq = []

    def sb(self, shape, dt=F32, name=None):
        self.n += 1
        nm = f"{name or 'sb'}_{self.n}"
        if self.stack:
            return self.stack[-1].enter_context(self.nc.sbuf_tensor(nm, list(shape), dt))
        return self.nc.alloc_sbuf_tensor(nm, list(shape), dt)

    def ps(self, shape, dt=F32, name=None):
        self.n += 1
        nm = f"{name or 'ps'}_{self.n}"
        if self.stack:
            return self.stack[-1].enter_context(self.nc.psum_tensor(nm, list(shape), dt))
        return self.nc.alloc_psum_tensor(nm, list(shape), dt)

    def scope(self):
        C = self

        class _Sc:
            def __enter__(s2):
                st = ExitStack(); st.__enter__(); C.stack.append(st); return st

            def __exit__(s2, *a):
                st = C.stack.pop()
                C.S.join()
                return st.__exit__(*a)
        return _Sc()

    def bg_step(self, n=1):
        for _ in range(n):
            if self.bgq:
                self.bgq.pop(0)()

    def bg_flush(self):
        while self.bgq:
            self.bgq.pop(0)()

    def uid(self, base):
        self.n += 1
        return f"{base}#{self.n}"

    def dram(self, name, shape, dt=F32, kind="Internal"):
        return self.nc.dram_tensor(name, list(shape), dt, kind=kind).ap()

    def ident(self, dt=F32):
        S = self.S
        t = self.sb([128, 128], F32, 'ident')
        k = f'ident{self.n}'
        S.op('pool', lambda e: e.memset(t[:], 0.0), writes=[k])
        S.op('pool', lambda e: e.affine_select(out=t[:], in_=t[:], pattern=[[-1, 128]], compare_op=ALU.not_equal,
                                               fill=1.0, base=0, channel_multiplier=1), reads=[k], writes=[k])
        if dt != F32:
            t2 = self.sb([128, 128], dt, 'identb')
            k2 = k + 'b'
            S.op('dve', lambda e: e.tensor_copy(out=t2[:], in_=t[:]), reads=[k], writes=[k2])
            return t2, k2
        return t, k


def emit_mods(C, adaw, adab, cin, nrm):
    S = C.S
    cT = C.sb([128, 16]); cS = C.sb([128, 16]); sg = C.sb([128, 16])
    bia = C.sb([128, 96]); nm = C.sb([128, 16])
    modT = C.sb([128, 96], name='modT')
    S.dma('sp', cT[:], cin, writes=['cT'])
    S.dma('sp', bia[:], adab, writes=['bia'])
    S.dma('sp', nm[:], nrm, writes=['nm'])
    S.op('act', lambda e: e.activation(out=sg[:], in_=cT[:], func=AF.Sigmoid), reads=['cT'], writes=['sg'])
    S.op('dve', lambda e: e.tensor_tensor(out=cS[:], in0=cT[:], in1=sg[:], op=ALU.mult), reads=['cT', 'sg'], writes=['cS'])
    pm = C.ps([128, 96], name='pm')
    wb = [C.sb([128, 8, 512], F32, 'adaw') for _ in range(2)]
    it = 0
    for j in range(6):
        for hf in range(2):
            w = wb[it % 2]; wk = f'adaw{it % 2}'
            for k in range(8):
                S.dma(['sp', 'act'][k % 2], w[:, k, :], adaw[k * 128:(k + 1) * 128, j * 1024 + hf * 512: j * 1024 + hf * 512 + 512],
                      writes=[(wk, k)])
            for mm in range(4):
                m = hf * 4 + mm
                col = (j * 8 + m) * 2
                for k in range(8):
                    S.op('pe', lambda e, k=k, mm=mm, col=col, w=w: e.matmul(pm[:, col:col + 2], lhsT=w[:, k, mm * 128:(mm + 1) * 128],
                                                                          rhs=cS[:, 2 * k:2 * k + 2], start=(k == 0), stop=(k == 7)),
                         reads=[(wk, k), 'cS'], writes=['pm'])
            it += 1
    S.op('dve', lambda e: e.tensor_tensor(out=modT[:], in0=pm[:], in1=bia[:], op=ALU.add), reads=['pm', 'bia'], writes=['modT'])
    A1 = C.sb([128, 8, 2], name='A1'); A2 = C.sb([128, 8, 2], name='A2')
    m3 = modT[:].rearrange("p (j m v) -> p j m v", j=6, m=8)
    S.op('dve', lambda e: e.scalar_tensor_tensor(out=A1[:], in0=m3[:, 1], scalar=1.0, in1=nm[:, 0:8].unsqueeze(2).broadcast_to([128, 8, 2]),
                                                 op0=ALU.add, op1=ALU.mult), reads=['modT', 'nm'], writes=['A1'])
    S.op('dve', lambda e: e.scalar_tensor_tensor(out=A2[:], in0=m3[:, 4], scalar=1.0, in1=nm[:, 8:16].unsqueeze(2).broadcast_to([128, 8, 2]),
                                                 op0=ALU.add, op1=ALU.mult), reads=['modT', 'nm'], writes=['A2'])
    return dict(modT=modT, m3=m3, A1=A1, A2=A2)


def emit_normT(C, xt, xkey, hT, hkey, col0, A, B, v, ident, idk, pT, pTk, tmp, rr):
    S = C.S
    junk, ss, xn = tmp['junk'], tmp['ss'], tmp['xn']
    S.op('act', lambda e: e.activation(out=junk[:], in_=xt, func=AF.Square, accum_out=ss[:, 0:1]), reads=[xkey], writes=['junk', 'ss'])
    S.op('dve', lambda e: e.tensor_scalar(out=ss[:, 1:2], in0=ss[:, 0:1], scalar1=1.0 / D, scalar2=1e-6, op0=ALU.mult, op1=ALU.add),
         reads=['ss'], writes=['ss'])
    S.op('act', lambda e: e.activation(out=ss[:, 2:3], in_=ss[:, 1:2], func=AF.Sqrt), reads=['ss'], writes=['ss'])
    S.op('dve', lambda e: e.reciprocal(out=ss[:, 3:4], in_=ss[:, 2:3]), reads=['ss'], writes=['ss'])
    S.op('dve', lambda e: e.tensor_scalar(out=xn[:], in0=xt, scalar1=ss[:, 3:4], scalar2=None, op0=ALU.mult),
         reads=[xkey, 'ss'], writes=['xn'])
    for k in range(8):
        S.op('pe', lambda e, k=k: e.transpose(out=pT[:, k * 128:(k + 1) * 128], in_=xn[:, k * 128:(k + 1) * 128], identity=ident[:]),
             reads=['xn', idk], writes=[pTk])
    t2 = tmp['t2']
    p3 = pT[:].rearrange("p (k t) -> p k t", k=8)
    S.op('dve', lambda e: e.tensor_tensor(out=t2[:], in0=p3, in1=A[:, :, v:v + 1].broadcast_to([128, 8, 128]), op=ALU.mult),
         reads=[pTk, 'A1', 'A2'], writes=['t2'])
    S.op('pool', lambda e: e.tensor_tensor(out=hT[:, :, col0:col0 + 128], in0=t2[:], in1=B[:, :, v:v + 1].broadcast_to([128, 8, 128]), op=ALU.add),
         reads=['t2', 'modT'], writes=[hkey])


def load_w_bf16(C, dst, dkey, src, rows, cols, queues=('pool',)):
    S = C.S
    nk = rows // 128
    i = 0
    for k in range(nk):
        c0 = 0
        while c0 < cols:
            cw = min(2048, cols - c0)
            S.dma(queues[i % len(queues)], dst[:, k, c0:c0 + cw], src[k * 128:(k + 1) * 128, c0:c0 + cw], writes=[(dkey, k)])
            c0 += cw
            i += 1


def build_pre(NT, NOUT):
    C = Ctx(); S = C.S; nc = C.nc
    xin = C.dram("xin", [NT * 128, D], kind="ExternalInput")
    cin = C.dram("cin", [128, 16], kind="ExternalInput")
    adaw = C.dram("adaw", [D, 6 * D], kind="ExternalInput")
    adab = C.dram("adab", [128, 96], kind="ExternalInput")
    nrm = C.dram("nrm", [128, 16], kind="ExternalInput")
    win = C.dram("win", [D, NOUT], kind="ExternalInput")
    y = C.dram("y", [NT * 128, NOUT], kind="ExternalOutput")
    modo = C.dram("modo", [128, 96], kind="ExternalOutput")
    ident, idk = C.ident()
    M = emit_mods(C, adaw, adab, cin, nrm)
    S.dma('sp', modo, M['modT'][:], reads=['modT'], writes=['modo'])
    wbf = C.sb([128, 8, NOUT], BF16, 'wbf')
    load_w_bf16(C, wbf, 'wbf', win, D, NOUT)
    hT = C.sb([128, 8, NT * 128], BF16, 'hT')
    tmp = dict(junk=C.sb([128, D]), ss=C.sb([128, 4]), xn=C.sb([128, D]), t2=C.sb([128, 8, 128]))
    xb = [C.sb([128, D], F32, 'xb') for _ in range(2)]
    pT = [C.ps([128, 1024], F32, 'pT') for _ in range(2)]
    B1 = M['m3'][:, 0]
    for t in range(NT):
        v = 1 if t == 0 else 0
        S.dma('sp', xb[t % 2][:], xin[t * 128:(t + 1) * 128, :], writes=[f'xb{t % 2}'])
        emit_normT(C, xb[t % 2][:], f'xb{t % 2}', hT, ('hT', t), t * 128, M['A1'], B1, v, ident, idk, pT[t % 2], f'pT{t % 2}', tmp, t)
    pY = [C.ps([128, 512], F32, 'pY') for _ in range(3)]
    yb = [C.sb([128, NOUT], F32, 'yb') for _ in range(2)]
    ncc = (NOUT + 511) // 512
    it = 0
    for t in range(NT):
        for c in range(ncc):
            cw = min(512, NOUT - c * 512)
            p = pY[it % 3]; pk = f'pY{it % 3}'
            for k in range(8):
                S.op('pe', lambda e, k=k, p=p, c=c, cw=cw, t=t: e.matmul(p[:, 0:cw], lhsT=hT[:, k, t * 128:(t + 1) * 128],
                                                                        rhs=wbf[:, k, c * 512:c * 512 + cw], start=(k == 0), stop=(k == 7)),
                     reads=[('hT', t), ('wbf', k)], writes=[pk])
            if it % 2 == 0:
                S.op('act', lambda e, p=p, c=c, cw=cw, t=t: e.copy(out=yb[t % 2][:, c * 512:c * 512 + cw], in_=p[:, 0:cw]),
                     reads=[pk], writes=[f'yb{t % 2}'])
            else:
                S.op('dve', lambda e, p=p, c=c, cw=cw, t=t: e.tensor_copy(out=yb[t % 2][:, c * 512:c * 512 + cw], in_=p[:, 0:cw]),
                     reads=[pk], writes=[f'yb{t % 2}'])
            it += 1
        S.dma(['sp', 'act'][t % 2], y[t * 128:(t + 1) * 128, :], yb[t % 2][:], reads=[f'yb{t % 2}'], writes=[('y', t)])
    S.finish()
    return C


def col_layout(vec, nchunk):
    return np.ascontiguousarray(np.asarray(vec, np.float32).reshape(nchunk, 128).T)


def mods_inputs(c_b, c_ctx, ada_b_l, norm_mix_l, norm_ffn_l):
    cin = np.empty((128, 8, 2), np.float32)
    cin[:, :, 0] = col_layout(c_b, 8)
    cin[:, :, 1] = col_layout(c_ctx, 8)
    adab = np.repeat(col_layout(ada_b_l, 48)[:, :, None], 2, axis=2).reshape(128, 96)
    nrm = np.concatenate([col_layout(norm_mix_l, 8), col_layout(norm_ffn_l, 8)], axis=1)
    return cin.reshape(128, 16), np.ascontiguousarray(adab), np.ascontiguousarray(nrm)


def tok_shard(x_lat_b, x_ctx_b, s):
    return np.ascontiguousarray(np.concatenate([x_ctx_b[128 * s:128 * (s + 1)], x_lat_b[2048 * s:2048 * (s + 1)]], axis=0))


_cache = {}


def run(name, builder, in_maps):
    if name not in _cache:
        _cache[name] = builder()
    C = _cache[name]
    res = run_bass_kernel_spmd(C.nc, in_maps, core_ids=list(range(NCORES)))
    return res.results


def make_masks(C):
    S = C.S
    out = {}
    for nm, pat, cm, cmp in [('lo_s', -1, 1, ALU.is_gt), ('up_s', 1, -1, ALU.is_gt), ('lo_i', -1, 1, ALU.is_ge), ('up_i', 1, -1, ALU.is_ge)]:
        t = C.sb([128, 128], F32, 'mask' + nm)
        S.op('pool', lambda e, t=t: e.memset(t[:], 1.0), writes=['mk' + nm])
        S.op('pool', lambda e, t=t, pat=pat, cm=cm, cmp=cmp: e.affine_select(out=t[:], in_=t[:], pattern=[[pat, 128]], compare_op=cmp,
                                                                             fill=0.0, base=0, channel_multiplier=cm),
             reads=['mk' + nm], writes=['mk' + nm])
        out[nm] = t
    return out


def chunk_order(NCH, d):
    if d == 0:
        return list(range(NCH))
    return [1, 0] + list(range(NCH - 1, 1, -1))


def emit_rwkv(C, ybT, mixo, prm, NCH, pairs, ident, idk, masks, scr):
    S = C.S
    NTOK = NCH * 128
    NT5 = (NTOK + 511) // 512
    with C.scope():
        cols = C.sb([128, 28], F32, 'rwcols')
        S.dma('sp', cols[:], prm['rw_cols'], writes=['rwcols'])
        bsel = C.sb([128, 2], F32, 'bsel'); bones = C.sb([128, 128], F32, 'bones')
        S.op('pool', lambda e: e.memset(bsel[:], 0.0), writes=['bsel'])
        S.op('pool', lambda e: e.memset(bsel[0:64, 0:1], 1.0), reads=['bsel'], writes=['bsel'])
        S.op('pool', lambda e: e.memset(bsel[64:128, 1:2], 1.0), reads=['bsel'], writes=['bsel'])
        S.op('pool', lambda e: e.memset(bones[:], 0.0), writes=['bones'])
        S.op('pool', lambda e: e.memset(bones[0:64, 0:64], 1.0), reads=['bones'], writes=['bones'])
        S.op('pool', lambda e: e.memset(bones[64:128, 64:128], 1.0), reads=['bones'], writes=['bones'])
        epsc = C.sb([128, 1], F32, 'epsc')
        S.op('pool', lambda e: e.memset(epsc[:], 1e-6), writes=['eps'])
        mX = []; mY = []
        for d in range(2):
            Ms, MsT, MiT = (masks['lo_s'], masks['up_s'], masks['up_i']) if d == 0 else (masks['up_s'], masks['lo_s'], masks['lo_i'])
            mx = C.sb([128, 512], BF16, 'mX'); my = C.sb([128, 128], BF16, 'mY')
            mkr = ['mklo_s', 'mkup_s', 'mklo_i', 'mkup_i']
            S.op('dve', lambda e, mx=mx, Ms=Ms: e.tensor_scalar(out=mx[:, 0:128], in0=Ms[:], scalar1=-1.0, scalar2=None, op0=ALU.mult), reads=mkr, writes=[f'mX{d}'])
            S.op('dve', lambda e, mx=mx, MsT=MsT: e.tensor_scalar(out=mx[:, 128:256], in0=MsT[:], scalar1=-1.0, scalar2=None, op0=ALU.mult), reads=mkr + [f'mX{d}'], writes=[f'mX{d}'])
            S.op('dve', lambda e, mx=mx, MsT=MsT: e.tensor_copy(out=mx[:, 256:384], in_=MsT[:]), reads=mkr + [f'mX{d}'], writes=[f'mX{d}'])
            S.op('dve', lambda e, mx=mx, MiT=MiT: e.tensor_copy(out=mx[:, 384:512], in_=MiT[:]), reads=mkr + [f'mX{d}'], writes=[f'mX{d}'])
            S.op('dve', lambda e, my=my, MiT=MiT: e.tensor_copy(out=my[:], in_=MiT[:]), reads=mkr, writes=[f'mY{d}'])
            mX.append(mx); mY.append(my)
        for P in pairs:
            with C.scope():
                kkT = C.sb([128, NTOK], F32, 'kkT'); aT = C.sb([128, NTOK], F32, 'aT')
                Lam = C.sb([128, NTOK], F32, 'Lam'); lam = C.sb([128, NTOK], F32, 'lam'); tmp = C.sb([128, NTOK], F32, 'tmp')
                ob = C.sb([128, NTOK], F32, 'ob')
                rmask = C.sb([128, NTOK], BF16, 'rmask')
                decup = C.sb([128, 512], F32, 'decup'); iclrup = C.sb([128, 512], F32, 'iclrup')
                S.dma('sp', decup[:], prm['dec_up'], writes=['decup'])
                S.dma('sp', iclrup[:], prm['iclr_up'], writes=['iclrup'])
                S.op('pool', lambda e: e.memset(rmask[:], 1.0), writes=['rmask'])
                S.op('pool', lambda e: e.memset(rmask[:].rearrange("p (c t) -> p c t", t=128)[:, :, 0:1], 0.0), reads=['rmask'], writes=['rmask'])
                pW = [C.ps([128, 512], F32, 'pW') for _ in range(2)]
                pTf = C.ps([128, 512], F32, 'pTf')
                pBn = C.ps([128, 512], F32, 'pBn')
                tokb = C.sb([128, NCH, 128], F32, 'tokb')
                bon = C.sb([128, NCH, 2], F32, 'bon')
                PC = C.sb([128, NCH], F32, 'PC')
                kcol = lambda j: cols[:, j:j + 1]
                c3 = lambda t_: t_[:].rearrange("p (c t) -> p c t", t=128)

                def transpose_store(src, skey, dst_dram):
                    for c0 in range(0, NCH, 4):
                        n = min(4, NCH - c0)
                        for c in range(c0, c0 + n):
                            S.op('pe', lambda e, c=c, c0=c0: e.transpose(out=pTf[:, (c - c0) * 128:(c - c0 + 1) * 128], in_=src[:, c * 128:(c + 1) * 128],
                                                                         identity=ident[:]), reads=[skey, idk], writes=['pTf'])
                        S.op('act', lambda e, c0=c0, n=n: e.copy(out=tokb[:, c0:c0 + n, :], in_=pTf[:, 0:n * 128].rearrange("p (c t) -> p c t", t=128)),
                             reads=['pTf'], writes=['tokb'])
                    S.dma('sp', dst_dram.rearrange("(c t) f -> t c f", t=128), tokb[:], reads=['tokb'], writes=[('scr', id(dst_dram))])

                S.dma('sp', tmp[:], ybT[1024 + P * 128:1024 + (P + 1) * 128, :], writes=['tmp'])
                transpose_store(tmp, 'tmp', scr['Vt'][P])
                S.dma('sp', ob[:], ybT[512 + P * 128:512 + (P + 1) * 128, :], writes=['ob'])
                S.op('dve', lambda e: e.tensor_scalar(out=kkT[:], in0=ob[:], scalar1=kcol(P), scalar2=None, op0=ALU.mult), reads=['ob', 'rwcols'], writes=['kkT'])
                S.op('pool', lambda e: e.tensor_tensor(out=tmp[:], in0=kkT[:], in1=kkT[:], op=ALU.mult), reads=['kkT'], writes=['tmp'])
                for i in range(NT5):
                    w = min(512, NTOK - i * 512)
                    S.op('pe', lambda e, i=i, w=w: e.matmul(pW[i % 2][:, 0:w], lhsT=bones[:], rhs=tmp[:, i * 512:i * 512 + w], start=True, stop=True),
                         reads=['tmp', 'bones'], writes=[f'pW{i % 2}'])
                    S.op('act', lambda e, i=i, w=w: e.activation(out=lam[:, i * 512:i * 512 + w], in_=pW[i % 2][:, 0:w], func=AF.Sqrt, bias=epsc[:, 0:1]),
                         reads=[f'pW{i % 2}', 'eps'], writes=['lam'])
                S.op('dve', lambda e: e.reciprocal(out=tmp[:], in_=lam[:]), reads=['lam'], writes=['tmp'])
                S.op('dve', lambda e: e.tensor_tensor(out=kkT[:], in0=kkT[:], in1=tmp[:], op=ALU.mult), reads=['kkT', 'tmp'], writes=['kkT'])
                S.dma('sp', tmp[:], ybT[P * 128:(P + 1) * 128, :], reads=['tmp'], writes=['tmp'])
                S.op('dve', lambda e: e.scalar_tensor_tensor(out=lam[:], in0=tmp[:], scalar=kcol(8 + P), in1=ob[:], op0=ALU.mult, op1=ALU.mult),
                     reads=['tmp', 'ob', 'rwcols'], writes=['lam'])
                for c in range(NCH):
                    S.op('pe', lambda e, c=c: e.matmul(pBn[:, 2 * c:2 * c + 2], lhsT=lam[:, c * 128:(c + 1) * 128], rhs=bsel[:], start=True, stop=True),
                         reads=['lam', 'bsel'], writes=['pBn'])
                S.op('dve', lambda e: e.tensor_copy(out=bon[:].rearrange("p c h -> p (c h)"), in_=pBn[:, 0:2 * NCH]), reads=['pBn'], writes=['bon'])
                S.dma('sp', scr['bon'][P], bon[:], reads=['bon'], writes=[('scr', 'bon', P)])
                for d in range(2):
                    S.dma('sp', ob[:], ybT[1664:1792, :], reads=['ob'], writes=['ob'])
                    S.dma('sp', tmp[:], ybT[1536:1664, :], reads=['tmp'], writes=['tmp'])
                    S.op('act', lambda e: e.activation(out=tmp[:], in_=tmp[:], func=AF.Tanh), reads=['tmp'], writes=['tmp'])
                    for i in range(NT5):
                        w = min(512, NTOK - i * 512)
                        S.op('pe', lambda e, i=i, w=w, d=d: e.matmul(pW[0][:, 0:w], lhsT=iclrup[d * 64:(d + 1) * 64, P * 128:(P + 1) * 128],
                                                                   rhs=ob[d * 64:(d + 1) * 64, i * 512:i * 512 + w], start=True, stop=True),
                             reads=['iclrup', 'ob'], writes=['pW0'])
                        S.op('act', lambda e, i=i, w=w, d=d: e.activation(out=aT[:, i * 512:i * 512 + w], in_=pW[0][:, 0:w], func=AF.Sigmoid,
                                                                        bias=kcol(20 + d * 4 + P)), reads=['pW0', 'rwcols'], writes=['aT'])
                        S.op('pe', lambda e, i=i, w=w, d=d: e.matmul(pW[1][:, 0:w], lhsT=decup[d * 64:(d + 1) * 64, P * 128:(P + 1) * 128],
                                                                   rhs=tmp[d * 64:(d + 1) * 64, i * 512:i * 512 + w], start=True, stop=True),
                             reads=['decup', 'tmp'], writes=['pW1'])
                        S.op('act', lambda e, i=i, w=w, d=d: e.activation(out=lam[:, i * 512:i * 512 + w], in_=pW[1][:, 0:w], func=AF.Sigmoid,
                                                                        bias=kcol(12 + d * 4 + P)), reads=['pW1', 'rwcols'], writes=['lam'])
                    S.op('dve', lambda e: e.tensor_scalar(out=lam[:], in0=lam[:], scalar1=-0.6065306597126334, scalar2=None, op0=ALU.mult), reads=['lam'], writes=['lam'])
                    S.op('dve', lambda e: e.tensor_tensor_scan(out=Lam[:], data0=rmask[:], data1=lam[:], initial=0.0, op0=ALU.mult, op1=ALU.add),
                         reads=['rmask', 'lam'], writes=['Lam'])
                    L3 = c3(Lam)
                    S.op('act', lambda e: e.activation(out=PC[:], in_=L3[:, :, 127], func=AF.Exp), reads=['Lam'], writes=['PC'])
                    S.dma('sp', scr['PC'][P][d], PC[:], reads=['PC'], writes=[('scr', 'PC', P, d)])
                    if d == 1:
                        S.op('dve', lambda e: e.tensor_tensor(out=tmp[:], in0=lam[:], in1=Lam[:], op=ALU.subtract), reads=['lam', 'Lam'], writes=['tmp'])
                        S.op('dve', lambda e: e.tensor_tensor(out=L3, in0=c3(tmp), in1=L3[:, :, 127:128].broadcast_to([128, NCH, 128]), op=ALU.add),
                             reads=['tmp', 'Lam'], writes=['Lam'])
                    PC3 = PC[:].unsqueeze(2).broadcast_to([128, NCH, 128])
                    S.op('dve', lambda e: e.tensor_tensor(out=tmp[:], in0=Lam[:], in1=lam[:], op=ALU.subtract), reads=['Lam', 'lam'], writes=['tmp'])
                    S.op('act', lambda e: e.activation(out=tmp[:], in_=tmp[:], func=AF.Exp), reads=['tmp'], writes=['tmp'])
                    S.op('dve', lambda e: e.tensor_tensor(out=tmp[:], in0=kkT[:], in1=tmp[:], op=ALU.mult), reads=['kkT', 'tmp'], writes=['tmp'])
                    S.dma('sp', scr['KQ'][P][d], tmp[:], reads=['tmp'], writes=[('scr', 'KQ', P, d)])
                    S.dma('sp', ob[:], ybT[P * 128:(P + 1) * 128, :], reads=['ob'], writes=['ob'])
                    S.op('act', lambda e: e.activation(out=lam[:], in_=Lam[:], func=AF.Exp), reads=['Lam', 'lam'], writes=['lam'])
                    S.op('dve', lambda e: e.tensor_tensor(out=ob[:], in0=ob[:], in1=lam[:], op=ALU.mult), reads=['ob', 'lam'], writes=['ob'])
                    S.dma('sp', scr['RQ'][P][d], ob[:], reads=['ob'], writes=[('scr', 'RQ', P, d)])
                    S.op('act', lambda e: e.activation(out=lam[:], in_=Lam[:], func=AF.Exp, scale=-1.0), reads=['Lam', 'lam'], writes=['lam'])
                    S.op('dve', lambda e: e.tensor_tensor(out=tmp[:], in0=kkT[:], in1=aT[:], op=ALU.mult), reads=['kkT', 'aT', 'tmp'], writes=['tmp'])
                    S.op('dve', lambda e: e.tensor_tensor(out=tmp[:], in0=tmp[:], in1=lam[:], op=ALU.mult), reads=['tmp', 'lam'], writes=['tmp'])
                    S.dma('sp', scr['BD'][P][d], tmp[:], reads=['tmp'], writes=[('scr', 'BD', P, d)])
                    S.op('dve', lambda e: e.tensor_tensor(out=c3(ob), in0=c3(tmp), in1=PC3, op=ALU.mult), reads=['tmp', 'PC', 'ob'], writes=['ob'])
                    transpose_store(ob, 'ob', scr['BT'][P][d])
                    S.dma('sp', ob[:], ybT[512 + P * 128:512 + (P + 1) * 128, :], reads=['ob'], writes=['ob'])
                    S.op('dve', lambda e: e.tensor_scalar(out=tmp[:], in0=aT[:], scalar1=-1.0, scalar2=kcol(4 + P), op0=ALU.add, op1=ALU.mult),
                         reads=['aT', 'rwcols', 'tmp'], writes=['tmp'])
                    S.op('dve', lambda e: e.scalar_tensor_tensor(out=tmp[:], in0=tmp[:], scalar=1.0, in1=ob[:], op0=ALU.add, op1=ALU.mult),
                         reads=['tmp', 'ob'], writes=['tmp'])
                    S.op('dve', lambda e: e.tensor_tensor(out=tmp[:], in0=tmp[:], in1=lam[:], op=ALU.mult), reads=['tmp', 'lam'], writes=['tmp'])
                    S.dma('sp', scr['KD'][P][d], tmp[:], reads=['tmp'], writes=[('scr', 'KD', P, d)])
                    S.op('dve', lambda e: e.tensor_tensor(out=c3(ob), in0=c3(tmp), in1=PC3, op=ALU.mult), reads=['tmp', 'PC', 'ob'], writes=['ob'])
                    transpose_store(ob, 'ob', scr['KT'][P][d])
            with C.scope():
                oacc = C.sb([128, NCH, 128], F32, 'oacc')
                Vt = C.sb([128, NCH, 128], F32, 'Vt')
                S.dma('sp', Vt[:], scr['Vt'][P].rearrange("(c t) f -> t c f", t=128), reads=[('scr', id(scr['Vt'][P]))], writes=['Vt'])
                for d in range(2):
                    with C.scope():
                        RQ, KQ, BD, KD = [C.sb([128, NTOK], F32, n) for n in ('RQ', 'KQ', 'BD', 'KD')]
                        for nm_, t_ in (('RQ', RQ), ('KQ', KQ), ('BD', BD), ('KD', KD)):
                            S.dma('act', t_[:], scr[nm_][P][d], reads=[('scr', nm_, P, d)], writes=[nm_])
                        BT = C.sb([128, NCH, 128], F32, 'BT'); KT = C.sb([128, NCH, 128], F32, 'KT')
                        S.dma('sp', BT[:], scr['BT'][P][d].rearrange("(c t) f -> t c f", t=128), reads=[('scr', id(scr['BT'][P][d]))], writes=['BT'])
                        S.dma('sp', KT[:], scr['KT'][P][d].rearrange("(c t) f -> t c f", t=128), reads=[('scr', id(scr['KT'][P][d]))], writes=['KT'])
                        PCs = C.sb([128, NCH], F32, 'PCs')
                        S.dma('sp', PCs[:], scr['PC'][P][d], reads=[('scr', 'PC', P, d)], writes=['PCs'])
                        H = C.sb([128, 64], F32, 'H'); Hb = H
                        S.op('pool', lambda e: e.memset(H[:], 0.0), writes=['H'])
                        pX = [C.ps([128, 512], F32, 'pX') for _ in range(2)]
                        pY = C.ps([128, 512], F32, 'pY'); pN = C.ps([128, 512], F32, 'pN'); pT = C.ps([128, 512], F32, 'pT')
                        pC = C.ps([128, 512], F32, 'pC'); pD = C.ps([128, 512], F32, 'pD')
                        XsA = [[C.sb([128, 512], F32, 'Xs') for _ in range(2)] for _ in range(2)]
                        YsA = [C.sb([128, 256], F32, 'Ys') for _ in range(2)]
                        NpA = [[C.sb([128, 512], F32, 'NpB') for _ in range(2)] for _ in range(2)]
                        TtA = [C.sb([128, 256], F32, 'Tt') for _ in range(2)]
                        Zs = C.sb([128, 128], F32, 'Zs'); Us = C.sb([128, 128], F32, 'Us')
                        corder = chunk_order(NCH, d)

                        def st_intra(c, q):
                            Xs, Ys, Tt = XsA[q], YsA[q], TtA[q]
                            ts = slice(c * 128, (c + 1) * 128)
                            for h in range(2):
                                pb = 64 * h
                                fm = lambda X, pb=pb: X[pb:pb + 64, ts]
                                for blk, (l_, r_) in enumerate([(KQ, BD), (BD, KQ), (KD, KQ), (BD, RQ)]):
                                    S.op('pe', lambda e, h=h, blk=blk, l_=l_, r_=r_, fm=fm: e.matmul(pX[h][:, blk * 128:(blk + 1) * 128], lhsT=fm(l_), rhs=fm(r_),
                                                                                                    start=True, stop=True),
                                         reads=['RQ', 'KQ', 'BD', 'KD'], writes=[f'pX{h}'])
                                S.op('pe', lambda e, h=h, fm=fm: e.matmul(pY[:, h * 128:(h + 1) * 128], lhsT=fm(KD), rhs=fm(RQ), start=True, stop=True),
                                     reads=['RQ', 'KD'], writes=['pY'])
                                S.op('dve', lambda e, h=h: e.tensor_tensor(out=Xs[h][:], in0=pX[h][:], in1=mX[d][:], op=ALU.mult),
                                     reads=[f'pX{h}', f'mX{d}'], writes=[f'Xs{q}{h}'])
                                S.op('pool', lambda e, h=h: e.tensor_tensor(out=Tt[:, h * 128:(h + 1) * 128], in0=Xs[h][:, 128:256], in1=ident[:], op=ALU.add),
                                     reads=[f'Xs{q}{h}', idk], writes=[f'Tt{q}{h}'])
                            S.op('dve', lambda e: e.tensor_tensor(out=Ys[:].rearrange("p (h t) -> p h t", h=2), in0=pY[:, 0:256].rearrange("p (h t) -> p h t", h=2),
                                                                  in1=mY[d][:].unsqueeze(1).broadcast_to([128, 2, 128]), op=ALU.mult),
                                 reads=['pY', f'mY{d}'], writes=[f'Ys{q}'])

                        def st_neumann(lv, q):
                            Xs, Tt = XsA[q], TtA[q]
                            if lv == 1:
                                prev = [(Xs[h][:, 0:128], Xs[h][:, 128:256], f'Xs{q}{h}') for h in range(2)]
                            else:
                                pb_ = NpA[q][(lv - 1) % 2]
                                prev = [(pb_[:, h * 256:h * 256 + 128], pb_[:, h * 256 + 128:h * 256 + 256], f'NpB{q}{(lv - 1) % 2}') for h in range(2)]
                            nb = NpA[q][lv % 2]; nk = f'NpB{q}{lv % 2}'
                            for h in range(2):
                                Nv, NTv, pk = prev[h]
                                S.op('pe', lambda e, h=h, Nv=Nv, NTv=NTv: e.matmul(pN[:, h * 256:h * 256 + 128], lhsT=NTv, rhs=Nv, start=True, stop=True),
                                     reads=[pk], writes=['pN'])
                                if lv < 6:
                                    S.op('pe', lambda e, h=h, Nv=Nv, NTv=NTv: e.matmul(pN[:, h * 256 + 128:h * 256 + 256], lhsT=Nv, rhs=NTv, start=True, stop=True),
                                         reads=[pk], writes=['pN'])
                            S.op('act', lambda e, nb=nb: e.copy(out=nb[:], in_=pN[:]), reads=['pN'], writes=[nk])
                            for h in range(2):
                                S.op('pe', lambda e, h=h, nb=nb: e.matmul(pT[:, h * 128:(h + 1) * 128], lhsT=nb[:, h * 256:h * 256 + 128], rhs=Tt[:, h * 128:(h + 1) * 128],
                                                                         start=True, stop=True), reads=[nk, f'Tt{q}{h}'], writes=['pT'])
                            S.op('dve', lambda e: e.tensor_tensor(out=Tt[:], in0=Tt[:], in1=pT[:, 0:256], op=ALU.add), reads=['pT', f'Tt{q}0', f'Tt{q}1'], writes=[f'Tt{q}0', f'Tt{q}1'])

                        def st_chain(k, c, q):
                            Xs, Ys, Tt = XsA[q], YsA[q], TtA[q]
                            ts = slice(c * 128, (c + 1) * 128)
                            if k == 0:
                                for h in range(2):
                                    pb = 64 * h
                                    S.op('pe', lambda e, h=h, pb=pb: e.matmul(pC[:, h * 64:(h + 1) * 64], lhsT=KQ[pb:pb + 64, ts], rhs=Hb[pb:pb + 64, :], start=True, stop=False),
                                         reads=['KQ', 'H'], writes=['pC'])
                                    S.op('pe', lambda e, h=h: e.matmul(pC[:, h * 64:(h + 1) * 64], lhsT=Xs[h][:, 256:384], rhs=Vt[:, c, h * 64:(h + 1) * 64], start=False, stop=True),
                                         reads=[f'Xs{q}{h}', 'Vt'], writes=['pC'])
                                S.op('act', lambda e: e.mul(out=Zs[:], in_=pC[:, 0:128], mul=-1.0), reads=['pC'], writes=['Zs'])
                            elif k == 1:
                                for h in range(2):
                                    S.op('pe', lambda e, h=h: e.matmul(pC[:, 128 + h * 64:128 + (h + 1) * 64], lhsT=Tt[:, h * 128:(h + 1) * 128], rhs=Zs[:, h * 64:(h + 1) * 64],
                                                                      start=True, stop=True), reads=[f'Tt{q}0', f'Tt{q}1', 'Zs'], writes=['pC'])
                                S.op('act', lambda e: e.copy(out=Us[:], in_=pC[:, 128:256]), reads=['pC'], writes=['Us'])
                            elif k == 2:
                                for h in range(2):
                                    pb = 64 * h
                                    S.op('pe', lambda e, h=h, pb=pb: e.matmul(pD[:, h * 64:(h + 1) * 64], lhsT=RQ[pb:pb + 64, ts], rhs=Hb[pb:pb + 64, :], start=True, stop=False),
                                         reads=['RQ', 'H'], writes=['pD'])
                                    S.op('pe', lambda e, h=h: e.matmul(pD[:, h * 64:(h + 1) * 64], lhsT=Xs[h][:, 384:512], rhs=Us[:, h * 64:(h + 1) * 64], start=False, stop=False),
                                         reads=[f'Xs{q}{h}', 'Us'], writes=['pD'])
                                    S.op('pe', lambda e, h=h: e.matmul(pD[:, h * 64:(h + 1) * 64], lhsT=Ys[:, h * 128:(h + 1) * 128], rhs=Vt[:, c, h * 64:(h + 1) * 64], start=False, stop=True),
                                         reads=[f'Ys{q}', 'Vt'], writes=['pD'])
                                if d == 0:
                                    S.op('dve', lambda e: e.tensor_copy(out=oacc[:, c, :], in_=pD[:, 0:128]), reads=['pD'], writes=['oacc'])
                                else:
                                    S.op('dve', lambda e: e.tensor_tensor(out=oacc[:, c, :], in0=oacc[:, c, :], in1=pD[:, 0:128], op=ALU.add), reads=['pD', 'oacc'], writes=['oacc'])
                            else:
                                for h in range(2):
                                    pb = 64 * h
                                    S.op('pe', lambda e, h=h, pb=pb: e.matmul(pD[pb:pb + 64, 128:192], lhsT=BT[:, c, pb:pb + 64], rhs=Us[:, h * 64:(h + 1) * 64], start=True, stop=False),
                                         reads=['BT', 'Us'], writes=['pD'])
                                    S.op('pe', lambda e, h=h, pb=pb: e.matmul(pD[pb:pb + 64, 128:192], lhsT=KT[:, c, pb:pb + 64], rhs=Vt[:, c, h * 64:(h + 1) * 64], start=False, stop=True),
                                         reads=['KT', 'Vt'], writes=['pD'])
                                S.op('dve', lambda e: e.scalar_tensor_tensor(out=H[:], in0=H[:], scalar=PCs[:, c:c + 1], in1=pD[:, 128:192], op0=ALU.mult, op1=ALU.add),
                                     reads=['H', 'PCs', 'pD'], writes=['H'])

                        st_intra(corder[0], 0)
                        for lv in range(1, 7):
                            st_neumann(lv, 0)
                        for i, c in enumerate(corder):
                            q = i % 2
                            C.bg_step()
                            nxt = corder[i + 1] if i + 1 < NCH else None
                            if nxt is not None:
                                st_intra(nxt, 1 - q)
                            for k in range(4):
                                if nxt is not None:
                                    st_neumann(k + 1, 1 - q)
                                st_chain(k, c, q)
                            if nxt is not None:
                                st_neumann(5, 1 - q)
                                st_neumann(6, 1 - q)
                with C.scope():
                    o4 = oacc[:].rearrange("p c (h v) -> p (c h) v", h=2)
                    red = C.sb([128, NCH * 2], F32, 'red'); cen = C.sb([128, NCH * 2, 64], F32, 'cen'); sq = C.sb([128, NCH * 2, 64], F32, 'sq')
                    bon = C.sb([128, NCH * 2], F32, 'bonl'); epsg = C.sb([128, 1], F32, 'epsg')
                    pG = [C.ps([128, 512], F32, 'pG') for _ in range(2)]
                    gnwb = C.sb([128, 2, 512], F32, 'gnwb')
                    S.dma('sp', gnwb[:], prm['gnwb'].partition_broadcast(128), writes=['gnwb'])
                    gateup = C.sb([128, 512], BF16, 'gateup')
                    S.dma('pool', gateup[:], prm['gate_up'], writes=['gateup'])
                    sgd = C.sb([128, NTOK], BF16, 'sgd'); tl = C.sb([128, NTOK], F32, 'tl')
                    S.dma('sp', tl[:], ybT[1792:1920, :], writes=['tl'])
                    S.op('act', lambda e: e.activation(out=sgd[:], in_=tl[:], func=AF.Sigmoid), reads=['tl'], writes=['sgd'])
                    S.op('pool', lambda e: e.memset(epsg[:], 64e-5), writes=['epsg'])
                    S.dma('sp', bon[:], scr['bon'][P].rearrange("p c h -> p (c h)"), reads=[('scr', 'bon', P)], writes=['bonl'])
                    bc = lambda t_: t_[:].unsqueeze(2).broadcast_to([128, NCH * 2, 64])
                    S.op('dve', lambda e: e.tensor_reduce(out=red[:], in_=o4, axis=AX.X, op=ALU.add), reads=['oacc'], writes=['red'])
                    S.op('dve', lambda e: e.tensor_scalar(out=red[:], in0=red[:], scalar1=1.0 / 64, scalar2=None, op0=ALU.mult), reads=['red'], writes=['red'])
                    S.op('dve', lambda e: e.tensor_tensor(out=cen[:], in0=o4, in1=bc(red), op=ALU.subtract), reads=['oacc', 'red'], writes=['cen'])
                    S.op('pool', lambda e: e.tensor_tensor(out=sq[:], in0=cen[:], in1=cen[:], op=ALU.mult), reads=['cen'], writes=['sq'])
                    S.op('dve', lambda e: e.tensor_reduce(out=red[:], in_=sq[:], axis=AX.X, op=ALU.add), reads=['sq', 'red'], writes=['red'])
                    S.op('act', lambda e: e.activation(out=red[:], in_=red[:], func=AF.Sqrt, scale=1.0 / 64, bias=epsg[:, 0:1]), reads=['red', 'epsg'], writes=['red'])
                    S.op('dve', lambda e: e.reciprocal(out=red[:], in_=red[:]), reads=['red'], writes=['red'])
                    S.op('dve', lambda e: e.tensor_tensor(out=cen[:], in0=cen[:], in1=bc(red), op=ALU.mult), reads=['cen', 'red'], writes=['cen'])
                    c4 = cen[:].rearrange("p (c h) v -> p c (h v)", h=2)
                    gw = gnwb[:, 0, P * 128:(P + 1) * 128].unsqueeze(1).broadcast_to([128, NCH, 128])
                    gb = gnwb[:, 1, P * 128:(P + 1) * 128].unsqueeze(1).broadcast_to([128, NCH, 128])
                    S.op('dve', lambda e: e.tensor_tensor(out=c4, in0=c4, in1=gw, op=ALU.mult), reads=['cen', 'gnwb'], writes=['cen'])
                    S.op('dve', lambda e: e.tensor_tensor(out=c4, in0=c4, in1=gb, op=ALU.add), reads=['cen', 'gnwb'], writes=['cen'])
                    S.op('dve', lambda e: e.tensor_tensor(out=sq[:], in0=Vt[:].rearrange("p c (h v) -> p (c h) v", h=2), in1=bc(bon), op=ALU.mult),
                         reads=['Vt', 'bonl', 'sq'], writes=['sq'])
                    S.op('dve', lambda e: e.tensor_tensor(out=cen[:], in0=cen[:], in1=sq[:], op=ALU.add), reads=['cen', 'sq'], writes=['cen'])
                    for g0 in range(0, NCH, 4):
                        n = min(4, NCH - g0)
                        pg = pG[(g0 // 4) % 2]; pgk = f'pG{(g0 // 4) % 2}'
                        for c in range(g0, g0 + n):
                            S.op('pe', lambda e, c=c, g0=g0, pg=pg: e.matmul(pg[:, (c - g0) * 128:(c - g0 + 1) * 128], lhsT=sgd[:, c * 128:(c + 1) * 128],
                                                                            rhs=gateup[:, P * 128:(P + 1) * 128], start=True, stop=True),
                                 reads=['sgd', 'gateup'], writes=[pgk])
                        S.op('dve', lambda e, g0=g0, n=n, pg=pg: e.tensor_tensor(out=c4[:, g0:g0 + n, :], in0=c4[:, g0:g0 + n, :],
                                                                                in1=pg[:, 0:n * 128].rearrange("p (c f) -> p c f", f=128), op=ALU.mult),
                             reads=[pgk, 'cen'], writes=['cen'])
                    S.dma('sp', mixo.rearrange("(c t) f -> t c f", t=128)[:, :, 512 + P * 128:512 + (P + 1) * 128], c4, reads=['cen'], writes=[('mixo', 'rw', P)])


def rwkv_scratch(C, NCH, pairs):
    NTOK = NCH * 128
    scr = {k: {} for k in ('Vt', 'bon', 'PC', 'RQ', 'KQ', 'BD', 'KD', 'BT', 'KT')}
    for P in pairs:
        scr['Vt'][P] = C.dram(f"rw_Vt{P}", [NTOK, 128], F32)
        scr['bon'][P] = C.dram(f"rw_bon{P}", [128, NCH, 2], F32)
        for k in ('PC', 'RQ', 'KQ', 'BD', 'KD', 'BT', 'KT'):
            scr[k][P] = {}
        for d in range(2):
            scr['PC'][P][d] = C.dram(f"rw_PC{P}{d}", [128, NCH], F32)
            for k in ('RQ', 'KQ', 'BD', 'KD'):
                scr[k][P][d] = C.dram(f"rw_{k}{P}{d}", [128, NTOK], F32)
            for k in ('BT', 'KT'):
                scr[k][P][d] = C.dram(f"rw_{k}{P}{d}", [NTOK, 128], F32)
    return scr


def rwkv_params_host(inp, j=0):
    cols = np.zeros((128, 28), np.float32)
    cols[:, 0:4] = col_layout(inp['ev_k_k'][j].reshape(512), 4)
    cols[:, 4:8] = col_layout(inp['ev_k_a'][j].reshape(512), 4)
    cols[:, 8:12] = col_layout(inp['ev_r_k'][j].reshape(512), 4)
    for d in range(2):
        cols[:, 12 + d * 4:16 + d * 4] = col_layout(inp['ev_dec0'][j][d], 4)
        cols[:, 20 + d * 4:24 + d * 4] = col_layout(inp['ev_iclr0'][j][d], 4)
    gnwb = np.stack([inp['ev_gn_w'][j].reshape(512), inp['ev_gn_b'][j].reshape(512)]).astype(np.float32)
    return dict(rw_cols=cols, gnwb=gnwb, dec_up=np.ascontiguousarray(inp['ev_dec_up'][j].reshape(128, 512)),
                iclr_up=np.ascontiguousarray(inp['ev_iclr_up'][j].reshape(128, 512)), gate_up=np.ascontiguousarray(inp['ev_gate_up'][j]))


def build_rwkv_test(NCH, pairs):
    C = Ctx(); S = C.S
    NTOK = NCH * 128
    ybT = C.dram("ybT", [1920, NTOK], kind="ExternalInput")
    prm = dict(rw_cols=C.dram("rw_cols", [128, 28], kind="ExternalInput"), gnwb=C.dram("gnwb", [2, 512], kind="ExternalInput"),
               dec_up=C.dram("dec_up", [128, 512], kind="ExternalInput"), iclr_up=C.dram("iclr_up", [128, 512], kind="ExternalInput"),
               gate_up=C.dram("gate_up", [128, 512], kind="ExternalInput"))
    mixo = C.dram("mixo", [NTOK, 1024], kind="ExternalOutput")
    ident, idk = C.ident()
    masks = make_masks(C)
    scr = rwkv_scratch(C, NCH, pairs)
    emit_rwkv(C, ybT, mixo, prm, NCH, pairs, ident, idk, masks, scr)
    S.finish()
    return C


def emit_attn(C, yatt, mixo, prm, NCH, ident, idk, masks):
    S = C.S
    NTOK = NCH * 128
    with C.scope():
        gq = C.sb([128, 64], F32, 'gq'); gk = C.sb([128, 64], F32, 'gk'); esk = C.sb([128, 8], F32, 'esk')
        S.dma('sp', gq[:], prm['q_norm'].partition_broadcast(128), writes=['gq'])
        S.dma('sp', gk[:], prm['k_norm'].partition_broadcast(128), writes=['gk'])
        S.dma('sp', esk[:], prm['sink'].partition_broadcast(128), writes=['esk'])
        S.op('act', lambda e: e.activation(out=esk[:], in_=esk[:], func=AF.Exp), reads=['esk'], writes=['esk'])
        epsa = C.sb([128, 1], F32, 'epsa')
        S.op('pool', lambda e: e.memset(epsa[:], 1e-6), writes=['epsa'])
        qT = C.sb([64, 8, NTOK], BF16, 'qT'); kT = C.sb([64, 2, NTOK], BF16, 'kTa')
        Va = C.sb([128, NCH, 2, 65], BF16, 'Va')
        S.op('pool', lambda e: e.memset(Va[:], 1.0), writes=['Va'])
        with C.scope():
            yb = [C.sb([128, 768], F32, 'ya') for _ in range(2)]
            rc = [C.sb([128, 128], F32, 'rc') for _ in range(2)]
            sq = C.sb([128, 640], F32, 'sqa'); ss = C.sb([128, 10], F32, 'ssa'); xr = C.sb([128, 640], F32, 'xr'); t2 = C.sb([128, 640], F32, 't2a')
            pTq = [C.ps([64, 1024], F32, 'pTq') for _ in range(2)]; pTk = [C.ps([64, 256], F32, 'pTk') for _ in range(2)]
            for t in range(NCH):
                y = yb[t % 2]; yk = f'ya{t % 2}'
                S.dma('sp', y[:], yatt[t * 128:(t + 1) * 128, :], writes=[yk])
                x3 = y[:, 0:640].rearrange("p (h f) -> p h f", f=64)
                S.op('pool', lambda e, y=y: e.tensor_tensor(out=sq[:], in0=y[:, 0:640], in1=y[:, 0:640], op=ALU.mult), reads=[yk], writes=['sqa'])
                S.op('dve', lambda e: e.tensor_reduce(out=ss[:], in_=sq[:].rearrange("p (h f) -> p h f", f=64), axis=AX.X, op=ALU.add), reads=['sqa'], writes=['ssa'])
                S.op('act', lambda e: e.activation(out=ss[:], in_=ss[:], func=AF.Sqrt, scale=1.0 / 64, bias=epsa[:, 0:1]), reads=['ssa', 'epsa'], writes=['ssa'])
                S.op('dve', lambda e: e.reciprocal(out=ss[:], in_=ss[:]), reads=['ssa'], writes=['ssa'])
                x3r = xr[:].rearrange("p (h f) -> p h f", f=64)
                S.op('dve', lambda e, x3=x3: e.tensor_tensor(out=x3r, in0=x3, in1=ss[:].unsqueeze(2).broadcast_to([128, 10, 64]), op=ALU.mult), reads=[yk, 'ssa'], writes=['xr'])
                S.op('dve', lambda e: e.tensor_tensor(out=x3r[:, 0:8], in0=x3r[:, 0:8], in1=gq[:].unsqueeze(1).broadcast_to([128, 8, 64]), op=ALU.mult), reads=['xr', 'gq'], writes=['xr'])
                S.op('dve', lambda e: e.tensor_tensor(out=x3r[:, 8:10], in0=x3r[:, 8:10], in1=gk[:].unsqueeze(1).broadcast_to([128, 2, 64]), op=ALU.mult), reads=['xr', 'gk'], writes=['xr'])
                src = xr
                if t >= 2:
                    r = rc[t % 2]; rk = f'rc{t % 2}'
                    S.dma('act', r[:], prm['rope'][(t - 2) * 128:(t - 1) * 128, :], writes=[rk])
                    S.op('dve', lambda e, r=r: e.tensor_tensor(out=t2[:].rearrange("p (h f) -> p h f", f=64), in0=x3r,
                                                               in1=r[:, 0:64].unsqueeze(1).broadcast_to([128, 10, 64]), op=ALU.mult), reads=['xr', rk], writes=['t2a'])
                    x5 = xr[:].rearrange("p (h a j m) -> p (h a) j m", a=2, j=2, m=16)
                    s5 = r[:, 64:128].rearrange("p (a j m) -> p a j m", a=2, j=2)
                    sqv = sq[:].rearrange("p (h a j m) -> p (h a) j m", a=2, j=2, m=16)
                    for j in range(2):
                        sj = s5[:, :, j, :].unsqueeze(1).broadcast_to([128, 10, 2, 16]).rearrange("p h a m -> p (h a) m") if False else None
                        for a in range(2):
                            S.op('pool', lambda e, j=j, a=a, r=r: e.tensor_tensor(
                                out=sq[:].rearrange("p (h a j m) -> p h a j m", a=2, j=2, m=16)[:, :, a, j, :],
                                in0=xr[:].rearrange("p (h a j m) -> p h a j m", a=2, j=2, m=16)[:, :, a, 1 - j, :],
                                in1=r[:, 64:128].rearrange("p (a j m) -> p a j m", a=2, j=2)[:, a, j, :].unsqueeze(1).broadcast_to([128, 10, 16]), op=ALU.mult),
                                 reads=['xr', rk], writes=['sqa'])
                    S.op('dve', lambda e: e.tensor_tensor(out=t2[:], in0=t2[:], in1=sq[:], op=ALU.add), reads=['t2a', 'sqa'], writes=['t2a'])
                    src = t2
                sk = 't2a' if t >= 2 else 'xr'
                pq = pTq[t % 2]; pk = pTk[t % 2]
                for h in range(8):
                    S.op('pe', lambda e, h=h, src=src, pq=pq: e.transpose(out=pq[:, h * 128:(h + 1) * 128], in_=src[:, h * 64:(h + 1) * 64], identity=ident[:]),
                         reads=[sk, idk], writes=[f'pTq{t % 2}'])
                for h in range(2):
                    S.op('pe', lambda e, h=h, src=src, pk=pk: e.transpose(out=pk[:, h * 128:(h + 1) * 128], in_=src[:, 512 + h * 64:512 + (h + 1) * 64], identity=ident[:]),
                         reads=[sk, idk], writes=[f'pTk{t % 2}'])
                S.op('act', lambda e, pq=pq, t=t: e.mul(out=qT[:, :, t * 128:(t + 1) * 128], in_=pq[:].rearrange("p (h t) -> p h t", t=128), mul=0.125),
                     reads=[f'pTq{t % 2}'], writes=[('qT', t)])
                S.op('act', lambda e, pk=pk, t=t: e.copy(out=kT[:, :, t * 128:(t + 1) * 128], in_=pk[:].rearrange("p (h t) -> p h t", t=128)),
                     reads=[f'pTk{t % 2}'], writes=[('kTa', t)])
                S.op('dve', lambda e, y=y, t=t: e.tensor_copy(out=Va[:, t, :, 0:64], in_=y[:, 640:768].rearrange("p (h f) -> p h f", f=64)), reads=[yk, 'Va'], writes=[('Va', t)])
        with C.scope():
            pS = [C.ps([128, 512], F32, 'pS') for _ in range(3)]
            pO = [C.ps([128, 260], F32, 'pO') for _ in range(2)]
            Pt = [C.sb([128, 5, 512], BF16, 'Pt') for _ in range(2)]
            den = C.sb([128, 4], F32, 'den'); ob = [C.sb([128, 512], F32, 'oba') for _ in range(2)]
            it = 0
            for tq in range(NCH):
                if tq < 2:
                    keys = [(0, None), (1, None)]
                else:
                    keys = [(0, None), (1, None)]
                    if tq - 1 >= 2:
                        keys.append((tq - 1, 'lo_i'))
                    keys.append((tq, None))
                    if tq + 1 < NCH:
                        keys.append((tq + 1, 'up_i'))
                o_t = ob[tq % 2]; ok_ = f'oba{tq % 2}'
                for g in range(2):
                    P_ = Pt[it % 2]; Pk = f'Pt{it % 2}'
                    for i, (tk, mk) in enumerate(keys):
                        ps = pS[i % 3]; psk = f'pS{i % 3}'
                        S.op('pe', lambda e, ps=ps, tk=tk, g=g, tq=tq: e.matmul(ps[:], lhsT=kT[:, g, tk * 128:(tk + 1) * 128], rhs=qT[:, 4 * g:4 * g + 4, tq * 128:(tq + 1) * 128],
                                                                             start=True, stop=True), reads=[('kTa', tk), ('qT', tq)], writes=[psk])
                        S.op('act', lambda e, ps=ps, i=i, P_=P_: e.activation(out=P_[:, i, :], in_=ps[:], func=AF.Exp), reads=[psk], writes=[(Pk, i)])
                        if mk:
                            S.op('pool', lambda e, i=i, P_=P_, mk=mk: e.tensor_tensor(out=P_[:, i, :].rearrange("p (h q) -> p h q", h=4), in0=P_[:, i, :].rearrange("p (h q) -> p h q", h=4),
                                                                                      in1=masks[mk][:].unsqueeze(1).broadcast_to([128, 4, 128]), op=ALU.mult),
                                 reads=[(Pk, i), 'mk' + mk], writes=[(Pk, i)])
                    po = pO[it % 2]; pok = f'pO{it % 2}'
                    for h in range(4):
                        for i, (tk, mk) in enumerate(keys):
                            S.op('pe', lambda e, h=h, i=i, tk=tk, po=po, P_=P_, g=g: e.matmul(po[:, h * 65:(h + 1) * 65], lhsT=P_[:, i, h * 128:(h + 1) * 128], rhs=Va[:, tk, g, :],
                                                                                            start=(i == 0), stop=(i == len(keys) - 1)),
                                 reads=[(Pk, i), ('Va', tk)], writes=[pok])
                    po3 = po[:].rearrange("p (h f) -> p h f", f=65)
                    S.op('dve', lambda e, po3=po3, g=g: e.tensor_tensor(out=den[:], in0=po3[:, :, 64], in1=esk[:, 4 * g:4 * g + 4], op=ALU.add), reads=[pok, 'esk'], writes=['den'])
                    S.op('dve', lambda e: e.reciprocal(out=den[:], in_=den[:]), reads=['den'], writes=['den'])
                    S.op('dve', lambda e, po3=po3, g=g, o_t=o_t: e.tensor_tensor(out=o_t[:, g * 256:(g + 1) * 256].rearrange("p (h f) -> p h f", f=64), in0=po3[:, :, 0:64],
                                                                               in1=den[:].unsqueeze(2).broadcast_to([128, 4, 64]), op=ALU.mult),
                         reads=[pok, 'den'], writes=[ok_])
                    it += 1
                S.dma('sp', mixo[tq * 128:(tq + 1) * 128, 0:512], o_t[:], reads=[ok_], writes=[('mixo', 'att', tq)])


def rope_table_host(nlat):
    pos = np.arange(nlat)
    row = (pos // 64).astype(np.float32); col = (pos % 64).astype(np.float32)
    inv = (10000.0 ** (-np.arange(0, 32, 2, dtype=np.float32) / 32)).astype(np.float32)
    ar = row[:, None] * inv[None, :]; ac = col[:, None] * inv[None, :]
    cr, sr, cc, sc = np.cos(ar), np.sin(ar), np.cos(ac), np.sin(ac)
    return np.ascontiguousarray(np.concatenate([cr, cr, cc, cc, -sr, sr, -sc, sc], axis=1).astype(np.float32))


def build_attn_test(NCH):
    C = Ctx(); S = C.S
    NTOK = NCH * 128
    yatt = C.dram("yatt", [NTOK, 768], kind="ExternalInput")
    prm = dict(q_norm=C.dram("q_norm", [64], kind="ExternalInput"), k_norm=C.dram("k_norm", [64], kind="ExternalInput"),
               sink=C.dram("sink", [8], kind="ExternalInput"), rope=C.dram("rope", [(NCH - 2) * 128, 128], kind="ExternalInput"))
    mixo = C.dram("mixo", [NTOK, 1024], kind="ExternalOutput")
    ident, idk = C.ident()
    masks = make_masks(C)
    emit_attn(C, yatt, mixo, prm, NCH, ident, idk, masks)
    S.finish()
    return C


def emit_rowbcast(C, col_ap, col_key, dst, dkey, scratch):
    S = C.S
    S.dma('sp', scratch.rearrange("(m p) -> p m", p=128), col_ap, reads=[col_key], writes=[('rowscr', id(scratch))], allow_slow_non_contiguous=True)
    S.dma('sp', dst, scratch.partition_broadcast(128), reads=[('rowscr', id(scratch))], writes=[dkey])


def emit_post(C, M, mixo, xs, frows, wout, prm, NT, ident, idk, R):
    S = C.S
    with C.scope():
        wbf = C.sb([128, 8, 1024], BF16, 'woutbf')
        load_w_bf16(C, wbf, 'woutbf', wout, D, 1024)
        wr = C.sb([128, 8, 36], F32, 'wr')
        S.dma('sp', wr[:, :, 0:4], prm['w_grp'].rearrange("(k p) g -> p k g", p=128), writes=['wr'])
        S.dma('sp', wr[:, :, 4:36], prm['w_exp'].rearrange("(k p) g -> p k g", p=128), reads=['wr'], writes=['wr'])
        brow = C.sb([128, 36], F32, 'brow')
        S.dma('sp', brow[:, 0:4], prm['b_grp'].partition_broadcast(128), writes=['brow'])
        S.dma('sp', brow[:, 4:36], prm['b_exp'].partition_broadcast(128), reads=['brow'], writes=['brow'])
        rows = {}
        for nm, col in (('G1', M['m3'][:, 2]), ('A2', None), ('B2', M['m3'][:, 3])):
            for v in range(2):
                t_ = C.sb([128, 1024], F32, f'row{nm}{v}')
                ca = M['A2'][:, :, v] if nm == 'A2' else col[:, :, v]
                emit_rowbcast(C, ca, 'A2' if nm == 'A2' else 'modT', t_[:], f'row{nm}{v}', prm['rowscr'][(nm, v)])
                rows[(nm, v)] = t_
        mt = [C.sb([128, 1024], F32, 'mt') for _ in range(2)]
        xt = [C.sb([128, 1024], F32, 'xt') for _ in range(2)]
        ft = [C.sb([128, 1024], F32, 'ft') for _ in range(2)]
        mT = C.sb([128, 8, 128], BF16, 'mT'); fT = C.sb([128, 8, 128], F32, 'fT')
        junk = C.sb([128, 1024], F32, 'junk'); ss = C.sb([128, 4], F32, 'ssp'); tmpm = C.sb([128, 1024], F32, 'tmpm')
        pT = C.ps([128, 1024], F32, 'pTp'); pM = [C.ps([128, 512], F32, 'pM') for _ in range(2)]
        pT2 = C.ps([128, 1024], F32, 'pTp2'); pL = C.ps([128, 512], F32, 'pL')
        for t in range(NT):
            v = 1 if t < 2 else 0
            m_ = mt[t % 2]; x_ = xt[t % 2]; f_ = ft[t % 2]
            mk, xk, fk = f'mt{t % 2}', f'xt{t % 2}', f'ft{t % 2}'
            S.dma('sp', m_[:], mixo[t * 128:(t + 1) * 128, :], reads=[('mixo', 'att', t)] + [('mixo', 'rw', P) for P in range(4)] + ['mixo'], writes=[mk])
            S.dma('act', x_[:], xs[t * 128:(t + 1) * 128, :], reads=[('xs', t)], writes=[xk])
            for k in range(8):
                S.op('pe', lambda e, k=k, m_=m_: e.transpose(out=pT[:, k * 128:(k + 1) * 128], in_=m_[:, k * 128:(k + 1) * 128], identity=ident[:]),
                     reads=[mk, idk], writes=['pTp'])
            S.op('act', lambda e: e.copy(out=mT[:], in_=pT[:].rearrange("p (k t) -> p k t", k=8)), reads=['pTp'], writes=['mT'])
            for hf in range(2):
                for k in range(8):
                    S.op('pe', lambda e, k=k, hf=hf: e.matmul(pM[hf][:], lhsT=mT[:, k, :], rhs=wbf[:, k, hf * 512:(hf + 1) * 512], start=(k == 0), stop=(k == 7)),
                         reads=['mT', ('woutbf', k)], writes=[f'pM{hf}'])
                S.op('dve', lambda e, hf=hf: e.tensor_tensor(out=tmpm[:, hf * 512:(hf + 1) * 512], in0=pM[hf][:], in1=rows[('G1', v)][:, hf * 512:(hf + 1) * 512], op=ALU.mult),
                     reads=[f'pM{hf}', f'rowG1{v}'], writes=['tmpm'])
            S.op('pool', lambda e, x_=x_: e.tensor_tensor(out=x_[:], in0=x_[:], in1=tmpm[:], op=ALU.add), reads=[xk, 'tmpm'], writes=[xk])
            S.dma('act', xs[t * 128:(t + 1) * 128, :], x_[:], reads=[xk], writes=[('xs', t)])
            S.op('act', lambda e, x_=x_: e.activation(out=junk[:], in_=x_[:], func=AF.Square, accum_out=ss[:, 0:1]), reads=[xk], writes=['junk', 'ssp'])
            S.op('dve', lambda e: e.tensor_scalar(out=ss[:, 1:2], in0=ss[:, 0:1], scalar1=1.0 / D, scalar2=1e-6, op0=ALU.mult, op1=ALU.add), reads=['ssp'], writes=['ssp'])
            S.op('act', lambda e: e.activation(out=ss[:, 2:3], in_=ss[:, 1:2], func=AF.Sqrt), reads=['ssp'], writes=['ssp'])
            S.op('dve', lambda e: e.reciprocal(out=ss[:, 3:4], in_=ss[:, 2:3]), reads=['ssp'], writes=['ssp'])
            S.op('dve', lambda e, x_=x_, f_=f_: e.scalar_tensor_tensor(out=f_[:], in0=x_[:], scalar=ss[:, 3:4], in1=rows[('A2', v)][:], op0=ALU.mult, op1=ALU.mult),
                 reads=[xk, 'ssp', f'rowA2{v}'], writes=[fk])
            S.op('pool', lambda e, f_=f_: e.tensor_tensor(out=f_[:], in0=f_[:], in1=rows[('B2', v)][:], op=ALU.add), reads=[fk, f'rowB2{v}'], writes=[fk])
            S.dma('sp', frows[t * 128:(t + 1) * 128, :], f_[:], reads=[fk], writes=[('frows', t)])
            for k in range(8):
                S.op('pe', lambda e, k=k, f_=f_: e.transpose(out=pT2[:, k * 128:(k + 1) * 128], in_=f_[:, k * 128:(k + 1) * 128], identity=ident[:]),
                     reads=[fk, idk], writes=['pTp2'])
            S.op('act', lambda e: e.copy(out=fT[:], in_=pT2[:].rearrange("p (k t) -> p k t", k=8)), reads=['pTp2'], writes=['fT'])
            for k in range(8):
                S.op('pe', lambda e, k=k: e.matmul(pL[:, 0:36], lhsT=fT[:, k, :], rhs=wr[:, k, :], start=(k == 0), stop=(k == 7)), reads=['fT', 'wr'], writes=['pL'])
            S.op('dve', lambda e, t=t: e.tensor_tensor(out=R['lgall'][:, t, :], in0=pL[:, 0:36], in1=brow[:], op=ALU.add), reads=['pL', 'brow'], writes=['lgall'])


def emit_route(C, R, NT, NBLK, masks):
    S = C.S
    lg = R['lgall']
    with C.scope():
        BIG = 1.0e30
        t4 = C.sb([128, NT, 4], F32, 't4'); ohg = C.sb([128, NT, 4], F32, 'ohg'); gmax = C.sb([128, NT], F32, 'gmax'); pg = C.sb([128, NT], F32, 'pg')
        me = C.sb([128, NT, 32], F32, 'me'); me2 = C.sb([128, NT, 32], F32, 'me2'); m1 = C.sb([128, NT], F32, 'm1'); m2 = C.sb([128, NT], F32, 'm2')
        oh1 = C.sb([128, NT, 32], F32, 'oh1'); oh2 = C.sb([128, NT, 32], F32, 'oh2'); ohb = C.sb([128, NT, 32], BF16, 'ohb')
        e21 = C.sb([128, NT], F32, 'e21'); g1 = C.sb([128, NT], F32, 'g1')
        lgg = lg[:, :, 0:4]; lge = lg[:, :, 4:36]
        bN = lambda t_, n: t_[:].unsqueeze(2).broadcast_to([128, NT, n])
        op = S.op
        op('dve', lambda e: e.tensor_reduce(out=gmax[:], in_=lgg, axis=AX.X, op=ALU.max), reads=['lgall'], writes=['gmax'])
        op('dve', lambda e: e.tensor_tensor(out=ohg[:], in0=lgg, in1=bN(gmax, 4), op=ALU.is_equal), reads=['lgall', 'gmax'], writes=['ohg'])
        op('dve', lambda e: e.tensor_tensor(out=t4[:], in0=lgg, in1=bN(gmax, 4), op=ALU.subtract), reads=['lgall', 'gmax'], writes=['t4'])
        op('act', lambda e: e.activation(out=t4[:], in_=t4[:], func=AF.Exp), reads=['t4'], writes=['t4'])
        op('dve', lambda e: e.tensor_reduce(out=pg[:], in_=t4[:], axis=AX.X, op=ALU.add), reads=['t4'], writes=['pg'])
        op('dve', lambda e: e.reciprocal(out=pg[:], in_=pg[:]), reads=['pg'], writes=['pg'])
        op('dve', lambda e: e.tensor_scalar(out=t4[:], in0=ohg[:], scalar1=-1.0, scalar2=BIG, op0=ALU.add, op1=ALU.mult), reads=['ohg', 't4'], writes=['t4'])
        op('dve', lambda e: e.tensor_tensor(out=me[:].rearrange("p t (g x) -> p t g x", g=4), in0=lge.rearrange("p t (g x) -> p t g x", g=4),
                                            in1=t4[:].unsqueeze(3).broadcast_to([128, NT, 4, 8]), op=ALU.add), reads=['lgall', 't4'], writes=['me'])
        op('dve', lambda e: e.tensor_reduce(out=m1[:], in_=me[:], axis=AX.X, op=ALU.max), reads=['me'], writes=['m1'])
        op('dve', lambda e: e.tensor_tensor(out=oh1[:], in0=me[:], in1=bN(m1, 32), op=ALU.is_equal), reads=['me', 'm1'], writes=['oh1'])
        op('dve', lambda e: e.scalar_tensor_tensor(out=me2[:], in0=oh1[:], scalar=-BIG, in1=me[:], op0=ALU.mult, op1=ALU.add), reads=['oh1', 'me'], writes=['me2'])
        op('dve', lambda e: e.tensor_reduce(out=m2[:], in_=me2[:], axis=AX.X, op=ALU.max), reads=['me2'], writes=['m2'])
        op('dve', lambda e: e.tensor_tensor(out=oh2[:], in0=me2[:], in1=bN(m2, 32), op=ALU.is_equal), reads=['me2', 'm2'], writes=['oh2'])
        op('dve', lambda e: e.tensor_tensor(out=e21[:], in0=m2[:], in1=m1[:], op=ALU.subtract), reads=['m1', 'm2'], writes=['e21'])
        op('act', lambda e: e.activation(out=e21[:], in_=e21[:], func=AF.Exp), reads=['e21'], writes=['e21'])
        op('dve', lambda e: e.tensor_scalar(out=g1[:], in0=e21[:], scalar1=1.0, scalar2=None, op0=ALU.add), reads=['e21'], writes=['g1'])
        op('dve', lambda e: e.reciprocal(out=g1[:], in_=g1[:]), reads=['g1'], writes=['g1'])
        gates = R['gates']
        op('dve', lambda e: e.tensor_tensor(out=gates[:, :, 0], in0=g1[:], in1=pg[:], op=ALU.mult), reads=['g1', 'pg'], writes=['gates'])
        op('dve', lambda e: e.tensor_tensor(out=e21[:], in0=e21[:], in1=g1[:], op=ALU.mult), reads=['e21', 'g1'], writes=['e21'])
        op('dve', lambda e: e.tensor_tensor(out=gates[:, :, 1], in0=e21[:], in1=pg[:], op=ALU.mult), reads=['e21', 'pg', 'gates'], writes=['gates'])
        op('dve', lambda e: e.tensor_tensor(out=ohb[:], in0=oh1[:], in1=oh2[:], op=ALU.add), reads=['oh1', 'oh2'], writes=['ohb'])
        onesb = C.sb([128, 128], BF16, 'onesb'); trib = C.sb([128, 128], BF16, 'trib')
        op('pool', lambda e: e.memset(onesb[:], 1.0), writes=['onesb'])
        op('dve', lambda e: e.tensor_copy(out=trib[:], in_=masks['up_s'][:]), reads=['mkup_s'], writes=['trib'])
        NG = (NT + 15) // 16
        pCS = [C.ps([128, 512], F32, 'pCS') for _ in range(NG)]; pRK = [C.ps([128, 512], F32, 'pRK') for _ in range(NG)]
        cs = C.sb([128, NT, 32], F32, 'cs'); rk = C.sb([128, NT, 32], F32, 'rk'); carry = C.sb([128, NT + 1, 32], F32, 'carry')
        for t in range(NT):
            g, o = t // 16, (t % 16) * 32
            op('pe', lambda e, g=g, o=o, t=t: e.matmul(pCS[g][:, o:o + 32], lhsT=onesb[:], rhs=ohb[:, t, :], start=True, stop=True), reads=['onesb', 'ohb'], writes=[f'pCS{g}'])
            op('pe', lambda e, g=g, o=o, t=t: e.matmul(pRK[g][:, o:o + 32], lhsT=trib[:], rhs=ohb[:, t, :], start=True, stop=True), reads=['trib', 'ohb'], writes=[f'pRK{g}'])
        for g in range(NG):
            n = min(16, NT - g * 16)
            op('dve', lambda e, g=g, n=n: e.tensor_copy(out=cs[:, g * 16:g * 16 + n, :], in_=pCS[g][:, 0:n * 32].rearrange("p (t x) -> p t x", x=32)), reads=[f'pCS{g}'], writes=['cs'])
            op('dve', lambda e, g=g, n=n: e.tensor_copy(out=rk[:, g * 16:g * 16 + n, :], in_=pRK[g][:, 0:n * 32].rearrange("p (t x) -> p t x", x=32)), reads=[f'pRK{g}'], writes=['rk'])
        op('pool', lambda e: e.memset(carry[:, 0, :], 0.0), writes=['carry'])
        for t in range(NT):
            op('dve', lambda e, t=t: e.tensor_tensor(out=carry[:, t + 1, :], in0=carry[:, t, :], in1=cs[:, t, :], op=ALU.add), reads=['carry', 'cs'], writes=['carry'])
        op('dve', lambda e: e.tensor_tensor(out=rk[:], in0=rk[:], in1=carry[:, 0:NT, :], op=ALU.add), reads=['rk', 'carry'], writes=['rk'])
        cnt = C.sb([128, 32], F32, 'cnt'); ci = C.sb([128, 32], I32, 'ci'); pad = C.sb([128, 32], F32, 'pad'); pend = C.sb([128, 32], F32, 'pend'); pst = C.sb([128, 32], F32, 'pst')
        ones32 = C.sb([128, 32], F32, 'ones32')
        op('pool', lambda e: e.memset(ones32[:], 1.0), writes=['ones32'])
        op('dve', lambda e: e.tensor_scalar(out=cnt[:], in0=carry[:, NT, :], scalar1=127.0, scalar2=None, op0=ALU.add), reads=['carry'], writes=['cnt'])
        op('dve', lambda e: e.tensor_copy(out=ci[:], in_=cnt[:]), reads=['cnt'], writes=['ci'])
        op('dve', lambda e: e.tensor_scalar(out=ci[:], in0=ci[:], scalar1=7, scalar2=7, op0=ALU.arith_shift_right, op1=ALU.logical_shift_left), reads=['ci'], writes=['ci'])
        op('dve', lambda e: e.tensor_copy(out=pad[:], in_=ci[:]), reads=['ci'], writes=['pad'])
        op('dve', lambda e: e.tensor_tensor_scan(out=pend[:], data0=ones32[:], data1=pad[:], initial=0.0, op0=ALU.mult, op1=ALU.add), reads=['ones32', 'pad'], writes=['pend'])
        op('dve', lambda e: e.tensor_tensor(out=pst[:], in0=pend[:], in1=pad[:], op=ALU.subtract), reads=['pend', 'pad'], writes=['pst'])
        op('dve', lambda e: e.tensor_tensor(out=rk[:], in0=rk[:], in1=pst[:].unsqueeze(1).broadcast_to([128, NT, 32]), op=ALU.add), reads=['rk', 'pst'], writes=['rk'])
        destf = C.sb([128, NT, 2], F32, 'destf')
        for j, oh in enumerate((oh1, oh2)):
            op('dve', lambda e, oh=oh: e.tensor_tensor(out=me[:], in0=oh[:], in1=rk[:], op=ALU.mult), reads=['oh1', 'oh2', 'rk', 'me'], writes=['me'])
            op('dve', lambda e, j=j: e.tensor_reduce(out=destf[:, :, j], in_=me[:], axis=AX.X, op=ALU.add), reads=['me', 'destf'], writes=['destf'])
        op('dve', lambda e: e.tensor_copy(out=R['dest'][:].rearrange("p (t j) -> p t j", j=2), in_=destf[:]), reads=['destf'], writes=['dest'])
        bpos = C.sb([128, NBLK], I32, 'bpos'); bposf = C.sb([128, NBLK], F32, 'bposf'); cmpb = C.sb([128, NBLK, 32], F32, 'cmpb'); eb = C.sb([128, NBLK], F32, 'eb')
        kp = C.sb([128, 8], I32, 'kp'); kpf = C.sb([128, 8], F32, 'kpf'); idf = C.sb([128, NBLK, 8], F32, 'idf')
        op('pool', lambda e: e.iota(bpos[:], pattern=[[128, NBLK]], base=0, channel_multiplier=0), writes=['bpos'])
        op('dve', lambda e: e.tensor_copy(out=bposf[:], in_=bpos[:]), reads=['bpos'], writes=['bposf'])
        op('dve', lambda e: e.tensor_tensor(out=cmpb[:], in0=pend[:].unsqueeze(1).broadcast_to([128, NBLK, 32]), in1=bposf[:].unsqueeze(2).broadcast_to([128, NBLK, 32]), op=ALU.is_le),
           reads=['pend', 'bposf'], writes=['cmpb'])
        op('dve', lambda e: e.tensor_reduce(out=eb[:], in_=cmpb[:], axis=AX.X, op=ALU.add), reads=['cmpb'], writes=['eb'])
        op('dve', lambda e: e.tensor_scalar(out=eb[:], in0=eb[:], scalar1=31.0, scalar2=None, op0=ALU.min), reads=['eb'], writes=['eb'])
        op('pool', lambda e: e.iota(kp[:], pattern=[[128, 8]], base=0, channel_multiplier=1), writes=['kp'])
        op('dve', lambda e: e.tensor_copy(out=kpf[:], in_=kp[:]), reads=['kp'], writes=['kpf'])
        op('dve', lambda e: e.scalar_tensor_tensor(out=idf[:], in0=eb[:].unsqueeze(2).broadcast_to([128, NBLK, 8]), scalar=1024.0, in1=kpf[:].unsqueeze(1).broadcast_to([128, NBLK, 8]),
                                                   op0=ALU.mult, op1=ALU.add), reads=['eb', 'kpf'], writes=['idf'])
        op('dve', lambda e: e.tensor_copy(out=R['idxg'][:].rearrange("p (b k) -> p b k", k=8), in_=idf[:]), reads=['idf'], writes=['idxg'])
        idf2 = C.sb([128, NBLK], F32, 'idf2')
        op('dve', lambda e: e.scalar_tensor_tensor(out=idf2[:], in0=eb[:], scalar=128.0, in1=kpf[:, 0:1].broadcast_to([128, NBLK]), op0=ALU.mult, op1=ALU.add), reads=['eb', 'kpf'], writes=['idf2'])
        op('dve', lambda e: e.tensor_copy(out=R['idxe'][:], in_=idf2[:]), reads=['idf2'], writes=['idxe'])
        op('dve', lambda e: e.scalar_tensor_tensor(out=idf[:, :, 0:4], in0=eb[:].unsqueeze(2).broadcast_to([128, NBLK, 4]), scalar=512.0, in1=kpf[:, 0:4].unsqueeze(1).broadcast_to([128, NBLK, 4]),
                                                   op0=ALU.mult, op1=ALU.add), reads=['eb', 'kpf', 'idf'], writes=['idf'])
        op('dve', lambda e: e.tensor_copy(out=R['idxd'][:].rearrange("p (b k) -> p b k", k=4), in_=idf[:, :, 0:4]), reads=['idf'], writes=['idxd'])


def emit_moe(C, M, R, frows, xs, xrows, yrows, wgu, wdn, prm, NT, NBLK, ident, idk, out_dram=None, out_tiles=None, wkeys=()):
    S = C.S
    IOA = bass.IndirectOffsetOnAxis
    with C.scope():
        z = C.sb([128, 2048], F32, 'zer')
        S.op('pool', lambda e: e.memset(z[:], 0.0), writes=['zer'])
        for b0 in range(0, NBLK, 2):
            n = min(2, NBLK - b0)
            S.dma('sp', xrows[b0 * 128:(b0 + n) * 128, :].rearrange("(b p) f -> p b f", p=128), z[:, 0:n * 1024].rearrange("p (b f) -> p b f", f=1024),
                  reads=['zer'], writes=['xrows'])
        ftb = [C.sb([128, 1024], F32, 'ftb') for _ in range(2)]
        for t in range(NT):
            f_ = ftb[t % 2]; fk = f'ftb{t % 2}'
            S.dma('sp', f_[:], frows[t * 128:(t + 1) * 128, :], reads=[('frows', t)], writes=[fk])
            for j in range(2):
                S.dma('pool', xrows, f_[:], reads=[fk, 'dest'], writes=['xrows'],
                      indirect=dict(out_offset=IOA(ap=R['dest'][:, 2 * t + j:2 * t + j + 1], axis=0), in_offset=None))
    with C.scope():
        identb = C.sb([128, 128], BF16, 'identb2')
        S.op('dve', lambda e: e.tensor_copy(out=identb[:], in_=ident[:]), reads=[idk], writes=['identb2'])
        wg = [C.sb([128, 8, 1024], BF16, 'wg') for _ in range(2)]; wd = [C.sb([128, 4, 1024], BF16, 'wd') for _ in range(2)]
        xb = [C.sb([128, 1024], F32, 'xb') for _ in range(2)]; yb = [C.sb([128, 1024], F32, 'ybm') for _ in range(2)]
        xT = C.sb([128, 8, 128], BF16, 'xTm'); sg = C.sb([128, 512], F32, 'sgm'); hb = C.sb([128, 512], BF16, 'hbm'); hT = C.sb([128, 4, 128], BF16, 'hTm')
        pTx = C.ps([128, 1024], F32, 'pTx'); pG = C.ps([128, 512], F32, 'pGm'); pU = C.ps([128, 512], F32, 'pUm')
        pTh = C.ps([128, 1024], BF16, 'pTh'); pD = [C.ps([128, 512], F32, 'pDm') for _ in range(2)]
        wflat = wgu.rearrange("e k n -> (e k) n") if len(wgu.shape) == 3 else wgu
        dflat = wdn.rearrange("e k n -> (e k) n") if len(wdn.shape) == 3 else wdn
        wkeys = list(wkeys)
        for b in range(NBLK):
            w_ = wg[b % 2]; d_ = wd[b % 2]; x_ = xb[b % 2]; y_ = yb[b % 2]
            if len(wgu.shape) == 2:
                S.dma('pool', w_[:].rearrange("p k n -> p (k n)"), wflat, reads=['idxe'] + wkeys, writes=[(f'wg{b % 2}', k) for k in range(8)],
                      indirect=dict(out_offset=None, in_offset=IOA(ap=R['idxe'][:, b:b + 1], axis=0)))
                S.dma('pool', d_[:].rearrange("p k n -> p (k n)"), dflat, reads=['idxe'] + wkeys, writes=[(f'wd{b % 2}', k) for k in range(4)],
                      indirect=dict(out_offset=None, in_offset=IOA(ap=R['idxe'][:, b:b + 1], axis=0)))
            else:
                for k in range(8):
                    S.dma('pool', w_[:, k, :], wflat, reads=['idxg'] + wkeys, writes=[(f'wg{b % 2}', k)],
                          indirect=dict(out_offset=None, in_offset=IOA(ap=R['idxg'][:, b * 8 + k:b * 8 + k + 1], axis=0)))
                for k in range(4):
                    S.dma('pool', d_[:, k, :], dflat, reads=['idxd'] + wkeys, writes=[(f'wd{b % 2}', k)],
                          indirect=dict(out_offset=None, in_offset=IOA(ap=R['idxd'][:, b * 4 + k:b * 4 + k + 1], axis=0)))
            S.dma('sp', x_[:], xrows[b * 128:(b + 1) * 128, :], reads=['xrows'], writes=[f'xb{b % 2}'])
            for k in range(8):
                S.op('pe', lambda e, k=k, x_=x_: e.transpose(out=pTx[:, k * 128:(k + 1) * 128], in_=x_[:, k * 128:(k + 1) * 128], identity=ident[:]),
                     reads=[f'xb{b % 2}', idk], writes=['pTx'])
            S.op('act', lambda e: e.copy(out=xT[:], in_=pTx[:].rearrange("p (k t) -> p k t", k=8)), reads=['pTx'], writes=['xTm'])
            for k in range(8):
                S.op('pe', lambda e, k=k, w_=w_: e.matmul(pG[:], lhsT=xT[:, k, :], rhs=w_[:, k, 0:512], start=(k == 0), stop=(k == 7)), reads=['xTm', (f'wg{b % 2}', k)], writes=['pGm'])
            for k in range(8):
                S.op('pe', lambda e, k=k, w_=w_: e.matmul(pU[:], lhsT=xT[:, k, :], rhs=w_[:, k, 512:1024], start=(k == 0), stop=(k == 7)), reads=['xTm', (f'wg{b % 2}', k)], writes=['pUm'])
            S.op('act', lambda e: e.activation(out=sg[:], in_=pG[:], func=AF.Silu), reads=['pGm'], writes=['sgm'])
            S.op('dve', lambda e: e.tensor_tensor(out=hb[:], in0=pU[:], in1=sg[:], op=ALU.mult), reads=['pUm', 'sgm'], writes=['hbm'])
            for k in range(4):
                S.op('pe', lambda e, k=k: e.transpose(out=pTh[:, k * 128:(k + 1) * 128], in_=hb[:, k * 128:(k + 1) * 128], identity=identb[:]), reads=['hbm', 'identb2'], writes=['pTh'])
            S.op('act', lambda e: e.copy(out=hT[:], in_=pTh[:, 0:512].rearrange("p (k t) -> p k t", k=4)), reads=['pTh'], writes=['hTm'])
            for hf in range(2):
                for k in range(4):
                    S.op('pe', lambda e, k=k, hf=hf, d_=d_: e.matmul(pD[hf][:], lhsT=hT[:, k, :], rhs=d_[:, k, hf * 512:(hf + 1) * 512], start=(k == 0), stop=(k == 3)),
                         reads=['hTm', (f'wd{b % 2}', k)], writes=[f'pDm{hf}'])
                if hf == 0:
                    S.op('act', lambda e, y_=y_: e.copy(out=y_[:, 0:512], in_=pD[0][:]), reads=['pDm0'], writes=[f'ybm{b % 2}'])
                else:
                    S.op('dve', lambda e, y_=y_: e.tensor_copy(out=y_[:, 512:1024], in_=pD[1][:]), reads=['pDm1'], writes=[f'ybm{b % 2}'])
            S.dma('sp', yrows[b * 128:(b + 1) * 128, :], y_[:], reads=[f'ybm{b % 2}'], writes=['yrows'])
    with C.scope():
        rowG2 = []
        for v in range(2):
            t_ = C.sb([128, 1024], F32, f'rowG2{v}')
            emit_rowbcast(C, M['m3'][:, 5][:, :, v], 'modT', t_[:], f'rowG2{v}', prm['rowscr'][('G2', v)])
            rowG2.append(t_)
        y1 = [C.sb([128, 1024], F32, 'y1') for _ in range(2)]; y2 = [C.sb([128, 1024], F32, 'y2') for _ in range(2)]
        xm = [C.sb([128, 1024], F32, 'xm') for _ in range(2)]
        tiles = out_tiles if out_tiles is not None else list(range(NT))
        for i, t in enumerate(tiles):
            v = 1 if t < 2 else 0
            a_, b_, x_ = y1[i % 2], y2[i % 2], xm[i % 2]
            ak, bk, xk = f'y1{i % 2}', f'y2{i % 2}', f'xm{i % 2}'
            S.dma('pool', a_[:], yrows, reads=['yrows', 'dest'], writes=[ak], indirect=dict(out_offset=None, in_offset=IOA(ap=R['dest'][:, 2 * t:2 * t + 1], axis=0)))
            S.dma('pool', b_[:], yrows, reads=['yrows', 'dest'], writes=[bk], indirect=dict(out_offset=None, in_offset=IOA(ap=R['dest'][:, 2 * t + 1:2 * t + 2], axis=0)))
            S.dma('sp', x_[:], xs[t * 128:(t + 1) * 128, :], reads=[('xs', t)], writes=[xk])
            S.op('dve', lambda e, a_=a_, t=t: e.tensor_scalar(out=a_[:], in0=a_[:], scalar1=R['gates'][:, t, 0:1], scalar2=None, op0=ALU.mult), reads=[ak, 'gates'], writes=[ak])
            S.op('dve', lambda e, a_=a_, b_=b_, t=t: e.scalar_tensor_tensor(out=a_[:], in0=b_[:], scalar=R['gates'][:, t, 1:2], in1=a_[:], op0=ALU.mult, op1=ALU.add),
                 reads=[ak, bk, 'gates'], writes=[ak])
            S.op('pool', lambda e, a_=a_, v=v: e.tensor_tensor(out=a_[:], in0=a_[:], in1=rowG2[v][:], op=ALU.mult), reads=[ak, f'rowG2{v}'], writes=[ak])
            S.op('dve', lambda e, a_=a_, x_=x_: e.tensor_tensor(out=x_[:], in0=x_[:], in1=a_[:], op=ALU.add), reads=[ak, xk], writes=[xk])
            if out_dram is None:
                S.dma('sp', xs[t * 128:(t + 1) * 128, :], x_[:], reads=[xk], writes=[('xs', t)])
            else:
                S.dma('sp', out_dram[(t - 2) * 128:(t - 1) * 128, :], x_[:], reads=[xk], writes=[('out', t)])


def route_alloc(C, NT, NBLK):
    return dict(lgall=C.sb([128, NT, 36], F32, 'lgall'), gates=C.sb([128, NT, 2], F32, 'gates'), dest=C.sb([128, NT * 2], I32, 'dest'),
                idxg=C.sb([128, NBLK * 8], I32, 'idxg'), idxd=C.sb([128, NBLK * 4], I32, 'idxd'), idxe=C.sb([128, NBLK], I32, 'idxe'))


def rowscr_alloc(C, tag):
    return {(nm, v): C.dram(f"rowscr_{tag}_{nm}{v}", [1024], F32) for nm in ('G1', 'A2', 'B2', 'G2') for v in range(2)}


def build_moe_test(NT):
    C = Ctx(); S = C.S
    NTOK = NT * 128; NBLK = 2 * NT + 32
    mixo = C.dram("mixo", [NTOK, 1024], kind="ExternalInput")
    xin = C.dram("xin", [NTOK, 1024], kind="ExternalInput")
    cin = C.dram("cin", [128, 16], kind="ExternalInput"); adaw = C.dram("adaw", [D, 6 * D], kind="ExternalInput")
    adab = C.dram("adab", [128, 96], kind="ExternalInput"); nrm = C.dram("nrm", [128, 16], kind="ExternalInput")
    wout = C.dram("wout", [D, D], kind="ExternalInput")
    prm = dict(w_grp=C.dram("w_grp", [D, 4], kind="ExternalInput"), b_grp=C.dram("b_grp", [4], kind="ExternalInput"),
               w_exp=C.dram("w_exp", [D, 32], kind="ExternalInput"), b_exp=C.dram("b_exp", [32], kind="ExternalInput"))
    wgu = C.dram("wgu", [32, 1024, 1024], kind="ExternalInput"); wdn = C.dram("wdn", [32, 512, 1024], kind="ExternalInput")
    xs = C.dram("xs", [NTOK, 1024], kind="ExternalOutput")
    frows = C.dram("frows", [NTOK, 1024], kind="ExternalOutput")
    xrows = C.dram("xrows", [NBLK * 128, 1024]); yrows = C.dram("yrows", [NBLK * 128, 1024])
    prm['rowscr'] = rowscr_alloc(C, 't')
    ident, idk = C.ident(); masks = make_masks(C)
    for t in range(NT):
        S.dma('sp', xs[t * 128:(t + 1) * 128, :], xin[t * 128:(t + 1) * 128, :], writes=[('xs', t)])
    M = emit_mods(C, adaw, adab, cin, nrm)
    R = route_alloc(C, NT, NBLK)
    emit_post(C, M, mixo, xs, frows, wout, prm, NT, ident, idk, R)
    emit_route(C, R, NT, NBLK, masks)
    emit_moe(C, M, R, frows, xs, xrows, yrows, wgu, wdn, prm, NT, NBLK, ident, idk)
    S.finish()
    return C


def emit_pre(C, M, xs, win, NOUT, NT, ident, idk, fm0, nfm, fm_out, tm0, tm1, tm_out, mode, pcols):
    S = C.S
    NTOK = NT * 128
    with C.scope():
        hT = C.sb([128, 8, NTOK], BF16, 'hT')
        with C.scope():
            tmp = dict(junk=C.sb([128, D]), ss=C.sb([128, 4]), xn=C.sb([128, D]), t2=C.sb([128, 8, 128]))
            xb = [C.sb([128, D], F32, 'xb') for _ in range(2)]
            pT = [C.ps([128, 1024], F32, 'pT') for _ in range(2)]
            B1 = M['m3'][:, 0]
            for t in range(NT):
                v = 1 if t < 2 else 0
                S.dma('sp', xb[t % 2][:], xs[t * 128:(t + 1) * 128, :], reads=[('xs', t)], writes=[f'xb{t % 2}'])
                emit_normT(C, xb[t % 2][:], f'xb{t % 2}', hT, ('hT', t), t * 128, M['A1'], B1, v, ident, idk, pT[t % 2], f'pT{t % 2}', tmp, t)
        hkeys = [('hT', t) for t in range(NT)]
        with C.scope():
            TW = tm1 - tm0
            wtm = C.sb([128, 8, TW], BF16, 'wtm')
            for k in range(8):
                S.dma('pool', wtm[:, k, :], win[k * 128:(k + 1) * 128, tm0:tm1], writes=[('wtm', k)])
            pY = [C.ps([128, 512], F32, 'pY') for _ in range(3)]
            yb = [C.sb([128, TW], F32, 'yb') for _ in range(2)]
            ncc = (TW + 511) // 512
            it = 0
            for t in range(NT):
                for c in range(ncc):
                    cw = min(512, TW - c * 512)
                    p = pY[it % 3]; pk = f'pY{it % 3}'
                    for k in range(8):
                        S.op('pe', lambda e, k=k, p=p, c=c, cw=cw, t=t: e.matmul(p[:, 0:cw], lhsT=hT[:, k, t * 128:(t + 1) * 128],
                                                                                rhs=wtm[:, k, c * 512:c * 512 + cw], start=(k == 0), stop=(k == 7)),
                             reads=[('hT', t), ('wtm', k)], writes=[pk])
                    eng = 'act' if it % 2 == 0 else 'dve'
                    if eng == 'act':
                        S.op('act', lambda e, p=p, c=c, cw=cw, t=t: e.copy(out=yb[t % 2][:, c * 512:c * 512 + cw], in_=p[:, 0:cw]), reads=[pk], writes=[f'yb{t % 2}'])
                    else:
                        S.op('dve', lambda e, p=p, c=c, cw=cw, t=t: e.tensor_copy(out=yb[t % 2][:, c * 512:c * 512 + cw], in_=p[:, 0:cw]), reads=[pk], writes=[f'yb{t % 2}'])
                    it += 1
                S.dma('sp', tm_out[t * 128:(t + 1) * 128, :], yb[t % 2][:], reads=[f'yb{t % 2}'], writes=[('tm_out', t)])
        with C.scope():
            PADW = NTOK + 8
            co, lo = 2, 262
            pb = [C.sb([128, PADW], F32, 'pbuf') for _ in range(2)]
            acc = [C.sb([128, PADW], F32, 'accb') for _ in range(2)]
            wc = [C.sb([128, 8, 128], BF16, 'wc') for _ in range(2)]
            pc = C.sb([128, pcols.shape[1]], F32, 'pcols')
            S.dma('sp', pc[:], pcols, writes=['pcols'])
            pF = [C.ps([128, 512], F32, 'pF') for _ in range(3)]
            for i in range(2):
                S.op('pool', lambda e, i=i: e.memset(pb[i][:], 0.0), writes=[f'pbuf{i}'])
            NT5 = (NTOK + 511) // 512
            it = 0
            for cc in range(nfm):
                P_ = pb[cc % 2]; A_ = acc[cc % 2]; W_ = wc[cc % 2]
                pk_, ak_, wk_ = f'pbuf{cc % 2}', f'accb{cc % 2}', f'wc{cc % 2}'
                S.dma('pool', W_[:], win[:, fm0 + cc * 128:fm0 + (cc + 1) * 128].rearrange("(k p) c -> p k c", p=128), writes=[wk_])
                for i in range(NT5):
                    w = min(512, NTOK - i * 512)
                    p = pF[it % 3]; pfk = f'pF{it % 3}'
                    for k in range(8):
                        S.op('pe', lambda e, k=k, p=p, i=i, w=w, W_=W_: e.matmul(p[:, 0:w], lhsT=W_[:, k, :], rhs=hT[:, k, i * 512:i * 512 + w], start=(k == 0), stop=(k == 7)),
                             reads=hkeys[i * 4:i * 4 + 4] + [wk_], writes=[pfk])
                    if i == 0:
                        S.op('act', lambda e, p=p, P_=P_: e.copy(out=P_[:, co:co + 256], in_=p[:, 0:256]), reads=[pfk], writes=[pk_])
                        S.op('dve', lambda e, p=p, P_=P_, w=w: e.tensor_copy(out=P_[:, lo:lo + w - 256], in_=p[:, 256:w]), reads=[pfk, pk_], writes=[pk_])
                    else:
                        o = lo + i * 512 - 256
                        if it % 2 == 0:
                            S.op('act', lambda e, p=p, P_=P_, w=w, o=o: e.copy(out=P_[:, o:o + w], in_=p[:, 0:w]), reads=[pfk, pk_], writes=[pk_])
                        else:
                            S.op('dve', lambda e, p=p, P_=P_, w=w, o=o: e.tensor_copy(out=P_[:, o:o + w], in_=p[:, 0:w]), reads=[pfk, pk_], writes=[pk_])
                    it += 1
                L = PADW - 4
                if mode == 'shift':
                    S.op('pool', lambda e, P_=P_, A_=A_: e.tensor_tensor(out=A_[:, 2:2 + L], in0=P_[:, 1:1 + L], in1=P_[:, 3:3 + L], op=ALU.add), reads=[pk_], writes=[ak_])
                    S.op('dve', lambda e, A_=A_, cc=cc: e.tensor_scalar(out=A_[:, 2:2 + L], in0=A_[:, 2:2 + L], scalar1=pc[:, 2 * nfm + cc:2 * nfm + cc + 1], scalar2=None, op0=ALU.mult),
                         reads=[ak_, 'pcols'], writes=[ak_])
                    S.op('dve', lambda e, A_=A_, P_=P_, cc=cc: e.scalar_tensor_tensor(out=A_[:, 2:2 + L], in0=P_[:, 2:2 + L], scalar=pc[:, nfm + cc:nfm + cc + 1], in1=A_[:, 2:2 + L],
                                                                                   op0=ALU.mult, op1=ALU.add), reads=[ak_, pk_, 'pcols'], writes=[ak_])
                else:
                    S.op('dve', lambda e, A_=A_, P_=P_, cc=cc: e.tensor_scalar(out=A_[:, 2:2 + L], in0=P_[:, 0:L], scalar1=pc[:, cc:cc + 1], scalar2=None, op0=ALU.mult),
                         reads=[pk_, 'pcols'], writes=[ak_])
                    for j in range(1, 5):
                        S.op('dve', lambda e, A_=A_, P_=P_, cc=cc, j=j: e.scalar_tensor_tensor(out=A_[:, 2:2 + L], in0=P_[:, j:j + L], scalar=pc[:, j * nfm + cc:j * nfm + cc + 1],
                                                                                              in1=A_[:, 2:2 + L], op0=ALU.mult, op1=ALU.add), reads=[ak_, pk_, 'pcols'], writes=[ak_])
                    S.op('act', lambda e, A_=A_: e.activation(out=A_[:, 2:2 + L], in_=A_[:, 2:2 + L], func=AF.Silu), reads=[ak_], writes=[ak_])
                S.dma('sp', fm_out[cc * 128:(cc + 1) * 128, 0:256], A_[:, co:co + 256], reads=[ak_], writes=[('fm_out', cc)])
                S.dma('act', fm_out[cc * 128:(cc + 1) * 128, 256:NTOK], A_[:, lo:lo + NTOK - 256], reads=[ak_], writes=[('fm_out', cc, 1)])


def emit_gdn(C, gT, yz, mixo, prm, NCH, heads, ident, idk, masks):
    S = C.S
    NTOK = NCH * 128
    NT5 = (NTOK + 511) // 512
    order = [chunk_order(NCH, 0), chunk_order(NCH, 1)]
    with C.scope():
        ab = C.sb([128, NCH, 32], F32, 'gab')
        S.dma('sp', ab[:], yz.rearrange("(c t) f -> t c f", t=128)[:, :, 1024:1056], reads=[('tm_out', t) for t in range(NCH)], writes=['gab'])
        rowp = C.sb([128, 48], F32, 'growp')
        S.dma('sp', rowp[:, 0:16], prm['dt_bias'].partition_broadcast(128), writes=['growp'])
        S.dma('sp', rowp[:, 16:32], prm['A_log'].partition_broadcast(128), reads=['growp'], writes=['growp'])
        onorm = C.sb([128, 128], F32, 'onorm')
        S.dma('sp', onorm[:], prm['out_norm'].partition_broadcast(128), writes=['onorm'])
        one1 = C.sb([128, 1], F32, 'one1'); eps6 = C.sb([128, 1], F32, 'eps6')
        S.op('pool', lambda e: e.memset(one1[:], 1.0), writes=['one1'])
        S.op('pool', lambda e: e.memset(eps6[:], 1e-6), writes=['eps6'])
        ones = C.sb([128, 128], F32, 'onesf')
        S.op('pool', lambda e: e.memset(ones[:], 1.0), writes=['onesf'])
        S.op('act', lambda e: e.activation(out=rowp[:, 16:32], in_=rowp[:, 16:32], func=AF.Exp), reads=['growp'], writes=['growp'])
        S.op('dve', lambda e: e.tensor_scalar(out=rowp[:, 16:32], in0=rowp[:, 16:32], scalar1=-1.0, scalar2=None, op0=ALU.mult), reads=['growp'], writes=['growp'])
        g = C.sb([128, NCH, 16], F32, 'gg'); beta = C.sb([128, NCH, 16], F32, 'gbeta')
        gam = C.sb([128, NCH, 16], F32, 'gam'); gtot = C.sb([128, NCH, 16], F32, 'gtot')
        bR = lambda a: a.unsqueeze(1).broadcast_to([128, NCH, 16])
        S.op('dve', lambda e: e.tensor_tensor(out=g[:], in0=ab[:, :, 0:16], in1=bR(rowp[:, 0:16]), op=ALU.add), reads=['gab', 'growp'], writes=['gg'])
        S.op('act', lambda e: e.activation(out=g[:], in_=g[:], func=AF.Exp), reads=['gg'], writes=['gg'])
        S.op('act', lambda e: e.activation(out=g[:], in_=g[:], func=AF.Ln, bias=one1[:, 0:1]), reads=['gg', 'one1'], writes=['gg'])
        S.op('dve', lambda e: e.tensor_tensor(out=g[:], in0=g[:], in1=bR(rowp[:, 16:32]), op=ALU.mult), reads=['gg', 'growp'], writes=['gg'])
        S.op('act', lambda e: e.activation(out=beta[:], in_=ab[:, :, 16:32], func=AF.Sigmoid), reads=['gab'], writes=['gbeta'])
        tri = [masks['up_i'], masks['lo_i']]
        trik = ['mkup_i', 'mklo_i']
        with C.scope():
            pGm = [C.ps([128, 512], F32, 'pGam') for _ in range(2)]; pGt = [C.ps([128, 512], F32, 'pGtot') for _ in range(2)]
            for c in range(NCH):
                b_, o_ = c // 32, (c % 32) * 16
                for d in range(2):
                    S.op('pe', lambda e, c=c, d=d, b_=b_, o_=o_: e.matmul(pGm[b_][:, o_ + d * 8:o_ + d * 8 + 8], lhsT=tri[d][:], rhs=g[:, c, d * 8:(d + 1) * 8], start=True, stop=True),
                         reads=['gg', trik[d]], writes=[f'pGam{b_}'])
                S.op('pe', lambda e, c=c, b_=b_, o_=o_: e.matmul(pGt[b_][:, o_:o_ + 16], lhsT=ones[:], rhs=g[:, c, :], start=True, stop=True), reads=['gg', 'onesf'], writes=[f'pGtot{b_}'])
            for b_ in range((NCH + 31) // 32):
                n = min(32, NCH - b_ * 32)
                S.op('dve', lambda e, b_=b_, n=n: e.tensor_copy(out=gam[:, b_ * 32:b_ * 32 + n, :], in_=pGm[b_][:, 0:n * 16].rearrange("p (c x) -> p c x", x=16)), reads=[f'pGam{b_}'], writes=['gam'])
                S.op('dve', lambda e, b_=b_, n=n: e.tensor_copy(out=gtot[:, b_ * 32:b_ * 32 + n, :], in_=pGt[b_][:, 0:n * 16].rearrange("p (c x) -> p c x", x=16)), reads=[f'pGtot{b_}'], writes=['gtot'])
        nbeg = C.sb([128, NCH, 16], F32, 'nbeg'); etail = C.sb([128, NCH, 16], F32, 'etail'); eC = C.sb([128, NCH, 16], F32, 'eC'); nbeta = C.sb([128, NCH, 16], F32, 'nbeta')
        S.op('act', lambda e: e.activation(out=nbeg[:], in_=gam[:], func=AF.Exp), reads=['gam'], writes=['nbeg'])
        S.op('dve', lambda e: e.tensor_tensor(out=nbeg[:], in0=nbeg[:], in1=beta[:], op=ALU.mult), reads=['nbeg', 'gbeta'], writes=['nbeg'])
        S.op('dve', lambda e: e.tensor_scalar(out=nbeg[:], in0=nbeg[:], scalar1=-1.0, scalar2=None, op0=ALU.mult), reads=['nbeg'], writes=['nbeg'])
        S.op('dve', lambda e: e.tensor_tensor(out=etail[:], in0=gtot[:], in1=gam[:], op=ALU.subtract), reads=['gtot', 'gam'], writes=['etail'])
        S.op('act', lambda e: e.activation(out=etail[:], in_=etail[:], func=AF.Exp), reads=['etail'], writes=['etail'])
        S.op('act', lambda e: e.activation(out=eC[:], in_=gtot[:], func=AF.Exp), reads=['gtot'], writes=['eC'])
        S.op('dve', lambda e: e.tensor_scalar(out=nbeta[:], in0=beta[:], scalar1=-1.0, scalar2=None, op0=ALU.mult), reads=['gbeta'], writes=['nbeta'])
        mS = [masks['lo_s'], masks['up_s']]; mSk = ['mklo_s', 'mkup_s']
        mIT = [masks['up_i'], masks['lo_i']]; mITk = ['mkup_i', 'mklo_i']
        for h in heads:
            with C.scope():
                qn = C.sb([128, NTOK], F32, 'qn'); kn = C.sb([128, NTOK], F32, 'kn')
                Vt = C.sb([128, NCH, 128], F32, 'gVt'); Kt = C.sb([128, NCH, 128], F32, 'gKt'); oacc = C.sb([128, NCH, 128], F32, 'goacc'); obw = C.sb([128, NCH, 128], F32, 'gobw')
                with C.scope():
                    tmp = C.sb([128, NTOK], F32, 'gtmp'); tmp2 = C.sb([128, NTOK], F32, 'gtmp2')
                    pW = [C.ps([128, 512], F32, 'pW') for _ in range(2)]; pTf = C.ps([128, 512], F32, 'pTf')

                    def l2n(dst, dkey, row0, scale):
                        S.dma('sp', dst[:], gT[row0:row0 + 128, :], reads=[('fm_out', row0 // 128), ('fm_out', row0 // 128, 1)], writes=[dkey])
                        S.op('pool', lambda e: e.tensor_tensor(out=tmp[:], in0=dst[:], in1=dst[:], op=ALU.mult), reads=[dkey], writes=['gtmp'])
                        for i in range(NT5):
                            w = min(512, NTOK - i * 512)
                            S.op('pe', lambda e, i=i, w=w: e.matmul(pW[i % 2][:, 0:w], lhsT=ones[:], rhs=tmp[:, i * 512:i * 512 + w], start=True, stop=True),
                                 reads=['gtmp', 'onesf'], writes=[f'pW{i % 2}'])
                            S.op('act', lambda e, i=i, w=w: e.activation(out=tmp2[:, i * 512:i * 512 + w], in_=pW[i % 2][:, 0:w], func=AF.Sqrt, bias=eps6[:, 0:1]),
                                 reads=[f'pW{i % 2}', 'eps6'], writes=['gtmp2'])
                        S.op('dve', lambda e: e.reciprocal(out=tmp2[:], in_=tmp2[:]), reads=['gtmp2'], writes=['gtmp2'])
                        S.op('dve', lambda e: e.scalar_tensor_tensor(out=dst[:], in0=dst[:], scalar=scale, in1=tmp2[:], op0=ALU.mult, op1=ALU.mult), reads=[dkey, 'gtmp2'], writes=[dkey])

                    def to_tok(src, skey, dst, dkey):
                        for c0 in range(0, NCH, 4):
                            n = min(4, NCH - c0)
                            for c in range(c0, c0 + n):
                                S.op('pe', lambda e, c=c, c0=c0: e.transpose(out=pTf[:, (c - c0) * 128:(c - c0 + 1) * 128], in_=src[:, c * 128:(c + 1) * 128], identity=ident[:]),
                                     reads=[skey, idk], writes=['pTf'])
                            S.op('act', lambda e, c0=c0, n=n: e.copy(out=dst[:, c0:c0 + n, :], in_=pTf[:, 0:n * 128].rearrange("p (c t) -> p c t", t=128)), reads=['pTf'], writes=[dkey])
                    l2n(qn, 'qn', h * 128, 128.0 ** -0.5)
                    l2n(kn, 'kn', 1024 + h * 128, 1.0)
                    to_tok(kn, 'kn', Kt, 'gKt')
                    S.dma('sp', tmp[:], gT[2048 + h * 128:2048 + (h + 1) * 128, :], reads=[('fm_out', 16 + h), ('fm_out', 16 + h, 1), 'gtmp'], writes=['gtmp'])
                    to_tok(tmp, 'gtmp', Vt, 'gVt')
                with C.scope():
                    St = [C.sb([128, 128], F32, 'gS') for _ in range(2)]
                    for d in range(2):
                        S.op('pool', lambda e, d=d: e.memset(St[d][:], 0.0), writes=[f'gS{d}'])
                    pGr = C.ps([128, 512], F32, 'pGr'); pKQ = C.ps([128, 512], F32, 'pKQ'); pN = C.ps([128, 512], F32, 'pN'); pT = C.ps([128, 512], F32, 'pT')
                    pC = C.ps([128, 512], F32, 'pC'); pD = C.ps([128, 512], F32, 'pD'); pNT = C.ps([128, 512], F32, 'pNT')
                    Gs = C.sb([128, 256], F32, 'Gs'); Dm = C.sb([128, 512], F32, 'Dm')
                    NsA = [C.sb([128, 512], F32, 'Ns') for _ in range(2)]; QKsA = [C.sb([128, 256], F32, 'QKs') for _ in range(2)]; qeA = [C.sb([128, 256], F32, 'qe') for _ in range(2)]
                    NpA = [[C.sb([128, 512], F32, 'NpB') for _ in range(2)] for _ in range(2)]; TtA = [C.sb([128, 256], F32, 'Tt') for _ in range(2)]
                    ktaA = [C.sb([128, 256], F32, 'kta') for _ in range(2)]; bVA = [C.sb([128, 256], F32, 'bV') for _ in range(2)]
                    Zs = C.sb([128, 256], F32, 'Zs'); Vn = C.sb([128, 256], F32, 'Vn')

                    def st_prep(i, q):
                        Ns, QKs, qe, Tt, kta, bV = NsA[q], QKsA[q], qeA[q], TtA[q], ktaA[q], bVA[q]
                        for d in range(2):
                            c = order[d][i]; ts = slice(c * 128, (c + 1) * 128); x = d * 8 + h
                            gcol = gam[:, c, x:x + 1]
                            S.op('dve', lambda e, d=d, c=c, x=x: e.tensor_scalar(out=Gs[:, d * 128:(d + 1) * 128], in0=tri[d][:], scalar1=g[:, c, x:x + 1], scalar2=None, op0=ALU.mult),
                                 reads=['gg', trik[d]], writes=[f'Gs{d}'])
                            S.op('pe', lambda e, d=d: e.matmul(pGr[:, d * 128:(d + 1) * 128], lhsT=ones[:], rhs=Gs[:, d * 128:(d + 1) * 128], start=True, stop=True),
                                 reads=[f'Gs{d}', 'onesf'], writes=['pGr'])
                            S.op('pe', lambda e, d=d, ts=ts: e.matmul(pKQ[:, d * 256:d * 256 + 128], lhsT=kn[:, ts], rhs=kn[:, ts], start=True, stop=True), reads=['kn'], writes=['pKQ'])
                            S.op('pe', lambda e, d=d, ts=ts: e.matmul(pKQ[:, d * 256 + 128:d * 256 + 256], lhsT=kn[:, ts], rhs=qn[:, ts], start=True, stop=True), reads=['kn', 'qn'], writes=['pKQ'])
                            S.op('dve', lambda e, d=d, gcol=gcol: e.tensor_scalar(out=Dm[:, d * 256:d * 256 + 128], in0=pGr[:, d * 128:(d + 1) * 128], scalar1=gcol, scalar2=0.0, op0=ALU.subtract, op1=ALU.max),
                                 reads=['pGr', 'gam'], writes=[f'Dm{d}'])
                            S.op('dve', lambda e, d=d, gcol=gcol: e.tensor_scalar(out=Dm[:, d * 256 + 128:d * 256 + 256], in0=pGr[:, d * 128:(d + 1) * 128], scalar1=gcol, scalar2=0.0, op0=ALU.subtract, op1=ALU.min),
                                 reads=['pGr', 'gam', f'Dm{d}'], writes=[f'Dm{d}'])
                            S.op('act', lambda e, d=d: e.activation(out=Dm[:, d * 256:d * 256 + 128], in_=Dm[:, d * 256:d * 256 + 128], func=AF.Exp, scale=-1.0), reads=[f'Dm{d}'], writes=[f'Dm{d}'])
                            S.op('act', lambda e, d=d: e.activation(out=Dm[:, d * 256 + 128:d * 256 + 256], in_=Dm[:, d * 256 + 128:d * 256 + 256], func=AF.Exp), reads=[f'Dm{d}'], writes=[f'Dm{d}'])
                            S.op('act', lambda e, d=d: e.activation(out=qe[:, d * 128:(d + 1) * 128], in_=pGr[:, d * 128:(d + 1) * 128], func=AF.Exp), reads=['pGr'], writes=[f'qe{q}{d}'])
                            S.op('dve', lambda e, d=d, ts=ts: e.tensor_tensor(out=qe[:, d * 128:(d + 1) * 128], in0=qe[:, d * 128:(d + 1) * 128], in1=qn[:, ts], op=ALU.mult), reads=[f'qe{q}{d}', 'qn'], writes=[f'qe{q}{d}'])
                            S.op('dve', lambda e, d=d: e.tensor_tensor(out=Dm[:, d * 256:d * 256 + 256], in0=pKQ[:, d * 256:d * 256 + 256], in1=Dm[:, d * 256:d * 256 + 256], op=ALU.mult),
                                 reads=['pKQ', f'Dm{d}'], writes=[f'Dm{d}'])
                            S.op('dve', lambda e, d=d, c=c, x=x: e.scalar_tensor_tensor(out=Ns[:, d * 256:d * 256 + 128], in0=Dm[:, d * 256:d * 256 + 128], scalar=nbeta[:, c, x:x + 1], in1=mS[d][:],
                                                                                       op0=ALU.mult, op1=ALU.mult), reads=[f'Dm{d}', 'nbeta', mSk[d]], writes=[f'Ns{q}{d}'])
                            S.op('pool', lambda e, d=d: e.tensor_tensor(out=QKs[:, d * 128:(d + 1) * 128], in0=Dm[:, d * 256 + 128:d * 256 + 256], in1=mIT[d][:], op=ALU.mult),
                                 reads=[f'Dm{d}', mITk[d]], writes=[f'QKs{q}{d}'])
                            S.op('pe', lambda e, d=d: e.transpose(out=pNT[:, d * 128:(d + 1) * 128], in_=Ns[:, d * 256:d * 256 + 128], identity=ident[:]), reads=[f'Ns{q}{d}', idk], writes=['pNT'])
                            S.op('act', lambda e, d=d: e.copy(out=Ns[:, d * 256 + 128:d * 256 + 256], in_=pNT[:, d * 128:(d + 1) * 128]), reads=['pNT', f'Ns{q}{d}'], writes=[f'Ns{q}{d}'])
                            S.op('pool', lambda e, d=d: e.tensor_tensor(out=Tt[:, d * 128:(d + 1) * 128], in0=Ns[:, d * 256 + 128:d * 256 + 256], in1=ident[:], op=ALU.add),
                                 reads=[f'Ns{q}{d}', idk], writes=[f'Tt{q}{d}'])
                            S.op('act', lambda e, d=d, c=c, x=x: e.activation(out=kta[:, d * 128:(d + 1) * 128], in_=Kt[:, c, :], func=AF.Copy, scale=etail[:, c, x:x + 1]),
                                 reads=['gKt', 'etail'], writes=[f'kta{q}{d}'])
                            S.op('act', lambda e, d=d, c=c, x=x: e.activation(out=bV[:, d * 128:(d + 1) * 128], in_=Vt[:, c, :], func=AF.Copy, scale=beta[:, c, x:x + 1]),
                                 reads=['gVt', 'gbeta'], writes=[f'bV{q}{d}'])

                    def st_neumann(lv, q):
                        Ns, Tt = NsA[q], TtA[q]
                        if lv == 1:
                            prev = [(Ns[:, d * 256:d * 256 + 128], Ns[:, d * 256 + 128:d * 256 + 256], f'Ns{q}{d}') for d in range(2)]
                        else:
                            pb_ = NpA[q][(lv - 1) % 2]
                            prev = [(pb_[:, d * 256:d * 256 + 128], pb_[:, d * 256 + 128:d * 256 + 256], f'NpB{q}{(lv - 1) % 2}') for d in range(2)]
                        nb = NpA[q][lv % 2]; nk = f'NpB{q}{lv % 2}'
                        for d in range(2):
                            Nv, NTv, pk = prev[d]
                            S.op('pe', lambda e, d=d, Nv=Nv, NTv=NTv: e.matmul(pN[:, d * 256:d * 256 + 128], lhsT=NTv, rhs=Nv, start=True, stop=True), reads=[pk], writes=['pN'])
                            if lv < 6:
                                S.op('pe', lambda e, d=d, Nv=Nv, NTv=NTv: e.matmul(pN[:, d * 256 + 128:d * 256 + 256], lhsT=Nv, rhs=NTv, start=True, stop=True), reads=[pk], writes=['pN'])
                        S.op('act', lambda e, nb=nb: e.copy(out=nb[:], in_=pN[:]), reads=['pN'], writes=[nk])
                        for d in range(2):
                            S.op('pe', lambda e, d=d, nb=nb: e.matmul(pT[:, d * 128:(d + 1) * 128], lhsT=nb[:, d * 256:d * 256 + 128], rhs=Tt[:, d * 128:(d + 1) * 128], start=True, stop=True),
                                 reads=[nk, f'Tt{q}{d}'], writes=['pT'])
                        S.op('dve', lambda e: e.tensor_tensor(out=Tt[:], in0=Tt[:], in1=pT[:, 0:256], op=ALU.add), reads=['pT', f'Tt{q}0', f'Tt{q}1'], writes=[f'Tt{q}0', f'Tt{q}1'])

                    def st_chain(k, i, q):
                        QKs, qe, Tt, kta, bV = QKsA[q], qeA[q], TtA[q], ktaA[q], bVA[q]
                        cs_ = [order[0][i], order[1][i]]
                        if k == 0:
                            for d in range(2):
                                c = cs_[d]; ts = slice(c * 128, (c + 1) * 128); x = d * 8 + h
                                S.op('pe', lambda e, d=d, ts=ts: e.matmul(pC[:, d * 128:(d + 1) * 128], lhsT=kn[:, ts], rhs=St[d][:], start=True, stop=True), reads=['kn', f'gS{d}'], writes=['pC'])
                                S.op('dve', lambda e, d=d, c=c, x=x: e.scalar_tensor_tensor(out=Zs[:, d * 128:(d + 1) * 128], in0=pC[:, d * 128:(d + 1) * 128], scalar=nbeg[:, c, x:x + 1],
                                                                                           in1=bV[:, d * 128:(d + 1) * 128], op0=ALU.mult, op1=ALU.add), reads=['pC', 'nbeg', f'bV{q}{d}'], writes=[f'Zs{d}'])
                        elif k == 1:
                            for d in range(2):
                                S.op('pe', lambda e, d=d: e.matmul(pC[:, 256 + d * 128:256 + (d + 1) * 128], lhsT=Tt[:, d * 128:(d + 1) * 128], rhs=Zs[:, d * 128:(d + 1) * 128], start=True, stop=True),
                                     reads=[f'Tt{q}0', f'Tt{q}1', f'Zs{d}'], writes=['pC'])
                            S.op('act', lambda e: e.copy(out=Vn[:], in_=pC[:, 256:512]), reads=['pC'], writes=['Vn'])
                        elif k == 2:
                            for d in range(2):
                                S.op('pe', lambda e, d=d: e.matmul(pD[:, d * 128:(d + 1) * 128], lhsT=qe[:, d * 128:(d + 1) * 128], rhs=St[d][:], start=True, stop=False), reads=[f'qe{q}{d}', f'gS{d}'], writes=['pD'])
                                S.op('pe', lambda e, d=d: e.matmul(pD[:, d * 128:(d + 1) * 128], lhsT=QKs[:, d * 128:(d + 1) * 128], rhs=Vn[:, d * 128:(d + 1) * 128], start=False, stop=True),
                                     reads=[f'QKs{q}{d}', 'Vn'], writes=['pD'])
                            S.op('dve', lambda e, c=cs_[0]: e.tensor_copy(out=oacc[:, c, :], in_=pD[:, 0:128]), reads=['pD'], writes=[('goacc', cs_[0], 0)])
                            S.op('dve', lambda e, c=cs_[1]: e.tensor_copy(out=obw[:, c, :], in_=pD[:, 128:256]), reads=['pD'], writes=[('gobw', cs_[1])])
                        else:
                            for d in range(2):
                                S.op('pe', lambda e, d=d: e.matmul(pD[:, 256 + d * 128:256 + (d + 1) * 128], lhsT=kta[:, d * 128:(d + 1) * 128], rhs=Vn[:, d * 128:(d + 1) * 128], start=True, stop=True),
                                     reads=[f'kta{q}{d}', 'Vn'], writes=['pD'])
                            for d in range(2):
                                c = cs_[d]; x = d * 8 + h
                                S.op('dve', lambda e, d=d, c=c, x=x: e.scalar_tensor_tensor(out=St[d][:], in0=St[d][:], scalar=eC[:, c, x:x + 1], in1=pD[:, 256 + d * 128:256 + (d + 1) * 128],
                                                                                           op0=ALU.mult, op1=ALU.add), reads=[f'gS{d}', 'eC', 'pD'], writes=[f'gS{d}'])

                    st_prep(0, 0)
                    for lv in range(1, 7):
                        st_neumann(lv, 0)
                    for i in range(NCH):
                        q = i % 2
                        C.bg_step()
                        has_next = i + 1 < NCH
                        if has_next:
                            st_prep(i + 1, 1 - q)
                        for k in range(4):
                            if has_next:
                                st_neumann(k + 1, 1 - q)
                            st_chain(k, i, q)
                        if has_next:
                            st_neumann(5, 1 - q)
                            st_neumann(6, 1 - q)
                with C.scope():
                    zt = C.sb([128, NCH, 128], F32, 'gzt'); sq = C.sb([128, NCH, 128], F32, 'gsq'); red = C.sb([128, NCH], F32, 'gred')
                    S.dma('sp', zt[:], yz.rearrange("(c t) f -> t c f", t=128)[:, :, h * 128:(h + 1) * 128], reads=[('tm_out', t) for t in range(NCH)], writes=['gzt'])
                    ok_ = [('goacc', c, 0) for c in range(NCH)]; bk_ = [('gobw', c) for c in range(NCH)]
                    S.op('dve', lambda e: e.tensor_tensor(out=oacc[:], in0=oacc[:], in1=obw[:], op=ALU.add), reads=ok_ + bk_, writes=['goall'])
                    S.op('pool', lambda e: e.tensor_tensor(out=sq[:], in0=oacc[:], in1=oacc[:], op=ALU.mult), reads=['goall'], writes=['gsq'])
                    S.op('dve', lambda e: e.tensor_reduce(out=red[:], in_=sq[:], axis=AX.X, op=ALU.add), reads=['gsq'], writes=['gred'])
                    S.op('act', lambda e: e.activation(out=red[:], in_=red[:], func=AF.Sqrt, scale=1.0 / 128, bias=eps6[:, 0:1]), reads=['gred', 'eps6'], writes=['gred'])
                    S.op('dve', lambda e: e.reciprocal(out=red[:], in_=red[:]), reads=['gred'], writes=['gred'])
                    S.op('dve', lambda e: e.tensor_tensor(out=oacc[:], in0=oacc[:], in1=red[:].unsqueeze(2).broadcast_to([128, NCH, 128]), op=ALU.mult), reads=['goall', 'gred'], writes=['goall'])
                    S.op('dve', lambda e: e.tensor_tensor(out=oacc[:], in0=oacc[:], in1=onorm[:].unsqueeze(1).broadcast_to([128, NCH, 128]), op=ALU.mult), reads=['goall', 'onorm'], writes=['goall'])
                    S.op('act', lambda e: e.activation(out=zt[:], in_=zt[:], func=AF.Silu), reads=['gzt'], writes=['gzt'])
                    S.op('dve', lambda e: e.tensor_tensor(out=oacc[:], in0=oacc[:], in1=zt[:], op=ALU.mult), reads=['goall', 'gzt'], writes=['goall'])
                    S.dma('sp', mixo.rearrange("(c t) f -> t c f", t=128)[:, :, h * 128:(h + 1) * 128], oacc[:], reads=['goall'], writes=[('mixo', 'att', 0), 'mixo'])


def build_gdn_test(NCH, heads):
    C = Ctx(); S = C.S
    NTOK = NCH * 128
    gT = C.dram("gT", [3072, NTOK], kind="ExternalInput"); yz = C.dram("yz", [NTOK, 1056], kind="ExternalInput")
    prm = dict(dt_bias=C.dram("dt_bias", [16], kind="ExternalInput"), A_log=C.dram("A_log", [16], kind="ExternalInput"), out_norm=C.dram("out_norm", [128], kind="ExternalInput"))
    mixo = C.dram("mixo", [NTOK, 1024], kind="ExternalOutput")
    ident, idk = C.ident(); masks = make_masks(C)
    emit_gdn(C, gT, yz, mixo, prm, NCH, heads, ident, idk, masks)
    S.finish()
    return C


def build_full(NCH=34):
    C = Ctx(); S = C.S
    NT = NCH; NTOK = NT * 128; NBLK = 2 * NT + 32
    ein = lambda n, s: C.dram(n, s, kind="ExternalInput")
    xin = ein("xin", [NTOK, D]); cin = ein("cin", [128, 16])
    adaw = [ein(f"adaw{l}", [D, 6 * D]) for l in range(2)]; adab = [ein(f"adab{l}", [128, 96]) for l in range(2)]; nrm = [ein(f"nrm{l}", [128, 16]) for l in range(2)]
    ev_w_in = ein("ev_w_in", [D, 2688]); mucol = ein("mucol", [128, 15])
    aprm = dict(q_norm=ein("q_norm", [64]), k_norm=ein("k_norm", [64]), sink=ein("sink", [8]), rope=ein("rope", [(NCH - 2) * 128, 128]))
    rprm = dict(rw_cols=ein("rw_cols", [128, 28]), gnwb=ein("gnwb", [2, 512]), dec_up=ein("dec_up", [128, 512]), iclr_up=ein("iclr_up", [128, 512]), gate_up=ein("gate_up", [128, 512]))
    wout = [ein("ev_w_out", [D, D]), ein("od_w_out", [D, D])]
    mprm = [dict(w_grp=ein(f"w_grp{l}", [D, 4]), b_grp=ein(f"b_grp{l}", [4]), w_exp=ein(f"w_exp{l}", [D, 32]), b_exp=ein(f"b_exp{l}", [32])) for l in range(2)]
    wgu = [ein(f"wgu{l}", [32, 1024, 1024]) for l in range(2)]; wdn = [ein(f"wdn{l}", [32, 512, 1024]) for l in range(2)]
    od_w_in = ein("od_w_in", [D, 4128]); convcol = ein("convcol", [128, 120])
    gprm = dict(dt_bias=ein("dt_bias", [16]), A_log=ein("A_log", [16]), out_norm=ein("out_norm", [128]))
    out = C.dram("out", [(NCH - 2) * 128, D], kind="ExternalOutput")
    xs = C.dram("xs", [NTOK, D]); mixo = C.dram("mixo_s", [NTOK, D]); frows = C.dram("frows", [NTOK, D])
    xrows = C.dram("xrows", [NBLK * 128, D]); yrows = C.dram("yrows", [NBLK * 128, D])
    yatt = C.dram("yatt", [NTOK, 768]); ybT = C.dram("ybT", [1920, NTOK]); gT = C.dram("gT", [3072, NTOK]); yz = C.dram("yz", [NTOK, 1056])
    mucols3 = C.dram("mucols3", [128, 45])
    ident, idk = C.ident(); masks = make_masks(C)
    wgub = [C.dram(f"wgub{l}", [32 * 128, 8 * 1024], BF16) for l in range(2)]; wdnb = [C.dram(f"wdnb{l}", [32 * 128, 4 * 1024], BF16) for l in range(2)]
    wck = [[], []]

    def convert_weights(l):
        for e_ in range(32):
            C.bgq.append(lambda e_=e_: S.dma('pool', wgub[l][e_ * 128:(e_ + 1) * 128, :].rearrange("p (k n) -> p k n", k=8), wgu[l][e_].rearrange("(k p) n -> p k n", p=128),
                                             writes=[('wconv', l, 'g', e_)])); wck[l].append(('wconv', l, 'g', e_))
            C.bgq.append(lambda e_=e_: S.dma('pool', wdnb[l][e_ * 128:(e_ + 1) * 128, :].rearrange("p (k n) -> p k n", k=4), wdn[l][e_].rearrange("(k p) n -> p k n", p=128),
                                             writes=[('wconv', l, 'd', e_)])); wck[l].append(('wconv', l, 'd', e_))
    convert_weights(0)
    for t in range(NT):
        S.dma(['sp', 'act'][t % 2], xs[t * 128:(t + 1) * 128, :], xin[t * 128:(t + 1) * 128, :], writes=[('xs', t)])
    with C.scope():
        M = emit_mods(C, adaw[0], adab[0], cin, nrm[0])
        with C.scope():
            mu3 = C.sb([128, 45], F32, 'mu3')
            S.dma('sp', mu3[:, 0:15], mucol, writes=['mu3'])
            S.op('dve', lambda e: e.tensor_scalar(out=mu3[:, 15:30], in0=mu3[:, 0:15], scalar1=-1.0, scalar2=1.0, op0=ALU.mult, op1=ALU.add), reads=['mu3'], writes=['mu3'])
            S.op('dve', lambda e: e.tensor_scalar(out=mu3[:, 30:45], in0=mu3[:, 0:15], scalar1=0.5, scalar2=None, op0=ALU.mult), reads=['mu3'], writes=['mu3'])
            S.dma('sp', mucols3, mu3[:], reads=['mu3'], writes=['mucols3'])
        emit_pre(C, M, xs, ev_w_in, 2688, NT, ident, idk, 768, 15, ybT, 0, 768, yatt, 'shift', mucols3)
        emit_attn(C, yatt, mixo, aprm, NCH, ident, idk, masks)
        scr = rwkv_scratch(C, NCH, [0])
        scr1 = {k: {P: v[0] for P in range(4)} for k, v in scr.items()}
        emit_rwkv(C, ybT, mixo, rprm, NCH, [0, 1, 2, 3], ident, idk, masks, scr1)
        C.bg_flush()
        prm = dict(mprm[0]); prm['rowscr'] = rowscr_alloc(C, 'l0')
        with C.scope():
            R = route_alloc(C, NT, NBLK)
            emit_post(C, M, mixo, xs, frows, wout[0], prm, NT, ident, idk, R)
            emit_route(C, R, NT, NBLK, masks)
            emit_moe(C, M, R, frows, xs, xrows, yrows, wgub[0], wdnb[0], prm, NT, NBLK, ident, idk, wkeys=wck[0])
    with C.scope():
        M = emit_mods(C, adaw[1], adab[1], cin, nrm[1])
        convert_weights(1)
        emit_pre(C, M, xs, od_w_in, 4128, NT, ident, idk, 0, 24, gT, 3072, 4128, yz, 'conv', convcol)
        emit_gdn(C, gT, yz, mixo, gprm, NCH, list(range(8)), ident, idk, masks)
        C.bg_flush()
        prm = dict(mprm[1]); prm['rowscr'] = rowscr_alloc(C, 'l1')
        with C.scope():
            R = route_alloc(C, NT, NBLK)
            emit_post(C, M, mixo, xs, frows, wout[1], prm, NT, ident, idk, R)
            emit_route(C, R, NT, NBLK, masks)
            emit_moe(C, M, R, frows, xs, xrows, yrows, wgub[1], wdnb[1], prm, NT, NBLK, ident, idk, out_dram=out, out_tiles=list(range(2, NT)), wkeys=wck[1])
    S.finish()
    return C


def full_inputs(inp, b, NCH=34):
    nl = (NCH - 2) * 128
    f32 = lambda a: np.ascontiguousarray(np.asarray(a, np.float32))
    m = {}
    m['xin'] = f32(np.concatenate([inp['ctx'][b], inp['x'][b][:nl]], axis=0))
    for l in range(2):
        cin, adab, nrm = mods_inputs(inp['c'][b], inp['c_ctx'], inp['ada_b'][l], inp['norm_mix'][l], inp['norm_ffn'][l])
        m['cin'] = cin; m[f'adab{l}'] = adab; m[f'nrm{l}'] = nrm
        m[f'adaw{l}'] = inp['ada_w'][l]
        m[f'w_grp{l}'] = inp['moe_w_grp'][l]; m[f'b_grp{l}'] = inp['moe_b_grp'][l]; m[f'w_exp{l}'] = inp['moe_w_exp'][l]; m[f'b_exp{l}'] = inp['moe_b_exp'][l]
        m[f'wgu{l}'] = inp['moe_w_gate_up'][l]; m[f'wdn{l}'] = inp['moe_w_down'][l]
    m['ev_w_in'] = inp['ev_w_in'][0]; m['mucol'] = col_layout(inp['ev_mu'][0], 15)
    m['q_norm'] = inp['ev_q_norm'][0]; m['k_norm'] = inp['ev_k_norm'][0]; m['sink'] = inp['ev_sink'][0]; m['rope'] = rope_table_host(nl)
    m.update(rwkv_params_host(inp, 0))
    m['ev_w_out'] = inp['ev_w_out'][0]; m['od_w_out'] = inp['od_w_out'][0]; m['od_w_in'] = inp['od_w_in'][0]
    cc = np.zeros((128, 120), np.float32)
    for j in range(5):
        cc[:, j * 24:(j + 1) * 24] = col_layout(inp['od_conv'][0][j], 24)
    m['convcol'] = cc
    m['dt_bias'] = f32(inp['od_dt_bias'][0].reshape(16)); m['A_log'] = f32(inp['od_A_log'][0].reshape(16)); m['out_norm'] = inp['od_out_norm'][0]
    return {k: f32(v) for k, v in m.items()}


_shared_cache = {}


def kernel(**inp):
    inp = {k: np.asarray(v) for k, v in inp.items()}
    C = build_full(34)
    maps = [full_inputs(inp, b) for b in range(4)]
    for b in range(1, 4):
        for k in maps[0]:
            if k not in ('xin', 'cin', 'adab0', 'adab1') and maps[b][k].shape == maps[0][k].shape and k not in ('xin',):
                if np.array_equal(maps[b][k], maps[0][k]):
                    maps[b][k] = maps[0][k]
    in_maps = [maps[c % 4] for c in range(NCORES)]
    res = run_bass_kernel_spmd(C.nc, in_maps, core_ids=list(range(NCORES))).results
    return np.stack([np.asarray(res[b]['out'], np.float32) for b in range(4)], axis=0)
```

```python
import numpy as np
import concourse.bass as bass
import concourse.mybir as mybir
from concourse.bass_utils import run_bass_kernel_spmd
from contextlib import ExitStack

F32 = mybir.dt.float32
BF16 = mybir.dt.bfloat16
I32 = mybir.dt.int32
U32 = mybir.dt.uint32
AF = mybir.ActivationFunctionType
ALU = mybir.AluOpType
AX = mybir.AxisListType

NDMA = 72
NPOOLSEM = 32
D = 1024
NCORES = 8


class Sched:
    def __init__(self, nc):
        self.nc = nc
        self.eng = {'pe': nc.tensor, 'act': nc.scalar, 'dve': nc.vector, 'pool': nc.gpsimd, 'sp': nc.sync}
        self.sem = {e: nc.alloc_semaphore(f"s_{e}") for e in self.eng}
        self.cnt = {e: 0 for e in self.eng}
        self.known = {e: {} for e in self.eng}
        self.snap = {e: [None] for e in self.eng}
        self.dsem = [nc.alloc_semaphore(f"d_{i}") for i in range(NDMA)]
        self.dcnt = [0] * NDMA
        self.dsnap = [[None] for _ in range(NDMA)]
        self.drr = 0
        self.prr = 0
        self.bufs = {}
        self.nwaits = 0
        self.nins = 0
        self._uid = 0

    def _sem_of(self, tok):
        if tok[0] == 'e':
            return self.sem[tok[1]], tok[2]
        return self.dsem[tok[1]], 16 * tok[2]

    def _snap_of(self, tok):
        if tok[0] == 'e':
            return self.snap[tok[1]][tok[2]]
        return self.dsnap[tok[1]][tok[2]]

    def _wait(self, e, tok):
        key = (tok[0], tok[1])
        kn = self.known[e]
        if kn.get(key, 0) >= tok[2]:
            return
        s, v = self._sem_of(tok)
        self.eng[e].wait_ge(s, v)
        self.nwaits += 1
        kn[key] = tok[2]
        sn = self._snap_of(tok)
        if sn:
            for k2, v2 in sn.items():
                if kn.get(k2, 0) < v2:
                    kn[k2] = v2

    def _deps(self, e, reads, writes):
        toks = []
        for r in reads:
            b = self.bufs.get(r)
            if b and b['w']:
                toks.append(b['w'])
        for w in writes:
            b = self.bufs.get(w)
            if b:
                if b['w']:
                    toks.append(b['w'])
                for t in b['r'].values():
                    toks.append(t)
        if e == 'pe':
            toks = [t for t in toks if not (t[0] == 'e' and t[1] == 'pe')]
        return toks

    def _record(self, tok, reads, writes):
        for r in reads:
            b = self.bufs.setdefault(r, {'w': None, 'r': {}})
            b['r'][(tok[0], tok[1])] = tok
        for w in writes:
            self.bufs[w] = {'w': tok, 'r': {}}

    def op(self, e, fn, reads=(), writes=()):
        for t in self._deps(e, reads, writes):
            self._wait(e, t)
        ins = fn(self.eng[e])
        self.cnt[e] += 1
        self.nins += 1
        ins.then_inc(self.sem[e], 1)
        tok = ('e', e, self.cnt[e])
        self.snap[e].append(dict(self.known[e]))
        self._record(tok, reads, writes)
        return tok

    def dma(self, q, out, in_, reads=(), writes=(), indirect=None, **kw):
        if q == 'pool':
            j = NDMA - NPOOLSEM + self.prr
            self.prr = (self.prr + 1) % NPOOLSEM
        else:
            j = self.drr
            self.drr = (self.drr + 1) % (NDMA - NPOOLSEM)
        if self.dcnt[j] > 0:
            self._wait(q, ('d', j, self.dcnt[j]))
        for t in self._deps(q, reads, writes):
            self._wait(q, t)
        if indirect is None:
            ins = self.eng[q].dma_start(out=out, in_=in_, **kw)
        else:
            ins = self.eng[q].indirect_dma_start(out=out, in_=in_, **indirect)
        self.dcnt[j] += 1
        self.nins += 1
        ins.then_inc(self.dsem[j], 16)
        tok = ('d', j, self.dcnt[j])
        self.dsnap[j].append(dict(self.known[q]))
        self._record(tok, reads, writes)
        return tok

    def join(self):
        toks = [('e', e2, self.cnt[e2]) for e2 in self.eng if self.cnt[e2] > 0]
        toks += [('d', j, self.dcnt[j]) for j in range(NDMA) if self.dcnt[j] > 0]
        for e in self.eng:
            for t in toks:
                self._wait(e, t)

    def finish(self, e='sp'):
        for e2 in self.eng:
            if self.cnt[e2] > 0:
                self._wait(e, ('e', e2, self.cnt[e2]))
        for j in range(NDMA):
            if self.dcnt[j] > 0:
                self._wait(e, ('d', j, self.dcnt[j]))


class Ctx:
    def __init__(self):
        self.nc = bass.Bass("TRN2", target_bir_lowering=False)
        self.S = Sched(self.nc)
        self.n = 0
        self.stack = []
        self.bgq = []

    def sb(self, shape, dt=F32, name=None):
        self.n += 1
        nm = f"{name or 'sb'}_{self.n}"
        if self.stack:
            return self.stack[-1].enter_context(self.nc.sbuf_tensor(nm, list(shape), dt))
        return self.nc.alloc_sbuf_tensor(nm, list(shape), dt)

    def ps(self, shape, dt=F32, name=None):
        self.n += 1
        nm = f"{name or 'ps'}_{self.n}"
        if self.stack:
            return self.stack[-1].enter_context(self.nc.psum_tensor(nm, list(shape), dt))
        return self.nc.alloc_psum_tensor(nm, list(shape), dt)

    def scope(self):
        C = self

        class _Sc:
            def __enter__(s2):
                st = ExitStack(); st.__enter__(); C.stack.append(st); return st

            def __exit__(s2, *a):
                st = C.stack.pop()
                C.S.join()
                return st.__exit__(*a)
        return _Sc()

    def bg_step(self, n=1):
        for _ in range(n):
            if self.bgq:
                self.bgq.pop(0)()

    def bg_flush(self):
        while self.bgq:
            self.bgq.pop(0)()

    def uid(self, base):
        self.n += 1
        return f"{base}#{self.n}"

    def dram(self, name, shape, dt=F32, kind="Internal"):
        return self.nc.dram_tensor(name, list(shape), dt, kind=kind).ap()

    def ident(self, dt=F32):
        S = self.S
        t = self.sb([128, 128], F32, 'ident')
        k = f'ident{self.n}'
        S.op('pool', lambda e: e.memset(t[:], 0.0), writes=[k])
        S.op('pool', lambda e: e.affine_select(out=t[:], in_=t[:], pattern=[[-1, 128]], compare_op=ALU.not_equal,
                                               fill=1.0, base=0, channel_multiplier=1), reads=[k], writes=[k])
        if dt != F32:
            t2 = self.sb([128, 128], dt, 'identb')
            k2 = k + 'b'
            S.op('dve', lambda e: e.tensor_copy(out=t2[:], in_=t[:]), reads=[k], writes=[k2])
            return t2, k2
        return t, k


def emit_mods(C, adaw, adab, cin, nrm):
    S = C.S
    cT = C.sb([128, 16]); cS = C.sb([128, 16]); sg = C.sb([128, 16])
    bia = C.sb([128, 96]); nm = C.sb([128, 16])
    modT = C.sb([128, 96], name='modT')
    S.dma('sp', cT[:], cin, writes=['cT'])
    S.dma('sp', bia[:], adab, writes=['bia'])
    S.dma('sp', nm[:], nrm, writes=['nm'])
    S.op('act', lambda e: e.activation(out=sg[:], in_=cT[:], func=AF.Sigmoid), reads=['cT'], writes=['sg'])
    S.op('dve', lambda e: e.tensor_tensor(out=cS[:], in0=cT[:], in1=sg[:], op=ALU.mult), reads=['cT', 'sg'], writes=['cS'])
    pm = C.ps([128, 96], name='pm')
    wb = [C.sb([128, 8, 512], F32, 'adaw') for _ in range(2)]
    it = 0
    for j in range(6):
        for hf in range(2):
            w = wb[it % 2]; wk = f'adaw{it % 2}'
            for k in range(8):
                S.dma(['sp', 'act'][k % 2], w[:, k, :], adaw[k * 128:(k + 1) * 128, j * 1024 + hf * 512: j * 1024 + hf * 512 + 512],
                      writes=[(wk, k)])
            for mm in range(4):
                m = hf * 4 + mm
                col = (j * 8 + m) * 2
                for k in range(8):
                    S.op('pe', lambda e, k=k, mm=mm, col=col, w=w: e.matmul(pm[:, col:col + 2], lhsT=w[:, k, mm * 128:(mm + 1) * 128],
                                                                          rhs=cS[:, 2 * k:2 * k + 2], start=(k == 0), stop=(k == 7)),
                         reads=[(wk, k), 'cS'], writes=['pm'])
            it += 1
    S.op('dve', lambda e: e.tensor_tensor(out=modT[:], in0=pm[:], in1=bia[:], op=ALU.add), reads=['pm', 'bia'], writes=['modT'])
    A1 = C.sb([128, 8, 2], name='A1'); A2 = C.sb([128, 8, 2], name='A2')
    m3 = modT[:].rearrange("p (j m v) -> p j m v", j=6, m=8)
    S.op('dve', lambda e: e.scalar_tensor_tensor(out=A1[:], in0=m3[:, 1], scalar=1.0, in1=nm[:, 0:8].unsqueeze(2).broadcast_to([128, 8, 2]),
                                                 op0=ALU.add, op1=ALU.mult), reads=['modT', 'nm'], writes=['A1'])
    S.op('dve', lambda e: e.scalar_tensor_tensor(out=A2[:], in0=m3[:, 4], scalar=1.0, in1=nm[:, 8:16].unsqueeze(2).broadcast_to([128, 8, 2]),
                                                 op0=ALU.add, op1=ALU.mult), reads=['modT', 'nm'], writes=['A2'])
    return dict(modT=modT, m3=m3, A1=A1, A2=A2)


def emit_normT(C, xt, xkey, hT, hkey, col0, A, B, v, ident, idk, pT, pTk, tmp, rr):
    S = C.S
    junk, ss, xn = tmp['junk'], tmp['ss'], tmp['xn']
    S.op('act', lambda e: e.activation(out=junk[:], in_=xt, func=AF.Square, accum_out=ss[:, 0:1]), reads=[xkey], writes=['junk', 'ss'])
    S.op('dve', lambda e: e.tensor_scalar(out=ss[:, 1:2], in0=ss[:, 0:1], scalar1=1.0 / D, scalar2=1e-6, op0=ALU.mult, op1=ALU.add),
         reads=['ss'], writes=['ss'])
    S.op('act', lambda e: e.activation(out=ss[:, 2:3], in_=ss[:, 1:2], func=AF.Sqrt), reads=['ss'], writes=['ss'])
    S.op('dve', lambda e: e.reciprocal(out=ss[:, 3:4], in_=ss[:, 2:3]), reads=['ss'], writes=['ss'])
    S.op('dve', lambda e: e.tensor_scalar(out=xn[:], in0=xt, scalar1=ss[:, 3:4], scalar2=None, op0=ALU.mult),
         reads=[xkey, 'ss'], writes=['xn'])
    for k in range(8):
        S.op('pe', lambda e, k=k: e.transpose(out=pT[:, k * 128:(k + 1) * 128], in_=xn[:, k * 128:(k + 1) * 128], identity=ident[:]),
             reads=['xn', idk], writes=[pTk])
    t2 = tmp['t2']
    p3 = pT[:].rearrange("p (k t) -> p k t", k=8)
    S.op('dve', lambda e: e.tensor_tensor(out=t2[:], in0=p3, in1=A[:, :, v:v + 1].broadcast_to([128, 8, 128]), op=ALU.mult),
         reads=[pTk, 'A1', 'A2'], writes=['t2'])
    S.op('pool', lambda e: e.tensor_tensor(out=hT[:, :, col0:col0 + 128], in0=t2[:], in1=B[:, :, v:v + 1].broadcast_to([128, 8, 128]), op=ALU.add),
         reads=['t2', 'modT'], writes=[hkey])


def load_w_bf16(C, dst, dkey, src, rows, cols, queues=('pool',)):
    S = C.S
    nk = rows // 128
    i = 0
    for k in range(nk):
        c0 = 0
        while c0 < cols:
            cw = min(2048, cols - c0)
            S.dma(queues[i % len(queues)], dst[:, k, c0:c0 + cw], src[k * 128:(k + 1) * 128, c0:c0 + cw], writes=[(dkey, k)])
            c0 += cw
            i += 1


def build_pre(NT, NOUT):
    C = Ctx(); S = C.S; nc = C.nc
    xin = C.dram("xin", [NT * 128, D], kind="ExternalInput")
    cin = C.dram("cin", [128, 16], kind="ExternalInput")
    adaw = C.dram("adaw", [D, 6 * D], kind="ExternalInput")
    adab = C.dram("adab", [128, 96], kind="ExternalInput")
    nrm = C.dram("nrm", [128, 16], kind="ExternalInput")
    win = C.dram("win", [D, NOUT], kind="ExternalInput")
    y = C.dram("y", [NT * 128, NOUT], kind="ExternalOutput")
    modo = C.dram("modo", [128, 96], kind="ExternalOutput")
    ident, idk = C.ident()
    M = emit_mods(C, adaw, adab, cin, nrm)
    S.dma('sp', modo, M['modT'][:], reads=['modT'], writes=['modo'])
    wbf = C.sb([128, 8, NOUT], BF16, 'wbf')
    load_w_bf16(C, wbf, 'wbf', win, D, NOUT)
    hT = C.sb([128, 8, NT * 128], BF16, 'hT')
    tmp = dict(junk=C.sb([128, D]), ss=C.sb([128, 4]), xn=C.sb([128, D]), t2=C.sb([128, 8, 128]))
    xb = [C.sb([128, D], F32, 'xb') for _ in range(2)]
    pT = [C.ps([128, 1024], F32, 'pT') for _ in range(2)]
    B1 = M['m3'][:, 0]
    for t in range(NT):
        v = 1 if t == 0 else 0
        S.dma('sp', xb[t % 2][:], xin[t * 128:(t + 1) * 128, :], writes=[f'xb{t % 2}'])
        emit_normT(C, xb[t % 2][:], f'xb{t % 2}', hT, ('hT', t), t * 128, M['A1'], B1, v, ident, idk, pT[t % 2], f'pT{t % 2}', tmp, t)
    pY = [C.ps([128, 512], F32, 'pY') for _ in range(3)]
    yb = [C.sb([128, NOUT], F32, 'yb') for _ in range(2)]
    ncc = (NOUT + 511) // 512
    it = 0
    for t in range(NT):
        for c in range(ncc):
            cw = min(512, NOUT - c * 512)
            p = pY[it % 3]; pk = f'pY{it % 3}'
            for k in range(8):
                S.op('pe', lambda e, k=k, p=p, c=c, cw=cw, t=t: e.matmul(p[:, 0:cw], lhsT=hT[:, k, t * 128:(t + 1) * 128],
                                                                        rhs=wbf[:, k, c * 512:c * 512 + cw], start=(k == 0), stop=(k == 7)),
                     reads=[('hT', t), ('wbf', k)], writes=[pk])
            if it % 2 == 0:
                S.op('act', lambda e, p=p, c=c, cw=cw, t=t: e.copy(out=yb[t % 2][:, c * 512:c * 512 + cw], in_=p[:, 0:cw]),
                     reads=[pk], writes=[f'yb{t % 2}'])
            else:
                S.op('dve', lambda e, p=p, c=c, cw=cw, t=t: e.tensor_copy(out=yb[t % 2][:, c * 512:c * 512 + cw], in_=p[:, 0:cw]),
                     reads=[pk], writes=[f'yb{t % 2}'])
            it += 1
        S.dma(['sp', 'act'][t % 2], y[t * 128:(t + 1) * 128, :], yb[t % 2][:], reads=[f'yb{t % 2}'], writes=[('y', t)])
    S.finish()
    return C


def col_layout(vec, nchunk):
    return np.ascontiguousarray(np.asarray(vec, np.float32).reshape(nchunk, 128).T)


def mods_inputs(c_b, c_ctx, ada_b_l, norm_mix_l, norm_ffn_l):
    cin = np.empty((128, 8, 2), np.float32)
    cin[:, :, 0] = col_layout(c_b, 8)
    cin[:, :, 1] = col_layout(c_ctx, 8)
    adab = np.repeat(col_layout(ada_b_l, 48)[:, :, None], 2, axis=2).reshape(128, 96)
    nrm = np.concatenate([col_layout(norm_mix_l, 8), col_layout(norm_ffn_l, 8)], axis=1)
    return cin.reshape(128, 16), np.ascontiguousarray(adab), np.ascontiguousarray(nrm)


def tok_shard(x_lat_b, x_ctx_b, s):
    return np.ascontiguousarray(np.concatenate([x_ctx_b[128 * s:128 * (s + 1)], x_lat_b[2048 * s:2048 * (s + 1)]], axis=0))


_cache = {}


def run(name, builder, in_maps):
    if name not in _cache:
        _cache[name] = builder()
    C = _cache[name]
    res = run_bass_kernel_spmd(C.nc, in_maps, core_ids=list(range(NCORES)))
    return res.results


def make_masks(C):
    S = C.S
    out = {}
    for nm, pat, cm, cmp in [('lo_s', -1, 1, ALU.is_gt), ('up_s', 1, -1, ALU.is_gt), ('lo_i', -1, 1, ALU.is_ge), ('up_i', 1, -1, ALU.is_ge)]:
        t = C.sb([128, 128], F32, 'mask' + nm)
        S.op('pool', lambda e, t=t: e.memset(t[:], 1.0), writes=['mk' + nm])
        S.op('pool', lambda e, t=t, pat=pat, cm=cm, cmp=cmp: e.affine_select(out=t[:], in_=t[:], pattern=[[pat, 128]], compare_op=cmp,
                                                                             fill=0.0, base=0, channel_multiplier=cm),
             reads=['mk' + nm], writes=['mk' + nm])
        out[nm] = t
    return out


def chunk_order(NCH, d):
    if d == 0:
        return list(range(NCH))
    return [1, 0] + list(range(NCH - 1, 1, -1))


def emit_rwkv(C, ybT, mixo, prm, NCH, pairs, ident, idk, masks, scr):
    S = C.S
    NTOK = NCH * 128
    NT5 = (NTOK + 511) // 512
    with C.scope():
        cols = C.sb([128, 28], F32, 'rwcols')
        S.dma('sp', cols[:], prm['rw_cols'], writes=['rwcols'])
        bsel = C.sb([128, 2], F32, 'bsel'); bones = C.sb([128, 128], F32, 'bones')
        S.op('pool', lambda e: e.memset(bsel[:], 0.0), writes=['bsel'])
        S.op('pool', lambda e: e.memset(bsel[0:64, 0:1], 1.0), reads=['bsel'], writes=['bsel'])
        S.op('pool', lambda e: e.memset(bsel[64:128, 1:2], 1.0), reads=['bsel'], writes=['bsel'])
        S.op('pool', lambda e: e.memset(bones[:], 0.0), writes=['bones'])
        S.op('pool', lambda e: e.memset(bones[0:64, 0:64], 1.0), reads=['bones'], writes=['bones'])
        S.op('pool', lambda e: e.memset(bones[64:128, 64:128], 1.0), reads=['bones'], writes=['bones'])
        epsc = C.sb([128, 1], F32, 'epsc')
        S.op('pool', lambda e: e.memset(epsc[:], 1e-6), writes=['eps'])
        mX = []; mY = []
        for d in range(2):
            Ms, MsT, MiT = (masks['lo_s'], masks['up_s'], masks['up_i']) if d == 0 else (masks['up_s'], masks['lo_s'], masks['lo_i'])
            mx = C.sb([128, 512], BF16, 'mX'); my = C.sb([128, 128], BF16, 'mY')
            mkr = ['mklo_s', 'mkup_s', 'mklo_i', 'mkup_i']
            S.op('dve', lambda e, mx=mx, Ms=Ms: e.tensor_scalar(out=mx[:, 0:128], in0=Ms[:], scalar1=-1.0, scalar2=None, op0=ALU.mult), reads=mkr, writes=[f'mX{d}'])
            S.op('dve', lambda e, mx=mx, MsT=MsT: e.tensor_scalar(out=mx[:, 128:256], in0=MsT[:], scalar1=-1.0, scalar2=None, op0=ALU.mult), reads=mkr + [f'mX{d}'], writes=[f'mX{d}'])
            S.op('dve', lambda e, mx=mx, MsT=MsT: e.tensor_copy(out=mx[:, 256:384], in_=MsT[:]), reads=mkr + [f'mX{d}'], writes=[f'mX{d}'])
            S.op('dve', lambda e, mx=mx, MiT=MiT: e.tensor_copy(out=mx[:, 384:512], in_=MiT[:]), reads=mkr + [f'mX{d}'], writes=[f'mX{d}'])
            S.op('dve', lambda e, my=my, MiT=MiT: e.tensor_copy(out=my[:], in_=MiT[:]), reads=mkr, writes=[f'mY{d}'])
            mX.append(mx); mY.append(my)
        for P in pairs:
            with C.scope():
                kkT = C.sb([128, NTOK], F32, 'kkT'); aT = C.sb([128, NTOK], F32, 'aT')
                Lam = C.sb([128, NTOK], F32, 'Lam'); lam = C.sb([128, NTOK], F32, 'lam'); tmp = C.sb([128, NTOK], F32, 'tmp')
                ob = C.sb([128, NTOK], F32, 'ob')
                rmask = C.sb([128, NTOK], BF16, 'rmask')
                decup = C.sb([128, 512], F32, 'decup'); iclrup = C.sb([128, 512], F32, 'iclrup')
                S.dma('sp', decup[:], prm['dec_up'], writes=['decup'])
                S.dma('sp', iclrup[:], prm['iclr_up'], writes=['iclrup'])
                S.op('pool', lambda e: e.memset(rmask[:], 1.0), writes=['rmask'])
                S.op('pool', lambda e: e.memset(rmask[:].rearrange("p (c t) -> p c t", t=128)[:, :, 0:1], 0.0), reads=['rmask'], writes=['rmask'])
                pW = [C.ps([128, 512], F32, 'pW') for _ in range(2)]
                pTf = C.ps([128, 512], F32, 'pTf')
                pBn = C.ps([128, 512], F32, 'pBn')
                tokb = C.sb([128, NCH, 128], F32, 'tokb')
                bon = C.sb([128, NCH, 2], F32, 'bon')
                PC = C.sb([128, NCH], F32, 'PC')
                kcol = lambda j: cols[:, j:j + 1]
                c3 = lambda t_: t_[:].rearrange("p (c t) -> p c t", t=128)

                def transpose_store(src, skey, dst_dram):
                    for c0 in range(0, NCH, 4):
                        n = min(4, NCH - c0)
                        for c in range(c0, c0 + n):
                            S.op('pe', lambda e, c=c, c0=c0: e.transpose(out=pTf[:, (c - c0) * 128:(c - c0 + 1) * 128], in_=src[:, c * 128:(c + 1) * 128],
                                                                         identity=ident[:]), reads=[skey, idk], writes=['pTf'])
                        S.op('act', lambda e, c0=c0, n=n: e.copy(out=tokb[:, c0:c0 + n, :], in_=pTf[:, 0:n * 128].rearrange("p (c t) -> p c t", t=128)),
                             reads=['pTf'], writes=['tokb'])
                    S.dma('sp', dst_dram.rearrange("(c t) f -> t c f", t=128), tokb[:], reads=['tokb'], writes=[('scr', id(dst_dram))])

                S.dma('sp', tmp[:], ybT[1024 + P * 128:1024 + (P + 1) * 128, :], writes=['tmp'])
                transpose_store(tmp, 'tmp', scr['Vt'][P])
                S.dma('sp', ob[:], ybT[512 + P * 128:512 + (P + 1) * 128, :], writes=['ob'])
                S.op('dve', lambda e: e.tensor_scalar(out=kkT[:], in0=ob[:], scalar1=kcol(P), scalar2=None, op0=ALU.mult), reads=['ob', 'rwcols'], writes=['kkT'])
                S.op('pool', lambda e: e.tensor_tensor(out=tmp[:], in0=kkT[:], in1=kkT[:], op=ALU.mult), reads=['kkT'], writes=['tmp'])
                for i in range(NT5):
                    w = min(512, NTOK - i * 512)
                    S.op('pe', lambda e, i=i, w=w: e.matmul(pW[i % 2][:, 0:w], lhsT=bones[:], rhs=tmp[:, i * 512:i * 512 + w], start=True, stop=True),
                         reads=['tmp', 'bones'], writes=[f'pW{i % 2}'])
                    S.op('act', lambda e, i=i, w=w: e.activation(out=lam[:, i * 512:i * 512 + w], in_=pW[i % 2][:, 0:w], func=AF.Sqrt, bias=epsc[:, 0:1]),
                         reads=[f'pW{i % 2}', 'eps'], writes=['lam'])
                S.op('dve', lambda e: e.reciprocal(out=tmp[:], in_=lam[:]), reads=['lam'], writes=['tmp'])
                S.op('dve', lambda e: e.tensor_tensor(out=kkT[:], in0=kkT[:], in1=tmp[:], op=ALU.mult), reads=['kkT', 'tmp'], writes=['kkT'])
                S.dma('sp', tmp[:], ybT[P * 128:(P + 1) * 128, :], reads=['tmp'], writes=['tmp'])
                S.op('dve', lambda e: e.scalar_tensor_tensor(out=lam[:], in0=tmp[:], scalar=kcol(8 + P), in1=ob[:], op0=ALU.mult, op1=ALU.mult),
                     reads=['tmp', 'ob', 'rwcols'], writes=['lam'])
                for c in range(NCH):
                    S.op('pe', lambda e, c=c: e.matmul(pBn[:, 2 * c:2 * c + 2], lhsT=lam[:, c * 128:(c + 1) * 128], rhs=bsel[:], start=True, stop=True),
                         reads=['lam', 'bsel'], writes=['pBn'])
                S.op('dve', lambda e: e.tensor_copy(out=bon[:].rearrange("p c h -> p (c h)"), in_=pBn[:, 0:2 * NCH]), reads=['pBn'], writes=['bon'])
                S.dma('sp', scr['bon'][P], bon[:], reads=['bon'], writes=[('scr', 'bon', P)])
                for d in range(2):
                    S.dma('sp', ob[:], ybT[1664:1792, :], reads=['ob'], writes=['ob'])
                    S.dma('sp', tmp[:], ybT[1536:1664, :], reads=['tmp'], writes=['tmp'])
                    S.op('act', lambda e: e.activation(out=tmp[:], in_=tmp[:], func=AF.Tanh), reads=['tmp'], writes=['tmp'])
                    for i in range(NT5):
                        w = min(512, NTOK - i * 512)
                        S.op('pe', lambda e, i=i, w=w, d=d: e.matmul(pW[0][:, 0:w], lhsT=iclrup[d * 64:(d + 1) * 64, P * 128:(P + 1) * 128],
                                                                   rhs=ob[d * 64:(d + 1) * 64, i * 512:i * 512 + w], start=True, stop=True),
                             reads=['iclrup', 'ob'], writes=['pW0'])
                        S.op('act', lambda e, i=i, w=w, d=d: e.activation(out=aT[:, i * 512:i * 512 + w], in_=pW[0][:, 0:w], func=AF.Sigmoid,
                                                                        bias=kcol(20 + d * 4 + P)), reads=['pW0', 'rwcols'], writes=['aT'])
                        S.op('pe', lambda e, i=i, w=w, d=d: e.matmul(pW[1][:, 0:w], lhsT=decup[d * 64:(d + 1) * 64, P * 128:(P + 1) * 128],
                                                                   rhs=tmp[d * 64:(d + 1) * 64, i * 512:i * 512 + w], start=True, stop=True),
                             reads=['decup', 'tmp'], writes=['pW1'])
                        S.op('act', lambda e, i=i, w=w, d=d: e.activation(out=lam[:, i * 512:i * 512 + w], in_=pW[1][:, 0:w], func=AF.Sigmoid,
                                                                        bias=kcol(12 + d * 4 + P)), reads=['pW1', 'rwcols'], writes=['lam'])
                    S.op('dve', lambda e: e.tensor_scalar(out=lam[:], in0=lam[:], scalar1=-0.6065306597126334, scalar2=None, op0=ALU.mult), reads=['lam'], writes=['lam'])
                    S.op('dve', lambda e: e.tensor_tensor_scan(out=Lam[:], data0=rmask[:], data1=lam[:], initial=0.0, op0=ALU.mult, op1=ALU.add),
                         reads=['rmask', 'lam'], writes=['Lam'])
                    L3 = c3(Lam)
                    S.op('act', lambda e: e.activation(out=PC[:], in_=L3[:, :, 127], func=AF.Exp), reads=['Lam'], writes=['PC'])
                    S.dma('sp', scr['PC'][P][d], PC[:], reads=['PC'], writes=[('scr', 'PC', P, d)])
                    if d == 1:
                        S.op('dve', lambda e: e.tensor_tensor(out=tmp[:], in0=lam[:], in1=Lam[:], op=ALU.subtract), reads=['lam', 'Lam'], writes=['tmp'])
                        S.op('dve', lambda e: e.tensor_tensor(out=L3, in0=c3(tmp), in1=L3[:, :, 127:128].broadcast_to([128, NCH, 128]), op=ALU.add),
                             reads=['tmp', 'Lam'], writes=['Lam'])
                    PC3 = PC[:].unsqueeze(2).broadcast_to([128, NCH, 128])
                    S.op('dve', lambda e: e.tensor_tensor(out=tmp[:], in0=Lam[:], in1=lam[:], op=ALU.subtract), reads=['Lam', 'lam'], writes=['tmp'])
                    S.op('act', lambda e: e.activation(out=tmp[:], in_=tmp[:], func=AF.Exp), reads=['tmp'], writes=['tmp'])
                    S.op('dve', lambda e: e.tensor_tensor(out=tmp[:], in0=kkT[:], in1=tmp[:], op=ALU.mult), reads=['kkT', 'tmp'], writes=['tmp'])
                    S.dma('sp', scr['KQ'][P][d], tmp[:], reads=['tmp'], writes=[('scr', 'KQ', P, d)])
                    S.dma('sp', ob[:], ybT[P * 128:(P + 1) * 128, :], reads=['ob'], writes=['ob'])
                    S.op('act', lambda e: e.activation(out=lam[:], in_=Lam[:], func=AF.Exp), reads=['Lam', 'lam'], writes=['lam'])
                    S.op('dve', lambda e: e.tensor_tensor(out=ob[:], in0=ob[:], in1=lam[:], op=ALU.mult), reads=['ob', 'lam'], writes=['ob'])
                    S.dma('sp', scr['RQ'][P][d], ob[:], reads=['ob'], writes=[('scr', 'RQ', P, d)])
                    S.op('act', lambda e: e.activation(out=lam[:], in_=Lam[:], func=AF.Exp, scale=-1.0), reads=['Lam', 'lam'], writes=['lam'])
                    S.op('dve', lambda e: e.tensor_tensor(out=tmp[:], in0=kkT[:], in1=aT[:], op=ALU.mult), reads=['kkT', 'aT', 'tmp'], writes=['tmp'])
                    S.op('dve', lambda e: e.tensor_tensor(out=tmp[:], in0=tmp[:], in1=lam[:], op=ALU.mult), reads=['tmp', 'lam'], writes=['tmp'])
                    S.dma('sp', scr['BD'][P][d], tmp[:], reads=['tmp'], writes=[('scr', 'BD', P, d)])
                    S.op('dve', lambda e: e.tensor_tensor(out=c3(ob), in0=c3(tmp), in1=PC3, op=ALU.mult), reads=['tmp', 'PC', 'ob'], writes=['ob'])
                    transpose_store(ob, 'ob', scr['BT'][P][d])
                    S.dma('sp', ob[:], ybT[512 + P * 128:512 + (P + 1) * 128, :], reads=['ob'], writes=['ob'])
                    S.op('dve', lambda e: e.tensor_scalar(out=tmp[:], in0=aT[:], scalar1=-1.0, scalar2=kcol(4 + P), op0=ALU.add, op1=ALU.mult),
                         reads=['aT', 'rwcols', 'tmp'], writes=['tmp'])
                    S.op('dve', lambda e: e.scalar_tensor_tensor(out=tmp[:], in0=tmp[:], scalar=1.0, in1=ob[:], op0=ALU.add, op1=ALU.mult),
                         reads=['tmp', 'ob'], writes=['tmp'])
                    S.op('dve', lambda e: e.tensor_tensor(out=tmp[:], in0=tmp[:], in1=lam[:], op=ALU.mult), reads=['tmp', 'lam'], writes=['tmp'])
                    S.dma('sp', scr['KD'][P][d], tmp[:], reads=['tmp'], writes=[('scr', 'KD', P, d)])
                    S.op('dve', lambda e: e.tensor_tensor(out=c3(ob), in0=c3(tmp), in1=PC3, op=ALU.mult), reads=['tmp', 'PC', 'ob'], writes=['ob'])
                    transpose_store(ob, 'ob', scr['KT'][P][d])
            with C.scope():
                oacc = C.sb([128, NCH, 128], F32, 'oacc')
                Vt = C.sb([128, NCH, 128], F32, 'Vt')
                S.dma('sp', Vt[:], scr['Vt'][P].rearrange("(c t) f -> t c f", t=128), reads=[('scr', id(scr['Vt'][P]))], writes=['Vt'])
                for d in range(2):
                    with C.scope():
                        RQ, KQ, BD, KD = [C.sb([128, NTOK], F32, n) for n in ('RQ', 'KQ', 'BD', 'KD')]
                        for nm_, t_ in (('RQ', RQ), ('KQ', KQ), ('BD', BD), ('KD', KD)):
                            S.dma('act', t_[:], scr[nm_][P][d], reads=[('scr', nm_, P, d)], writes=[nm_])
                        BT = C.sb([128, NCH, 128], F32, 'BT'); KT = C.sb([128, NCH, 128], F32, 'KT')
                        S.dma('sp', BT[:], scr['BT'][P][d].rearrange("(c t) f -> t c f", t=128), reads=[('scr', id(scr['BT'][P][d]))], writes=['BT'])
                        S.dma('sp', KT[:], scr['KT'][P][d].rearrange("(c t) f -> t c f", t=128), reads=[('scr', id(scr['KT'][P][d]))], writes=['KT'])
                        PCs = C.sb([128, NCH], F32, 'PCs')
                        S.dma('sp', PCs[:], scr['PC'][P][d], reads=[('scr', 'PC', P, d)], writes=['PCs'])
                        H = C.sb([128, 64], F32, 'H'); Hb = H
                        S.op('pool', lambda e: e.memset(H[:], 0.0), writes=['H'])
                        pX = [C.ps([128, 512], F32, 'pX') for _ in range(2)]
                        pY = C.ps([128, 512], F32, 'pY'); pN = C.ps([128, 512], F32, 'pN'); pT = C.ps([128, 512], F32, 'pT')
                        pC = C.ps([128, 512], F32, 'pC'); pD = C.ps([128, 512], F32, 'pD')
                        XsA = [[C.sb([128, 512], F32, 'Xs') for _ in range(2)] for _ in range(2)]
                        YsA = [C.sb([128, 256], F32, 'Ys') for _ in range(2)]
                        NpA = [[C.sb([128, 512], F32, 'NpB') for _ in range(2)] for _ in range(2)]
                        TtA = [C.sb([128, 256], F32, 'Tt') for _ in range(2)]
                        Zs = C.sb([128, 128], F32, 'Zs'); Us = C.sb([128, 128], F32, 'Us')
                        corder = chunk_order(NCH, d)

                        def st_intra(c, q):
                            Xs, Ys, Tt = XsA[q], YsA[q], TtA[q]
                            ts = slice(c * 128, (c + 1) * 128)
                            for h in range(2):
                                pb = 64 * h
                                fm = lambda X, pb=pb: X[pb:pb + 64, ts]
                                for blk, (l_, r_) in enumerate([(KQ, BD), (BD, KQ), (KD, KQ), (BD, RQ)]):
                                    S.op('pe', lambda e, h=h, blk=blk, l_=l_, r_=r_, fm=fm: e.matmul(pX[h][:, blk * 128:(blk + 1) * 128], lhsT=fm(l_), rhs=fm(r_),
                                                                                                    start=True, stop=True),
                                         reads=['RQ', 'KQ', 'BD', 'KD'], writes=[f'pX{h}'])
                                S.op('pe', lambda e, h=h, fm=fm: e.matmul(pY[:, h * 128:(h + 1) * 128], lhsT=fm(KD), rhs=fm(RQ), start=True, stop=True),
                                     reads=['RQ', 'KD'], writes=['pY'])
                                S.op('dve', lambda e, h=h: e.tensor_tensor(out=Xs[h][:], in0=pX[h][:], in1=mX[d][:], op=ALU.mult),
                                     reads=[f'pX{h}', f'mX{d}'], writes=[f'Xs{q}{h}'])
                                S.op('pool', lambda e, h=h: e.tensor_tensor(out=Tt[:, h * 128:(h + 1) * 128], in0=Xs[h][:, 128:256], in1=ident[:], op=ALU.add),
                                     reads=[f'Xs{q}{h}', idk], writes=[f'Tt{q}{h}'])
                            S.op('dve', lambda e: e.tensor_tensor(out=Ys[:].rearrange("p (h t) -> p h t", h=2), in0=pY[:, 0:256].rearrange("p (h t) -> p h t", h=2),
                                                                  in1=mY[d][:].unsqueeze(1).broadcast_to([128, 2, 128]), op=ALU.mult),
                                 reads=['pY', f'mY{d}'], writes=[f'Ys{q}'])

                        def st_neumann(lv, q):
                            Xs, Tt = XsA[q], TtA[q]
                            if lv == 1:
                                prev = [(Xs[h][:, 0:128], Xs[h][:, 128:256], f'Xs{q}{h}') for h in range(2)]
                            else:
                                pb_ = NpA[q][(lv - 1) % 2]
                                prev = [(pb_[:, h * 256:h * 256 + 128], pb_[:, h * 256 + 128:h * 256 + 256], f'NpB{q}{(lv - 1) % 2}') for h in range(2)]
                            nb = NpA[q][lv % 2]; nk = f'NpB{q}{lv % 2}'
                            for h in range(2):
                                Nv, NTv, pk = prev[h]
                                S.op('pe', lambda e, h=h, Nv=Nv, NTv=NTv: e.matmul(pN[:, h * 256:h * 256 + 128], lhsT=NTv, rhs=Nv, start=True, stop=True),
                                     reads=[pk], writes=['pN'])
                                if lv < 6:
                                    S.op('pe', lambda e, h=h, Nv=Nv, NTv=NTv: e.matmul(pN[:, h * 256 + 128:h * 256 + 256], lhsT=Nv, rhs=NTv, start=True, stop=True),
                                         reads=[pk], writes=['pN'])
                            S.op('act', lambda e, nb=nb: e.copy(out=nb[:], in_=pN[:]), reads=['pN'], writes=[nk])
                            for h in range(2):
                                S.op('pe', lambda e, h=h, nb=nb: e.matmul(pT[:, h * 128:(h + 1) * 128], lhsT=nb[:, h * 256:h * 256 + 128], rhs=Tt[:, h * 128:(h + 1) * 128],
                                                                         start=True, stop=True), reads=[nk, f'Tt{q}{h}'], writes=['pT'])
                            S.op('dve', lambda e: e.tensor_tensor(out=Tt[:], in0=Tt[:], in1=pT[:, 0:256], op=ALU.add), reads=['pT', f'Tt{q}0', f'Tt{q}1'], writes=[f'Tt{q}0', f'Tt{q}1'])

                        def st_chain(k, c, q):
                            Xs, Ys, Tt = XsA[q], YsA[q], TtA[q]
                            ts = slice(c * 128, (c + 1) * 128)
                            if k == 0:
                                for h in range(2):
                                    pb = 64 * h
                                    S.op('pe', lambda e, h=h, pb=pb: e.matmul(pC[:, h * 64:(h + 1) * 64], lhsT=KQ[pb:pb + 64, ts], rhs=Hb[pb:pb + 64, :], start=True, stop=False),
                                         reads=['KQ', 'H'], writes=['pC'])
                                    S.op('pe', lambda e, h=h: e.matmul(pC[:, h * 64:(h + 1) * 64], lhsT=Xs[h][:, 256:384], rhs=Vt[:, c, h * 64:(h + 1) * 64], start=False, stop=True),
                                         reads=[f'Xs{q}{h}', 'Vt'], writes=['pC'])
                                S.op('act', lambda e: e.mul(out=Zs[:], in_=pC[:, 0:128], mul=-1.0), reads=['pC'], writes=['Zs'])
                            elif k == 1:
                                for h in range(2):
                                    S.op('pe', lambda e, h=h: e.matmul(pC[:, 128 + h * 64:128 + (h + 1) * 64], lhsT=Tt[:, h * 128:(h + 1) * 128], rhs=Zs[:, h * 64:(h + 1) * 64],
                                                                      start=True, stop=True), reads=[f'Tt{q}0', f'Tt{q}1', 'Zs'], writes=['pC'])
                                S.op('act', lambda e: e.copy(out=Us[:], in_=pC[:, 128:256]), reads=['pC'], writes=['Us'])
                            elif k == 2:
                                for h in range(2):
                                    pb = 64 * h
                                    S.op('pe', lambda e, h=h, pb=pb: e.matmul(pD[:, h * 64:(h + 1) * 64], lhsT=RQ[pb:pb + 64, ts], rhs=Hb[pb:pb + 64, :], start=True, stop=False),
                                         reads=['RQ', 'H'], writes=['pD'])
                                    S.op('pe', lambda e, h=h: e.matmul(pD[:, h * 64:(h + 1) * 64], lhsT=Xs[h][:, 384:512], rhs=Us[:, h * 64:(h + 1) * 64], start=False, stop=False),
                                         reads=[f'Xs{q}{h}', 'Us'], writes=['pD'])
                                    S.op('pe', lambda e, h=h: e.matmul(pD[:, h * 64:(h + 1) * 64], lhsT=Ys[:, h * 128:(h + 1) * 128], rhs=Vt[:, c, h * 64:(h + 1) * 64], start=False, stop=True),
                                         reads=[f'Ys{q}', 'Vt'], writes=['pD'])
                                if d == 0:
                                    S.op('dve', lambda e: e.tensor_copy(out=oacc[:, c, :], in_=pD[:, 0:128]), reads=['pD'], writes=['oacc'])
                                else:
                                    S.op('dve', lambda e: e.tensor_tensor(out=oacc[:, c, :], in0=oacc[:, c, :], in1=pD[:, 0:128], op=ALU.add), reads=['pD', 'oacc'], writes=['oacc'])
                            else:
                                for h in range(2):
                                    pb = 64 * h
                                    S.op('pe', lambda e, h=h, pb=pb: e.matmul(pD[pb:pb + 64, 128:192], lhsT=BT[:, c, pb:pb + 64], rhs=Us[:, h * 64:(h + 1) * 64], start=True, stop=False),
                                         reads=['BT', 'Us'], writes=['pD'])
                                    S.op('pe', lambda e, h=h, pb=pb: e.matmul(pD[pb:pb + 64, 128:192], lhsT=KT[:, c, pb:pb + 64], rhs=Vt[:, c, h * 64:(h + 1) * 64], start=False, stop=True),
                                         reads=['KT', 'Vt'], writes=['pD'])
                                S.op('dve', lambda e: e.scalar_tensor_tensor(out=H[:], in0=H[:], scalar=PCs[:, c:c + 1], in1=pD[:, 128:192], op0=ALU.mult, op1=ALU.add),
                                     reads=['H', 'PCs', 'pD'], writes=['H'])

                        st_intra(corder[0], 0)
                        for lv in range(1, 7):
                            st_neumann(lv, 0)
                        for i, c in enumerate(corder):
                            q = i % 2
                            C.bg_step()
                            nxt = corder[i + 1] if i + 1 < NCH else None
                            if nxt is not None:
                                st_intra(nxt, 1 - q)
                            for k in range(4):
                                if nxt is not None:
                                    st_neumann(k + 1, 1 - q)
                                st_chain(k, c, q)
                            if nxt is not None:
                                st_neumann(5, 1 - q)
                                st_neumann(6, 1 - q)
                with C.scope():
                    o4 = oacc[:].rearrange("p c (h v) -> p (c h) v", h=2)
                    red = C.sb([128, NCH * 2], F32, 'red'); cen = C.sb([128, NCH * 2, 64], F32, 'cen'); sq = C.sb([128, NCH * 2, 64], F32, 'sq')
                    bon = C.sb([128, NCH * 2], F32, 'bonl'); epsg = C.sb([128, 1], F32, 'epsg')
                    pG = [C.ps([128, 512], F32, 'pG') for _ in range(2)]
                    gnwb = C.sb([128, 2, 512], F32, 'gnwb')
                    S.dma('sp', gnwb[:], prm['gnwb'].partition_broadcast(128), writes=['gnwb'])
                    gateup = C.sb([128, 512], BF16, 'gateup')
                    S.dma('pool', gateup[:], prm['gate_up'], writes=['gateup'])
                    sgd = C.sb([128, NTOK], BF16, 'sgd'); tl = C.sb([128, NTOK], F32, 'tl')
                    S.dma('sp', tl[:], ybT[1792:1920, :], writes=['tl'])
                    S.op('act', lambda e: e.activation(out=sgd[:], in_=tl[:], func=AF.Sigmoid), reads=['tl'], writes=['sgd'])
                    S.op('pool', lambda e: e.memset(epsg[:], 64e-5), writes=['epsg'])
                    S.dma('sp', bon[:], scr['bon'][P].rearrange("p c h -> p (c h)"), reads=[('scr', 'bon', P)], writes=['bonl'])
                    bc = lambda t_: t_[:].unsqueeze(2).broadcast_to([128, NCH * 2, 64])
                    S.op('dve', lambda e: e.tensor_reduce(out=red[:], in_=o4, axis=AX.X, op=ALU.add), reads=['oacc'], writes=['red'])
                    S.op('dve', lambda e: e.tensor_scalar(out=red[:], in0=red[:], scalar1=1.0 / 64, scalar2=None, op0=ALU.mult), reads=['red'], writes=['red'])
                    S.op('dve', lambda e: e.tensor_tensor(out=cen[:], in0=o4, in1=bc(red), op=ALU.subtract), reads=['oacc', 'red'], writes=['cen'])
                    S.op('pool', lambda e: e.tensor_tensor(out=sq[:], in0=cen[:], in1=cen[:], op=ALU.mult), reads=['cen'], writes=['sq'])
                    S.op('dve', lambda e: e.tensor_reduce(out=red[:], in_=sq[:], axis=AX.X, op=ALU.add), reads=['sq', 'red'], writes=['red'])
                    S.op('act', lambda e: e.activation(out=red[:], in_=red[:], func=AF.Sqrt, scale=1.0 / 64, bias=epsg[:, 0:1]), reads=['red', 'epsg'], writes=['red'])
                    S.op('dve', lambda e: e.reciprocal(out=red[:], in_=red[:]), reads=['red'], writes=['red'])
                    S.op('dve', lambda e: e.tensor_tensor(out=cen[:], in0=cen[:], in1=bc(red), op=ALU.mult), reads=['cen', 'red'], writes=['cen'])
                    c4 = cen[:].rearrange("p (c h) v -> p c (h v)", h=2)
                    gw = gnwb[:, 0, P * 128:(P + 1) * 128].unsqueeze(1).broadcast_to([128, NCH, 128])
                    gb = gnwb[:, 1, P * 128:(P + 1) * 128].unsqueeze(1).broadcast_to([128, NCH, 128])
                    S.op('dve', lambda e: e.tensor_tensor(out=c4, in0=c4, in1=gw, op=ALU.mult), reads=['cen', 'gnwb'], writes=['cen'])
                    S.op('dve', lambda e: e.tensor_tensor(out=c4, in0=c4, in1=gb, op=ALU.add), reads=['cen', 'gnwb'], writes=['cen'])
                    S.op('dve', lambda e: e.tensor_tensor(out=sq[:], in0=Vt[:].rearrange("p c (h v) -> p (c h) v", h=2), in1=bc(bon), op=ALU.mult),
                         reads=['Vt', 'bonl', 'sq'], writes=['sq'])
                    S.op('dve', lambda e: e.tensor_tensor(out=cen[:], in0=cen[:], in1=sq[:], op=ALU.add), reads=['cen', 'sq'], writes=['cen'])
                    for g0 in range(0, NCH, 4):
                        n = min(4, NCH - g0)
                        pg = pG[(g0 // 4) % 2]; pgk = f'pG{(g0 // 4) % 2}'
                        for c in range(g0, g0 + n):
                            S.op('pe', lambda e, c=c, g0=g0, pg=pg: e.matmul(pg[:, (c - g0) * 128:(c - g0 + 1) * 128], lhsT=sgd[:, c * 128:(c + 1) * 128],
                                                                            rhs=gateup[:, P * 128:(P + 1) * 128], start=True, stop=True),
                                 reads=['sgd', 'gateup'], writes=[pgk])
                        S.op('dve', lambda e, g0=g0, n=n, pg=pg: e.tensor_tensor(out=c4[:, g0:g0 + n, :], in0=c4[:, g0:g0 + n, :],
                                                                                in1=pg[:, 0:n * 128].rearrange("p (c f) -> p c f", f=128), op=ALU.mult),
                             reads=[pgk, 'cen'], writes=['cen'])
                    S.dma('sp', mixo.rearrange("(c t) f -> t c f", t=128)[:, :, 512 + P * 128:512 + (P + 1) * 128], c4, reads=['cen'], writes=[('mixo', 'rw', P)])


def rwkv_scratch(C, NCH, pairs):
    NTOK = NCH * 128
    scr = {k: {} for k in ('Vt', 'bon', 'PC', 'RQ', 'KQ', 'BD', 'KD', 'BT', 'KT')}
    for P in pairs:
        scr['Vt'][P] = C.dram(f"rw_Vt{P}", [NTOK, 128], F32)
        scr['bon'][P] = C.dram(f"rw_bon{P}", [128, NCH, 2], F32)
        for k in ('PC', 'RQ', 'KQ', 'BD', 'KD', 'BT', 'KT'):
            scr[k][P] = {}
        for d in range(2):
            scr['PC'][P][d] = C.dram(f"rw_PC{P}{d}", [128, NCH], F32)
            for k in ('RQ', 'KQ', 'BD', 'KD'):
                scr[k][P][d] = C.dram(f"rw_{k}{P}{d}", [128, NTOK], F32)
            for k in ('BT', 'KT'):
                scr[k][P][d] = C.dram(f"rw_{k}{P}{d}", [NTOK, 128], F32)
    return scr


def rwkv_params_host(inp, j=0):
    cols = np.zeros((128, 28), np.float32)
    cols[:, 0:4] = col_layout(inp['ev_k_k'][j].reshape(512), 4)
    cols[:, 4:8] = col_layout(inp['ev_k_a'][j].reshape(512), 4)
    cols[:, 8:12] = col_layout(inp['ev_r_k'][j].reshape(512), 4)
    for d in range(2):
        cols[:, 12 + d * 4:16 + d * 4] = col_layout(inp['ev_dec0'][j][d], 4)
        cols[:, 20 + d * 4:24 + d * 4] = col_layout(inp['ev_iclr0'][j][d], 4)
    gnwb = np.stack([inp['ev_gn_w'][j].reshape(512), inp['ev_gn_b'][j].reshape(512)]).astype(np.float32)
    return dict(rw_cols=cols, gnwb=gnwb, dec_up=np.ascontiguousarray(inp['ev_dec_up'][j].reshape(128, 512)),
                iclr_up=np.ascontiguousarray(inp['ev_iclr_up'][j].reshape(128, 512)), gate_up=np.ascontiguousarray(inp['ev_gate_up'][j]))


def build_rwkv_test(NCH, pairs):
    C = Ctx(); S = C.S
    NTOK = NCH * 128
    ybT = C.dram("ybT", [1920, NTOK], kind="ExternalInput")
    prm = dict(rw_cols=C.dram("rw_cols", [128, 28], kind="ExternalInput"), gnwb=C.dram("gnwb", [2, 512], kind="ExternalInput"),
               dec_up=C.dram("dec_up", [128, 512], kind="ExternalInput"), iclr_up=C.dram("iclr_up", [128, 512], kind="ExternalInput"),
               gate_up=C.dram("gate_up", [128, 512], kind="ExternalInput"))
    mixo = C.dram("mixo", [NTOK, 1024], kind="ExternalOutput")
    ident, idk = C.ident()
    masks = make_masks(C)
    scr = rwkv_scratch(C, NCH, pairs)
    emit_rwkv(C, ybT, mixo, prm, NCH, pairs, ident, idk, masks, scr)
    S.finish()
    return C


def emit_attn(C, yatt, mixo, prm, NCH, ident, idk, masks):
    S = C.S
    NTOK = NCH * 128
    with C.scope():
        gq = C.sb([128, 64], F32, 'gq'); gk = C.sb([128, 64], F32, 'gk'); esk = C.sb([128, 8], F32, 'esk')
        S.dma('sp', gq[:], prm['q_norm'].partition_broadcast(128), writes=['gq'])
        S.dma('sp', gk[:], prm['k_norm'].partition_broadcast(128), writes=['gk'])
        S.dma('sp', esk[:], prm['sink'].partition_broadcast(128), writes=['esk'])
        S.op('act', lambda e: e.activation(out=esk[:], in_=esk[:], func=AF.Exp), reads=['esk'], writes=['esk'])
        epsa = C.sb([128, 1], F32, 'epsa')
        S.op('pool', lambda e: e.memset(epsa[:], 1e-6), writes=['epsa'])
        qT = C.sb([64, 8, NTOK], BF16, 'qT'); kT = C.sb([64, 2, NTOK], BF16, 'kTa')
        Va = C.sb([128, NCH, 2, 65], BF16, 'Va')
        S.op('pool', lambda e: e.memset(Va[:], 1.0), writes=['Va'])
        with C.scope():
            yb = [C.sb([128, 768], F32, 'ya') for _ in range(2)]
            rc = [C.sb([128, 128], F32, 'rc') for _ in range(2)]
            sq = C.sb([128, 640], F32, 'sqa'); ss = C.sb([128, 10], F32, 'ssa'); xr = C.sb([128, 640], F32, 'xr'); t2 = C.sb([128, 640], F32, 't2a')
            pTq = [C.ps([64, 1024], F32, 'pTq') for _ in range(2)]; pTk = [C.ps([64, 256], F32, 'pTk') for _ in range(2)]
            for t in range(NCH):
                y = yb[t % 2]; yk = f'ya{t % 2}'
                S.dma('sp', y[:], yatt[t * 128:(t + 1) * 128, :], writes=[yk])
                x3 = y[:, 0:640].rearrange("p (h f) -> p h f", f=64)
                S.op('pool', lambda e, y=y: e.tensor_tensor(out=sq[:], in0=y[:, 0:640], in1=y[:, 0:640], op=ALU.mult), reads=[yk], writes=['sqa'])
                S.op('dve', lambda e: e.tensor_reduce(out=ss[:], in_=sq[:].rearrange("p (h f) -> p h f", f=64), axis=AX.X, op=ALU.add), reads=['sqa'], writes=['ssa'])
                S.op('act', lambda e: e.activation(out=ss[:], in_=ss[:], func=AF.Sqrt, scale=1.0 / 64, bias=epsa[:, 0:1]), reads=['ssa', 'epsa'], writes=['ssa'])
                S.op('dve', lambda e: e.reciprocal(out=ss[:], in_=ss[:]), reads=['ssa'], writes=['ssa'])
                x3r = xr[:].rearrange("p (h f) -> p h f", f=64)
                S.op('dve', lambda e, x3=x3: e.tensor_tensor(out=x3r, in0=x3, in1=ss[:].unsqueeze(2).broadcast_to([128, 10, 64]), op=ALU.mult), reads=[yk, 'ssa'], writes=['xr'])
                S.op('dve', lambda e: e.tensor_tensor(out=x3r[:, 0:8], in0=x3r[:, 0:8], in1=gq[:].unsqueeze(1).broadcast_to([128, 8, 64]), op=ALU.mult), reads=['xr', 'gq'], writes=['xr'])
                S.op('dve', lambda e: e.tensor_tensor(out=x3r[:, 8:10], in0=x3r[:, 8:10], in1=gk[:].unsqueeze(1).broadcast_to([128, 2, 64]), op=ALU.mult), reads=['xr', 'gk'], writes=['xr'])
                src = xr
                if t >= 2:
                    r = rc[t % 2]; rk = f'rc{t % 2}'
                    S.dma('act', r[:], prm['rope'][(t - 2) * 128:(t - 1) * 128, :], writes=[rk])
                    S.op('dve', lambda e, r=r: e.tensor_tensor(out=t2[:].rearrange("p (h f) -> p h f", f=64), in0=x3r,
                                                               in1=r[:, 0:64].unsqueeze(1).broadcast_to([128, 10, 64]), op=ALU.mult), reads=['xr', rk], writes=['t2a'])
                    x5 = xr[:].rearrange("p (h a j m) -> p (h a) j m", a=2, j=2, m=16)
                    s5 = r[:, 64:128].rearrange("p (a j m) -> p a j m", a=2, j=2)
                    sqv = sq[:].rearrange("p (h a j m) -> p (h a) j m", a=2, j=2, m=16)
                    for j in range(2):
                        sj = s5[:, :, j, :].unsqueeze(1).broadcast_to([128, 10, 2, 16]).rearrange("p h a m -> p (h a) m") if False else None
                        for a in range(2):
                            S.op('pool', lambda e, j=j, a=a, r=r: e.tensor_tensor(
                                out=sq[:].rearrange("p (h a j m) -> p h a j m", a=2, j=2, m=16)[:, :, a, j, :],
                                in0=xr[:].rearrange("p (h a j m) -> p h a j m", a=2, j=2, m=16)[:, :, a, 1 - j, :],
                                in1=r[:, 64:128].rearrange("p (a j m) -> p a j m", a=2, j=2)[:, a, j, :].unsqueeze(1).broadcast_to([128, 10, 16]), op=ALU.mult),
                                 reads=['xr', rk], writes=['sqa'])
                    S.op('dve', lambda e: e.tensor_tensor(out=t2[:], in0=t2[:], in1=sq[:], op=ALU.add), reads=['t2a', 'sqa'], writes=['t2a'])
                    src = t2
                sk = 't2a' if t >= 2 else 'xr'
                pq = pTq[t % 2]; pk = pTk[t % 2]
                for h in range(8):
                    S.op('pe', lambda e, h=h, src=src, pq=pq: e.transpose(out=pq[:, h * 128:(h + 1) * 128], in_=src[:, h * 64:(h + 1) * 64], identity=ident[:]),
                         reads=[sk, idk], writes=[f'pTq{t % 2}'])
                for h in range(2):
                    S.op('pe', lambda e, h=h, src=src, pk=pk: e.transpose(out=pk[:, h * 128:(h + 1) * 128], in_=src[:, 512 + h * 64:512 + (h + 1) * 64], identity=ident[:]),
                         reads=[sk, idk], writes=[f'pTk{t % 2}'])
                S.op('act', lambda e, pq=pq, t=t: e.mul(out=qT[:, :, t * 128:(t + 1) * 128], in_=pq[:].rearrange("p (h t) -> p h t", t=128), mul=0.125),
                     reads=[f'pTq{t % 2}'], writes=[('qT', t)])
                S.op('act', lambda e, pk=pk, t=t: e.copy(out=kT[:, :, t * 128:(t + 1) * 128], in_=pk[:].rearrange("p (h t) -> p h t", t=128)),
                     reads=[f'pTk{t % 2}'], writes=[('kTa', t)])
                S.op('dve', lambda e, y=y, t=t: e.tensor_copy(out=Va[:, t, :, 0:64], in_=y[:, 640:768].rearrange("p (h f) -> p h f", f=64)), reads=[yk, 'Va'], writes=[('Va', t)])
        with C.scope():
            pS = [C.ps([128, 512], F32, 'pS') for _ in range(3)]
            pO = [C.ps([128, 260], F32, 'pO') for _ in range(2)]
            Pt = [C.sb([128, 5, 512], BF16, 'Pt') for _ in range(2)]
            den = C.sb([128, 4], F32, 'den'); ob = [C.sb([128, 512], F32, 'oba') for _ in range(2)]
            it = 0
            for tq in range(NCH):
                if tq < 2:
                    keys = [(0, None), (1, None)]
                else:
                    keys = [(0, None), (1, None)]
                    if tq - 1 >= 2:
                        keys.append((tq - 1, 'lo_i'))
                    keys.append((tq, None))
                    if tq + 1 < NCH:
                        keys.append((tq + 1, 'up_i'))
                o_t = ob[tq % 2]; ok_ = f'oba{tq % 2}'
                for g in range(2):
                    P_ = Pt[it % 2]; Pk = f'Pt{it % 2}'
                    for i, (tk, mk) in enumerate(keys):
                        ps = pS[i % 3]; psk = f'pS{i % 3}'
                        S.op('pe', lambda e, ps=ps, tk=tk, g=g, tq=tq: e.matmul(ps[:], lhsT=kT[:, g, tk * 128:(tk + 1) * 128], rhs=qT[:, 4 * g:4 * g + 4, tq * 128:(tq + 1) * 128],
                                                                             start=True, stop=True), reads=[('kTa', tk), ('qT', tq)], writes=[psk])
                        S.op('act', lambda e, ps=ps, i=i, P_=P_: e.activation(out=P_[:, i, :], in_=ps[:], func=AF.Exp), reads=[psk], writes=[(Pk, i)])
                        if mk:
                            S.op('pool', lambda e, i=i, P_=P_, mk=mk: e.tensor_tensor(out=P_[:, i, :].rearrange("p (h q) -> p h q", h=4), in0=P_[:, i, :].rearrange("p (h q) -> p h q", h=4),
                                                                                      in1=masks[mk][:].unsqueeze(1).broadcast_to([128, 4, 128]), op=ALU.mult),
                                 reads=[(Pk, i), 'mk' + mk], writes=[(Pk, i)])
                    po = pO[it % 2]; pok = f'pO{it % 2}'
                    for h in range(4):
                        for i, (tk, mk) in enumerate(keys):
                            S.op('pe', lambda e, h=h, i=i, tk=tk, po=po, P_=P_, g=g: e.matmul(po[:, h * 65:(h + 1) * 65], lhsT=P_[:, i, h * 128:(h + 1) * 128], rhs=Va[:, tk, g, :],
                                                                                            start=(i == 0), stop=(i == len(keys) - 1)),
                                 reads=[(Pk, i), ('Va', tk)], writes=[pok])
                    po3 = po[:].rearrange("p (h f) -> p h f", f=65)
                    S.op('dve', lambda e, po3=po3, g=g: e.tensor_tensor(out=den[:], in0=po3[:, :, 64], in1=esk[:, 4 * g:4 * g + 4], op=ALU.add), reads=[pok, 'esk'], writes=['den'])
                    S.op('dve', lambda e: e.reciprocal(out=den[:], in_=den[:]), reads=['den'], writes=['den'])
                    S.op('dve', lambda e, po3=po3, g=g, o_t=o_t: e.tensor_tensor(out=o_t[:, g * 256:(g + 1) * 256].rearrange("p (h f) -> p h f", f=64), in0=po3[:, :, 0:64],
                                                                               in1=den[:].unsqueeze(2).broadcast_to([128, 4, 64]), op=ALU.mult),
                         reads=[pok, 'den'], writes=[ok_])
                    it += 1
                S.dma('sp', mixo[tq * 128:(tq + 1) * 128, 0:512], o_t[:], reads=[ok_], writes=[('mixo', 'att', tq)])


def rope_table_host(nlat):
    pos = np.arange(nlat)
    row = (pos // 64).astype(np.float32); col = (pos % 64).astype(np.float32)
    inv = (10000.0 ** (-np.arange(0, 32, 2, dtype=np.float32) / 32)).astype(np.float32)
    ar = row[:, None] * inv[None, :]; ac = col[:, None] * inv[None, :]
    cr, sr, cc, sc = np.cos(ar), np.sin(ar), np.cos(ac), np.sin(ac)
    return np.ascontiguousarray(np.concatenate([cr, cr, cc, cc, -sr, sr, -sc, sc], axis=1).astype(np.float32))


def build_attn_test(NCH):
    C = Ctx(); S = C.S
    NTOK = NCH * 128
    yatt = C.dram("yatt", [NTOK, 768], kind="ExternalInput")
    prm = dict(q_norm=C.dram("q_norm", [64], kind="ExternalInput"), k_norm=C.dram("k_norm", [64], kind="ExternalInput"),
               sink=C.dram("sink", [8], kind="ExternalInput"), rope=C.dram("rope", [(NCH - 2) * 128, 128], kind="ExternalInput"))
    mixo = C.dram("mixo", [NTOK, 1024], kind="ExternalOutput")
    ident, idk = C.ident()
    masks = make_masks(C)
    emit_attn(C, yatt, mixo, prm, NCH, ident, idk, masks)
    S.finish()
    return C


def emit_rowbcast(C, col_ap, col_key, dst, dkey, scratch):
    S = C.S
    S.dma('sp', scratch.rearrange("(m p) -> p m", p=128), col_ap, reads=[col_key], writes=[('rowscr', id(scratch))], allow_slow_non_contiguous=True)
    S.dma('sp', dst, scratch.partition_broadcast(128), reads=[('rowscr', id(scratch))], writes=[dkey])


def emit_post(C, M, mixo, xs, frows, wout, prm, NT, ident, idk, R):
    S = C.S
    with C.scope():
        wbf = C.sb([128, 8, 1024], BF16, 'woutbf')
        load_w_bf16(C, wbf, 'woutbf', wout, D, 1024)
        wr = C.sb([128, 8, 36], F32, 'wr')
        S.dma('sp', wr[:, :, 0:4], prm['w_grp'].rearrange("(k p) g -> p k g", p=128), writes=['wr'])
        S.dma('sp', wr[:, :, 4:36], prm['w_exp'].rearrange("(k p) g -> p k g", p=128), reads=['wr'], writes=['wr'])
        brow = C.sb([128, 36], F32, 'brow')
        S.dma('sp', brow[:, 0:4], prm['b_grp'].partition_broadcast(128), writes=['brow'])
        S.dma('sp', brow[:, 4:36], prm['b_exp'].partition_broadcast(128), reads=['brow'], writes=['brow'])
        rows = {}
        for nm, col in (('G1', M['m3'][:, 2]), ('A2', None), ('B2', M['m3'][:, 3])):
            for v in range(2):
                t_ = C.sb([128, 1024], F32, f'row{nm}{v}')
                ca = M['A2'][:, :, v] if nm == 'A2' else col[:, :, v]
                emit_rowbcast(C, ca, 'A2' if nm == 'A2' else 'modT', t_[:], f'row{nm}{v}', prm['rowscr'][(nm, v)])
                rows[(nm, v)] = t_
        mt = [C.sb([128, 1024], F32, 'mt') for _ in range(2)]
        xt = [C.sb([128, 1024], F32, 'xt') for _ in range(2)]
        ft = [C.sb([128, 1024], F32, 'ft') for _ in range(2)]
        mT = C.sb([128, 8, 128], BF16, 'mT'); fT = C.sb([128, 8, 128], F32, 'fT')
        junk = C.sb([128, 1024], F32, 'junk'); ss = C.sb([128, 4], F32, 'ssp'); tmpm = C.sb([128, 1024], F32, 'tmpm')
        pT = C.ps([128, 1024], F32, 'pTp'); pM = [C.ps([128, 512], F32, 'pM') for _ in range(2)]
        pT2 = C.ps([128, 1024], F32, 'pTp2'); pL = C.ps([128, 512], F32, 'pL')
        for t in range(NT):
            v = 1 if t < 2 else 0
            m_ = mt[t % 2]; x_ = xt[t % 2]; f_ = ft[t % 2]
            mk, xk, fk = f'mt{t % 2}', f'xt{t % 2}', f'ft{t % 2}'
            S.dma('sp', m_[:], mixo[t * 128:(t + 1) * 128, :], reads=[('mixo', 'att', t)] + [('mixo', 'rw', P) for P in range(4)] + ['mixo'], writes=[mk])
            S.dma('act', x_[:], xs[t * 128:(t + 1) * 128, :], reads=[('xs', t)], writes=[xk])
            for k in range(8):
                S.op('pe', lambda e, k=k, m_=m_: e.transpose(out=pT[:, k * 128:(k + 1) * 128], in_=m_[:, k * 128:(k + 1) * 128], identity=ident[:]),
                     reads=[mk, idk], writes=['pTp'])
            S.op('act', lambda e: e.copy(out=mT[:], in_=pT[:].rearrange("p (k t) -> p k t", k=8)), reads=['pTp'], writes=['mT'])
            for hf in range(2):
                for k in range(8):
                    S.op('pe', lambda e, k=k, hf=hf: e.matmul(pM[hf][:], lhsT=mT[:, k, :], rhs=wbf[:, k, hf * 512:(hf + 1) * 512], start=(k == 0), stop=(k == 7)),
                         reads=['mT', ('woutbf', k)], writes=[f'pM{hf}'])
                S.op('dve', lambda e, hf=hf: e.tensor_tensor(out=tmpm[:, hf * 512:(hf + 1) * 512], in0=pM[hf][:], in1=rows[('G1', v)][:, hf * 512:(hf + 1) * 512], op=ALU.mult),
                     reads=[f'pM{hf}', f'rowG1{v}'], writes=['tmpm'])
            S.op('pool', lambda e, x_=x_: e.tensor_tensor(out=x_[:], in0=x_[:], in1=tmpm[:], op=ALU.add), reads=[xk, 'tmpm'], writes=[xk])
            S.dma('act', xs[t * 128:(t + 1) * 128, :], x_[:], reads=[xk], writes=[('xs', t)])
            S.op('act', lambda e, x_=x_: e.activation(out=junk[:], in_=x_[:], func=AF.Square, accum_out=ss[:, 0:1]), reads=[xk], writes=['junk', 'ssp'])
            S.op('dve', lambda e: e.tensor_scalar(out=ss[:, 1:2], in0=ss[:, 0:1], scalar1=1.0 / D, scalar2=1e-6, op0=ALU.mult, op1=ALU.add), reads=['ssp'], writes=['ssp'])
            S.op('act', lambda e: e.activation(out=ss[:, 2:3], in_=ss[:, 1:2], func=AF.Sqrt), reads=['ssp'], writes=['ssp'])
            S.op('dve', lambda e: e.reciprocal(out=ss[:, 3:4], in_=ss[:, 2:3]), reads=['ssp'], writes=['ssp'])
            S.op('dve', lambda e, x_=x_, f_=f_: e.scalar_tensor_tensor(out=f_[:], in0=x_[:], scalar=ss[:, 3:4], in1=rows[('A2', v)][:], op0=ALU.mult, op1=ALU.mult),
                 reads=[xk, 'ssp', f'rowA2{v}'], writes=[fk])
            S.op('pool', lambda e, f_=f_: e.tensor_tensor(out=f_[:], in0=f_[:], in1=rows[('B2', v)][:], op=ALU.add), reads=[fk, f'rowB2{v}'], writes=[fk])
            S.dma('sp', frows[t * 128:(t + 1) * 128, :], f_[:], reads=[fk], writes=[('frows', t)])
            for k in range(8):
                S.op('pe', lambda e, k=k, f_=f_: e.transpose(out=pT2[:, k * 128:(k + 1) * 128], in_=f_[:, k * 128:(k + 1) * 128], identity=ident[:]),
                     reads=[fk, idk], writes=['pTp2'])
            S.op('act', lambda e: e.copy(out=fT[:], in_=pT2[:].rearrange("p (k t) -> p k t", k=8)), reads=['pTp2'], writes=['fT'])
            for k in range(8):
                S.op('pe', lambda e, k=k: e.matmul(pL[:, 0:36], lhsT=fT[:, k, :], rhs=wr[:, k, :], start=(k == 0), stop=(k == 7)), reads=['fT', 'wr'], writes=['pL'])
            S.op('dve', lambda e, t=t: e.tensor_tensor(out=R['lgall'][:, t, :], in0=pL[:, 0:36], in1=brow[:], op=ALU.add), reads=['pL', 'brow'], writes=['lgall'])


def emit_route(C, R, NT, NBLK, masks):
    S = C.S
    lg = R['lgall']
    with C.scope():
        BIG = 1.0e30
        t4 = C.sb([128, NT, 4], F32, 't4'); ohg = C.sb([128, NT, 4], F32, 'ohg'); gmax = C.sb([128, NT], F32, 'gmax'); pg = C.sb([128, NT], F32, 'pg')
        me = C.sb([128, NT, 32], F32, 'me'); me2 = C.sb([128, NT, 32], F32, 'me2'); m1 = C.sb([128, NT], F32, 'm1'); m2 = C.sb([128, NT], F32, 'm2')
        oh1 = C.sb([128, NT, 32], F32, 'oh1'); oh2 = C.sb([128, NT, 32], F32, 'oh2'); ohb = C.sb([128, NT, 32], BF16, 'ohb')
        e21 = C.sb([128, NT], F32, 'e21'); g1 = C.sb([128, NT], F32, 'g1')
        lgg = lg[:, :, 0:4]; lge = lg[:, :, 4:36]
        bN = lambda t_, n: t_[:].unsqueeze(2).broadcast_to([128, NT, n])
        op = S.op
        op('dve', lambda e: e.tensor_reduce(out=gmax[:], in_=lgg, axis=AX.X, op=ALU.max), reads=['lgall'], writes=['gmax'])
        op('dve', lambda e: e.tensor_tensor(out=ohg[:], in0=lgg, in1=bN(gmax, 4), op=ALU.is_equal), reads=['lgall', 'gmax'], writes=['ohg'])
        op('dve', lambda e: e.tensor_tensor(out=t4[:], in0=lgg, in1=bN(gmax, 4), op=ALU.subtract), reads=['lgall', 'gmax'], writes=['t4'])
        op('act', lambda e: e.activation(out=t4[:], in_=t4[:], func=AF.Exp), reads=['t4'], writes=['t4'])
        op('dve', lambda e: e.tensor_reduce(out=pg[:], in_=t4[:], axis=AX.X, op=ALU.add), reads=['t4'], writes=['pg'])
        op('dve', lambda e: e.reciprocal(out=pg[:], in_=pg[:]), reads=['pg'], writes=['pg'])
        op('dve', lambda e: e.tensor_scalar(out=t4[:], in0=ohg[:], scalar1=-1.0, scalar2=BIG, op0=ALU.add, op1=ALU.mult), reads=['ohg', 't4'], writes=['t4'])
        op('dve', lambda e: e.tensor_tensor(out=me[:].rearrange("p t (g x) -> p t g x", g=4), in0=lge.rearrange("p t (g x) -> p t g x", g=4),
                                            in1=t4[:].unsqueeze(3).broadcast_to([128, NT, 4, 8]), op=ALU.add), reads=['lgall', 't4'], writes=['me'])
        op('dve', lambda e: e.tensor_reduce(out=m1[:], in_=me[:], axis=AX.X, op=ALU.max), reads=['me'], writes=['m1'])
        op('dve', lambda e: e.tensor_tensor(out=oh1[:], in0=me[:], in1=bN(m1, 32), op=ALU.is_equal), reads=['me', 'm1'], writes=['oh1'])
        op('dve', lambda e: e.scalar_tensor_tensor(out=me2[:], in0=oh1[:], scalar=-BIG, in1=me[:], op0=ALU.mult, op1=ALU.add), reads=['oh1', 'me'], writes=['me2'])
        op('dve', lambda e: e.tensor_reduce(out=m2[:], in_=me2[:], axis=AX.X, op=ALU.max), reads=['me2'], writes=['m2'])
        op('dve', lambda e: e.tensor_tensor(out=oh2[:], in0=me2[:], in1=bN(m2, 32), op=ALU.is_equal), reads=['me2', 'm2'], writes=['oh2'])
        op('dve', lambda e: e.tensor_tensor(out=e21[:], in0=m2[:], in1=m1[:], op=ALU.subtract), reads=['m1', 'm2'], writes=['e21'])
        op('act', lambda e: e.activation(out=e21[:], in_=e21[:], func=AF.Exp), reads=['e21'], writes=['e21'])
        op('dve', lambda e: e.tensor_scalar(out=g1[:], in0=e21[:], scalar1=1.0, scalar2=None, op0=ALU.add), reads=['e21'], writes=['g1'])
        op('dve', lambda e: e.reciprocal(out=g1[:], in_=g1[:]), reads=['g1'], writes=['g1'])
        gates = R['gates']
        op('dve', lambda e: e.tensor_tensor(out=gates[:, :, 0], in0=g1[:], in1=pg[:], op=ALU.mult), reads=['g1', 'pg'], writes=['gates'])
        op('dve', lambda e: e.tensor_tensor(out=e21[:], in0=e21[:], in1=g1[:], op=ALU.mult), reads=['e21', 'g1'], writes=['e21'])
        op('dve', lambda e: e.tensor_tensor(out=gates[:, :, 1], in0=e21[:], in1=pg[:], op=ALU.mult), reads=['e21', 'pg', 'gates'], writes=['gates'])
        op('dve', lambda e: e.tensor_tensor(out=ohb[:], in0=oh1[:], in1=oh2[:], op=ALU.add), reads=['oh1', 'oh2'], writes=['ohb'])
        onesb = C.sb([128, 128], BF16, 'onesb'); trib = C.sb([128, 128], BF16, 'trib')
        op('pool', lambda e: e.memset(onesb[:], 1.0), writes=['onesb'])
        op('dve', lambda e: e.tensor_copy(out=trib[:], in_=masks['up_s'][:]), reads=['mkup_s'], writes=['trib'])
        NG = (NT + 15) // 16
        pCS = [C.ps([128, 512], F32, 'pCS') for _ in range(NG)]; pRK = [C.ps([128, 512], F32, 'pRK') for _ in range(NG)]
        cs = C.sb([128, NT, 32], F32, 'cs'); rk = C.sb([128, NT, 32], F32, 'rk'); carry = C.sb([128, NT + 1, 32], F32, 'carry')
        for t in range(NT):
            g, o = t // 16, (t % 16) * 32
            op('pe', lambda e, g=g, o=o, t=t: e.matmul(pCS[g][:, o:o + 32], lhsT=onesb[:], rhs=ohb[:, t, :], start=True, stop=True), reads=['onesb', 'ohb'], writes=[f'pCS{g}'])
            op('pe', lambda e, g=g, o=o, t=t: e.matmul(pRK[g][:, o:o + 32], lhsT=trib[:], rhs=ohb[:, t, :], start=True, stop=True), reads=['trib', 'ohb'], writes=[f'pRK{g}'])
        for g in range(NG):
            n = min(16, NT - g * 16)
            op('dve', lambda e, g=g, n=n: e.tensor_copy(out=cs[:, g * 16:g * 16 + n, :], in_=pCS[g][:, 0:n * 32].rearrange("p (t x) -> p t x", x=32)), reads=[f'pCS{g}'], writes=['cs'])
            op('dve', lambda e, g=g, n=n: e.tensor_copy(out=rk[:, g * 16:g * 16 + n, :], in_=pRK[g][:, 0:n * 32].rearrange("p (t x) -> p t x", x=32)), reads=[f'pRK{g}'], writes=['rk'])
        op('pool', lambda e: e.memset(carry[:, 0, :], 0.0), writes=['carry'])
        for t in range(NT):
            op('dve', lambda e, t=t: e.tensor_tensor(out=carry[:, t + 1, :], in0=carry[:, t, :], in1=cs[:, t, :], op=ALU.add), reads=['carry', 'cs'], writes=['carry'])
        op('dve', lambda e: e.tensor_tensor(out=rk[:], in0=rk[:], in1=carry[:, 0:NT, :], op=ALU.add), reads=['rk', 'carry'], writes=['rk'])
        cnt = C.sb([128, 32], F32, 'cnt'); ci = C.sb([128, 32], I32, 'ci'); pad = C.sb([128, 32], F32, 'pad'); pend = C.sb([128, 32], F32, 'pend'); pst = C.sb([128, 32], F32, 'pst')
        ones32 = C.sb([128, 32], F32, 'ones32')
        op('pool', lambda e: e.memset(ones32[:], 1.0), writes=['ones32'])
        op('dve', lambda e: e.tensor_scalar(out=cnt[:], in0=carry[:, NT, :], scalar1=127.0, scalar2=None, op0=ALU.add), reads=['carry'], writes=['cnt'])
        op('dve', lambda e: e.tensor_copy(out=ci[:], in_=cnt[:]), reads=['cnt'], writes=['ci'])
        op('dve', lambda e: e.tensor_scalar(out=ci[:], in0=ci[:], scalar1=7, scalar2=7, op0=ALU.arith_shift_right, op1=ALU.logical_shift_left), reads=['ci'], writes=['ci'])
        op('dve', lambda e: e.tensor_copy(out=pad[:], in_=ci[:]), reads=['ci'], writes=['pad'])
        op('dve', lambda e: e.tensor_tensor_scan(out=pend[:], data0=ones32[:], data1=pad[:], initial=0.0, op0=ALU.mult, op1=ALU.add), reads=['ones32', 'pad'], writes=['pend'])
        op('dve', lambda e: e.tensor_tensor(out=pst[:], in0=pend[:], in1=pad[:], op=ALU.subtract), reads=['pend', 'pad'], writes=['pst'])
        op('dve', lambda e: e.tensor_tensor(out=rk[:], in0=rk[:], in1=pst[:].unsqueeze(1).broadcast_to([128, NT, 32]), op=ALU.add), reads=['rk', 'pst'], writes=['rk'])
        destf = C.sb([128, NT, 2], F32, 'destf')
        for j, oh in enumerate((oh1, oh2)):
            op('dve', lambda e, oh=oh: e.tensor_tensor(out=me[:], in0=oh[:], in1=rk[:], op=ALU.mult), reads=['oh1', 'oh2', 'rk', 'me'], writes=['me'])
            op('dve', lambda e, j=j: e.tensor_reduce(out=destf[:, :, j], in_=me[:], axis=AX.X, op=ALU.add), reads=['me', 'destf'], writes=['destf'])
        op('dve', lambda e: e.tensor_copy(out=R['dest'][:].rearrange("p (t j) -> p t j", j=2), in_=destf[:]), reads=['destf'], writes=['dest'])
        bpos = C.sb([128, NBLK], I32, 'bpos'); bposf = C.sb([128, NBLK], F32, 'bposf'); cmpb = C.sb([128, NBLK, 32], F32, 'cmpb'); eb = C.sb([128, NBLK], F32, 'eb')
        kp = C.sb([128, 8], I32, 'kp'); kpf = C.sb([128, 8], F32, 'kpf'); idf = C.sb([128, NBLK, 8], F32, 'idf')
        op('pool', lambda e: e.iota(bpos[:], pattern=[[128, NBLK]], base=0, channel_multiplier=0), writes=['bpos'])
        op('dve', lambda e: e.tensor_copy(out=bposf[:], in_=bpos[:]), reads=['bpos'], writes=['bposf'])
        op('dve', lambda e: e.tensor_tensor(out=cmpb[:], in0=pend[:].unsqueeze(1).broadcast_to([128, NBLK, 32]), in1=bposf[:].unsqueeze(2).broadcast_to([128, NBLK, 32]), op=ALU.is_le),
           reads=['pend', 'bposf'], writes=['cmpb'])
        op('dve', lambda e: e.tensor_reduce(out=eb[:], in_=cmpb[:], axis=AX.X, op=ALU.add), reads=['cmpb'], writes=['eb'])
        op('dve', lambda e: e.tensor_scalar(out=eb[:], in0=eb[:], scalar1=31.0, scalar2=None, op0=ALU.min), reads=['eb'], writes=['eb'])
        op('pool', lambda e: e.iota(kp[:], pattern=[[128, 8]], base=0, channel_multiplier=1), writes=['kp'])
        op('dve', lambda e: e.tensor_copy(out=kpf[:], in_=kp[:]), reads=['kp'], writes=['kpf'])
        op('dve', lambda e: e.scalar_tensor_tensor(out=idf[:], in0=eb[:].unsqueeze(2).broadcast_to([128, NBLK, 8]), scalar=1024.0, in1=kpf[:].unsqueeze(1).broadcast_to([128, NBLK, 8]),
                                                   op0=ALU.mult, op1=ALU.add), reads=['eb', 'kpf'], writes=['idf'])
        op('dve', lambda e: e.tensor_copy(out=R['idxg'][:].rearrange("p (b k) -> p b k", k=8), in_=idf[:]), reads=['idf'], writes=['idxg'])
        idf2 = C.sb([128, NBLK], F32, 'idf2')
        op('dve', lambda e: e.scalar_tensor_tensor(out=idf2[:], in0=eb[:], scalar=128.0, in1=kpf[:, 0:1].broadcast_to([128, NBLK]), op0=ALU.mult, op1=ALU.add), reads=['eb', 'kpf'], writes=['idf2'])
        op('dve', lambda e: e.tensor_copy(out=R['idxe'][:], in_=idf2[:]), reads=['idf2'], writes=['idxe'])
        op('dve', lambda e: e.scalar_tensor_tensor(out=idf[:, :, 0:4], in0=eb[:].unsqueeze(2).broadcast_to([128, NBLK, 4]), scalar=512.0, in1=kpf[:, 0:4].unsqueeze(1).broadcast_to([128, NBLK, 4]),
                                                   op0=ALU.mult, op1=ALU.add), reads=['eb', 'kpf', 'idf'], writes=['idf'])
        op('dve', lambda e: e.tensor_copy(out=R['idxd'][:].rearrange("p (b k) -> p b k", k=4), in_=idf[:, :, 0:4]), reads=['idf'], writes=['idxd'])


def emit_moe(C, M, R, frows, xs, xrows, yrows, wgu, wdn, prm, NT, NBLK, ident, idk, out_dram=None, out_tiles=None, wkeys=()):
    S = C.S
    IOA = bass.IndirectOffsetOnAxis
    with C.scope():
        z = C.sb([128, 2048], F32, 'zer')
        S.op('pool', lambda e: e.memset(z[:], 0.0), writes=['zer'])
        for b0 in range(0, NBLK, 2):
            n = min(2, NBLK - b0)
            S.dma('sp', xrows[b0 * 128:(b0 + n) * 128, :].rearrange("(b p) f -> p b f", p=128), z[:, 0:n * 1024].rearrange("p (b f) -> p b f", f=1024),
                  reads=['zer'], writes=['xrows'])
        ftb = [C.sb([128, 1024], F32, 'ftb') for _ in range(2)]
        for t in range(NT):
            f_ = ftb[t % 2]; fk = f'ftb{t % 2}'
            S.dma('sp', f_[:], frows[t * 128:(t + 1) * 128, :], reads=[('frows', t)], writes=[fk])
            for j in range(2):
                S.dma('pool', xrows, f_[:], reads=[fk, 'dest'], writes=['xrows'],
                      indirect=dict(out_offset=IOA(ap=R['dest'][:, 2 * t + j:2 * t + j + 1], axis=0), in_offset=None))
    with C.scope():
        identb = C.sb([128, 128], BF16, 'identb2')
        S.op('dve', lambda e: e.tensor_copy(out=identb[:], in_=ident[:]), reads=[idk], writes=['identb2'])
        wg = [C.sb([128, 8, 1024], BF16, 'wg') for _ in range(2)]; wd = [C.sb([128, 4, 1024], BF16, 'wd') for _ in range(2)]
        xb = [C.sb([128, 1024], F32, 'xb') for _ in range(2)]; yb = [C.sb([128, 1024], F32, 'ybm') for _ in range(2)]
        xT = C.sb([128, 8, 128], BF16, 'xTm'); sg = C.sb([128, 512], F32, 'sgm'); hb = C.sb([128, 512], BF16, 'hbm'); hT = C.sb([128, 4, 128], BF16, 'hTm')
        pTx = C.ps([128, 1024], F32, 'pTx'); pG = C.ps([128, 512], F32, 'pGm'); pU = C.ps([128, 512], F32, 'pUm')
        pTh = C.ps([128, 1024], BF16, 'pTh'); pD = [C.ps([128, 512], F32, 'pDm') for _ in range(2)]
        wflat = wgu.rearrange("e k n -> (e k) n") if len(wgu.shape) == 3 else wgu
        dflat = wdn.rearrange("e k n -> (e k) n") if len(wdn.shape) == 3 else wdn
        wkeys = list(wkeys)
        xTA = [xT, C.sb([128, 8, 128], BF16, 'xTm2')]; hbA = [hb, C.sb([128, 512], BF16, 'hbm2')]; hTA = [hT, C.sb([128, 4, 128], BF16, 'hTm2')]

        def stA(b):
            w_ = wg[b % 2]; x_ = xb[b % 2]; xT_ = xTA[b % 2]
            S.dma('pool', w_[:].rearrange("p k n -> p (k n)"), wflat, reads=['idxe'] + wkeys, writes=[(f'wg{b % 2}', k) for k in range(8)],
                  indirect=dict(out_offset=None, in_offset=IOA(ap=R['idxe'][:, b:b + 1], axis=0)))
            S.dma('sp', x_[:], xrows[b * 128:(b + 1) * 128, :], reads=['xrows'], writes=[f'xb{b % 2}'])
            for k in range(8):
                S.op('pe', lambda e, k=k, x_=x_: e.transpose(out=pTx[:, k * 128:(k + 1) * 128], in_=x_[:, k * 128:(k + 1) * 128], identity=ident[:]),
                     reads=[f'xb{b % 2}', idk], writes=['pTx'])
            S.op('act', lambda e: e.copy(out=xT_[:], in_=pTx[:].rearrange("p (k t) -> p k t", k=8)), reads=['pTx'], writes=[f'xTm{b % 2}'])

        def stB(b):
            w_ = wg[b % 2]; xT_ = xTA[b % 2]; hb_ = hbA[b % 2]
            for k in range(8):
                S.op('pe', lambda e, k=k: e.matmul(pG[:], lhsT=xT_[:, k, :], rhs=w_[:, k, 0:512], start=(k == 0), stop=(k == 7)), reads=[f'xTm{b % 2}', (f'wg{b % 2}', k)], writes=['pGm'])
            for k in range(8):
                S.op('pe', lambda e, k=k: e.matmul(pU[:], lhsT=xT_[:, k, :], rhs=w_[:, k, 512:1024], start=(k == 0), stop=(k == 7)), reads=[f'xTm{b % 2}', (f'wg{b % 2}', k)], writes=['pUm'])
            S.op('act', lambda e: e.activation(out=sg[:], in_=pG[:], func=AF.Silu), reads=['pGm'], writes=['sgm'])
            S.op('dve', lambda e: e.tensor_tensor(out=hb_[:], in0=pU[:], in1=sg[:], op=ALU.mult), reads=['pUm', 'sgm'], writes=[f'hbm{b % 2}'])

        def stC(b):
            d_ = wd[b % 2]; hb_ = hbA[b % 2]; hT_ = hTA[b % 2]
            S.dma('pool', d_[:].rearrange("p k n -> p (k n)"), dflat, reads=['idxe'] + wkeys, writes=[(f'wd{b % 2}', k) for k in range(4)],
                  indirect=dict(out_offset=None, in_offset=IOA(ap=R['idxe'][:, b:b + 1], axis=0)))
            for k in range(4):
                S.op('pe', lambda e, k=k: e.transpose(out=pTh[:, k * 128:(k + 1) * 128], in_=hb_[:, k * 128:(k + 1) * 128], identity=identb[:]), reads=[f'hbm{b % 2}', 'identb2'], writes=['pTh'])
            S.op('act', lambda e: e.copy(out=hT_[:], in_=pTh[:, 0:512].rearrange("p (k t) -> p k t", k=4)), reads=['pTh'], writes=[f'hTm{b % 2}'])

        def stD(b):
            d_ = wd[b % 2]; hT_ = hTA[b % 2]; y_ = yb[b % 2]
            for hf in range(2):
                for k in range(4):
                    S.op('pe', lambda e, k=k, hf=hf: e.matmul(pD[hf][:], lhsT=hT_[:, k, :], rhs=d_[:, k, hf * 512:(hf + 1) * 512], start=(k == 0), stop=(k == 3)),
                         reads=[f'hTm{b % 2}', (f'wd{b % 2}', k)], writes=[f'pDm{hf}'])
                if hf == 0:
                    S.op('act', lambda e: e.copy(out=y_[:, 0:512], in_=pD[0][:]), reads=['pDm0'], writes=[f'ybm{b % 2}'])
                else:
                    S.op('dve', lambda e: e.tensor_copy(out=y_[:, 512:1024], in_=pD[1][:]), reads=['pDm1'], writes=[f'ybm{b % 2}'])
            S.dma('sp', yrows[b * 128:(b + 1) * 128, :], y_[:], reads=[f'ybm{b % 2}'], writes=['yrows'])

        assert len(wgu.shape) == 2
        for it in range(NBLK + 3):
            if it < NBLK:
                stA(it)
            if 0 <= it - 1 < NBLK:
                stB(it - 1)
            if 0 <= it - 2 < NBLK:
                stC(it - 2)
            if 0 <= it - 3 < NBLK:
                stD(it - 3)
    with C.scope():
        rowG2 = []
        for v in range(2):
            t_ = C.sb([128, 1024], F32, f'rowG2{v}')
            emit_rowbcast(C, M['m3'][:, 5][:, :, v], 'modT', t_[:], f'rowG2{v}', prm['rowscr'][('G2', v)])
            rowG2.append(t_)
        y1 = [C.sb([128, 1024], F32, 'y1') for _ in range(2)]; y2 = [C.sb([128, 1024], F32, 'y2') for _ in range(2)]
        xm = [C.sb([128, 1024], F32, 'xm') for _ in range(2)]
        tiles = out_tiles if out_tiles is not None else list(range(NT))
        for i, t in enumerate(tiles):
            v = 1 if t < 2 else 0
            a_, b_, x_ = y1[i % 2], y2[i % 2], xm[i % 2]
            ak, bk, xk = f'y1{i % 2}', f'y2{i % 2}', f'xm{i % 2}'
            S.dma('pool', a_[:], yrows, reads=['yrows', 'dest'], writes=[ak], indirect=dict(out_offset=None, in_offset=IOA(ap=R['dest'][:, 2 * t:2 * t + 1], axis=0)))
            S.dma('pool', b_[:], yrows, reads=['yrows', 'dest'], writes=[bk], indirect=dict(out_offset=None, in_offset=IOA(ap=R['dest'][:, 2 * t + 1:2 * t + 2], axis=0)))
            S.dma('sp', x_[:], xs[t * 128:(t + 1) * 128, :], reads=[('xs', t)], writes=[xk])
            S.op('dve', lambda e, a_=a_, t=t: e.tensor_scalar(out=a_[:], in0=a_[:], scalar1=R['gates'][:, t, 0:1], scalar2=None, op0=ALU.mult), reads=[ak, 'gates'], writes=[ak])
            S.op('dve', lambda e, a_=a_, b_=b_, t=t: e.scalar_tensor_tensor(out=a_[:], in0=b_[:], scalar=R['gates'][:, t, 1:2], in1=a_[:], op0=ALU.mult, op1=ALU.add),
                 reads=[ak, bk, 'gates'], writes=[ak])
            S.op('pool', lambda e, a_=a_, v=v: e.tensor_tensor(out=a_[:], in0=a_[:], in1=rowG2[v][:], op=ALU.mult), reads=[ak, f'rowG2{v}'], writes=[ak])
            S.op('dve', lambda e, a_=a_, x_=x_: e.tensor_tensor(out=x_[:], in0=x_[:], in1=a_[:], op=ALU.add), reads=[ak, xk], writes=[xk])
            if out_dram is None:
                S.dma('sp', xs[t * 128:(t + 1) * 128, :], x_[:], reads=[xk], writes=[('xs', t)])
            else:
                S.dma('sp', out_dram[(t - 2) * 128:(t - 1) * 128, :], x_[:], reads=[xk], writes=[('out', t)])


def route_alloc(C, NT, NBLK):
    return dict(lgall=C.sb([128, NT, 36], F32, 'lgall'), gates=C.sb([128, NT, 2], F32, 'gates'), dest=C.sb([128, NT * 2], I32, 'dest'),
                idxg=C.sb([128, NBLK * 8], I32, 'idxg'), idxd=C.sb([128, NBLK * 4], I32, 'idxd'), idxe=C.sb([128, NBLK], I32, 'idxe'))


def rowscr_alloc(C, tag):
    return {(nm, v): C.dram(f"rowscr_{tag}_{nm}{v}", [1024], F32) for nm in ('G1', 'A2', 'B2', 'G2') for v in range(2)}


def build_moe_test(NT):
    C = Ctx(); S = C.S
    NTOK = NT * 128; NBLK = 2 * NT + 32
    mixo = C.dram("mixo", [NTOK, 1024], kind="ExternalInput")
    xin = C.dram("xin", [NTOK, 1024], kind="ExternalInput")
    cin = C.dram("cin", [128, 16], kind="ExternalInput"); adaw = C.dram("adaw", [D, 6 * D], kind="ExternalInput")
    adab = C.dram("adab", [128, 96], kind="ExternalInput"); nrm = C.dram("nrm", [128, 16], kind="ExternalInput")
    wout = C.dram("wout", [D, D], kind="ExternalInput")
    prm = dict(w_grp=C.dram("w_grp", [D, 4], kind="ExternalInput"), b_grp=C.dram("b_grp", [4], kind="ExternalInput"),
               w_exp=C.dram("w_exp", [D, 32], kind="ExternalInput"), b_exp=C.dram("b_exp", [32], kind="ExternalInput"))
    wgu = C.dram("wgu", [32, 1024, 1024], kind="ExternalInput"); wdn = C.dram("wdn", [32, 512, 1024], kind="ExternalInput")
    xs = C.dram("xs", [NTOK, 1024], kind="ExternalOutput")
    frows = C.dram("frows", [NTOK, 1024], kind="ExternalOutput")
    xrows = C.dram("xrows", [NBLK * 128, 1024]); yrows = C.dram("yrows", [NBLK * 128, 1024])
    prm['rowscr'] = rowscr_alloc(C, 't')
    ident, idk = C.ident(); masks = make_masks(C)
    for t in range(NT):
        S.dma('sp', xs[t * 128:(t + 1) * 128, :], xin[t * 128:(t + 1) * 128, :], writes=[('xs', t)])
    M = emit_mods(C, adaw, adab, cin, nrm)
    R = route_alloc(C, NT, NBLK)
    emit_post(C, M, mixo, xs, frows, wout, prm, NT, ident, idk, R)
    emit_route(C, R, NT, NBLK, masks)
    emit_moe(C, M, R, frows, xs, xrows, yrows, wgu, wdn, prm, NT, NBLK, ident, idk)
    S.finish()
    return C


def emit_pre(C, M, xs, win, NOUT, NT, ident, idk, fm0, nfm, fm_out, tm0, tm1, tm_out, mode, pcols):
    S = C.S
    NTOK = NT * 128
    with C.scope():
        hT = C.sb([128, 8, NTOK], BF16, 'hT')
        with C.scope():
            tmp = dict(junk=C.sb([128, D]), ss=C.sb([128, 4]), xn=C.sb([128, D]), t2=C.sb([128, 8, 128]))
            xb = [C.sb([128, D], F32, 'xb') for _ in range(2)]
            pT = [C.ps([128, 1024], F32, 'pT') for _ in range(2)]
            B1 = M['m3'][:, 0]
            for t in range(NT):
                v = 1 if t < 2 else 0
                S.dma('sp', xb[t % 2][:], xs[t * 128:(t + 1) * 128, :], reads=[('xs', t)], writes=[f'xb{t % 2}'])
                emit_normT(C, xb[t % 2][:], f'xb{t % 2}', hT, ('hT', t), t * 128, M['A1'], B1, v, ident, idk, pT[t % 2], f'pT{t % 2}', tmp, t)
        hkeys = [('hT', t) for t in range(NT)]
        with C.scope():
            TW = tm1 - tm0
            wtm = C.sb([128, 8, TW], BF16, 'wtm')
            for k in range(8):
                S.dma('pool', wtm[:, k, :], win[k * 128:(k + 1) * 128, tm0:tm1], writes=[('wtm', k)])
            pY = [C.ps([128, 512], F32, 'pY') for _ in range(3)]
            yb = [C.sb([128, TW], F32, 'yb') for _ in range(2)]
            ncc = (TW + 511) // 512
            it = 0
            for t in range(NT):
                for c in range(ncc):
                    cw = min(512, TW - c * 512)
                    p = pY[it % 3]; pk = f'pY{it % 3}'
                    for k in range(8):
                        S.op('pe', lambda e, k=k, p=p, c=c, cw=cw, t=t: e.matmul(p[:, 0:cw], lhsT=hT[:, k, t * 128:(t + 1) * 128],
                                                                                rhs=wtm[:, k, c * 512:c * 512 + cw], start=(k == 0), stop=(k == 7)),
                             reads=[('hT', t), ('wtm', k)], writes=[pk])
                    eng = 'act' if it % 2 == 0 else 'dve'
                    if eng == 'act':
                        S.op('act', lambda e, p=p, c=c, cw=cw, t=t: e.copy(out=yb[t % 2][:, c * 512:c * 512 + cw], in_=p[:, 0:cw]), reads=[pk], writes=[f'yb{t % 2}'])
                    else:
                        S.op('dve', lambda e, p=p, c=c, cw=cw, t=t: e.tensor_copy(out=yb[t % 2][:, c * 512:c * 512 + cw], in_=p[:, 0:cw]), reads=[pk], writes=[f'yb{t % 2}'])
                    it += 1
                S.dma('sp', tm_out[t * 128:(t + 1) * 128, :], yb[t % 2][:], reads=[f'yb{t % 2}'], writes=[('tm_out', t)])
        with C.scope():
            PADW = NTOK + 8
            co, lo = 2, 262
            pb = [C.sb([128, PADW], F32, 'pbuf') for _ in range(2)]
            acc = [C.sb([128, PADW], F32, 'accb') for _ in range(2)]
            wc = [C.sb([128, 8, 128], BF16, 'wc') for _ in range(2)]
            pc = C.sb([128, pcols.shape[1]], F32, 'pcols')
            S.dma('sp', pc[:], pcols, writes=['pcols'])
            pF = [C.ps([128, 512], F32, 'pF') for _ in range(3)]
            for i in range(2):
                S.op('pool', lambda e, i=i: e.memset(pb[i][:], 0.0), writes=[f'pbuf{i}'])
            NT5 = (NTOK + 511) // 512
            it = 0
            for cc in range(nfm):
                P_ = pb[cc % 2]; A_ = acc[cc % 2]; W_ = wc[cc % 2]
                pk_, ak_, wk_ = f'pbuf{cc % 2}', f'accb{cc % 2}', f'wc{cc % 2}'
                S.dma('pool', W_[:], win[:, fm0 + cc * 128:fm0 + (cc + 1) * 128].rearrange("(k p) c -> p k c", p=128), writes=[wk_])
                for i in range(NT5):
                    w = min(512, NTOK - i * 512)
                    p = pF[it % 3]; pfk = f'pF{it % 3}'
                    for k in range(8):
                        S.op('pe', lambda e, k=k, p=p, i=i, w=w, W_=W_: e.matmul(p[:, 0:w], lhsT=W_[:, k, :], rhs=hT[:, k, i * 512:i * 512 + w], start=(k == 0), stop=(k == 7)),
                             reads=hkeys[i * 4:i * 4 + 4] + [wk_], writes=[pfk])
                    if i == 0:
                        S.op('act', lambda e, p=p, P_=P_: e.copy(out=P_[:, co:co + 256], in_=p[:, 0:256]), reads=[pfk], writes=[pk_])
                        S.op('dve', lambda e, p=p, P_=P_, w=w: e.tensor_copy(out=P_[:, lo:lo + w - 256], in_=p[:, 256:w]), reads=[pfk, pk_], writes=[pk_])
                    else:
                        o = lo + i * 512 - 256
                        if it % 2 == 0:
                            S.op('act', lambda e, p=p, P_=P_, w=w, o=o: e.copy(out=P_[:, o:o + w], in_=p[:, 0:w]), reads=[pfk, pk_], writes=[pk_])
                        else:
                            S.op('dve', lambda e, p=p, P_=P_, w=w, o=o: e.tensor_copy(out=P_[:, o:o + w], in_=p[:, 0:w]), reads=[pfk, pk_], writes=[pk_])
                    it += 1
                L = PADW - 4
                if mode == 'shift':
                    S.op('pool', lambda e, P_=P_, A_=A_: e.tensor_tensor(out=A_[:, 2:2 + L], in0=P_[:, 1:1 + L], in1=P_[:, 3:3 + L], op=ALU.add), reads=[pk_], writes=[ak_])
                    S.op('dve', lambda e, A_=A_, cc=cc: e.tensor_scalar(out=A_[:, 2:2 + L], in0=A_[:, 2:2 + L], scalar1=pc[:, 2 * nfm + cc:2 * nfm + cc + 1], scalar2=None, op0=ALU.mult),
                         reads=[ak_, 'pcols'], writes=[ak_])
                    S.op('dve', lambda e, A_=A_, P_=P_, cc=cc: e.scalar_tensor_tensor(out=A_[:, 2:2 + L], in0=P_[:, 2:2 + L], scalar=pc[:, nfm + cc:nfm + cc + 1], in1=A_[:, 2:2 + L],
                                                                                   op0=ALU.mult, op1=ALU.add), reads=[ak_, pk_, 'pcols'], writes=[ak_])
                else:
                    S.op('dve', lambda e, A_=A_, P_=P_, cc=cc: e.tensor_scalar(out=A_[:, 2:2 + L], in0=P_[:, 0:L], scalar1=pc[:, cc:cc + 1], scalar2=None, op0=ALU.mult),
                         reads=[pk_, 'pcols'], writes=[ak_])
                    for j in range(1, 5):
                        S.op('dve', lambda e, A_=A_, P_=P_, cc=cc, j=j: e.scalar_tensor_tensor(out=A_[:, 2:2 + L], in0=P_[:, j:j + L], scalar=pc[:, j * nfm + cc:j * nfm + cc + 1],
                                                                                              in1=A_[:, 2:2 + L], op0=ALU.mult, op1=ALU.add), reads=[ak_, pk_, 'pcols'], writes=[ak_])
                    S.op('act', lambda e, A_=A_: e.activation(out=A_[:, 2:2 + L], in_=A_[:, 2:2 + L], func=AF.Silu), reads=[ak_], writes=[ak_])
                S.dma('sp', fm_out[cc * 128:(cc + 1) * 128, 0:256], A_[:, co:co + 256], reads=[ak_], writes=[('fm_out', cc)])
                S.dma('act', fm_out[cc * 128:(cc + 1) * 128, 256:NTOK], A_[:, lo:lo + NTOK - 256], reads=[ak_], writes=[('fm_out', cc, 1)])


def emit_gdn(C, gT, yz, mixo, prm, NCH, heads, ident, idk, masks):
    S = C.S
    NTOK = NCH * 128
    NT5 = (NTOK + 511) // 512
    order = [chunk_order(NCH, 0), chunk_order(NCH, 1)]
    with C.scope():
        ab = C.sb([128, NCH, 32], F32, 'gab')
        S.dma('sp', ab[:], yz.rearrange("(c t) f -> t c f", t=128)[:, :, 1024:1056], reads=[('tm_out', t) for t in range(NCH)], writes=['gab'])
        rowp = C.sb([128, 48], F32, 'growp')
        S.dma('sp', rowp[:, 0:16], prm['dt_bias'].partition_broadcast(128), writes=['growp'])
        S.dma('sp', rowp[:, 16:32], prm['A_log'].partition_broadcast(128), reads=['growp'], writes=['growp'])
        onorm = C.sb([128, 128], F32, 'onorm')
        S.dma('sp', onorm[:], prm['out_norm'].partition_broadcast(128), writes=['onorm'])
        one1 = C.sb([128, 1], F32, 'one1'); eps6 = C.sb([128, 1], F32, 'eps6')
        S.op('pool', lambda e: e.memset(one1[:], 1.0), writes=['one1'])
        S.op('pool', lambda e: e.memset(eps6[:], 1e-6), writes=['eps6'])
        ones = C.sb([128, 128], F32, 'onesf')
        S.op('pool', lambda e: e.memset(ones[:], 1.0), writes=['onesf'])
        S.op('act', lambda e: e.activation(out=rowp[:, 16:32], in_=rowp[:, 16:32], func=AF.Exp), reads=['growp'], writes=['growp'])
        S.op('dve', lambda e: e.tensor_scalar(out=rowp[:, 16:32], in0=rowp[:, 16:32], scalar1=-1.0, scalar2=None, op0=ALU.mult), reads=['growp'], writes=['growp'])
        g = C.sb([128, NCH, 16], F32, 'gg'); beta = C.sb([128, NCH, 16], F32, 'gbeta')
        gam = C.sb([128, NCH, 16], F32, 'gam'); gtot = C.sb([128, NCH, 16], F32, 'gtot')
        bR = lambda a: a.unsqueeze(1).broadcast_to([128, NCH, 16])
        S.op('dve', lambda e: e.tensor_tensor(out=g[:], in0=ab[:, :, 0:16], in1=bR(rowp[:, 0:16]), op=ALU.add), reads=['gab', 'growp'], writes=['gg'])
        S.op('act', lambda e: e.activation(out=g[:], in_=g[:], func=AF.Exp), reads=['gg'], writes=['gg'])
        S.op('act', lambda e: e.activation(out=g[:], in_=g[:], func=AF.Ln, bias=one1[:, 0:1]), reads=['gg', 'one1'], writes=['gg'])
        S.op('dve', lambda e: e.tensor_tensor(out=g[:], in0=g[:], in1=bR(rowp[:, 16:32]), op=ALU.mult), reads=['gg', 'growp'], writes=['gg'])
        S.op('act', lambda e: e.activation(out=beta[:], in_=ab[:, :, 16:32], func=AF.Sigmoid), reads=['gab'], writes=['gbeta'])
        tri = [masks['up_i'], masks['lo_i']]
        trik = ['mkup_i', 'mklo_i']
        with C.scope():
            pGm = [C.ps([128, 512], F32, 'pGam') for _ in range(2)]; pGt = [C.ps([128, 512], F32, 'pGtot') for _ in range(2)]
            for c in range(NCH):
                b_, o_ = c // 32, (c % 32) * 16
                for d in range(2):
                    S.op('pe', lambda e, c=c, d=d, b_=b_, o_=o_: e.matmul(pGm[b_][:, o_ + d * 8:o_ + d * 8 + 8], lhsT=tri[d][:], rhs=g[:, c, d * 8:(d + 1) * 8], start=True, stop=True),
                         reads=['gg', trik[d]], writes=[f'pGam{b_}'])
                S.op('pe', lambda e, c=c, b_=b_, o_=o_: e.matmul(pGt[b_][:, o_:o_ + 16], lhsT=ones[:], rhs=g[:, c, :], start=True, stop=True), reads=['gg', 'onesf'], writes=[f'pGtot{b_}'])
            for b_ in range((NCH + 31) // 32):
                n = min(32, NCH - b_ * 32)
                S.op('dve', lambda e, b_=b_, n=n: e.tensor_copy(out=gam[:, b_ * 32:b_ * 32 + n, :], in_=pGm[b_][:, 0:n * 16].rearrange("p (c x) -> p c x", x=16)), reads=[f'pGam{b_}'], writes=['gam'])
                S.op('dve', lambda e, b_=b_, n=n: e.tensor_copy(out=gtot[:, b_ * 32:b_ * 32 + n, :], in_=pGt[b_][:, 0:n * 16].rearrange("p (c x) -> p c x", x=16)), reads=[f'pGtot{b_}'], writes=['gtot'])
        nbeg = C.sb([128, NCH, 16], F32, 'nbeg'); etail = C.sb([128, NCH, 16], F32, 'etail'); eC = C.sb([128, NCH, 16], F32, 'eC'); nbeta = C.sb([128, NCH, 16], F32, 'nbeta')
        S.op('act', lambda e: e.activation(out=nbeg[:], in_=gam[:], func=AF.Exp), reads=['gam'], writes=['nbeg'])
        S.op('dve', lambda e: e.tensor_tensor(out=nbeg[:], in0=nbeg[:], in1=beta[:], op=ALU.mult), reads=['nbeg', 'gbeta'], writes=['nbeg'])
        S.op('dve', lambda e: e.tensor_scalar(out=nbeg[:], in0=nbeg[:], scalar1=-1.0, scalar2=None, op0=ALU.mult), reads=['nbeg'], writes=['nbeg'])
        S.op('dve', lambda e: e.tensor_tensor(out=etail[:], in0=gtot[:], in1=gam[:], op=ALU.subtract), reads=['gtot', 'gam'], writes=['etail'])
        S.op('act', lambda e: e.activation(out=etail[:], in_=etail[:], func=AF.Exp), reads=['etail'], writes=['etail'])
        S.op('act', lambda e: e.activation(out=eC[:], in_=gtot[:], func=AF.Exp), reads=['gtot'], writes=['eC'])
        S.op('dve', lambda e: e.tensor_scalar(out=nbeta[:], in0=beta[:], scalar1=-1.0, scalar2=None, op0=ALU.mult), reads=['gbeta'], writes=['nbeta'])
        mS = [masks['lo_s'], masks['up_s']]; mSk = ['mklo_s', 'mkup_s']
        mIT = [masks['up_i'], masks['lo_i']]; mITk = ['mkup_i', 'mklo_i']
        for h in heads:
            with C.scope():
                qn = C.sb([128, NTOK], F32, 'qn'); kn = C.sb([128, NTOK], F32, 'kn')
                Vt = C.sb([128, NCH, 128], F32, 'gVt'); Kt = C.sb([128, NCH, 128], F32, 'gKt'); oacc = C.sb([128, NCH, 128], F32, 'goacc'); obw = C.sb([128, NCH, 128], F32, 'gobw')
                with C.scope():
                    tmp = C.sb([128, NTOK], F32, 'gtmp'); tmp2 = C.sb([128, NTOK], F32, 'gtmp2')
                    pW = [C.ps([128, 512], F32, 'pW') for _ in range(2)]; pTf = C.ps([128, 512], F32, 'pTf')

                    def l2n(dst, dkey, row0, scale):
                        S.dma('sp', dst[:], gT[row0:row0 + 128, :], reads=[('fm_out', row0 // 128), ('fm_out', row0 // 128, 1)], writes=[dkey])
                        S.op('pool', lambda e: e.tensor_tensor(out=tmp[:], in0=dst[:], in1=dst[:], op=ALU.mult), reads=[dkey], writes=['gtmp'])
                        for i in range(NT5):
                            w = min(512, NTOK - i * 512)
                            S.op('pe', lambda e, i=i, w=w: e.matmul(pW[i % 2][:, 0:w], lhsT=ones[:], rhs=tmp[:, i * 512:i * 512 + w], start=True, stop=True),
                                 reads=['gtmp', 'onesf'], writes=[f'pW{i % 2}'])
                            S.op('act', lambda e, i=i, w=w: e.activation(out=tmp2[:, i * 512:i * 512 + w], in_=pW[i % 2][:, 0:w], func=AF.Sqrt, bias=eps6[:, 0:1]),
                                 reads=[f'pW{i % 2}', 'eps6'], writes=['gtmp2'])
                        S.op('dve', lambda e: e.reciprocal(out=tmp2[:], in_=tmp2[:]), reads=['gtmp2'], writes=['gtmp2'])
                        S.op('dve', lambda e: e.scalar_tensor_tensor(out=dst[:], in0=dst[:], scalar=scale, in1=tmp2[:], op0=ALU.mult, op1=ALU.mult), reads=[dkey, 'gtmp2'], writes=[dkey])

                    def to_tok(src, skey, dst, dkey):
                        for c0 in range(0, NCH, 4):
                            n = min(4, NCH - c0)
                            for c in range(c0, c0 + n):
                                S.op('pe', lambda e, c=c, c0=c0: e.transpose(out=pTf[:, (c - c0) * 128:(c - c0 + 1) * 128], in_=src[:, c * 128:(c + 1) * 128], identity=ident[:]),
                                     reads=[skey, idk], writes=['pTf'])
                            S.op('act', lambda e, c0=c0, n=n: e.copy(out=dst[:, c0:c0 + n, :], in_=pTf[:, 0:n * 128].rearrange("p (c t) -> p c t", t=128)), reads=['pTf'], writes=[dkey])
                    l2n(qn, 'qn', h * 128, 128.0 ** -0.5)
                    l2n(kn, 'kn', 1024 + h * 128, 1.0)
                    to_tok(kn, 'kn', Kt, 'gKt')
                    S.dma('sp', tmp[:], gT[2048 + h * 128:2048 + (h + 1) * 128, :], reads=[('fm_out', 16 + h), ('fm_out', 16 + h, 1), 'gtmp'], writes=['gtmp'])
                    to_tok(tmp, 'gtmp', Vt, 'gVt')
                with C.scope():
                    St = [C.sb([128, 128], F32, 'gS') for _ in range(2)]
                    for d in range(2):
                        S.op('pool', lambda e, d=d: e.memset(St[d][:], 0.0), writes=[f'gS{d}'])
                    pGr = C.ps([128, 512], F32, 'pGr'); pKQ = C.ps([128, 512], F32, 'pKQ'); pN = C.ps([128, 512], F32, 'pN'); pT = C.ps([128, 512], F32, 'pT')
                    pC = C.ps([128, 512], F32, 'pC'); pD = C.ps([128, 512], F32, 'pD'); pNT = C.ps([128, 512], F32, 'pNT')
                    Gs = C.sb([128, 256], F32, 'Gs'); Dm = C.sb([128, 512], F32, 'Dm')
                    NsA = [C.sb([128, 512], F32, 'Ns') for _ in range(2)]; QKsA = [C.sb([128, 256], F32, 'QKs') for _ in range(2)]; qeA = [C.sb([128, 256], F32, 'qe') for _ in range(2)]
                    NpA = [[C.sb([128, 512], F32, 'NpB') for _ in range(2)] for _ in range(2)]; TtA = [C.sb([128, 256], F32, 'Tt') for _ in range(2)]
                    ktaA = [C.sb([128, 256], F32, 'kta') for _ in range(2)]; bVA = [C.sb([128, 256], F32, 'bV') for _ in range(2)]
                    Zs = C.sb([128, 256], F32, 'Zs'); Vn = C.sb([128, 256], F32, 'Vn')

                    def st_prep(i, q):
                        Ns, QKs, qe, Tt, kta, bV = NsA[q], QKsA[q], qeA[q], TtA[q], ktaA[q], bVA[q]
                        for d in range(2):
                            c = order[d][i]; ts = slice(c * 128, (c + 1) * 128); x = d * 8 + h
                            gcol = gam[:, c, x:x + 1]
                            S.op('dve', lambda e, d=d, c=c, x=x: e.tensor_scalar(out=Gs[:, d * 128:(d + 1) * 128], in0=tri[d][:], scalar1=g[:, c, x:x + 1], scalar2=None, op0=ALU.mult),
                                 reads=['gg', trik[d]], writes=[f'Gs{d}'])
                            S.op('pe', lambda e, d=d: e.matmul(pGr[:, d * 128:(d + 1) * 128], lhsT=ones[:], rhs=Gs[:, d * 128:(d + 1) * 128], start=True, stop=True),
                                 reads=[f'Gs{d}', 'onesf'], writes=['pGr'])
                            S.op('pe', lambda e, d=d, ts=ts: e.matmul(pKQ[:, d * 256:d * 256 + 128], lhsT=kn[:, ts], rhs=kn[:, ts], start=True, stop=True), reads=['kn'], writes=['pKQ'])
                            S.op('pe', lambda e, d=d, ts=ts: e.matmul(pKQ[:, d * 256 + 128:d * 256 + 256], lhsT=kn[:, ts], rhs=qn[:, ts], start=True, stop=True), reads=['kn', 'qn'], writes=['pKQ'])
                            S.op('dve', lambda e, d=d, gcol=gcol: e.tensor_scalar(out=Dm[:, d * 256:d * 256 + 128], in0=pGr[:, d * 128:(d + 1) * 128], scalar1=gcol, scalar2=0.0, op0=ALU.subtract, op1=ALU.max),
                                 reads=['pGr', 'gam'], writes=[f'Dm{d}'])
                            S.op('dve', lambda e, d=d, gcol=gcol: e.tensor_scalar(out=Dm[:, d * 256 + 128:d * 256 + 256], in0=pGr[:, d * 128:(d + 1) * 128], scalar1=gcol, scalar2=0.0, op0=ALU.subtract, op1=ALU.min),
                                 reads=['pGr', 'gam', f'Dm{d}'], writes=[f'Dm{d}'])
                            S.op('act', lambda e, d=d: e.activation(out=Dm[:, d * 256:d * 256 + 128], in_=Dm[:, d * 256:d * 256 + 128], func=AF.Exp, scale=-1.0), reads=[f'Dm{d}'], writes=[f'Dm{d}'])
                            S.op('act', lambda e, d=d: e.activation(out=Dm[:, d * 256 + 128:d * 256 + 256], in_=Dm[:, d * 256 + 128:d * 256 + 256], func=AF.Exp), reads=[f'Dm{d}'], writes=[f'Dm{d}'])
                            S.op('act', lambda e, d=d: e.activation(out=qe[:, d * 128:(d + 1) * 128], in_=pGr[:, d * 128:(d + 1) * 128], func=AF.Exp), reads=['pGr'], writes=[f'qe{q}{d}'])
                            S.op('dve', lambda e, d=d, ts=ts: e.tensor_tensor(out=qe[:, d * 128:(d + 1) * 128], in0=qe[:, d * 128:(d + 1) * 128], in1=qn[:, ts], op=ALU.mult), reads=[f'qe{q}{d}', 'qn'], writes=[f'qe{q}{d}'])
                            S.op('dve', lambda e, d=d: e.tensor_tensor(out=Dm[:, d * 256:d * 256 + 256], in0=pKQ[:, d * 256:d * 256 + 256], in1=Dm[:, d * 256:d * 256 + 256], op=ALU.mult),
                                 reads=['pKQ', f'Dm{d}'], writes=[f'Dm{d}'])
                            S.op('dve', lambda e, d=d, c=c, x=x: e.scalar_tensor_tensor(out=Ns[:, d * 256:d * 256 + 128], in0=Dm[:, d * 256:d * 256 + 128], scalar=nbeta[:, c, x:x + 1], in1=mS[d][:],
                                                                                       op0=ALU.mult, op1=ALU.mult), reads=[f'Dm{d}', 'nbeta', mSk[d]], writes=[f'Ns{q}{d}'])
                            S.op('pool', lambda e, d=d: e.tensor_tensor(out=QKs[:, d * 128:(d + 1) * 128], in0=Dm[:, d * 256 + 128:d * 256 + 256], in1=mIT[d][:], op=ALU.mult),
                                 reads=[f'Dm{d}', mITk[d]], writes=[f'QKs{q}{d}'])
                            S.op('pe', lambda e, d=d: e.transpose(out=pNT[:, d * 128:(d + 1) * 128], in_=Ns[:, d * 256:d * 256 + 128], identity=ident[:]), reads=[f'Ns{q}{d}', idk], writes=['pNT'])
                            S.op('act', lambda e, d=d: e.copy(out=Ns[:, d * 256 + 128:d * 256 + 256], in_=pNT[:, d * 128:(d + 1) * 128]), reads=['pNT', f'Ns{q}{d}'], writes=[f'Ns{q}{d}'])
                            S.op('pool', lambda e, d=d: e.tensor_tensor(out=Tt[:, d * 128:(d + 1) * 128], in0=Ns[:, d * 256 + 128:d * 256 + 256], in1=ident[:], op=ALU.add),
                                 reads=[f'Ns{q}{d}', idk], writes=[f'Tt{q}{d}'])
                            S.op('act', lambda e, d=d, c=c, x=x: e.activation(out=kta[:, d * 128:(d + 1) * 128], in_=Kt[:, c, :], func=AF.Copy, scale=etail[:, c, x:x + 1]),
                                 reads=['gKt', 'etail'], writes=[f'kta{q}{d}'])
                            S.op('act', lambda e, d=d, c=c, x=x: e.activation(out=bV[:, d * 128:(d + 1) * 128], in_=Vt[:, c, :], func=AF.Copy, scale=beta[:, c, x:x + 1]),
                                 reads=['gVt', 'gbeta'], writes=[f'bV{q}{d}'])

                    def st_neumann(lv, q):
                        Ns, Tt = NsA[q], TtA[q]
                        if lv == 1:
                            prev = [(Ns[:, d * 256:d * 256 + 128], Ns[:, d * 256 + 128:d * 256 + 256], f'Ns{q}{d}') for d in range(2)]
                        else:
                            pb_ = NpA[q][(lv - 1) % 2]
                            prev = [(pb_[:, d * 256:d * 256 + 128], pb_[:, d * 256 + 128:d * 256 + 256], f'NpB{q}{(lv - 1) % 2}') for d in range(2)]
                        nb = NpA[q][lv % 2]; nk = f'NpB{q}{lv % 2}'
                        for d in range(2):
                            Nv, NTv, pk = prev[d]
                            S.op('pe', lambda e, d=d, Nv=Nv, NTv=NTv: e.matmul(pN[:, d * 256:d * 256 + 128], lhsT=NTv, rhs=Nv, start=True, stop=True), reads=[pk], writes=['pN'])
                            if lv < 6:
                                S.op('pe', lambda e, d=d, Nv=Nv, NTv=NTv: e.matmul(pN[:, d * 256 + 128:d * 256 + 256], lhsT=Nv, rhs=NTv, start=True, stop=True), reads=[pk], writes=['pN'])
                        S.op('act', lambda e, nb=nb: e.copy(out=nb[:], in_=pN[:]), reads=['pN'], writes=[nk])
                        for d in range(2):
                            S.op('pe', lambda e, d=d, nb=nb: e.matmul(pT[:, d * 128:(d + 1) * 128], lhsT=nb[:, d * 256:d * 256 + 128], rhs=Tt[:, d * 128:(d + 1) * 128], start=True, stop=True),
                                 reads=[nk, f'Tt{q}{d}'], writes=['pT'])
                        S.op('dve', lambda e: e.tensor_tensor(out=Tt[:], in0=Tt[:], in1=pT[:, 0:256], op=ALU.add), reads=['pT', f'Tt{q}0', f'Tt{q}1'], writes=[f'Tt{q}0', f'Tt{q}1'])

                    def st_chain(k, i, q):
                        QKs, qe, Tt, kta, bV = QKsA[q], qeA[q], TtA[q], ktaA[q], bVA[q]
                        cs_ = [order[0][i], order[1][i]]
                        if k == 0:
                            for d in range(2):
                                c = cs_[d]; ts = slice(c * 128, (c + 1) * 128); x = d * 8 + h
                                S.op('pe', lambda e, d=d, ts=ts: e.matmul(pC[:, d * 128:(d + 1) * 128], lhsT=kn[:, ts], rhs=St[d][:], start=True, stop=True), reads=['kn', f'gS{d}'], writes=['pC'])
                                S.op('dve', lambda e, d=d, c=c, x=x: e.scalar_tensor_tensor(out=Zs[:, d * 128:(d + 1) * 128], in0=pC[:, d * 128:(d + 1) * 128], scalar=nbeg[:, c, x:x + 1],
                                                                                           in1=bV[:, d * 128:(d + 1) * 128], op0=ALU.mult, op1=ALU.add), reads=['pC', 'nbeg', f'bV{q}{d}'], writes=[f'Zs{d}'])
                        elif k == 1:
                            for d in range(2):
                                S.op('pe', lambda e, d=d: e.matmul(pC[:, 256 + d * 128:256 + (d + 1) * 128], lhsT=Tt[:, d * 128:(d + 1) * 128], rhs=Zs[:, d * 128:(d + 1) * 128], start=True, stop=True),
                                     reads=[f'Tt{q}0', f'Tt{q}1', f'Zs{d}'], writes=['pC'])
                            S.op('act', lambda e: e.copy(out=Vn[:], in_=pC[:, 256:512]), reads=['pC'], writes=['Vn'])
                        elif k == 2:
                            for d in range(2):
                                S.op('pe', lambda e, d=d: e.matmul(pD[:, d * 128:(d + 1) * 128], lhsT=qe[:, d * 128:(d + 1) * 128], rhs=St[d][:], start=True, stop=False), reads=[f'qe{q}{d}', f'gS{d}'], writes=['pD'])
                                S.op('pe', lambda e, d=d: e.matmul(pD[:, d * 128:(d + 1) * 128], lhsT=QKs[:, d * 128:(d + 1) * 128], rhs=Vn[:, d * 128:(d + 1) * 128], start=False, stop=True),
                                     reads=[f'QKs{q}{d}', 'Vn'], writes=['pD'])
                            S.op('dve', lambda e, c=cs_[0]: e.tensor_copy(out=oacc[:, c, :], in_=pD[:, 0:128]), reads=['pD'], writes=[('goacc', cs_[0], 0)])
                            S.op('dve', lambda e, c=cs_[1]: e.tensor_copy(out=obw[:, c, :], in_=pD[:, 128:256]), reads=['pD'], writes=[('gobw', cs_[1])])
                        else:
                            for d in range(2):
                                S.op('pe', lambda e, d=d: e.matmul(pD[:, 256 + d * 128:256 + (d + 1) * 128], lhsT=kta[:, d * 128:(d + 1) * 128], rhs=Vn[:, d * 128:(d + 1) * 128], start=True, stop=True),
                                     reads=[f'kta{q}{d}', 'Vn'], writes=['pD'])
                            for d in range(2):
                                c = cs_[d]; x = d * 8 + h
                                S.op('dve', lambda e, d=d, c=c, x=x: e.scalar_tensor_tensor(out=St[d][:], in0=St[d][:], scalar=eC[:, c, x:x + 1], in1=pD[:, 256 + d * 128:256 + (d + 1) * 128],
                                                                                           op0=ALU.mult, op1=ALU.add), reads=[f'gS{d}', 'eC', 'pD'], writes=[f'gS{d}'])

                    st_prep(0, 0)
                    for lv in range(1, 7):
                        st_neumann(lv, 0)
                    for i in range(NCH):
                        q = i % 2
                        C.bg_step()
                        has_next = i + 1 < NCH
                        if has_next:
                            st_prep(i + 1, 1 - q)
                        for k in range(4):
                            if has_next:
                                st_neumann(k + 1, 1 - q)
                            st_chain(k, i, q)
                        if has_next:
                            st_neumann(5, 1 - q)
                            st_neumann(6, 1 - q)
                with C.scope():
                    zt = C.sb([128, NCH, 128], F32, 'gzt'); sq = C.sb([128, NCH, 128], F32, 'gsq'); red = C.sb([128, NCH], F32, 'gred')
                    S.dma('sp', zt[:], yz.rearrange("(c t) f -> t c f", t=128)[:, :, h * 128:(h + 1) * 128], reads=[('tm_out', t) for t in range(NCH)], writes=['gzt'])
                    ok_ = [('goacc', c, 0) for c in range(NCH)]; bk_ = [('gobw', c) for c in range(NCH)]
                    S.op('dve', lambda e: e.tensor_tensor(out=oacc[:], in0=oacc[:], in1=obw[:], op=ALU.add), reads=ok_ + bk_, writes=['goall'])
                    S.op('pool', lambda e: e.tensor_tensor(out=sq[:], in0=oacc[:], in1=oacc[:], op=ALU.mult), reads=['goall'], writes=['gsq'])
                    S.op('dve', lambda e: e.tensor_reduce(out=red[:], in_=sq[:], axis=AX.X, op=ALU.add), reads=['gsq'], writes=['gred'])
                    S.op('act', lambda e: e.activation(out=red[:], in_=red[:], func=AF.Sqrt, scale=1.0 / 128, bias=eps6[:, 0:1]), reads=['gred', 'eps6'], writes=['gred'])
                    S.op('dve', lambda e: e.reciprocal(out=red[:], in_=red[:]), reads=['gred'], writes=['gred'])
                    S.op('dve', lambda e: e.tensor_tensor(out=oacc[:], in0=oacc[:], in1=red[:].unsqueeze(2).broadcast_to([128, NCH, 128]), op=ALU.mult), reads=['goall', 'gred'], writes=['goall'])
                    S.op('dve', lambda e: e.tensor_tensor(out=oacc[:], in0=oacc[:], in1=onorm[:].unsqueeze(1).broadcast_to([128, NCH, 128]), op=ALU.mult), reads=['goall', 'onorm'], writes=['goall'])
                    S.op('act', lambda e: e.activation(out=zt[:], in_=zt[:], func=AF.Silu), reads=['gzt'], writes=['gzt'])
                    S.op('dve', lambda e: e.tensor_tensor(out=oacc[:], in0=oacc[:], in1=zt[:], op=ALU.mult), reads=['goall', 'gzt'], writes=['goall'])
                    S.dma('sp', mixo.rearrange("(c t) f -> t c f", t=128)[:, :, h * 128:(h + 1) * 128], oacc[:], reads=['goall'], writes=[('mixo', 'att', 0), 'mixo'])


def build_gdn_test(NCH, heads):
    C = Ctx(); S = C.S
    NTOK = NCH * 128
    gT = C.dram("gT", [3072, NTOK], kind="ExternalInput"); yz = C.dram("yz", [NTOK, 1056], kind="ExternalInput")
    prm = dict(dt_bias=C.dram("dt_bias", [16], kind="ExternalInput"), A_log=C.dram("A_log", [16], kind="ExternalInput"), out_norm=C.dram("out_norm", [128], kind="ExternalInput"))
    mixo = C.dram("mixo", [NTOK, 1024], kind="ExternalOutput")
    ident, idk = C.ident(); masks = make_masks(C)
    emit_gdn(C, gT, yz, mixo, prm, NCH, heads, ident, idk, masks)
    S.finish()
    return C


def build_full(NCH=34):
    C = Ctx(); S = C.S
    NT = NCH; NTOK = NT * 128; NBLK = 2 * NT + 32
    ein = lambda n, s: C.dram(n, s, kind="ExternalInput")
    xin = ein("xin", [NTOK, D]); cin = ein("cin", [128, 16])
    adaw = [ein(f"adaw{l}", [D, 6 * D]) for l in range(2)]; adab = [ein(f"adab{l}", [128, 96]) for l in range(2)]; nrm = [ein(f"nrm{l}", [128, 16]) for l in range(2)]
    ev_w_in = ein("ev_w_in", [D, 2688]); mucol = ein("mucol", [128, 15])
    aprm = dict(q_norm=ein("q_norm", [64]), k_norm=ein("k_norm", [64]), sink=ein("sink", [8]), rope=ein("rope", [(NCH - 2) * 128, 128]))
    rprm = dict(rw_cols=ein("rw_cols", [128, 28]), gnwb=ein("gnwb", [2, 512]), dec_up=ein("dec_up", [128, 512]), iclr_up=ein("iclr_up", [128, 512]), gate_up=ein("gate_up", [128, 512]))
    wout = [ein("ev_w_out", [D, D]), ein("od_w_out", [D, D])]
    mprm = [dict(w_grp=ein(f"w_grp{l}", [D, 4]), b_grp=ein(f"b_grp{l}", [4]), w_exp=ein(f"w_exp{l}", [D, 32]), b_exp=ein(f"b_exp{l}", [32])) for l in range(2)]
    wgu = [ein(f"wgu{l}", [32, 1024, 1024]) for l in range(2)]; wdn = [ein(f"wdn{l}", [32, 512, 1024]) for l in range(2)]
    od_w_in = ein("od_w_in", [D, 4128]); convcol = ein("convcol", [128, 120])
    gprm = dict(dt_bias=ein("dt_bias", [16]), A_log=ein("A_log", [16]), out_norm=ein("out_norm", [128]))
    out = C.dram("out", [(NCH - 2) * 128, D], kind="ExternalOutput")
    xs = C.dram("xs", [NTOK, D]); mixo = C.dram("mixo_s", [NTOK, D]); frows = C.dram("frows", [NTOK, D])
    xrows = C.dram("xrows", [NBLK * 128, D]); yrows = C.dram("yrows", [NBLK * 128, D])
    yatt = C.dram("yatt", [NTOK, 768]); ybT = C.dram("ybT", [1920, NTOK]); gT = C.dram("gT", [3072, NTOK]); yz = C.dram("yz", [NTOK, 1056])
    mucols3 = C.dram("mucols3", [128, 45])
    ident, idk = C.ident(); masks = make_masks(C)
    wgub = [C.dram(f"wgub{l}", [32 * 128, 8 * 1024], BF16) for l in range(2)]; wdnb = [C.dram(f"wdnb{l}", [32 * 128, 4 * 1024], BF16) for l in range(2)]
    wck = [[], []]

    def convert_weights(l):
        for e_ in range(32):
            C.bgq.append(lambda e_=e_: S.dma('pool', wgub[l][e_ * 128:(e_ + 1) * 128, :].rearrange("p (k n) -> p k n", k=8), wgu[l][e_].rearrange("(k p) n -> p k n", p=128),
                                             writes=[('wconv', l, 'g', e_)])); wck[l].append(('wconv', l, 'g', e_))
            C.bgq.append(lambda e_=e_: S.dma('pool', wdnb[l][e_ * 128:(e_ + 1) * 128, :].rearrange("p (k n) -> p k n", k=4), wdn[l][e_].rearrange("(k p) n -> p k n", p=128),
                                             writes=[('wconv', l, 'd', e_)])); wck[l].append(('wconv', l, 'd', e_))
    convert_weights(0)
    for t in range(NT):
        S.dma(['sp', 'act'][t % 2], xs[t * 128:(t + 1) * 128, :], xin[t * 128:(t + 1) * 128, :], writes=[('xs', t)])
    with C.scope():
        M = emit_mods(C, adaw[0], adab[0], cin, nrm[0])
        with C.scope():
            mu3 = C.sb([128, 45], F32, 'mu3')
            S.dma('sp', mu3[:, 0:15], mucol, writes=['mu3'])
            S.op('dve', lambda e: e.tensor_scalar(out=mu3[:, 15:30], in0=mu3[:, 0:15], scalar1=-1.0, scalar2=1.0, op0=ALU.mult, op1=ALU.add), reads=['mu3'], writes=['mu3'])
            S.op('dve', lambda e: e.tensor_scalar(out=mu3[:, 30:45], in0=mu3[:, 0:15], scalar1=0.5, scalar2=None, op0=ALU.mult), reads=['mu3'], writes=['mu3'])
            S.dma('sp', mucols3, mu3[:], reads=['mu3'], writes=['mucols3'])
        emit_pre(C, M, xs, ev_w_in, 2688, NT, ident, idk, 768, 15, ybT, 0, 768, yatt, 'shift', mucols3)
        emit_attn(C, yatt, mixo, aprm, NCH, ident, idk, masks)
        scr = rwkv_scratch(C, NCH, [0])
        scr1 = {k: {P: v[0] for P in range(4)} for k, v in scr.items()}
        emit_rwkv(C, ybT, mixo, rprm, NCH, [0, 1, 2, 3], ident, idk, masks, scr1)
        C.bg_flush()
        prm = dict(mprm[0]); prm['rowscr'] = rowscr_alloc(C, 'l0')
        with C.scope():
            R = route_alloc(C, NT, NBLK)
            emit_post(C, M, mixo, xs, frows, wout[0], prm, NT, ident, idk, R)
            emit_route(C, R, NT, NBLK, masks)
            emit_moe(C, M, R, frows, xs, xrows, yrows, wgub[0], wdnb[0], prm, NT, NBLK, ident, idk, wkeys=wck[0])
    with C.scope():
        M = emit_mods(C, adaw[1], adab[1], cin, nrm[1])
        convert_weights(1)
        emit_pre(C, M, xs, od_w_in, 4128, NT, ident, idk, 0, 24, gT, 3072, 4128, yz, 'conv', convcol)
        emit_gdn(C, gT, yz, mixo, gprm, NCH, list(range(8)), ident, idk, masks)
        C.bg_flush()
        prm = dict(mprm[1]); prm['rowscr'] = rowscr_alloc(C, 'l1')
        with C.scope():
            R = route_alloc(C, NT, NBLK)
            emit_post(C, M, mixo, xs, frows, wout[1], prm, NT, ident, idk, R)
            emit_route(C, R, NT, NBLK, masks)
            emit_moe(C, M, R, frows, xs, xrows, yrows, wgub[1], wdnb[1], prm, NT, NBLK, ident, idk, out_dram=out, out_tiles=list(range(2, NT)), wkeys=wck[1])
    S.finish()
    return C


def full_inputs(inp, b, NCH=34):
    nl = (NCH - 2) * 128
    f32 = lambda a: np.ascontiguousarray(np.asarray(a, np.float32))
    m = {}
    m['xin'] = f32(np.concatenate([inp['ctx'][b], inp['x'][b][:nl]], axis=0))
    for l in range(2):
        cin, adab, nrm = mods_inputs(inp['c'][b], inp['c_ctx'], inp['ada_b'][l], inp['norm_mix'][l], inp['norm_ffn'][l])
        m['cin'] = cin; m[f'adab{l}'] = adab; m[f'nrm{l}'] = nrm
        m[f'adaw{l}'] = inp['ada_w'][l]
        m[f'w_grp{l}'] = inp['moe_w_grp'][l]; m[f'b_grp{l}'] = inp['moe_b_grp'][l]; m[f'w_exp{l}'] = inp['moe_w_exp'][l]; m[f'b_exp{l}'] = inp['moe_b_exp'][l]
        m[f'wgu{l}'] = inp['moe_w_gate_up'][l]; m[f'wdn{l}'] = inp['moe_w_down'][l]
    m['ev_w_in'] = inp['ev_w_in'][0]; m['mucol'] = col_layout(inp['ev_mu'][0], 15)
    m['q_norm'] = inp['ev_q_norm'][0]; m['k_norm'] = inp['ev_k_norm'][0]; m['sink'] = inp['ev_sink'][0]; m['rope'] = rope_table_host(nl)
    m.update(rwkv_params_host(inp, 0))
    m['ev_w_out'] = inp['ev_w_out'][0]; m['od_w_out'] = inp['od_w_out'][0]; m['od_w_in'] = inp['od_w_in'][0]
    cc = np.zeros((128, 120), np.float32)
    for j in range(5):
        cc[:, j * 24:(j + 1) * 24] = col_layout(inp['od_conv'][0][j], 24)
    m['convcol'] = cc
    m['dt_bias'] = f32(inp['od_dt_bias'][0].reshape(16)); m['A_log'] = f32(inp['od_A_log'][0].reshape(16)); m['out_norm'] = inp['od_out_norm'][0]
    return {k: f32(v) for k, v in m.items()}


_shared_cache = {}


def kernel(**inp):
    inp = {k: np.asarray(v) for k, v in inp.items()}
    C = build_full(34)
    maps = [full_inputs(inp, b) for b in range(4)]
    for b in range(1, 4):
        for k in maps[0]:
            if k not in ('xin', 'cin', 'adab0', 'adab1') and maps[b][k].shape == maps[0][k].shape and k not in ('xin',):
                if np.array_equal(maps[b][k], maps[0][k]):
                    maps[b][k] = maps[0][k]
    in_maps = [maps[c % 4] for c in range(NCORES)]
    res = run_bass_kernel_spmd(C.nc, in_maps, core_ids=list(range(NCORES))).results
    return np.stack([np.asarray(res[b]['out'], np.float32) for b in range(4)], axis=0)
```

```python
import numpy as np
import concourse.bass as bass
import concourse.mybir as mybir
from concourse.bass_utils import run_bass_kernel_spmd
from contextlib import ExitStack

F32 = mybir.dt.float32
BF16 = mybir.dt.bfloat16
I32 = mybir.dt.int32
U32 = mybir.dt.uint32
AF = mybir.ActivationFunctionType
ALU = mybir.AluOpType
AX = mybir.AxisListType

NDMA = 72
NPOOLSEM = 32
D = 1024
NCORES = 8


class Sched:
    def __init__(self, nc):
        self.nc = nc
        self.eng = {'pe': nc.tensor, 'act': nc.scalar, 'dve': nc.vector, 'pool': nc.gpsimd, 'sp': nc.sync}
        self.sem = {e: nc.alloc_semaphore(f"s_{e}") for e in self.eng}
        self.cnt = {e: 0 for e in self.eng}
        self.known = {e: {} for e in self.eng}
        self.snap = {e: [None] for e in self.eng}
        self.dsem = [nc.alloc_semaphore(f"d_{i}") for i in range(NDMA)]
        self.dcnt = [0] * NDMA
        self.dsnap = [[None] for _ in range(NDMA)]
        self.drr = 0
        self.prr = 0
        self.bufs = {}
        self.nwaits = 0
        self.nins = 0
        self._uid = 0

    def _sem_of(self, tok):
        if tok[0] == 'e':
            return self.sem[tok[1]], tok[2]
        return self.dsem[tok[1]], 16 * tok[2]

    def _snap_of(self, tok):
        if tok[0] == 'e':
            return self.snap[tok[1]][tok[2]]
        return self.dsnap[tok[1]][tok[2]]

    def _wait(self, e, tok):
        key = (tok[0], tok[1])
        kn = self.known[e]
        if kn.get(key, 0) >= tok[2]:
            return
        s, v = self._sem_of(tok)
        self.eng[e].wait_ge(s, v)
        self.nwaits += 1
        kn[key] = tok[2]
        sn = self._snap_of(tok)
        if sn:
            for k2, v2 in sn.items():
                if kn.get(k2, 0) < v2:
                    kn[k2] = v2

    def _deps(self, e, reads, writes):
        toks = []
        for r in reads:
            b = self.bufs.get(r)
            if b and b['w']:
                toks.append(b['w'])
        for w in writes:
            b = self.bufs.get(w)
            if b:
                if b['w']:
                    toks.append(b['w'])
                for t in b['r'].values():
                    toks.append(t)
        if e == 'pe':
            toks = [t for t in toks if not (t[0] == 'e' and t[1] == 'pe')]
        return toks

    def _record(self, tok, reads, writes):
        for r in reads:
            b = self.bufs.setdefault(r, {'w': None, 'r': {}})
            b['r'][(tok[0], tok[1])] = tok
        for w in writes:
            self.bufs[w] = {'w': tok, 'r': {}}

    def op(self, e, fn, reads=(), writes=()):
        for t in self._deps(e, reads, writes):
            self._wait(e, t)
        ins = fn(self.eng[e])
        self.cnt[e] += 1
        self.nins += 1
        ins.then_inc(self.sem[e], 1)
        tok = ('e', e, self.cnt[e])
        self.snap[e].append(dict(self.known[e]))
        self._record(tok, reads, writes)
        return tok

    def dma(self, q, out, in_, reads=(), writes=(), indirect=None, **kw):
        if q == 'pool':
            j = NDMA - NPOOLSEM + self.prr
            self.prr = (self.prr + 1) % NPOOLSEM
        else:
            j = self.drr
            self.drr = (self.drr + 1) % (NDMA - NPOOLSEM)
        if self.dcnt[j] > 0:
            self._wait(q, ('d', j, self.dcnt[j]))
        for t in self._deps(q, reads, writes):
            self._wait(q, t)
        if indirect is None:
            ins = self.eng[q].dma_start(out=out, in_=in_, **kw)
        else:
            ins = self.eng[q].indirect_dma_start(out=out, in_=in_, **indirect)
        self.dcnt[j] += 1
        self.nins += 1
        ins.then_inc(self.dsem[j], 16)
        tok = ('d', j, self.dcnt[j])
        self.dsnap[j].append(dict(self.known[q]))
        self._record(tok, reads, writes)
        return tok

    def join(self):
        toks = [('e', e2, self.cnt[e2]) for e2 in self.eng if self.cnt[e2] > 0]
        toks += [('d', j, self.dcnt[j]) for j in range(NDMA) if self.dcnt[j] > 0]
        for e in self.eng:
            for t in toks:
                self._wait(e, t)

    def finish(self, e='sp'):
        for e2 in self.eng:
            if self.cnt[e2] > 0:
                self._wait(e, ('e', e2, self.cnt[e2]))
        for j in range(NDMA):
            if self.dcnt[j] > 0:
                self._wait(e, ('d', j, self.dcnt[j]))


class Ctx:
    def __init__(self):
        self.nc = bass.Bass("TRN2", target_bir_lowering=False)
        self.S = Sched(self.nc)
        self.n = 0
        self.stack = []
        self.bgq = []

    def sb(self, shape, dt=F32, name=None):
        self.n += 1
        nm = f"{name or 'sb'}_{self.n}"
        if self.stack:
            return self.stack[-1].enter_context(self.nc.sbuf_tensor(nm, list(shape), dt))
        return self.nc.alloc_sbuf_tensor(nm, list(shape), dt)

    def ps(self, shape, dt=F32, name=None):
        self.n += 1
        nm = f"{name or 'ps'}_{self.n}"
        if self.stack:
            return self.stack[-1].enter_context(self.nc.psum_tensor(nm, list(shape), dt))
        return self.nc.alloc_psum_tensor(nm, list(shape), dt)

    def scope(self):
        C = self

        class _Sc:
            def __enter__(s2):
                st = ExitStack(); st.__enter__(); C.stack.append(st); return st

            def __exit__(s2, *a):
                st = C.stack.pop()
                C.S.join()
                return st.__exit__(*a)
        return _Sc()

    def bg_step(self, n=1):
        for _ in range(n):
            if self.bgq:
                self.bgq.pop(0)()

    def bg_flush(self):
        while self.bgq:
            self.bgq.pop(0)()

    def uid(self, base):
        self.n += 1
        return f"{base}#{self.n}"

    def dram(self, name, shape, dt=F32, kind="Internal"):
        return self.nc.dram_tensor(name, list(shape), dt, kind=kind).ap()

    def ident(self, dt=F32):
        S = self.S
        t = self.sb([128, 128], F32, 'ident')
        k = f'ident{self.n}'
        S.op('pool', lambda e: e.memset(t[:], 0.0), writes=[k])
        S.op('pool', lambda e: e.affine_select(out=t[:], in_=t[:], pattern=[[-1, 128]], compare_op=ALU.not_equal,
                                               fill=1.0, base=0, channel_multiplier=1), reads=[k], writes=[k])
        if dt != F32:
            t2 = self.sb([128, 128], dt, 'identb')
            k2 = k + 'b'
            S.op('dve', lambda e: e.tensor_copy(out=t2[:], in_=t[:]), reads=[k], writes=[k2])
            return t2, k2
        return t, k


def emit_mods(C, adaw, adab, cin, nrm):
    S = C.S
    cT = C.sb([128, 16]); cS = C.sb([128, 16]); sg = C.sb([128, 16])
    bia = C.sb([128, 96]); nm = C.sb([128, 16])
    modT = C.sb([128, 96], name='modT')
    S.dma('sp', cT[:], cin, writes=['cT'])
    S.dma('sp', bia[:], adab, writes=['bia'])
    S.dma('sp', nm[:], nrm, writes=['nm'])
    S.op('act', lambda e: e.activation(out=sg[:], in_=cT[:], func=AF.Sigmoid), reads=['cT'], writes=['sg'])
    S.op('dve', lambda e: e.tensor_tensor(out=cS[:], in0=cT[:], in1=sg[:], op=ALU.mult), reads=['cT', 'sg'], writes=['cS'])
    pm = C.ps([128, 96], name='pm')
    wb = [C.sb([128, 8, 512], F32, 'adaw') for _ in range(2)]
    it = 0
    for j in range(6):
        for hf in range(2):
            w = wb[it % 2]; wk = f'adaw{it % 2}'
            for k in range(8):
                S.dma(['sp', 'act'][k % 2], w[:, k, :], adaw[k * 128:(k + 1) * 128, j * 1024 + hf * 512: j * 1024 + hf * 512 + 512],
                      writes=[(wk, k)])
            for mm in range(4):
                m = hf * 4 + mm
                col = (j * 8 + m) * 2
                for k in range(8):
                    S.op('pe', lambda e, k=k, mm=mm, col=col, w=w: e.matmul(pm[:, col:col + 2], lhsT=w[:, k, mm * 128:(mm + 1) * 128],
                                                                          rhs=cS[:, 2 * k:2 * k + 2], start=(k == 0), stop=(k == 7)),
                         reads=[(wk, k), 'cS'], writes=['pm'])
            it += 1
    S.op('dve', lambda e: e.tensor_tensor(out=modT[:], in0=pm[:], in1=bia[:], op=ALU.add), reads=['pm', 'bia'], writes=['modT'])
    A1 = C.sb([128, 8, 2], name='A1'); A2 = C.sb([128, 8, 2], name='A2')
    m3 = modT[:].rearrange("p (j m v) -> p j m v", j=6, m=8)
    S.op('dve', lambda e: e.scalar_tensor_tensor(out=A1[:], in0=m3[:, 1], scalar=1.0, in1=nm[:, 0:8].unsqueeze(2).broadcast_to([128, 8, 2]),
                                                 op0=ALU.add, op1=ALU.mult), reads=['modT', 'nm'], writes=['A1'])
    S.op('dve', lambda e: e.scalar_tensor_tensor(out=A2[:], in0=m3[:, 4], scalar=1.0, in1=nm[:, 8:16].unsqueeze(2).broadcast_to([128, 8, 2]),
                                                 op0=ALU.add, op1=ALU.mult), reads=['modT', 'nm'], writes=['A2'])
    return dict(modT=modT, m3=m3, A1=A1, A2=A2)


def emit_normT(C, xt, xkey, hT, hkey, col0, A, B, v, ident, idk, pT, pTk, tmp, rr):
    S = C.S
    junk, ss, xn = tmp['junk'], tmp['ss'], tmp['xn']
    S.op('act', lambda e: e.activation(out=junk[:], in_=xt, func=AF.Square, accum_out=ss[:, 0:1]), reads=[xkey], writes=['junk', 'ss'])
    S.op('dve', lambda e: e.tensor_scalar(out=ss[:, 1:2], in0=ss[:, 0:1], scalar1=1.0 / D, scalar2=1e-6, op0=ALU.mult, op1=ALU.add),
         reads=['ss'], writes=['ss'])
    S.op('act', lambda e: e.activation(out=ss[:, 2:3], in_=ss[:, 1:2], func=AF.Sqrt), reads=['ss'], writes=['ss'])
    S.op('dve', lambda e: e.reciprocal(out=ss[:, 3:4], in_=ss[:, 2:3]), reads=['ss'], writes=['ss'])
    S.op('dve', lambda e: e.tensor_scalar(out=xn[:], in0=xt, scalar1=ss[:, 3:4], scalar2=None, op0=ALU.mult),
         reads=[xkey, 'ss'], writes=['xn'])
    for k in range(8):
        S.op('pe', lambda e, k=k: e.transpose(out=pT[:, k * 128:(k + 1) * 128], in_=xn[:, k * 128:(k + 1) * 128], identity=ident[:]),
             reads=['xn', idk], writes=[pTk])
    t2 = tmp['t2']
    p3 = pT[:].rearrange("p (k t) -> p k t", k=8)
    S.op('dve', lambda e: e.tensor_tensor(out=t2[:], in0=p3, in1=A[:, :, v:v + 1].broadcast_to([128, 8, 128]), op=ALU.mult),
         reads=[pTk, 'A1', 'A2'], writes=['t2'])
    S.op('pool', lambda e: e.tensor_tensor(out=hT[:, :, col0:col0 + 128], in0=t2[:], in1=B[:, :, v:v + 1].broadcast_to([128, 8, 128]), op=ALU.add),
         reads=['t2', 'modT'], writes=[hkey])


def load_w_bf16(C, dst, dkey, src, rows, cols, queues=('pool',)):
    S = C.S
    nk = rows // 128
    i = 0
    for k in range(nk):
        c0 = 0
        while c0 < cols:
            cw = min(2048, cols - c0)
            S.dma(queues[i % len(queues)], dst[:, k, c0:c0 + cw], src[k * 128:(k + 1) * 128, c0:c0 + cw], writes=[(dkey, k)])
            c0 += cw
            i += 1


def build_pre(NT, NOUT):
    C = Ctx(); S = C.S; nc = C.nc
    xin = C.dram("xin", [NT * 128, D], kind="ExternalInput")
    cin = C.dram("cin", [128, 16], kind="ExternalInput")
    adaw = C.dram("adaw", [D, 6 * D], kind="ExternalInput")
    adab = C.dram("adab", [128, 96], kind="ExternalInput")
    nrm = C.dram("nrm", [128, 16], kind="ExternalInput")
    win = C.dram("win", [D, NOUT], kind="ExternalInput")
    y = C.dram("y", [NT * 128, NOUT], kind="ExternalOutput")
    modo = C.dram("modo", [128, 96], kind="ExternalOutput")
    ident, idk = C.ident()
    M = emit_mods(C, adaw, adab, cin, nrm)
    S.dma('sp', modo, M['modT'][:], reads=['modT'], writes=['modo'])
    wbf = C.sb([128, 8, NOUT], BF16, 'wbf')
    load_w_bf16(C, wbf, 'wbf', win, D, NOUT)
    hT = C.sb([128, 8, NT * 128], BF16, 'hT')
    tmp = dict(junk=C.sb([128, D]), ss=C.sb([128, 4]), xn=C.sb([128, D]), t2=C.sb([128, 8, 128]))
    xb = [C.sb([128, D], F32, 'xb') for _ in range(2)]
    pT = [C.ps([128, 1024], F32, 'pT') for _ in range(2)]
    B1 = M['m3'][:, 0]
    for t in range(NT):
        v = 1 if t == 0 else 0
        S.dma('sp', xb[t % 2][:], xin[t * 128:(t + 1) * 128, :], writes=[f'xb{t % 2}'])
        emit_normT(C, xb[t % 2][:], f'xb{t % 2}', hT, ('hT', t), t * 128, M['A1'], B1, v, ident, idk, pT[t % 2], f'pT{t % 2}', tmp, t)
    pY = [C.ps([128, 512], F32, 'pY') for _ in range(3)]
    yb = [C.sb([128, NOUT], F32, 'yb') for _ in range(2)]
    ncc = (NOUT + 511) // 512
    it = 0
    for t in range(NT):
        for c in range(ncc):
            cw = min(512, NOUT - c * 512)
            p = pY[it % 3]; pk = f'pY{it % 3}'
            for k in range(8):
                S.op('pe', lambda e, k=k, p=p, c=c, cw=cw, t=t: e.matmul(p[:, 0:cw], lhsT=hT[:, k, t * 128:(t + 1) * 128],
                                                                        rhs=wbf[:, k, c * 512:c * 512 + cw], start=(k == 0), stop=(k == 7)),
                     reads=[('hT', t), ('wbf', k)], writes=[pk])
            if it % 2 == 0:
                S.op('act', lambda e, p=p, c=c, cw=cw, t=t: e.copy(out=yb[t % 2][:, c * 512:c * 512 + cw], in_=p[:, 0:cw]),
                     reads=[pk], writes=[f'yb{t % 2}'])
            else:
                S.op('dve', lambda e, p=p, c=c, cw=cw, t=t: e.tensor_copy(out=yb[t % 2][:, c * 512:c * 512 + cw], in_=p[:, 0:cw]),
                     reads=[pk], writes=[f'yb{t % 2}'])
            it += 1
        S.dma(['sp', 'act'][t % 2], y[t * 128:(t + 1) * 128, :], yb[t % 2][:], reads=[f'yb{t % 2}'], writes=[('y', t)])
    S.finish()
    return C


def col_layout(vec, nchunk):
    return np.ascontiguousarray(np.asarray(vec, np.float32).reshape(nchunk, 128).T)


def mods_inputs(c_b, c_ctx, ada_b_l, norm_mix_l, norm_ffn_l):
    cin = np.empty((128, 8, 2), np.float32)
    cin[:, :, 0] = col_layout(c_b, 8)
    cin[:, :, 1] = col_layout(c_ctx, 8)
    adab = np.repeat(col_layout(ada_b_l, 48)[:, :, None], 2, axis=2).reshape(128, 96)
    nrm = np.concatenate([col_layout(norm_mix_l, 8), col_layout(norm_ffn_l, 8)], axis=1)
    return cin.reshape(128, 16), np.ascontiguousarray(adab), np.ascontiguousarray(nrm)


def tok_shard(x_lat_b, x_ctx_b, s):
    return np.ascontiguousarray(np.concatenate([x_ctx_b[128 * s:128 * (s + 1)], x_lat_b[2048 * s:2048 * (s + 1)]], axis=0))


_cache = {}


def run(name, builder, in_maps):
    if name not in _cache:
        _cache[name] = builder()
    C = _cache[name]
    res = run_bass_kernel_spmd(C.nc, in_maps, core_ids=list(range(NCORES)))
    return res.results


def make_masks(C):
    S = C.S
    out = {}
    for nm, pat, cm, cmp in [('lo_s', -1, 1, ALU.is_gt), ('up_s', 1, -1, ALU.is_gt), ('lo_i', -1, 1, ALU.is_ge), ('up_i', 1, -1, ALU.is_ge)]:
        t = C.sb([128, 128], F32, 'mask' + nm)
        S.op('pool', lambda e, t=t: e.memset(t[:], 1.0), writes=['mk' + nm])
        S.op('pool', lambda e, t=t, pat=pat, cm=cm, cmp=cmp: e.affine_select(out=t[:], in_=t[:], pattern=[[pat, 128]], compare_op=cmp,
                                                                             fill=0.0, base=0, channel_multiplier=cm),
             reads=['mk' + nm], writes=['mk' + nm])
        out[nm] = t
    return out


def chunk_order(NCH, d):
    if d == 0:
        return list(range(NCH))
    return [1, 0] + list(range(NCH - 1, 1, -1))


def emit_rwkv(C, ybT, mixo, prm, NCH, pairs, ident, idk, masks, scr):
    S = C.S
    NTOK = NCH * 128
    NT5 = (NTOK + 511) // 512
    with C.scope():
        cols = C.sb([128, 28], F32, 'rwcols')
        S.dma('sp', cols[:], prm['rw_cols'], writes=['rwcols'])
        bsel = C.sb([128, 2], F32, 'bsel'); bones = C.sb([128, 128], F32, 'bones')
        S.op('pool', lambda e: e.memset(bsel[:], 0.0), writes=['bsel'])
        S.op('pool', lambda e: e.memset(bsel[0:64, 0:1], 1.0), reads=['bsel'], writes=['bsel'])
        S.op('pool', lambda e: e.memset(bsel[64:128, 1:2], 1.0), reads=['bsel'], writes=['bsel'])
        S.op('pool', lambda e: e.memset(bones[:], 0.0), writes=['bones'])
        S.op('pool', lambda e: e.memset(bones[0:64, 0:64], 1.0), reads=['bones'], writes=['bones'])
        S.op('pool', lambda e: e.memset(bones[64:128, 64:128], 1.0), reads=['bones'], writes=['bones'])
        epsc = C.sb([128, 1], F32, 'epsc')
        S.op('pool', lambda e: e.memset(epsc[:], 1e-6), writes=['eps'])
        mX = []; mY = []
        for d in range(2):
            Ms, MsT, MiT = (masks['lo_s'], masks['up_s'], masks['up_i']) if d == 0 else (masks['up_s'], masks['lo_s'], masks['lo_i'])
            mx = C.sb([128, 512], BF16, 'mX'); my = C.sb([128, 128], BF16, 'mY')
            mkr = ['mklo_s', 'mkup_s', 'mklo_i', 'mkup_i']
            S.op('dve', lambda e, mx=mx, Ms=Ms: e.tensor_scalar(out=mx[:, 0:128], in0=Ms[:], scalar1=-1.0, scalar2=None, op0=ALU.mult), reads=mkr, writes=[f'mX{d}'])
            S.op('dve', lambda e, mx=mx, MsT=MsT: e.tensor_scalar(out=mx[:, 128:256], in0=MsT[:], scalar1=-1.0, scalar2=None, op0=ALU.mult), reads=mkr + [f'mX{d}'], writes=[f'mX{d}'])
            S.op('dve', lambda e, mx=mx, MsT=MsT: e.tensor_copy(out=mx[:, 256:384], in_=MsT[:]), reads=mkr + [f'mX{d}'], writes=[f'mX{d}'])
            S.op('dve', lambda e, mx=mx, MiT=MiT: e.tensor_copy(out=mx[:, 384:512], in_=MiT[:]), reads=mkr + [f'mX{d}'], writes=[f'mX{d}'])
            S.op('dve', lambda e, my=my, MiT=MiT: e.tensor_copy(out=my[:], in_=MiT[:]), reads=mkr, writes=[f'mY{d}'])
            mX.append(mx); mY.append(my)
        for P in pairs:
            with C.scope():
                kkT = C.sb([128, NTOK], F32, 'kkT'); aT = C.sb([128, NTOK], F32, 'aT')
                Lam = C.sb([128, NTOK], F32, 'Lam'); lam = C.sb([128, NTOK], F32, 'lam'); tmp = C.sb([128, NTOK], F32, 'tmp')
                ob = C.sb([128, NTOK], F32, 'ob')
                rmask = C.sb([128, NTOK], BF16, 'rmask')
                decup = C.sb([128, 512], F32, 'decup'); iclrup = C.sb([128, 512], F32, 'iclrup')
                S.dma('sp', decup[:], prm['dec_up'], writes=['decup'])
                S.dma('sp', iclrup[:], prm['iclr_up'], writes=['iclrup'])
                S.op('pool', lambda e: e.memset(rmask[:], 1.0), writes=['rmask'])
                S.op('pool', lambda e: e.memset(rmask[:].rearrange("p (c t) -> p c t", t=128)[:, :, 0:1], 0.0), reads=['rmask'], writes=['rmask'])
                pW = [C.ps([128, 512], F32, 'pW') for _ in range(2)]
                pTf = C.ps([128, 512], F32, 'pTf')
                pBn = C.ps([128, 512], F32, 'pBn')
                tokb = C.sb([128, NCH, 128], F32, 'tokb')
                bon = C.sb([128, NCH, 2], F32, 'bon')
                PC = C.sb([128, NCH], F32, 'PC')
                kcol = lambda j: cols[:, j:j + 1]
                c3 = lambda t_: t_[:].rearrange("p (c t) -> p c t", t=128)

                def transpose_store(src, skey, dst_dram):
                    for c0 in range(0, NCH, 4):
                        n = min(4, NCH - c0)
                        for c in range(c0, c0 + n):
                            S.op('pe', lambda e, c=c, c0=c0: e.transpose(out=pTf[:, (c - c0) * 128:(c - c0 + 1) * 128], in_=src[:, c * 128:(c + 1) * 128],
                                                                         identity=ident[:]), reads=[skey, idk], writes=['pTf'])
                        S.op('act', lambda e, c0=c0, n=n: e.copy(out=tokb[:, c0:c0 + n, :], in_=pTf[:, 0:n * 128].rearrange("p (c t) -> p c t", t=128)),
                             reads=['pTf'], writes=['tokb'])
                    S.dma('sp', dst_dram.rearrange("(c t) f -> t c f", t=128), tokb[:], reads=['tokb'], writes=[('scr', id(dst_dram))])

                S.dma('sp', tmp[:], ybT[1024 + P * 128:1024 + (P + 1) * 128, :], writes=['tmp'])
                transpose_store(tmp, 'tmp', scr['Vt'][P])
                S.dma('sp', ob[:], ybT[512 + P * 128:512 + (P + 1) * 128, :], writes=['ob'])
                S.op('dve', lambda e: e.tensor_scalar(out=kkT[:], in0=ob[:], scalar1=kcol(P), scalar2=None, op0=ALU.mult), reads=['ob', 'rwcols'], writes=['kkT'])
                S.op('pool', lambda e: e.tensor_tensor(out=tmp[:], in0=kkT[:], in1=kkT[:], op=ALU.mult), reads=['kkT'], writes=['tmp'])
                for i in range(NT5):
                    w = min(512, NTOK - i * 512)
                    S.op('pe', lambda e, i=i, w=w: e.matmul(pW[i % 2][:, 0:w], lhsT=bones[:], rhs=tmp[:, i * 512:i * 512 + w], start=True, stop=True),
                         reads=['tmp', 'bones'], writes=[f'pW{i % 2}'])
                    S.op('act', lambda e, i=i, w=w: e.activation(out=lam[:, i * 512:i * 512 + w], in_=pW[i % 2][:, 0:w], func=AF.Sqrt, bias=epsc[:, 0:1]),
                         reads=[f'pW{i % 2}', 'eps'], writes=['lam'])
                S.op('dve', lambda e: e.reciprocal(out=tmp[:], in_=lam[:]), reads=['lam'], writes=['tmp'])
                S.op('dve', lambda e: e.tensor_tensor(out=kkT[:], in0=kkT[:], in1=tmp[:], op=ALU.mult), reads=['kkT', 'tmp'], writes=['kkT'])
                S.dma('sp', tmp[:], ybT[P * 128:(P + 1) * 128, :], reads=['tmp'], writes=['tmp'])
                S.op('dve', lambda e: e.scalar_tensor_tensor(out=lam[:], in0=tmp[:], scalar=kcol(8 + P), in1=ob[:], op0=ALU.mult, op1=ALU.mult),
                     reads=['tmp', 'ob', 'rwcols'], writes=['lam'])
                for c in range(NCH):
                    S.op('pe', lambda e, c=c: e.matmul(pBn[:, 2 * c:2 * c + 2], lhsT=lam[:, c * 128:(c + 1) * 128], rhs=bsel[:], start=True, stop=True),
                         reads=['lam', 'bsel'], writes=['pBn'])
                S.op('dve', lambda e: e.tensor_copy(out=bon[:].rearrange("p c h -> p (c h)"), in_=pBn[:, 0:2 * NCH]), reads=['pBn'], writes=['bon'])
                S.dma('sp', scr['bon'][P], bon[:], reads=['bon'], writes=[('scr', 'bon', P)])
                for d in range(2):
                    S.dma('sp', ob[:], ybT[1664:1792, :], reads=['ob'], writes=['ob'])
                    S.dma('sp', tmp[:], ybT[1536:1664, :], reads=['tmp'], writes=['tmp'])
                    S.op('act', lambda e: e.activation(out=tmp[:], in_=tmp[:], func=AF.Tanh), reads=['tmp'], writes=['tmp'])
                    for i in range(NT5):
                        w = min(512, NTOK - i * 512)
                        S.op('pe', lambda e, i=i, w=w, d=d: e.matmul(pW[0][:, 0:w], lhsT=iclrup[d * 64:(d + 1) * 64, P * 128:(P + 1) * 128],
                                                                   rhs=ob[d * 64:(d + 1) * 64, i * 512:i * 512 + w], start=True, stop=True),
                             reads=['iclrup', 'ob'], writes=['pW0'])
                        S.op('act', lambda e, i=i, w=w, d=d: e.activation(out=aT[:, i * 512:i * 512 + w], in_=pW[0][:, 0:w], func=AF.Sigmoid,
                                                                        bias=kcol(20 + d * 4 + P)), reads=['pW0', 'rwcols'], writes=['aT'])
                        S.op('pe', lambda e, i=i, w=w, d=d: e.matmul(pW[1][:, 0:w], lhsT=decup[d * 64:(d + 1) * 64, P * 128:(P + 1) * 128],
                                                                   rhs=tmp[d * 64:(d + 1) * 64, i * 512:i * 512 + w], start=True, stop=True),
                             reads=['decup', 'tmp'], writes=['pW1'])
                        S.op('act', lambda e, i=i, w=w, d=d: e.activation(out=lam[:, i * 512:i * 512 + w], in_=pW[1][:, 0:w], func=AF.Sigmoid,
                                                                        bias=kcol(12 + d * 4 + P)), reads=['pW1', 'rwcols'], writes=['lam'])
                    S.op('dve', lambda e: e.tensor_scalar(out=lam[:], in0=lam[:], scalar1=-0.6065306597126334, scalar2=None, op0=ALU.mult), reads=['lam'], writes=['lam'])
                    S.op('dve', lambda e: e.tensor_tensor_scan(out=Lam[:], data0=rmask[:], data1=lam[:], initial=0.0, op0=ALU.mult, op1=ALU.add),
                         reads=['rmask', 'lam'], writes=['Lam'])
                    L3 = c3(Lam)
                    S.op('act', lambda e: e.activation(out=PC[:], in_=L3[:, :, 127], func=AF.Exp), reads=['Lam'], writes=['PC'])
                    S.dma('sp', scr['PC'][P][d], PC[:], reads=['PC'], writes=[('scr', 'PC', P, d)])
                    if d == 1:
                        S.op('dve', lambda e: e.tensor_tensor(out=tmp[:], in0=lam[:], in1=Lam[:], op=ALU.subtract), reads=['lam', 'Lam'], writes=['tmp'])
                        S.op('dve', lambda e: e.tensor_tensor(out=L3, in0=c3(tmp), in1=L3[:, :, 127:128].broadcast_to([128, NCH, 128]), op=ALU.add),
                             reads=['tmp', 'Lam'], writes=['Lam'])
                    PC3 = PC[:].unsqueeze(2).broadcast_to([128, NCH, 128])
                    S.op('dve', lambda e: e.tensor_tensor(out=tmp[:], in0=Lam[:], in1=lam[:], op=ALU.subtract), reads=['Lam', 'lam'], writes=['tmp'])
                    S.op('act', lambda e: e.activation(out=tmp[:], in_=tmp[:], func=AF.Exp), reads=['tmp'], writes=['tmp'])
                    S.op('dve', lambda e: e.tensor_tensor(out=tmp[:], in0=kkT[:], in1=tmp[:], op=ALU.mult), reads=['kkT', 'tmp'], writes=['tmp'])
                    S.dma('sp', scr['KQ'][P][d], tmp[:], reads=['tmp'], writes=[('scr', 'KQ', P, d)])
                    S.dma('sp', ob[:], ybT[P * 128:(P + 1) * 128, :], reads=['ob'], writes=['ob'])
                    S.op('act', lambda e: e.activation(out=lam[:], in_=Lam[:], func=AF.Exp), reads=['Lam', 'lam'], writes=['lam'])
                    S.op('dve', lambda e: e.tensor_tensor(out=ob[:], in0=ob[:], in1=lam[:], op=ALU.mult), reads=['ob', 'lam'], writes=['ob'])
                    S.dma('sp', scr['RQ'][P][d], ob[:], reads=['ob'], writes=[('scr', 'RQ', P, d)])
                    S.op('act', lambda e: e.activation(out=lam[:], in_=Lam[:], func=AF.Exp, scale=-1.0), reads=['Lam', 'lam'], writes=['lam'])
                    S.op('dve', lambda e: e.tensor_tensor(out=tmp[:], in0=kkT[:], in1=aT[:], op=ALU.mult), reads=['kkT', 'aT', 'tmp'], writes=['tmp'])
                    S.op('dve', lambda e: e.tensor_tensor(out=tmp[:], in0=tmp[:], in1=lam[:], op=ALU.mult), reads=['tmp', 'lam'], writes=['tmp'])
                    S.dma('sp', scr['BD'][P][d], tmp[:], reads=['tmp'], writes=[('scr', 'BD', P, d)])
                    S.op('dve', lambda e: e.tensor_tensor(out=c3(ob), in0=c3(tmp), in1=PC3, op=ALU.mult), reads=['tmp', 'PC', 'ob'], writes=['ob'])
                    transpose_store(ob, 'ob', scr['BT'][P][d])
                    S.dma('sp', ob[:], ybT[512 + P * 128:512 + (P + 1) * 128, :], reads=['ob'], writes=['ob'])
                    S.op('dve', lambda e: e.tensor_scalar(out=tmp[:], in0=aT[:], scalar1=-1.0, scalar2=kcol(4 + P), op0=ALU.add, op1=ALU.mult),
                         reads=['aT', 'rwcols', 'tmp'], writes=['tmp'])
                    S.op('dve', lambda e: e.scalar_tensor_tensor(out=tmp[:], in0=tmp[:], scalar=1.0, in1=ob[:], op0=ALU.add, op1=ALU.mult),
                         reads=['tmp', 'ob'], writes=['tmp'])
                    S.op('dve', lambda e: e.tensor_tensor(out=tmp[:], in0=tmp[:], in1=lam[:], op=ALU.mult), reads=['tmp', 'lam'], writes=['tmp'])
                    S.dma('sp', scr['KD'][P][d], tmp[:], reads=['tmp'], writes=[('scr', 'KD', P, d)])
                    S.op('dve', lambda e: e.tensor_tensor(out=c3(ob), in0=c3(tmp), in1=PC3, op=ALU.mult), reads=['tmp', 'PC', 'ob'], writes=['ob'])
                    transpose_store(ob, 'ob', scr['KT'][P][d])
            with C.scope():
                oacc = C.sb([128, NCH, 128], F32, 'oacc')
                Vt = C.sb([128, NCH, 128], F32, 'Vt')
                S.dma('sp', Vt[:], scr['Vt'][P].rearrange("(c t) f -> t c f", t=128), reads=[('scr', id(scr['Vt'][P]))], writes=['Vt'])
                for d in range(2):
                    with C.scope():
                        RQ, KQ, BD, KD = [C.sb([128, NTOK], F32, n) for n in ('RQ', 'KQ', 'BD', 'KD')]
                        for nm_, t_ in (('RQ', RQ), ('KQ', KQ), ('BD', BD), ('KD', KD)):
                            S.dma('act', t_[:], scr[nm_][P][d], reads=[('scr', nm_, P, d)], writes=[nm_])
                        BT = C.sb([128, NCH, 128], F32, 'BT'); KT = C.sb([128, NCH, 128], F32, 'KT')
                        S.dma('sp', BT[:], scr['BT'][P][d].rearrange("(c t) f -> t c f", t=128), reads=[('scr', id(scr['BT'][P][d]))], writes=['BT'])
                        S.dma('sp', KT[:], scr['KT'][P][d].rearrange("(c t) f -> t c f", t=128), reads=[('scr', id(scr['KT'][P][d]))], writes=['KT'])
                        PCs = C.sb([128, NCH], F32, 'PCs')
                        S.dma('sp', PCs[:], scr['PC'][P][d], reads=[('scr', 'PC', P, d)], writes=['PCs'])
                        H = C.sb([128, 64], F32, 'H'); Hb = H
                        S.op('pool', lambda e: e.memset(H[:], 0.0), writes=['H'])
                        pX = [C.ps([128, 512], F32, 'pX') for _ in range(2)]
                        pY = C.ps([128, 512], F32, 'pY'); pN = C.ps([128, 512], F32, 'pN'); pT = C.ps([128, 512], F32, 'pT')
                        pC = C.ps([128, 512], F32, 'pC'); pD = C.ps([128, 512], F32, 'pD')
                        XsA = [[C.sb([128, 512], F32, 'Xs') for _ in range(2)] for _ in range(2)]
                        YsA = [C.sb([128, 256], F32, 'Ys') for _ in range(2)]
                        NpA = [[C.sb([128, 512], F32, 'NpB') for _ in range(2)] for _ in range(2)]
                        TtA = [C.sb([128, 256], F32, 'Tt') for _ in range(2)]
                        Zs = C.sb([128, 128], F32, 'Zs'); Us = C.sb([128, 128], F32, 'Us')
                        corder = chunk_order(NCH, d)

                        def st_intra(c, q):
                            Xs, Ys, Tt = XsA[q], YsA[q], TtA[q]
                            ts = slice(c * 128, (c + 1) * 128)
                            for h in range(2):
                                pb = 64 * h
                                fm = lambda X, pb=pb: X[pb:pb + 64, ts]
                                for blk, (l_, r_) in enumerate([(KQ, BD), (BD, KQ), (KD, KQ), (BD, RQ)]):
                                    S.op('pe', lambda e, h=h, blk=blk, l_=l_, r_=r_, fm=fm: e.matmul(pX[h][:, blk * 128:(blk + 1) * 128], lhsT=fm(l_), rhs=fm(r_),
                                                                                                    start=True, stop=True),
                                         reads=['RQ', 'KQ', 'BD', 'KD'], writes=[f'pX{h}'])
                                S.op('pe', lambda e, h=h, fm=fm: e.matmul(pY[:, h * 128:(h + 1) * 128], lhsT=fm(KD), rhs=fm(RQ), start=True, stop=True),
                                     reads=['RQ', 'KD'], writes=['pY'])
                                S.op('dve', lambda e, h=h: e.tensor_tensor(out=Xs[h][:], in0=pX[h][:], in1=mX[d][:], op=ALU.mult),
                                     reads=[f'pX{h}', f'mX{d}'], writes=[f'Xs{q}{h}'])
                                S.op('pool', lambda e, h=h: e.tensor_tensor(out=Tt[:, h * 128:(h + 1) * 128], in0=Xs[h][:, 128:256], in1=ident[:], op=ALU.add),
                                     reads=[f'Xs{q}{h}', idk], writes=[f'Tt{q}{h}'])
                            S.op('dve', lambda e: e.tensor_tensor(out=Ys[:].rearrange("p (h t) -> p h t", h=2), in0=pY[:, 0:256].rearrange("p (h t) -> p h t", h=2),
                                                                  in1=mY[d][:].unsqueeze(1).broadcast_to([128, 2, 128]), op=ALU.mult),
                                 reads=['pY', f'mY{d}'], writes=[f'Ys{q}'])

                        def st_neumann(lv, q):
                            Xs, Tt = XsA[q], TtA[q]
                            if lv == 1:
                                prev = [(Xs[h][:, 0:128], Xs[h][:, 128:256], f'Xs{q}{h}') for h in range(2)]
                            else:
                                pb_ = NpA[q][(lv - 1) % 2]
                                prev = [(pb_[:, h * 256:h * 256 + 128], pb_[:, h * 256 + 128:h * 256 + 256], f'NpB{q}{(lv - 1) % 2}') for h in range(2)]
                            nb = NpA[q][lv % 2]; nk = f'NpB{q}{lv % 2}'
                            for h in range(2):
                                Nv, NTv, pk = prev[h]
                                S.op('pe', lambda e, h=h, Nv=Nv, NTv=NTv: e.matmul(pN[:, h * 256:h * 256 + 128], lhsT=NTv, rhs=Nv, start=True, stop=True),
                                     reads=[pk], writes=['pN'])
                                if lv < 6:
                                    S.op('pe', lambda e, h=h, Nv=Nv, NTv=NTv: e.matmul(pN[:, h * 256 + 128:h * 256 + 256], lhsT=Nv, rhs=NTv, start=True, stop=True),
                                         reads=[pk], writes=['pN'])
                            S.op('act', lambda e, nb=nb: e.copy(out=nb[:], in_=pN[:]), reads=['pN'], writes=[nk])
                            for h in range(2):
                                S.op('pe', lambda e, h=h, nb=nb: e.matmul(pT[:, h * 128:(h + 1) * 128], lhsT=nb[:, h * 256:h * 256 + 128], rhs=Tt[:, h * 128:(h + 1) * 128],
                                                                         start=True, stop=True), reads=[nk, f'Tt{q}{h}'], writes=['pT'])
                            S.op('dve', lambda e: e.tensor_tensor(out=Tt[:], in0=Tt[:], in1=pT[:, 0:256], op=ALU.add), reads=['pT', f'Tt{q}0', f'Tt{q}1'], writes=[f'Tt{q}0', f'Tt{q}1'])

                        def st_chain(k, c, q):
                            Xs, Ys, Tt = XsA[q], YsA[q], TtA[q]
                            ts = slice(c * 128, (c + 1) * 128)
                            if k == 0:
                                for h in range(2):
                                    pb = 64 * h
                                    S.op('pe', lambda e, h=h, pb=pb: e.matmul(pC[:, h * 64:(h + 1) * 64], lhsT=KQ[pb:pb + 64, ts], rhs=Hb[pb:pb + 64, :], start=True, stop=False),
                                         reads=['KQ', 'H'], writes=['pC'])
                                    S.op('pe', lambda e, h=h: e.matmul(pC[:, h * 64:(h + 1) * 64], lhsT=Xs[h][:, 256:384], rhs=Vt[:, c, h * 64:(h + 1) * 64], start=False, stop=True),
                                         reads=[f'Xs{q}{h}', 'Vt'], writes=['pC'])
                                S.op('act', lambda e: e.mul(out=Zs[:], in_=pC[:, 0:128], mul=-1.0), reads=['pC'], writes=['Zs'])
                            elif k == 1:
                                for h in range(2):
                                    S.op('pe', lambda e, h=h: e.matmul(pC[:, 128 + h * 64:128 + (h + 1) * 64], lhsT=Tt[:, h * 128:(h + 1) * 128], rhs=Zs[:, h * 64:(h + 1) * 64],
                                                                      start=True, stop=True), reads=[f'Tt{q}0', f'Tt{q}1', 'Zs'], writes=['pC'])
                                S.op('act', lambda e: e.copy(out=Us[:], in_=pC[:, 128:256]), reads=['pC'], writes=['Us'])
                            elif k == 2:
                                for h in range(2):
                                    pb = 64 * h
                                    S.op('pe', lambda e, h=h, pb=pb: e.matmul(pD[:, h * 64:(h + 1) * 64], lhsT=RQ[pb:pb + 64, ts], rhs=Hb[pb:pb + 64, :], start=True, stop=False),
                                         reads=['RQ', 'H'], writes=['pD'])
                                    S.op('pe', lambda e, h=h: e.matmul(pD[:, h * 64:(h + 1) * 64], lhsT=Xs[h][:, 384:512], rhs=Us[:, h * 64:(h + 1) * 64], start=False, stop=False),
                                         reads=[f'Xs{q}{h}', 'Us'], writes=['pD'])
                                    S.op('pe', lambda e, h=h: e.matmul(pD[:, h * 64:(h + 1) * 64], lhsT=Ys[:, h * 128:(h + 1) * 128], rhs=Vt[:, c, h * 64:(h + 1) * 64], start=False, stop=True),
                                         reads=[f'Ys{q}', 'Vt'], writes=['pD'])
                                if d == 0:
                                    S.op('dve', lambda e: e.tensor_copy(out=oacc[:, c, :], in_=pD[:, 0:128]), reads=['pD'], writes=['oacc'])
                                else:
                                    S.op('dve', lambda e: e.tensor_tensor(out=oacc[:, c, :], in0=oacc[:, c, :], in1=pD[:, 0:128], op=ALU.add), reads=['pD', 'oacc'], writes=['oacc'])
                            else:
                                for h in range(2):
                                    pb = 64 * h
                                    S.op('pe', lambda e, h=h, pb=pb: e.matmul(pD[pb:pb + 64, 128:192], lhsT=BT[:, c, pb:pb + 64], rhs=Us[:, h * 64:(h + 1) * 64], start=True, stop=False),
                                         reads=['BT', 'Us'], writes=['pD'])
                                    S.op('pe', lambda e, h=h, pb=pb: e.matmul(pD[pb:pb + 64, 128:192], lhsT=KT[:, c, pb:pb + 64], rhs=Vt[:, c, h * 64:(h + 1) * 64], start=False, stop=True),
                                         reads=['KT', 'Vt'], writes=['pD'])
                                S.op('dve', lambda e: e.scalar_tensor_tensor(out=H[:], in0=H[:], scalar=PCs[:, c:c + 1], in1=pD[:, 128:192], op0=ALU.mult, op1=ALU.add),
                                     reads=['H', 'PCs', 'pD'], writes=['H'])

                        st_intra(corder[0], 0)
                        for lv in range(1, 7):
                            st_neumann(lv, 0)
                        for i, c in enumerate(corder):
                            q = i % 2
                            C.bg_step()
                            nxt = corder[i + 1] if i + 1 < NCH else None
                            if nxt is not None:
                                st_intra(nxt, 1 - q)
                            for k in range(4):
                                if nxt is not None:
                                    st_neumann(k + 1, 1 - q)
                                st_chain(k, c, q)
                            if nxt is not None:
                                st_neumann(5, 1 - q)
                                st_neumann(6, 1 - q)
                with C.scope():
                    o4 = oacc[:].rearrange("p c (h v) -> p (c h) v", h=2)
                    red = C.sb([128, NCH * 2], F32, 'red'); cen = C.sb([128, NCH * 2, 64], F32, 'cen'); sq = C.sb([128, NCH * 2, 64], F32, 'sq')
                    bon = C.sb([128, NCH * 2], F32, 'bonl'); epsg = C.sb([128, 1], F32, 'epsg')
                    pG = [C.ps([128, 512], F32, 'pG') for _ in range(2)]
                    gnwb = C.sb([128, 2, 512], F32, 'gnwb')
                    S.dma('sp', gnwb[:], prm['gnwb'].partition_broadcast(128), writes=['gnwb'])
                    gateup = C.sb([128, 512], BF16, 'gateup')
                    S.dma('pool', gateup[:], prm['gate_up'], writes=['gateup'])
                    sgd = C.sb([128, NTOK], BF16, 'sgd'); tl = C.sb([128, NTOK], F32, 'tl')
                    S.dma('sp', tl[:], ybT[1792:1920, :], writes=['tl'])
                    S.op('act', lambda e: e.activation(out=sgd[:], in_=tl[:], func=AF.Sigmoid), reads=['tl'], writes=['sgd'])
                    S.op('pool', lambda e: e.memset(epsg[:], 64e-5), writes=['epsg'])
                    S.dma('sp', bon[:], scr['bon'][P].rearrange("p c h -> p (c h)"), reads=[('scr', 'bon', P)], writes=['bonl'])
                    bc = lambda t_: t_[:].unsqueeze(2).broadcast_to([128, NCH * 2, 64])
                    S.op('dve', lambda e: e.tensor_reduce(out=red[:], in_=o4, axis=AX.X, op=ALU.add), reads=['oacc'], writes=['red'])
                    S.op('dve', lambda e: e.tensor_scalar(out=red[:], in0=red[:], scalar1=1.0 / 64, scalar2=None, op0=ALU.mult), reads=['red'], writes=['red'])
                    S.op('dve', lambda e: e.tensor_tensor(out=cen[:], in0=o4, in1=bc(red), op=ALU.subtract), reads=['oacc', 'red'], writes=['cen'])
                    S.op('pool', lambda e: e.tensor_tensor(out=sq[:], in0=cen[:], in1=cen[:], op=ALU.mult), reads=['cen'], writes=['sq'])
                    S.op('dve', lambda e: e.tensor_reduce(out=red[:], in_=sq[:], axis=AX.X, op=ALU.add), reads=['sq', 'red'], writes=['red'])
                    S.op('act', lambda e: e.activation(out=red[:], in_=red[:], func=AF.Sqrt, scale=1.0 / 64, bias=epsg[:, 0:1]), reads=['red', 'epsg'], writes=['red'])
                    S.op('dve', lambda e: e.reciprocal(out=red[:], in_=red[:]), reads=['red'], writes=['red'])
                    S.op('dve', lambda e: e.tensor_tensor(out=cen[:], in0=cen[:], in1=bc(red), op=ALU.mult), reads=['cen', 'red'], writes=['cen'])
                    c4 = cen[:].rearrange("p (c h) v -> p c (h v)", h=2)
                    gw = gnwb[:, 0, P * 128:(P + 1) * 128].unsqueeze(1).broadcast_to([128, NCH, 128])
                    gb = gnwb[:, 1, P * 128:(P + 1) * 128].unsqueeze(1).broadcast_to([128, NCH, 128])
                    S.op('dve', lambda e: e.tensor_tensor(out=c4, in0=c4, in1=gw, op=ALU.mult), reads=['cen', 'gnwb'], writes=['cen'])
                    S.op('dve', lambda e: e.tensor_tensor(out=c4, in0=c4, in1=gb, op=ALU.add), reads=['cen', 'gnwb'], writes=['cen'])
                    S.op('dve', lambda e: e.tensor_tensor(out=sq[:], in0=Vt[:].rearrange("p c (h v) -> p (c h) v", h=2), in1=bc(bon), op=ALU.mult),
                         reads=['Vt', 'bonl', 'sq'], writes=['sq'])
                    S.op('dve', lambda e: e.tensor_tensor(out=cen[:], in0=cen[:], in1=sq[:], op=ALU.add), reads=['cen', 'sq'], writes=['cen'])
                    for g0 in range(0, NCH, 4):
                        n = min(4, NCH - g0)
                        pg = pG[(g0 // 4) % 2]; pgk = f'pG{(g0 // 4) % 2}'
                        for c in range(g0, g0 + n):
                            S.op('pe', lambda e, c=c, g0=g0, pg=pg: e.matmul(pg[:, (c - g0) * 128:(c - g0 + 1) * 128], lhsT=sgd[:, c * 128:(c + 1) * 128],
                                                                            rhs=gateup[:, P * 128:(P + 1) * 128], start=True, stop=True),
                                 reads=['sgd', 'gateup'], writes=[pgk])
                        S.op('dve', lambda e, g0=g0, n=n, pg=pg: e.tensor_tensor(out=c4[:, g0:g0 + n, :], in0=c4[:, g0:g0 + n, :],
                                                                                in1=pg[:, 0:n * 128].rearrange("p (c f) -> p c f", f=128), op=ALU.mult),
                             reads=[pgk, 'cen'], writes=['cen'])
                    S.dma('sp', mixo.rearrange("(c t) f -> t c f", t=128)[:, :, 512 + P * 128:512 + (P + 1) * 128], c4, reads=['cen'], writes=[('mixo', 'rw', P)])


def rwkv_scratch(C, NCH, pairs):
    NTOK = NCH * 128
    scr = {k: {} for k in ('Vt', 'bon', 'PC', 'RQ', 'KQ', 'BD', 'KD', 'BT', 'KT')}
    for P in pairs:
        scr['Vt'][P] = C.dram(f"rw_Vt{P}", [NTOK, 128], F32)
        scr['bon'][P] = C.dram(f"rw_bon{P}", [128, NCH, 2], F32)
        for k in ('PC', 'RQ', 'KQ', 'BD', 'KD', 'BT', 'KT'):
            scr[k][P] = {}
        for d in range(2):
            scr['PC'][P][d] = C.dram(f"rw_PC{P}{d}", [128, NCH], F32)
            for k in ('RQ', 'KQ', 'BD', 'KD'):
                scr[k][P][d] = C.dram(f"rw_{k}{P}{d}", [128, NTOK], F32)
            for k in ('BT', 'KT'):
                scr[k][P][d] = C.dram(f"rw_{k}{P}{d}", [NTOK, 128], F32)
    return scr


def rwkv_params_host(inp, j=0):
    cols = np.zeros((128, 28), np.float32)
    cols[:, 0:4] = col_layout(inp['ev_k_k'][j].reshape(512), 4)
    cols[:, 4:8] = col_layout(inp['ev_k_a'][j].reshape(512), 4)
    cols[:, 8:12] = col_layout(inp['ev_r_k'][j].reshape(512), 4)
    for d in range(2):
        cols[:, 12 + d * 4:16 + d * 4] = col_layout(inp['ev_dec0'][j][d], 4)
        cols[:, 20 + d * 4:24 + d * 4] = col_layout(inp['ev_iclr0'][j][d], 4)
    gnwb = np.stack([inp['ev_gn_w'][j].reshape(512), inp['ev_gn_b'][j].reshape(512)]).astype(np.float32)
    return dict(rw_cols=cols, gnwb=gnwb, dec_up=np.ascontiguousarray(inp['ev_dec_up'][j].reshape(128, 512)),
                iclr_up=np.ascontiguousarray(inp['ev_iclr_up'][j].reshape(128, 512)), gate_up=np.ascontiguousarray(inp['ev_gate_up'][j]))


def build_rwkv_test(NCH, pairs):
    C = Ctx(); S = C.S
    NTOK = NCH * 128
    ybT = C.dram("ybT", [1920, NTOK], kind="ExternalInput")
    prm = dict(rw_cols=C.dram("rw_cols", [128, 28], kind="ExternalInput"), gnwb=C.dram("gnwb", [2, 512], kind="ExternalInput"),
               dec_up=C.dram("dec_up", [128, 512], kind="ExternalInput"), iclr_up=C.dram("iclr_up", [128, 512], kind="ExternalInput"),
               gate_up=C.dram("gate_up", [128, 512], kind="ExternalInput"))
    mixo = C.dram("mixo", [NTOK, 1024], kind="ExternalOutput")
    ident, idk = C.ident()
    masks = make_masks(C)
    scr = rwkv_scratch(C, NCH, pairs)
    emit_rwkv(C, ybT, mixo, prm, NCH, pairs, ident, idk, masks, scr)
    S.finish()
    return C


def emit_attn(C, yatt, mixo, prm, NCH, ident, idk, masks):
    S = C.S
    NTOK = NCH * 128
    with C.scope():
        gq = C.sb([128, 64], F32, 'gq'); gk = C.sb([128, 64], F32, 'gk'); esk = C.sb([128, 8], F32, 'esk')
        S.dma('sp', gq[:], prm['q_norm'].partition_broadcast(128), writes=['gq'])
        S.dma('sp', gk[:], prm['k_norm'].partition_broadcast(128), writes=['gk'])
        S.dma('sp', esk[:], prm['sink'].partition_broadcast(128), writes=['esk'])
        S.op('act', lambda e: e.activation(out=esk[:], in_=esk[:], func=AF.Exp), reads=['esk'], writes=['esk'])
        epsa = C.sb([128, 1], F32, 'epsa')
        S.op('pool', lambda e: e.memset(epsa[:], 1e-6), writes=['epsa'])
        qT = C.sb([64, 8, NTOK], BF16, 'qT'); kT = C.sb([64, 2, NTOK], BF16, 'kTa')
        Va = C.sb([128, NCH, 2, 65], BF16, 'Va')
        S.op('pool', lambda e: e.memset(Va[:], 1.0), writes=['Va'])
        with C.scope():
            yb = [C.sb([128, 768], F32, 'ya') for _ in range(2)]
            rc = [C.sb([128, 128], F32, 'rc') for _ in range(2)]
            sq = C.sb([128, 640], F32, 'sqa'); ss = C.sb([128, 10], F32, 'ssa'); xr = C.sb([128, 640], F32, 'xr'); t2 = C.sb([128, 640], F32, 't2a')
            pTq = [C.ps([64, 1024], F32, 'pTq') for _ in range(2)]; pTk = [C.ps([64, 256], F32, 'pTk') for _ in range(2)]
            for t in range(NCH):
                y = yb[t % 2]; yk = f'ya{t % 2}'
                S.dma('sp', y[:], yatt[t * 128:(t + 1) * 128, :], writes=[yk])
                x3 = y[:, 0:640].rearrange("p (h f) -> p h f", f=64)
                S.op('pool', lambda e, y=y: e.tensor_tensor(out=sq[:], in0=y[:, 0:640], in1=y[:, 0:640], op=ALU.mult), reads=[yk], writes=['sqa'])
                S.op('dve', lambda e: e.tensor_reduce(out=ss[:], in_=sq[:].rearrange("p (h f) -> p h f", f=64), axis=AX.X, op=ALU.add), reads=['sqa'], writes=['ssa'])
                S.op('act', lambda e: e.activation(out=ss[:], in_=ss[:], func=AF.Sqrt, scale=1.0 / 64, bias=epsa[:, 0:1]), reads=['ssa', 'epsa'], writes=['ssa'])
                S.op('dve', lambda e: e.reciprocal(out=ss[:], in_=ss[:]), reads=['ssa'], writes=['ssa'])
                x3r = xr[:].rearrange("p (h f) -> p h f", f=64)
                S.op('dve', lambda e, x3=x3: e.tensor_tensor(out=x3r, in0=x3, in1=ss[:].unsqueeze(2).broadcast_to([128, 10, 64]), op=ALU.mult), reads=[yk, 'ssa'], writes=['xr'])
                S.op('dve', lambda e: e.tensor_tensor(out=x3r[:, 0:8], in0=x3r[:, 0:8], in1=gq[:].unsqueeze(1).broadcast_to([128, 8, 64]), op=ALU.mult), reads=['xr', 'gq'], writes=['xr'])
                S.op('dve', lambda e: e.tensor_tensor(out=x3r[:, 8:10], in0=x3r[:, 8:10], in1=gk[:].unsqueeze(1).broadcast_to([128, 2, 64]), op=ALU.mult), reads=['xr', 'gk'], writes=['xr'])
                src = xr
                if t >= 2:
                    r = rc[t % 2]; rk = f'rc{t % 2}'
                    S.dma('act', r[:], prm['rope'][(t - 2) * 128:(t - 1) * 128, :], writes=[rk])
                    S.op('dve', lambda e, r=r: e.tensor_tensor(out=t2[:].rearrange("p (h f) -> p h f", f=64), in0=x3r,
                                                               in1=r[:, 0:64].unsqueeze(1).broadcast_to([128, 10, 64]), op=ALU.mult), reads=['xr', rk], writes=['t2a'])
                    x5 = xr[:].rearrange("p (h a j m) -> p (h a) j m", a=2, j=2, m=16)
                    s5 = r[:, 64:128].rearrange("p (a j m) -> p a j m", a=2, j=2)
                    sqv = sq[:].rearrange("p (h a j m) -> p (h a) j m", a=2, j=2, m=16)
                    for j in range(2):
                        sj = s5[:, :, j, :].unsqueeze(1).broadcast_to([128, 10, 2, 16]).rearrange("p h a m -> p (h a) m") if False else None
                        for a in range(2):
                            S.op('pool', lambda e, j=j, a=a, r=r: e.tensor_tensor(
                                out=sq[:].rearrange("p (h a j m) -> p h a j m", a=2, j=2, m=16)[:, :, a, j, :],
                                in0=xr[:].rearrange("p (h a j m) -> p h a j m", a=2, j=2, m=16)[:, :, a, 1 - j, :],
                                in1=r[:, 64:128].rearrange("p (a j m) -> p a j m", a=2, j=2)[:, a, j, :].unsqueeze(1).broadcast_to([128, 10, 16]), op=ALU.mult),
                                 reads=['xr', rk], writes=['sqa'])
                    S.op('dve', lambda e: e.tensor_tensor(out=t2[:], in0=t2[:], in1=sq[:], op=ALU.add), reads=['t2a', 'sqa'], writes=['t2a'])
                    src = t2
                sk = 't2a' if t >= 2 else 'xr'
                pq = pTq[t % 2]; pk = pTk[t % 2]
                for h in range(8):
                    S.op('pe', lambda e, h=h, src=src, pq=pq: e.transpose(out=pq[:, h * 128:(h + 1) * 128], in_=src[:, h * 64:(h + 1) * 64], identity=ident[:]),
                         reads=[sk, idk], writes=[f'pTq{t % 2}'])
                for h in range(2):
                    S.op('pe', lambda e, h=h, src=src, pk=pk: e.transpose(out=pk[:, h * 128:(h + 1) * 128], in_=src[:, 512 + h * 64:512 + (h + 1) * 64], identity=ident[:]),
                         reads=[sk, idk], writes=[f'pTk{t % 2}'])
                S.op('act', lambda e, pq=pq, t=t: e.mul(out=qT[:, :, t * 128:(t + 1) * 128], in_=pq[:].rearrange("p (h t) -> p h t", t=128), mul=0.125),
                     reads=[f'pTq{t % 2}'], writes=[('qT', t)])
                S.op('act', lambda e, pk=pk, t=t: e.copy(out=kT[:, :, t * 128:(t + 1) * 128], in_=pk[:].rearrange("p (h t) -> p h t", t=128)),
                     reads=[f'pTk{t % 2}'], writes=[('kTa', t)])
                S.op('dve', lambda e, y=y, t=t: e.tensor_copy(out=Va[:, t, :, 0:64], in_=y[:, 640:768].rearrange("p (h f) -> p h f", f=64)), reads=[yk, 'Va'], writes=[('Va', t)])
        with C.scope():
            pS = [C.ps([128, 512], F32, 'pS') for _ in range(3)]
            pO = [C.ps([128, 260], F32, 'pO') for _ in range(2)]
            Pt = [C.sb([128, 5, 512], BF16, 'Pt') for _ in range(2)]
            den = C.sb([128, 4], F32, 'den'); ob = [C.sb([128, 512], F32, 'oba') for _ in range(2)]
            it = 0
            for tq in range(NCH):
                if tq < 2:
                    keys = [(0, None), (1, None)]
                else:
                    keys = [(0, None), (1, None)]
                    if tq - 1 >= 2:
                        keys.append((tq - 1, 'lo_i'))
                    keys.append((tq, None))
                    if tq + 1 < NCH:
                        keys.append((tq + 1, 'up_i'))
                o_t = ob[tq % 2]; ok_ = f'oba{tq % 2}'
                for g in range(2):
                    P_ = Pt[it % 2]; Pk = f'Pt{it % 2}'
                    for i, (tk, mk) in enumerate(keys):
                        ps = pS[i % 3]; psk = f'pS{i % 3}'
                        S.op('pe', lambda e, ps=ps, tk=tk, g=g, tq=tq: e.matmul(ps[:], lhsT=kT[:, g, tk * 128:(tk + 1) * 128], rhs=qT[:, 4 * g:4 * g + 4, tq * 128:(tq + 1) * 128],
                                                                             start=True, stop=True), reads=[('kTa', tk), ('qT', tq)], writes=[psk])
                        S.op('act', lambda e, ps=ps, i=i, P_=P_: e.activation(out=P_[:, i, :], in_=ps[:], func=AF.Exp), reads=[psk], writes=[(Pk, i)])
                        if mk:
                            S.op('pool', lambda e, i=i, P_=P_, mk=mk: e.tensor_tensor(out=P_[:, i, :].rearrange("p (h q) -> p h q", h=4), in0=P_[:, i, :].rearrange("p (h q) -> p h q", h=4),
                                                                                      in1=masks[mk][:].unsqueeze(1).broadcast_to([128, 4, 128]), op=ALU.mult),
                                 reads=[(Pk, i), 'mk' + mk], writes=[(Pk, i)])
                    po = pO[it % 2]; pok = f'pO{it % 2}'
                    for h in range(4):
                        for i, (tk, mk) in enumerate(keys):
                            S.op('pe', lambda e, h=h, i=i, tk=tk, po=po, P_=P_, g=g: e.matmul(po[:, h * 65:(h + 1) * 65], lhsT=P_[:, i, h * 128:(h + 1) * 128], rhs=Va[:, tk, g, :],
                                                                                            start=(i == 0), stop=(i == len(keys) - 1)),
                                 reads=[(Pk, i), ('Va', tk)], writes=[pok])
                    po3 = po[:].rearrange("p (h f) -> p h f", f=65)
                    S.op('dve', lambda e, po3=po3, g=g: e.tensor_tensor(out=den[:], in0=po3[:, :, 64], in1=esk[:, 4 * g:4 * g + 4], op=ALU.add), reads=[pok, 'esk'], writes=['den'])
                    S.op('dve', lambda e: e.reciprocal(out=den[:], in_=den[:]), reads=['den'], writes=['den'])
                    S.op('dve', lambda e, po3=po3, g=g, o_t=o_t: e.tensor_tensor(out=o_t[:, g * 256:(g + 1) * 256].rearrange("p (h f) -> p h f", f=64), in0=po3[:, :, 0:64],
                                                                               in1=den[:].unsqueeze(2).broadcast_to([128, 4, 64]), op=ALU.mult),
                         reads=[pok, 'den'], writes=[ok_])
                    it += 1
                S.dma('sp', mixo[tq * 128:(tq + 1) * 128, 0:512], o_t[:], reads=[ok_], writes=[('mixo', 'att', tq)])


def rope_table_host(nlat):
    pos = np.arange(nlat)
    row = (pos // 64).astype(np.float32); col = (pos % 64).astype(np.float32)
    inv = (10000.0 ** (-np.arange(0, 32, 2, dtype=np.float32) / 32)).astype(np.float32)
    ar = row[:, None] * inv[None, :]; ac = col[:, None] * inv[None, :]
    cr, sr, cc, sc = np.cos(ar), np.sin(ar), np.cos(ac), np.sin(ac)
    return np.ascontiguousarray(np.concatenate([cr, cr, cc, cc, -sr, sr, -sc, sc], axis=1).astype(np.float32))


def build_attn_test(NCH):
    C = Ctx(); S = C.S
    NTOK = NCH * 128
    yatt = C.dram("yatt", [NTOK, 768], kind="ExternalInput")
    prm = dict(q_norm=C.dram("q_norm", [64], kind="ExternalInput"), k_norm=C.dram("k_norm", [64], kind="ExternalInput"),
               sink=C.dram("sink", [8], kind="ExternalInput"), rope=C.dram("rope", [(NCH - 2) * 128, 128], kind="ExternalInput"))
    mixo = C.dram("mixo", [NTOK, 1024], kind="ExternalOutput")
    ident, idk = C.ident()
    masks = make_masks(C)
    emit_attn(C, yatt, mixo, prm, NCH, ident, idk, masks)
    S.finish()
    return C


def emit_rowbcast(C, col_ap, col_key, dst, dkey, scratch):
    S = C.S
    S.dma('sp', scratch.rearrange("(m p) -> p m", p=128), col_ap, reads=[col_key], writes=[('rowscr', id(scratch))], allow_slow_non_contiguous=True)
    S.dma('sp', dst, scratch.partition_broadcast(128), reads=[('rowscr', id(scratch))], writes=[dkey])


def emit_post(C, M, mixo, xs, frows, wout, prm, NT, ident, idk, R):
    S = C.S
    with C.scope():
        wbf = C.sb([128, 8, 1024], BF16, 'woutbf')
        load_w_bf16(C, wbf, 'woutbf', wout, D, 1024)
        wr = C.sb([128, 8, 36], F32, 'wr')
        S.dma('sp', wr[:, :, 0:4], prm['w_grp'].rearrange("(k p) g -> p k g", p=128), writes=['wr'])
        S.dma('sp', wr[:, :, 4:36], prm['w_exp'].rearrange("(k p) g -> p k g", p=128), reads=['wr'], writes=['wr'])
        brow = C.sb([128, 36], F32, 'brow')
        S.dma('sp', brow[:, 0:4], prm['b_grp'].partition_broadcast(128), writes=['brow'])
        S.dma('sp', brow[:, 4:36], prm['b_exp'].partition_broadcast(128), reads=['brow'], writes=['brow'])
        rows = {}
        for nm, col in (('G1', M['m3'][:, 2]), ('A2', None), ('B2', M['m3'][:, 3])):
            for v in range(2):
                t_ = C.sb([128, 1024], F32, f'row{nm}{v}')
                ca = M['A2'][:, :, v] if nm == 'A2' else col[:, :, v]
                emit_rowbcast(C, ca, 'A2' if nm == 'A2' else 'modT', t_[:], f'row{nm}{v}', prm['rowscr'][(nm, v)])
                rows[(nm, v)] = t_
        mt = [C.sb([128, 1024], F32, 'mt') for _ in range(2)]
        xt = [C.sb([128, 1024], F32, 'xt') for _ in range(2)]
        ft = [C.sb([128, 1024], F32, 'ft') for _ in range(2)]
        mT = C.sb([128, 8, 128], BF16, 'mT'); fT = C.sb([128, 8, 128], F32, 'fT')
        junk = C.sb([128, 1024], F32, 'junk'); ss = C.sb([128, 4], F32, 'ssp'); tmpm = C.sb([128, 1024], F32, 'tmpm')
        pT = C.ps([128, 1024], F32, 'pTp'); pM = [C.ps([128, 512], F32, 'pM') for _ in range(2)]
        pT2 = C.ps([128, 1024], F32, 'pTp2'); pL = C.ps([128, 512], F32, 'pL')
        for t in range(NT):
            v = 1 if t < 2 else 0
            m_ = mt[t % 2]; x_ = xt[t % 2]; f_ = ft[t % 2]
            mk, xk, fk = f'mt{t % 2}', f'xt{t % 2}', f'ft{t % 2}'
            S.dma('sp', m_[:], mixo[t * 128:(t + 1) * 128, :], reads=[('mixo', 'att', t)] + [('mixo', 'rw', P) for P in range(4)] + ['mixo'], writes=[mk])
            S.dma('act', x_[:], xs[t * 128:(t + 1) * 128, :], reads=[('xs', t)], writes=[xk])
            for k in range(8):
                S.op('pe', lambda e, k=k, m_=m_: e.transpose(out=pT[:, k * 128:(k + 1) * 128], in_=m_[:, k * 128:(k + 1) * 128], identity=ident[:]),
                     reads=[mk, idk], writes=['pTp'])
            S.op('act', lambda e: e.copy(out=mT[:], in_=pT[:].rearrange("p (k t) -> p k t", k=8)), reads=['pTp'], writes=['mT'])
            for hf in range(2):
                for k in range(8):
                    S.op('pe', lambda e, k=k, hf=hf: e.matmul(pM[hf][:], lhsT=mT[:, k, :], rhs=wbf[:, k, hf * 512:(hf + 1) * 512], start=(k == 0), stop=(k == 7)),
                         reads=['mT', ('woutbf', k)], writes=[f'pM{hf}'])
                S.op('dve', lambda e, hf=hf: e.tensor_tensor(out=tmpm[:, hf * 512:(hf + 1) * 512], in0=pM[hf][:], in1=rows[('G1', v)][:, hf * 512:(hf + 1) * 512], op=ALU.mult),
                     reads=[f'pM{hf}', f'rowG1{v}'], writes=['tmpm'])
            S.op('pool', lambda e, x_=x_: e.tensor_tensor(out=x_[:], in0=x_[:], in1=tmpm[:], op=ALU.add), reads=[xk, 'tmpm'], writes=[xk])
            S.dma('act', xs[t * 128:(t + 1) * 128, :], x_[:], reads=[xk], writes=[('xs', t)])
            S.op('act', lambda e, x_=x_: e.activation(out=junk[:], in_=x_[:], func=AF.Square, accum_out=ss[:, 0:1]), reads=[xk], writes=['junk', 'ssp'])
            S.op('dve', lambda e: e.tensor_scalar(out=ss[:, 1:2], in0=ss[:, 0:1], scalar1=1.0 / D, scalar2=1e-6, op0=ALU.mult, op1=ALU.add), reads=['ssp'], writes=['ssp'])
            S.op('act', lambda e: e.activation(out=ss[:, 2:3], in_=ss[:, 1:2], func=AF.Sqrt), reads=['ssp'], writes=['ssp'])
            S.op('dve', lambda e: e.reciprocal(out=ss[:, 3:4], in_=ss[:, 2:3]), reads=['ssp'], writes=['ssp'])
            S.op('dve', lambda e, x_=x_, f_=f_: e.scalar_tensor_tensor(out=f_[:], in0=x_[:], scalar=ss[:, 3:4], in1=rows[('A2', v)][:], op0=ALU.mult, op1=ALU.mult),
                 reads=[xk, 'ssp', f'rowA2{v}'], writes=[fk])
            S.op('pool', lambda e, f_=f_: e.tensor_tensor(out=f_[:], in0=f_[:], in1=rows[('B2', v)][:], op=ALU.add), reads=[fk, f'rowB2{v}'], writes=[fk])
            S.dma('sp', frows[t * 128:(t + 1) * 128, :], f_[:], reads=[fk], writes=[('frows', t)])
            for k in range(8):
                S.op('pe', lambda e, k=k, f_=f_: e.transpose(out=pT2[:, k * 128:(k + 1) * 128], in_=f_[:, k * 128:(k + 1) * 128], identity=ident[:]),
                     reads=[fk, idk], writes=['pTp2'])
            S.op('act', lambda e: e.copy(out=fT[:], in_=pT2[:].rearrange("p (k t) -> p k t", k=8)), reads=['pTp2'], writes=['fT'])
            for k in range(8):
                S.op('pe', lambda e, k=k: e.matmul(pL[:, 0:36], lhsT=fT[:, k, :], rhs=wr[:, k, :], start=(k == 0), stop=(k == 7)), reads=['fT', 'wr'], writes=['pL'])
            S.op('dve', lambda e, t=t: e.tensor_tensor(out=R['lgall'][:, t, :], in0=pL[:, 0:36], in1=brow[:], op=ALU.add), reads=['pL', 'brow'], writes=['lgall'])


def emit_route(C, R, NT, NBLK, masks):
    S = C.S
    lg = R['lgall']
    with C.scope():
        BIG = 1.0e30
        t4 = C.sb([128, NT, 4], F32, 't4'); ohg = C.sb([128, NT, 4], F32, 'ohg'); gmax = C.sb([128, NT], F32, 'gmax'); pg = C.sb([128, NT], F32, 'pg')
        me = C.sb([128, NT, 32], F32, 'me'); me2 = C.sb([128, NT, 32], F32, 'me2'); m1 = C.sb([128, NT], F32, 'm1'); m2 = C.sb([128, NT], F32, 'm2')
        oh1 = C.sb([128, NT, 32], F32, 'oh1'); oh2 = C.sb([128, NT, 32], F32, 'oh2'); ohb = C.sb([128, NT, 32], BF16, 'ohb')
        e21 = C.sb([128, NT], F32, 'e21'); g1 = C.sb([128, NT], F32, 'g1')
        lgg = lg[:, :, 0:4]; lge = lg[:, :, 4:36]
        bN = lambda t_, n: t_[:].unsqueeze(2).broadcast_to([128, NT, n])
        op = S.op
        op('dve', lambda e: e.tensor_reduce(out=gmax[:], in_=lgg, axis=AX.X, op=ALU.max), reads=['lgall'], writes=['gmax'])
        op('dve', lambda e: e.tensor_tensor(out=ohg[:], in0=lgg, in1=bN(gmax, 4), op=ALU.is_equal), reads=['lgall', 'gmax'], writes=['ohg'])
        op('dve', lambda e: e.tensor_tensor(out=t4[:], in0=lgg, in1=bN(gmax, 4), op=ALU.subtract), reads=['lgall', 'gmax'], writes=['t4'])
        op('act', lambda e: e.activation(out=t4[:], in_=t4[:], func=AF.Exp), reads=['t4'], writes=['t4'])
        op('dve', lambda e: e.tensor_reduce(out=pg[:], in_=t4[:], axis=AX.X, op=ALU.add), reads=['t4'], writes=['pg'])
        op('dve', lambda e: e.reciprocal(out=pg[:], in_=pg[:]), reads=['pg'], writes=['pg'])
        op('dve', lambda e: e.tensor_scalar(out=t4[:], in0=ohg[:], scalar1=-1.0, scalar2=BIG, op0=ALU.add, op1=ALU.mult), reads=['ohg', 't4'], writes=['t4'])
        op('dve', lambda e: e.tensor_tensor(out=me[:].rearrange("p t (g x) -> p t g x", g=4), in0=lge.rearrange("p t (g x) -> p t g x", g=4),
                                            in1=t4[:].unsqueeze(3).broadcast_to([128, NT, 4, 8]), op=ALU.add), reads=['lgall', 't4'], writes=['me'])
        op('dve', lambda e: e.tensor_reduce(out=m1[:], in_=me[:], axis=AX.X, op=ALU.max), reads=['me'], writes=['m1'])
        op('dve', lambda e: e.tensor_tensor(out=oh1[:], in0=me[:], in1=bN(m1, 32), op=ALU.is_equal), reads=['me', 'm1'], writes=['oh1'])
        op('dve', lambda e: e.scalar_tensor_tensor(out=me2[:], in0=oh1[:], scalar=-BIG, in1=me[:], op0=ALU.mult, op1=ALU.add), reads=['oh1', 'me'], writes=['me2'])
        op('dve', lambda e: e.tensor_reduce(out=m2[:], in_=me2[:], axis=AX.X, op=ALU.max), reads=['me2'], writes=['m2'])
        op('dve', lambda e: e.tensor_tensor(out=oh2[:], in0=me2[:], in1=bN(m2, 32), op=ALU.is_equal), reads=['me2', 'm2'], writes=['oh2'])
        op('dve', lambda e: e.tensor_tensor(out=e21[:], in0=m2[:], in1=m1[:], op=ALU.subtract), reads=['m1', 'm2'], writes=['e21'])
        op('act', lambda e: e.activation(out=e21[:], in_=e21[:], func=AF.Exp), reads=['e21'], writes=['e21'])
        op('dve', lambda e: e.tensor_scalar(out=g1[:], in0=e21[:], scalar1=1.0, scalar2=None, op0=ALU.add), reads=['e21'], writes=['g1'])
        op('dve', lambda e: e.reciprocal(out=g1[:], in_=g1[:]), reads=['g1'], writes=['g1'])
        gates = R['gates']
        op('dve', lambda e: e.tensor_tensor(out=gates[:, :, 0], in0=g1[:], in1=pg[:], op=ALU.mult), reads=['g1', 'pg'], writes=['gates'])
        op('dve', lambda e: e.tensor_tensor(out=e21[:], in0=e21[:], in1=g1[:], op=ALU.mult), reads=['e21', 'g1'], writes=['e21'])
        op('dve', lambda e: e.tensor_tensor(out=gates[:, :, 1], in0=e21[:], in1=pg[:], op=ALU.mult), reads=['e21', 'pg', 'gates'], writes=['gates'])
        op('dve', lambda e: e.tensor_tensor(out=ohb[:], in0=oh1[:], in1=oh2[:], op=ALU.add), reads=['oh1', 'oh2'], writes=['ohb'])
        onesb = C.sb([128, 128], BF16, 'onesb'); trib = C.sb([128, 128], BF16, 'trib')
        op('pool', lambda e: e.memset(onesb[:], 1.0), writes=['onesb'])
        op('dve', lambda e: e.tensor_copy(out=trib[:], in_=masks['up_s'][:]), reads=['mkup_s'], writes=['trib'])
        NG = (NT + 15) // 16
        pCS = [C.ps([128, 512], F32, 'pCS') for _ in range(NG)]; pRK = [C.ps([128, 512], F32, 'pRK') for _ in range(NG)]
        cs = C.sb([128, NT, 32], F32, 'cs'); rk = C.sb([128, NT, 32], F32, 'rk'); carry = C.sb([128, NT + 1, 32], F32, 'carry')
        for t in range(NT):
            g, o = t // 16, (t % 16) * 32
            op('pe', lambda e, g=g, o=o, t=t: e.matmul(pCS[g][:, o:o + 32], lhsT=onesb[:], rhs=ohb[:, t, :], start=True, stop=True), reads=['onesb', 'ohb'], writes=[f'pCS{g}'])
            op('pe', lambda e, g=g, o=o, t=t: e.matmul(pRK[g][:, o:o + 32], lhsT=trib[:], rhs=ohb[:, t, :], start=True, stop=True), reads=['trib', 'ohb'], writes=[f'pRK{g}'])
        for g in range(NG):
            n = min(16, NT - g * 16)
            op('dve', lambda e, g=g, n=n: e.tensor_copy(out=cs[:, g * 16:g * 16 + n, :], in_=pCS[g][:, 0:n * 32].rearrange("p (t x) -> p t x", x=32)), reads=[f'pCS{g}'], writes=['cs'])
            op('dve', lambda e, g=g, n=n: e.tensor_copy(out=rk[:, g * 16:g * 16 + n, :], in_=pRK[g][:, 0:n * 32].rearrange("p (t x) -> p t x", x=32)), reads=[f'pRK{g}'], writes=['rk'])
        op('pool', lambda e: e.memset(carry[:, 0, :], 0.0), writes=['carry'])
        for t in range(NT):
            op('dve', lambda e, t=t: e.tensor_tensor(out=carry[:, t + 1, :], in0=carry[:, t, :], in1=cs[:, t, :], op=ALU.add), reads=['carry', 'cs'], writes=['carry'])
        op('dve', lambda e: e.tensor_tensor(out=rk[:], in0=rk[:], in1=carry[:, 0:NT, :], op=ALU.add), reads=['rk', 'carry'], writes=['rk'])
        cnt = C.sb([128, 32], F32, 'cnt'); ci = C.sb([128, 32], I32, 'ci'); pad = C.sb([128, 32], F32, 'pad'); pend = C.sb([128, 32], F32, 'pend'); pst = C.sb([128, 32], F32, 'pst')
        ones32 = C.sb([128, 32], F32, 'ones32')
        op('pool', lambda e: e.memset(ones32[:], 1.0), writes=['ones32'])
        op('dve', lambda e: e.tensor_scalar(out=cnt[:], in0=carry[:, NT, :], scalar1=127.0, scalar2=None, op0=ALU.add), reads=['carry'], writes=['cnt'])
        op('dve', lambda e: e.tensor_copy(out=ci[:], in_=cnt[:]), reads=['cnt'], writes=['ci'])
        op('dve', lambda e: e.tensor_scalar(out=ci[:], in0=ci[:], scalar1=7, scalar2=7, op0=ALU.arith_shift_right, op1=ALU.logical_shift_left), reads=['ci'], writes=['ci'])
        op('dve', lambda e: e.tensor_copy(out=pad[:], in_=ci[:]), reads=['ci'], writes=['pad'])
        op('dve', lambda e: e.tensor_tensor_scan(out=pend[:], data0=ones32[:], data1=pad[:], initial=0.0, op0=ALU.mult, op1=ALU.add), reads=['ones32', 'pad'], writes=['pend'])
        op('dve', lambda e: e.tensor_tensor(out=pst[:], in0=pend[:], in1=pad[:], op=ALU.subtract), reads=['pend', 'pad'], writes=['pst'])
        op('dve', lambda e: e.tensor_tensor(out=rk[:], in0=rk[:], in1=pst[:].unsqueeze(1).broadcast_to([128, NT, 32]), op=ALU.add), reads=['rk', 'pst'], writes=['rk'])
        destf = C.sb([128, NT, 2], F32, 'destf')
        for j, oh in enumerate((oh1, oh2)):
            op('dve', lambda e, oh=oh: e.tensor_tensor(out=me[:], in0=oh[:], in1=rk[:], op=ALU.mult), reads=['oh1', 'oh2', 'rk', 'me'], writes=['me'])
            op('dve', lambda e, j=j: e.tensor_reduce(out=destf[:, :, j], in_=me[:], axis=AX.X, op=ALU.add), reads=['me', 'destf'], writes=['destf'])
        op('dve', lambda e: e.tensor_copy(out=R['dest'][:].rearrange("p (t j) -> p t j", j=2), in_=destf[:]), reads=['destf'], writes=['dest'])
        bpos = C.sb([128, NBLK], I32, 'bpos'); bposf = C.sb([128, NBLK], F32, 'bposf'); cmpb = C.sb([128, NBLK, 32], F32, 'cmpb'); eb = C.sb([128, NBLK], F32, 'eb')
        kp = C.sb([128, 8], I32, 'kp'); kpf = C.sb([128, 8], F32, 'kpf'); idf = C.sb([128, NBLK, 8], F32, 'idf')
        op('pool', lambda e: e.iota(bpos[:], pattern=[[128, NBLK]], base=0, channel_multiplier=0), writes=['bpos'])
        op('dve', lambda e: e.tensor_copy(out=bposf[:], in_=bpos[:]), reads=['bpos'], writes=['bposf'])
        op('dve', lambda e: e.tensor_tensor(out=cmpb[:], in0=pend[:].unsqueeze(1).broadcast_to([128, NBLK, 32]), in1=bposf[:].unsqueeze(2).broadcast_to([128, NBLK, 32]), op=ALU.is_le),
           reads=['pend', 'bposf'], writes=['cmpb'])
        op('dve', lambda e: e.tensor_reduce(out=eb[:], in_=cmpb[:], axis=AX.X, op=ALU.add), reads=['cmpb'], writes=['eb'])
        op('dve', lambda e: e.tensor_scalar(out=eb[:], in0=eb[:], scalar1=31.0, scalar2=None, op0=ALU.min), reads=['eb'], writes=['eb'])
        op('pool', lambda e: e.iota(kp[:], pattern=[[128, 8]], base=0, channel_multiplier=1), writes=['kp'])
        op('dve', lambda e: e.tensor_copy(out=kpf[:], in_=kp[:]), reads=['kp'], writes=['kpf'])
        op('dve', lambda e: e.scalar_tensor_tensor(out=idf[:], in0=eb[:].unsqueeze(2).broadcast_to([128, NBLK, 8]), scalar=1024.0, in1=kpf[:].unsqueeze(1).broadcast_to([128, NBLK, 8]),
                                                   op0=ALU.mult, op1=ALU.add), reads=['eb', 'kpf'], writes=['idf'])
        op('dve', lambda e: e.tensor_copy(out=R['idxg'][:].rearrange("p (b k) -> p b k", k=8), in_=idf[:]), reads=['idf'], writes=['idxg'])
        idf2 = C.sb([128, NBLK], F32, 'idf2')
        op('dve', lambda e: e.scalar_tensor_tensor(out=idf2[:], in0=eb[:], scalar=128.0, in1=kpf[:, 0:1].broadcast_to([128, NBLK]), op0=ALU.mult, op1=ALU.add), reads=['eb', 'kpf'], writes=['idf2'])
        op('dve', lambda e: e.tensor_copy(out=R['idxe'][:], in_=idf2[:]), reads=['idf2'], writes=['idxe'])
        op('dve', lambda e: e.scalar_tensor_tensor(out=idf[:, :, 0:4], in0=eb[:].unsqueeze(2).broadcast_to([128, NBLK, 4]), scalar=512.0, in1=kpf[:, 0:4].unsqueeze(1).broadcast_to([128, NBLK, 4]),
                                                   op0=ALU.mult, op1=ALU.add), reads=['eb', 'kpf', 'idf'], writes=['idf'])
        op('dve', lambda e: e.tensor_copy(out=R['idxd'][:].rearrange("p (b k) -> p b k", k=4), in_=idf[:, :, 0:4]), reads=['idf'], writes=['idxd'])


def emit_moe(C, M, R, frows, xs, xrows, yrows, wgu, wdn, prm, NT, NBLK, ident, idk, out_dram=None, out_tiles=None, wkeys=()):
    S = C.S
    IOA = bass.IndirectOffsetOnAxis
    with C.scope():
        z = C.sb([128, 2048], F32, 'zer')
        S.op('pool', lambda e: e.memset(z[:], 0.0), writes=['zer'])
        for b0 in range(0, NBLK, 2):
            n = min(2, NBLK - b0)
            S.dma(['sp', 'act'][(b0 // 2) % 2], xrows[b0 * 128:(b0 + n) * 128, :].rearrange("(b p) f -> p b f", p=128), z[:, 0:n * 1024].rearrange("p (b f) -> p b f", f=1024),
                  reads=['zer'], writes=[('xrz', b0)])
        zkeys = [('xrz', b0) for b0 in range(0, NBLK, 2)]
        skeys = []
        ftb = [C.sb([128, 1024], F32, 'ftb') for _ in range(2)]
        for t in range(NT):
            f_ = ftb[t % 2]; fk = f'ftb{t % 2}'
            S.dma('sp', f_[:], frows[t * 128:(t + 1) * 128, :], reads=[('frows', t)], writes=[fk])
            for j in range(2):
                S.dma('pool', xrows, f_[:], reads=[fk, 'dest'] + zkeys, writes=[('xrs', t, j)],
                      indirect=dict(out_offset=IOA(ap=R['dest'][:, 2 * t + j:2 * t + j + 1], axis=0), in_offset=None))
                skeys.append(('xrs', t, j))
    with C.scope():
        identb = C.sb([128, 128], BF16, 'identb2')
        S.op('dve', lambda e: e.tensor_copy(out=identb[:], in_=ident[:]), reads=[idk], writes=['identb2'])
        wg = [C.sb([128, 8, 1024], BF16, 'wg') for _ in range(2)]; wd = [C.sb([128, 4, 1024], BF16, 'wd') for _ in range(2)]
        xb = [C.sb([128, 1024], F32, 'xb') for _ in range(2)]; yb = [C.sb([128, 1024], F32, 'ybm') for _ in range(2)]
        xT = C.sb([128, 8, 128], BF16, 'xTm'); sg = C.sb([128, 512], F32, 'sgm'); hb = C.sb([128, 512], BF16, 'hbm'); hT = C.sb([128, 4, 128], BF16, 'hTm')
        pTx = C.ps([128, 1024], F32, 'pTx'); pG = C.ps([128, 512], F32, 'pGm'); pU = C.ps([128, 512], F32, 'pUm')
        pTh = C.ps([128, 1024], BF16, 'pTh'); pD = [C.ps([128, 512], F32, 'pDm') for _ in range(2)]
        wflat = wgu.rearrange("e k n -> (e k) n") if len(wgu.shape) == 3 else wgu
        dflat = wdn.rearrange("e k n -> (e k) n") if len(wdn.shape) == 3 else wdn
        wkeys = list(wkeys)
        xTA = [xT, C.sb([128, 8, 128], BF16, 'xTm2')]; hbA = [hb, C.sb([128, 512], BF16, 'hbm2')]; hTA = [hT, C.sb([128, 4, 128], BF16, 'hTm2')]

        def stA(b):
            w_ = wg[b % 2]; x_ = xb[b % 2]; xT_ = xTA[b % 2]
            S.dma('pool', w_[:].rearrange("p k n -> p (k n)"), wflat, reads=['idxe'] + wkeys, writes=[(f'wg{b % 2}', k) for k in range(8)],
                  indirect=dict(out_offset=None, in_offset=IOA(ap=R['idxe'][:, b:b + 1], axis=0)))
            S.dma('sp', x_[:], xrows[b * 128:(b + 1) * 128, :], reads=skeys + zkeys, writes=[f'xb{b % 2}'])
            for k in range(8):
                S.op('pe', lambda e, k=k, x_=x_: e.transpose(out=pTx[:, k * 128:(k + 1) * 128], in_=x_[:, k * 128:(k + 1) * 128], identity=ident[:]),
                     reads=[f'xb{b % 2}', idk], writes=['pTx'])
            S.op('act', lambda e: e.copy(out=xT_[:], in_=pTx[:].rearrange("p (k t) -> p k t", k=8)), reads=['pTx'], writes=[f'xTm{b % 2}'])

        def stB(b):
            w_ = wg[b % 2]; xT_ = xTA[b % 2]; hb_ = hbA[b % 2]
            for k in range(8):
                S.op('pe', lambda e, k=k: e.matmul(pG[:], lhsT=xT_[:, k, :], rhs=w_[:, k, 0:512], start=(k == 0), stop=(k == 7)), reads=[f'xTm{b % 2}', (f'wg{b % 2}', k)], writes=['pGm'])
            for k in range(8):
                S.op('pe', lambda e, k=k: e.matmul(pU[:], lhsT=xT_[:, k, :], rhs=w_[:, k, 512:1024], start=(k == 0), stop=(k == 7)), reads=[f'xTm{b % 2}', (f'wg{b % 2}', k)], writes=['pUm'])
            S.op('act', lambda e: e.activation(out=sg[:], in_=pG[:], func=AF.Silu), reads=['pGm'], writes=['sgm'])
            S.op('dve', lambda e: e.tensor_tensor(out=hb_[:], in0=pU[:], in1=sg[:], op=ALU.mult), reads=['pUm', 'sgm'], writes=[f'hbm{b % 2}'])

        def stC(b):
            d_ = wd[b % 2]; hb_ = hbA[b % 2]; hT_ = hTA[b % 2]
            S.dma('pool', d_[:].rearrange("p k n -> p (k n)"), dflat, reads=['idxe'] + wkeys, writes=[(f'wd{b % 2}', k) for k in range(4)],
                  indirect=dict(out_offset=None, in_offset=IOA(ap=R['idxe'][:, b:b + 1], axis=0)))
            for k in range(4):
                S.op('pe', lambda e, k=k: e.transpose(out=pTh[:, k * 128:(k + 1) * 128], in_=hb_[:, k * 128:(k + 1) * 128], identity=identb[:]), reads=[f'hbm{b % 2}', 'identb2'], writes=['pTh'])
            S.op('act', lambda e: e.copy(out=hT_[:], in_=pTh[:, 0:512].rearrange("p (k t) -> p k t", k=4)), reads=['pTh'], writes=[f'hTm{b % 2}'])

        def stD(b):
            d_ = wd[b % 2]; hT_ = hTA[b % 2]; y_ = yb[b % 2]
            for hf in range(2):
                for k in range(4):
                    S.op('pe', lambda e, k=k, hf=hf: e.matmul(pD[hf][:], lhsT=hT_[:, k, :], rhs=d_[:, k, hf * 512:(hf + 1) * 512], start=(k == 0), stop=(k == 3)),
                         reads=[f'hTm{b % 2}', (f'wd{b % 2}', k)], writes=[f'pDm{hf}'])
                if hf == 0:
                    S.op('act', lambda e: e.copy(out=y_[:, 0:512], in_=pD[0][:]), reads=['pDm0'], writes=[f'ybm{b % 2}'])
                else:
                    S.op('dve', lambda e: e.tensor_copy(out=y_[:, 512:1024], in_=pD[1][:]), reads=['pDm1'], writes=[f'ybm{b % 2}'])
            S.dma('sp', yrows[b * 128:(b + 1) * 128, :], y_[:], reads=[f'ybm{b % 2}'], writes=['yrows'])

        assert len(wgu.shape) == 2
        for it in range(NBLK + 3):
            if it < NBLK:
                stA(it)
            if 0 <= it - 1 < NBLK:
                stB(it - 1)
            if 0 <= it - 2 < NBLK:
                stC(it - 2)
            if 0 <= it - 3 < NBLK:
                stD(it - 3)
    with C.scope():
        rowG2 = []
        for v in range(2):
            t_ = C.sb([128, 1024], F32, f'rowG2{v}')
            emit_rowbcast(C, M['m3'][:, 5][:, :, v], 'modT', t_[:], f'rowG2{v}', prm['rowscr'][('G2', v)])
            rowG2.append(t_)
        y1 = [C.sb([128, 1024], F32, 'y1') for _ in range(2)]; y2 = [C.sb([128, 1024], F32, 'y2') for _ in range(2)]
        xm = [C.sb([128, 1024], F32, 'xm') for _ in range(2)]
        tiles = out_tiles if out_tiles is not None else list(range(NT))
        for i, t in enumerate(tiles):
            v = 1 if t < 2 else 0
            a_, b_, x_ = y1[i % 2], y2[i % 2], xm[i % 2]
            ak, bk, xk = f'y1{i % 2}', f'y2{i % 2}', f'xm{i % 2}'
            S.dma('pool', a_[:], yrows, reads=['yrows', 'dest'], writes=[ak], indirect=dict(out_offset=None, in_offset=IOA(ap=R['dest'][:, 2 * t:2 * t + 1], axis=0)))
            S.dma('pool', b_[:], yrows, reads=['yrows', 'dest'], writes=[bk], indirect=dict(out_offset=None, in_offset=IOA(ap=R['dest'][:, 2 * t + 1:2 * t + 2], axis=0)))
            S.dma('sp', x_[:], xs[t * 128:(t + 1) * 128, :], reads=[('xs', t)], writes=[xk])
            S.op('dve', lambda e, a_=a_, t=t: e.tensor_scalar(out=a_[:], in0=a_[:], scalar1=R['gates'][:, t, 0:1], scalar2=None, op0=ALU.mult), reads=[ak, 'gates'], writes=[ak])
            S.op('dve', lambda e, a_=a_, b_=b_, t=t: e.scalar_tensor_tensor(out=a_[:], in0=b_[:], scalar=R['gates'][:, t, 1:2], in1=a_[:], op0=ALU.mult, op1=ALU.add),
                 reads=[ak, bk, 'gates'], writes=[ak])
            S.op('pool', lambda e, a_=a_, v=v: e.tensor_tensor(out=a_[:], in0=a_[:], in1=rowG2[v][:], op=ALU.mult), reads=[ak, f'rowG2{v}'], writes=[ak])
            S.op('dve', lambda e, a_=a_, x_=x_: e.tensor_tensor(out=x_[:], in0=x_[:], in1=a_[:], op=ALU.add), reads=[ak, xk], writes=[xk])
            if out_dram is None:
                S.dma('sp', xs[t * 128:(t + 1) * 128, :], x_[:], reads=[xk], writes=[('xs', t)])
            else:
                S.dma('sp', out_dram[(t - 2) * 128:(t - 1) * 128, :], x_[:], reads=[xk], writes=[('out', t)])


def route_alloc(C, NT, NBLK):
    return dict(lgall=C.sb([128, NT, 36], F32, 'lgall'), gates=C.sb([128, NT, 2], F32, 'gates'), dest=C.sb([128, NT * 2], I32, 'dest'),
                idxg=C.sb([128, NBLK * 8], I32, 'idxg'), idxd=C.sb([128, NBLK * 4], I32, 'idxd'), idxe=C.sb([128, NBLK], I32, 'idxe'))


def rowscr_alloc(C, tag):
    return {(nm, v): C.dram(f"rowscr_{tag}_{nm}{v}", [1024], F32) for nm in ('G1', 'A2', 'B2', 'G2') for v in range(2)}


def build_moe_test(NT):
    C = Ctx(); S = C.S
    NTOK = NT * 128; NBLK = 2 * NT + 32
    mixo = C.dram("mixo", [NTOK, 1024], kind="ExternalInput")
    xin = C.dram("xin", [NTOK, 1024], kind="ExternalInput")
    cin = C.dram("cin", [128, 16], kind="ExternalInput"); adaw = C.dram("adaw", [D, 6 * D], kind="ExternalInput")
    adab = C.dram("adab", [128, 96], kind="ExternalInput"); nrm = C.dram("nrm", [128, 16], kind="ExternalInput")
    wout = C.dram("wout", [D, D], kind="ExternalInput")
    prm = dict(w_grp=C.dram("w_grp", [D, 4], kind="ExternalInput"), b_grp=C.dram("b_grp", [4], kind="ExternalInput"),
               w_exp=C.dram("w_exp", [D, 32], kind="ExternalInput"), b_exp=C.dram("b_exp", [32], kind="ExternalInput"))
    wgu = C.dram("wgu", [32, 1024, 1024], kind="ExternalInput"); wdn = C.dram("wdn", [32, 512, 1024], kind="ExternalInput")
    xs = C.dram("xs", [NTOK, 1024], kind="ExternalOutput")
    frows = C.dram("frows", [NTOK, 1024], kind="ExternalOutput")
    xrows = C.dram("xrows", [NBLK * 128, 1024]); yrows = C.dram("yrows", [NBLK * 128, 1024])
    prm['rowscr'] = rowscr_alloc(C, 't')
    ident, idk = C.ident(); masks = make_masks(C)
    for t in range(NT):
        S.dma('sp', xs[t * 128:(t + 1) * 128, :], xin[t * 128:(t + 1) * 128, :], writes=[('xs', t)])
    M = emit_mods(C, adaw, adab, cin, nrm)
    R = route_alloc(C, NT, NBLK)
    emit_post(C, M, mixo, xs, frows, wout, prm, NT, ident, idk, R)
    emit_route(C, R, NT, NBLK, masks)
    emit_moe(C, M, R, frows, xs, xrows, yrows, wgu, wdn, prm, NT, NBLK, ident, idk)
    S.finish()
    return C


def emit_pre(C, M, xs, win, NOUT, NT, ident, idk, fm0, nfm, fm_out, tm0, tm1, tm_out, mode, pcols):
    S = C.S
    NTOK = NT * 128
    with C.scope():
        hT = C.sb([128, 8, NTOK], BF16, 'hT')
        with C.scope():
            tmp = dict(junk=C.sb([128, D]), ss=C.sb([128, 4]), xn=C.sb([128, D]), t2=C.sb([128, 8, 128]))
            xb = [C.sb([128, D], F32, 'xb') for _ in range(2)]
            pT = [C.ps([128, 1024], F32, 'pT') for _ in range(2)]
            B1 = M['m3'][:, 0]
            for t in range(NT):
                v = 1 if t < 2 else 0
                S.dma('sp', xb[t % 2][:], xs[t * 128:(t + 1) * 128, :], reads=[('xs', t)], writes=[f'xb{t % 2}'])
                emit_normT(C, xb[t % 2][:], f'xb{t % 2}', hT, ('hT', t), t * 128, M['A1'], B1, v, ident, idk, pT[t % 2], f'pT{t % 2}', tmp, t)
        hkeys = [('hT', t) for t in range(NT)]
        with C.scope():
            TW = tm1 - tm0
            wtm = C.sb([128, 8, TW], BF16, 'wtm')
            for k in range(8):
                S.dma('pool', wtm[:, k, :], win[k * 128:(k + 1) * 128, tm0:tm1], writes=[('wtm', k)])
            pY = [C.ps([128, 512], F32, 'pY') for _ in range(3)]
            yb = [C.sb([128, TW], F32, 'yb') for _ in range(2)]
            ncc = (TW + 511) // 512
            it = 0
            for t in range(NT):
                for c in range(ncc):
                    cw = min(512, TW - c * 512)
                    p = pY[it % 3]; pk = f'pY{it % 3}'
                    for k in range(8):
                        S.op('pe', lambda e, k=k, p=p, c=c, cw=cw, t=t: e.matmul(p[:, 0:cw], lhsT=hT[:, k, t * 128:(t + 1) * 128],
                                                                                rhs=wtm[:, k, c * 512:c * 512 + cw], start=(k == 0), stop=(k == 7)),
                             reads=[('hT', t), ('wtm', k)], writes=[pk])
                    eng = 'act' if it % 2 == 0 else 'dve'
                    if eng == 'act':
                        S.op('act', lambda e, p=p, c=c, cw=cw, t=t: e.copy(out=yb[t % 2][:, c * 512:c * 512 + cw], in_=p[:, 0:cw]), reads=[pk], writes=[f'yb{t % 2}'])
                    else:
                        S.op('dve', lambda e, p=p, c=c, cw=cw, t=t: e.tensor_copy(out=yb[t % 2][:, c * 512:c * 512 + cw], in_=p[:, 0:cw]), reads=[pk], writes=[f'yb{t % 2}'])
                    it += 1
                S.dma('sp', tm_out[t * 128:(t + 1) * 128, :], yb[t % 2][:], reads=[f'yb{t % 2}'], writes=[('tm_out', t)])
        with C.scope():
            PADW = NTOK + 8
            co, lo = 2, 262
            pb = [C.sb([128, PADW], F32, 'pbuf') for _ in range(2)]
            acc = [C.sb([128, PADW], F32, 'accb') for _ in range(2)]
            wc = [C.sb([128, 8, 128], BF16, 'wc') for _ in range(2)]
            pc = C.sb([128, pcols.shape[1]], F32, 'pcols')
            S.dma('sp', pc[:], pcols, writes=['pcols'])
            pF = [C.ps([128, 512], F32, 'pF') for _ in range(3)]
            for i in range(2):
                S.op('pool', lambda e, i=i: e.memset(pb[i][:], 0.0), writes=[f'pbuf{i}'])
            NT5 = (NTOK + 511) // 512
            it = 0
            for cc in range(nfm):
                P_ = pb[cc % 2]; A_ = acc[cc % 2]; W_ = wc[cc % 2]
                pk_, ak_, wk_ = f'pbuf{cc % 2}', f'accb{cc % 2}', f'wc{cc % 2}'
                S.dma('pool', W_[:], win[:, fm0 + cc * 128:fm0 + (cc + 1) * 128].rearrange("(k p) c -> p k c", p=128), writes=[wk_])
                for i in range(NT5):
                    w = min(512, NTOK - i * 512)
                    p = pF[it % 3]; pfk = f'pF{it % 3}'
                    for k in range(8):
                        S.op('pe', lambda e, k=k, p=p, i=i, w=w, W_=W_: e.matmul(p[:, 0:w], lhsT=W_[:, k, :], rhs=hT[:, k, i * 512:i * 512 + w], start=(k == 0), stop=(k == 7)),
                             reads=hkeys[i * 4:i * 4 + 4] + [wk_], writes=[pfk])
                    if i == 0:
                        S.op('act', lambda e, p=p, P_=P_: e.copy(out=P_[:, co:co + 256], in_=p[:, 0:256]), reads=[pfk], writes=[pk_])
                        S.op('dve', lambda e, p=p, P_=P_, w=w: e.tensor_copy(out=P_[:, lo:lo + w - 256], in_=p[:, 256:w]), reads=[pfk, pk_], writes=[pk_])
                    else:
                        o = lo + i * 512 - 256
                        if it % 2 == 0:
                            S.op('act', lambda e, p=p, P_=P_, w=w, o=o: e.copy(out=P_[:, o:o + w], in_=p[:, 0:w]), reads=[pfk, pk_], writes=[pk_])
                        else:
                            S.op('dve', lambda e, p=p, P_=P_, w=w, o=o: e.tensor_copy(out=P_[:, o:o + w], in_=p[:, 0:w]), reads=[pfk, pk_], writes=[pk_])
                    it += 1
                L = PADW - 4
                if mode == 'shift':
                    S.op('pool', lambda e, P_=P_, A_=A_: e.tensor_tensor(out=A_[:, 2:2 + L], in0=P_[:, 1:1 + L], in1=P_[:, 3:3 + L], op=ALU.add), reads=[pk_], writes=[ak_])
                    S.op('dve', lambda e, A_=A_, cc=cc: e.tensor_scalar(out=A_[:, 2:2 + L], in0=A_[:, 2:2 + L], scalar1=pc[:, 2 * nfm + cc:2 * nfm + cc + 1], scalar2=None, op0=ALU.mult),
                         reads=[ak_, 'pcols'], writes=[ak_])
                    S.op('dve', lambda e, A_=A_, P_=P_, cc=cc: e.scalar_tensor_tensor(out=A_[:, 2:2 + L], in0=P_[:, 2:2 + L], scalar=pc[:, nfm + cc:nfm + cc + 1], in1=A_[:, 2:2 + L],
                                                                                   op0=ALU.mult, op1=ALU.add), reads=[ak_, pk_, 'pcols'], writes=[ak_])
                else:
                    S.op('dve', lambda e, A_=A_, P_=P_, cc=cc: e.tensor_scalar(out=A_[:, 2:2 + L], in0=P_[:, 0:L], scalar1=pc[:, cc:cc + 1], scalar2=None, op0=ALU.mult),
                         reads=[pk_, 'pcols'], writes=[ak_])
                    for j in range(1, 5):
                        S.op('dve', lambda e, A_=A_, P_=P_, cc=cc, j=j: e.scalar_tensor_tensor(out=A_[:, 2:2 + L], in0=P_[:, j:j + L], scalar=pc[:, j * nfm + cc:j * nfm + cc + 1],
                                                                                              in1=A_[:, 2:2 + L], op0=ALU.mult, op1=ALU.add), reads=[ak_, pk_, 'pcols'], writes=[ak_])
                    S.op('act', lambda e, A_=A_: e.activation(out=A_[:, 2:2 + L], in_=A_[:, 2:2 + L], func=AF.Silu), reads=[ak_], writes=[ak_])
                S.dma('sp', fm_out[cc * 128:(cc + 1) * 128, 0:256], A_[:, co:co + 256], reads=[ak_], writes=[('fm_out', cc)])
                S.dma('act', fm_out[cc * 128:(cc + 1) * 128, 256:NTOK], A_[:, lo:lo + NTOK - 256], reads=[ak_], writes=[('fm_out', cc, 1)])


def emit_gdn(C, gT, yz, mixo, prm, NCH, heads, ident, idk, masks):
    S = C.S
    NTOK = NCH * 128
    NT5 = (NTOK + 511) // 512
    order = [chunk_order(NCH, 0), chunk_order(NCH, 1)]
    with C.scope():
        ab = C.sb([128, NCH, 32], F32, 'gab')
        S.dma('sp', ab[:], yz.rearrange("(c t) f -> t c f", t=128)[:, :, 1024:1056], reads=[('tm_out', t) for t in range(NCH)], writes=['gab'])
        rowp = C.sb([128, 48], F32, 'growp')
        S.dma('sp', rowp[:, 0:16], prm['dt_bias'].partition_broadcast(128), writes=['growp'])
        S.dma('sp', rowp[:, 16:32], prm['A_log'].partition_broadcast(128), reads=['growp'], writes=['growp'])
        onorm = C.sb([128, 128], F32, 'onorm')
        S.dma('sp', onorm[:], prm['out_norm'].partition_broadcast(128), writes=['onorm'])
        one1 = C.sb([128, 1], F32, 'one1'); eps6 = C.sb([128, 1], F32, 'eps6')
        S.op('pool', lambda e: e.memset(one1[:], 1.0), writes=['one1'])
        S.op('pool', lambda e: e.memset(eps6[:], 1e-6), writes=['eps6'])
        ones = C.sb([128, 128], F32, 'onesf')
        S.op('pool', lambda e: e.memset(ones[:], 1.0), writes=['onesf'])
        S.op('act', lambda e: e.activation(out=rowp[:, 16:32], in_=rowp[:, 16:32], func=AF.Exp), reads=['growp'], writes=['growp'])
        S.op('dve', lambda e: e.tensor_scalar(out=rowp[:, 16:32], in0=rowp[:, 16:32], scalar1=-1.0, scalar2=None, op0=ALU.mult), reads=['growp'], writes=['growp'])
        g = C.sb([128, NCH, 16], F32, 'gg'); beta = C.sb([128, NCH, 16], F32, 'gbeta')
        gam = C.sb([128, NCH, 16], F32, 'gam'); gtot = C.sb([128, NCH, 16], F32, 'gtot')
        bR = lambda a: a.unsqueeze(1).broadcast_to([128, NCH, 16])
        S.op('dve', lambda e: e.tensor_tensor(out=g[:], in0=ab[:, :, 0:16], in1=bR(rowp[:, 0:16]), op=ALU.add), reads=['gab', 'growp'], writes=['gg'])
        S.op('act', lambda e: e.activation(out=g[:], in_=g[:], func=AF.Exp), reads=['gg'], writes=['gg'])
        S.op('act', lambda e: e.activation(out=g[:], in_=g[:], func=AF.Ln, bias=one1[:, 0:1]), reads=['gg', 'one1'], writes=['gg'])
        S.op('dve', lambda e: e.tensor_tensor(out=g[:], in0=g[:], in1=bR(rowp[:, 16:32]), op=ALU.mult), reads=['gg', 'growp'], writes=['gg'])
        S.op('act', lambda e: e.activation(out=beta[:], in_=ab[:, :, 16:32], func=AF.Sigmoid), reads=['gab'], writes=['gbeta'])
        tri = [masks['up_i'], masks['lo_i']]
        trik = ['mkup_i', 'mklo_i']
        with C.scope():
            pGm = [C.ps([128, 512], F32, 'pGam') for _ in range(2)]; pGt = [C.ps([128, 512], F32, 'pGtot') for _ in range(2)]
            for c in range(NCH):
                b_, o_ = c // 32, (c % 32) * 16
                for d in range(2):
                    S.op('pe', lambda e, c=c, d=d, b_=b_, o_=o_: e.matmul(pGm[b_][:, o_ + d * 8:o_ + d * 8 + 8], lhsT=tri[d][:], rhs=g[:, c, d * 8:(d + 1) * 8], start=True, stop=True),
                         reads=['gg', trik[d]], writes=[f'pGam{b_}'])
                S.op('pe', lambda e, c=c, b_=b_, o_=o_: e.matmul(pGt[b_][:, o_:o_ + 16], lhsT=ones[:], rhs=g[:, c, :], start=True, stop=True), reads=['gg', 'onesf'], writes=[f'pGtot{b_}'])
            for b_ in range((NCH + 31) // 32):
                n = min(32, NCH - b_ * 32)
                S.op('dve', lambda e, b_=b_, n=n: e.tensor_copy(out=gam[:, b_ * 32:b_ * 32 + n, :], in_=pGm[b_][:, 0:n * 16].rearrange("p (c x) -> p c x", x=16)), reads=[f'pGam{b_}'], writes=['gam'])
                S.op('dve', lambda e, b_=b_, n=n: e.tensor_copy(out=gtot[:, b_ * 32:b_ * 32 + n, :], in_=pGt[b_][:, 0:n * 16].rearrange("p (c x) -> p c x", x=16)), reads=[f'pGtot{b_}'], writes=['gtot'])
        nbeg = C.sb([128, NCH, 16], F32, 'nbeg'); etail = C.sb([128, NCH, 16], F32, 'etail'); eC = C.sb([128, NCH, 16], F32, 'eC'); nbeta = C.sb([128, NCH, 16], F32, 'nbeta')
        S.op('act', lambda e: e.activation(out=nbeg[:], in_=gam[:], func=AF.Exp), reads=['gam'], writes=['nbeg'])
        S.op('dve', lambda e: e.tensor_tensor(out=nbeg[:], in0=nbeg[:], in1=beta[:], op=ALU.mult), reads=['nbeg', 'gbeta'], writes=['nbeg'])
        S.op('dve', lambda e: e.tensor_scalar(out=nbeg[:], in0=nbeg[:], scalar1=-1.0, scalar2=None, op0=ALU.mult), reads=['nbeg'], writes=['nbeg'])
        S.op('dve', lambda e: e.tensor_tensor(out=etail[:], in0=gtot[:], in1=gam[:], op=ALU.subtract), reads=['gtot', 'gam'], writes=['etail'])
        S.op('act', lambda e: e.activation(out=etail[:], in_=etail[:], func=AF.Exp), reads=['etail'], writes=['etail'])
        S.op('act', lambda e: e.activation(out=eC[:], in_=gtot[:], func=AF.Exp), reads=['gtot'], writes=['eC'])
        S.op('dve', lambda e: e.tensor_scalar(out=nbeta[:], in0=beta[:], scalar1=-1.0, scalar2=None, op0=ALU.mult), reads=['gbeta'], writes=['nbeta'])
        mS = [masks['lo_s'], masks['up_s']]; mSk = ['mklo_s', 'mkup_s']
        mIT = [masks['up_i'], masks['lo_i']]; mITk = ['mkup_i', 'mklo_i']
        for h in heads:
            with C.scope():
                qn = C.sb([128, NTOK], F32, 'qn'); kn = C.sb([128, NTOK], F32, 'kn')
                Vt = C.sb([128, NCH, 128], F32, 'gVt'); Kt = C.sb([128, NCH, 128], F32, 'gKt'); oacc = C.sb([128, NCH, 128], F32, 'goacc'); obw = C.sb([128, NCH, 128], F32, 'gobw')
                with C.scope():
                    tmp = C.sb([128, NTOK], F32, 'gtmp'); tmp2 = C.sb([128, NTOK], F32, 'gtmp2')
                    pW = [C.ps([128, 512], F32, 'pW') for _ in range(2)]; pTf = C.ps([128, 512], F32, 'pTf')

                    def l2n(dst, dkey, row0, scale):
                        S.dma('sp', dst[:], gT[row0:row0 + 128, :], reads=[('fm_out', row0 // 128), ('fm_out', row0 // 128, 1)], writes=[dkey])
                        S.op('pool', lambda e: e.tensor_tensor(out=tmp[:], in0=dst[:], in1=dst[:], op=ALU.mult), reads=[dkey], writes=['gtmp'])
                        for i in range(NT5):
                            w = min(512, NTOK - i * 512)
                            S.op('pe', lambda e, i=i, w=w: e.matmul(pW[i % 2][:, 0:w], lhsT=ones[:], rhs=tmp[:, i * 512:i * 512 + w], start=True, stop=True),
                                 reads=['gtmp', 'onesf'], writes=[f'pW{i % 2}'])
                            S.op('act', lambda e, i=i, w=w: e.activation(out=tmp2[:, i * 512:i * 512 + w], in_=pW[i % 2][:, 0:w], func=AF.Sqrt, bias=eps6[:, 0:1]),
                                 reads=[f'pW{i % 2}', 'eps6'], writes=['gtmp2'])
                        S.op('dve', lambda e: e.reciprocal(out=tmp2[:], in_=tmp2[:]), reads=['gtmp2'], writes=['gtmp2'])
                        S.op('dve', lambda e: e.scalar_tensor_tensor(out=dst[:], in0=dst[:], scalar=scale, in1=tmp2[:], op0=ALU.mult, op1=ALU.mult), reads=[dkey, 'gtmp2'], writes=[dkey])

                    def to_tok(src, skey, dst, dkey):
                        for c0 in range(0, NCH, 4):
                            n = min(4, NCH - c0)
                            for c in range(c0, c0 + n):
                                S.op('pe', lambda e, c=c, c0=c0: e.transpose(out=pTf[:, (c - c0) * 128:(c - c0 + 1) * 128], in_=src[:, c * 128:(c + 1) * 128], identity=ident[:]),
                                     reads=[skey, idk], writes=['pTf'])
                            S.op('act', lambda e, c0=c0, n=n: e.copy(out=dst[:, c0:c0 + n, :], in_=pTf[:, 0:n * 128].rearrange("p (c t) -> p c t", t=128)), reads=['pTf'], writes=[dkey])
                    l2n(qn, 'qn', h * 128, 128.0 ** -0.5)
                    l2n(kn, 'kn', 1024 + h * 128, 1.0)
                    to_tok(kn, 'kn', Kt, 'gKt')
                    S.dma('sp', tmp[:], gT[2048 + h * 128:2048 + (h + 1) * 128, :], reads=[('fm_out', 16 + h), ('fm_out', 16 + h, 1), 'gtmp'], writes=['gtmp'])
                    to_tok(tmp, 'gtmp', Vt, 'gVt')
                with C.scope():
                    St = [C.sb([128, 128], F32, 'gS') for _ in range(2)]
                    for d in range(2):
                        S.op('pool', lambda e, d=d: e.memset(St[d][:], 0.0), writes=[f'gS{d}'])
                    pGr = C.ps([128, 512], F32, 'pGr'); pKQ = C.ps([128, 512], F32, 'pKQ'); pN = C.ps([128, 512], F32, 'pN'); pT = C.ps([128, 512], F32, 'pT')
                    pC = C.ps([128, 512], F32, 'pC'); pD = C.ps([128, 512], F32, 'pD'); pNT = C.ps([128, 512], F32, 'pNT')
                    Gs = C.sb([128, 256], F32, 'Gs'); Dm = C.sb([128, 512], F32, 'Dm')
                    NsA = [C.sb([128, 512], F32, 'Ns') for _ in range(2)]; QKsA = [C.sb([128, 256], F32, 'QKs') for _ in range(2)]; qeA = [C.sb([128, 256], F32, 'qe') for _ in range(2)]
                    NpA = [[C.sb([128, 512], F32, 'NpB') for _ in range(2)] for _ in range(2)]; TtA = [C.sb([128, 256], F32, 'Tt') for _ in range(2)]
                    ktaA = [C.sb([128, 256], F32, 'kta') for _ in range(2)]; bVA = [C.sb([128, 256], F32, 'bV') for _ in range(2)]
                    Zs = C.sb([128, 256], F32, 'Zs'); Vn = C.sb([128, 256], F32, 'Vn')

                    def st_prep(i, q):
                        Ns, QKs, qe, Tt, kta, bV = NsA[q], QKsA[q], qeA[q], TtA[q], ktaA[q], bVA[q]
                        for d in range(2):
                            c = order[d][i]; ts = slice(c * 128, (c + 1) * 128); x = d * 8 + h
                            gcol = gam[:, c, x:x + 1]
                            S.op('dve', lambda e, d=d, c=c, x=x: e.tensor_scalar(out=Gs[:, d * 128:(d + 1) * 128], in0=tri[d][:], scalar1=g[:, c, x:x + 1], scalar2=None, op0=ALU.mult),
                                 reads=['gg', trik[d]], writes=[f'Gs{d}'])
                            S.op('pe', lambda e, d=d: e.matmul(pGr[:, d * 128:(d + 1) * 128], lhsT=ones[:], rhs=Gs[:, d * 128:(d + 1) * 128], start=True, stop=True),
                                 reads=[f'Gs{d}', 'onesf'], writes=['pGr'])
                            S.op('pe', lambda e, d=d, ts=ts: e.matmul(pKQ[:, d * 256:d * 256 + 128], lhsT=kn[:, ts], rhs=kn[:, ts], start=True, stop=True), reads=['kn'], writes=['pKQ'])
                            S.op('pe', lambda e, d=d, ts=ts: e.matmul(pKQ[:, d * 256 + 128:d * 256 + 256], lhsT=kn[:, ts], rhs=qn[:, ts], start=True, stop=True), reads=['kn', 'qn'], writes=['pKQ'])
                            S.op('dve', lambda e, d=d, gcol=gcol: e.tensor_scalar(out=Dm[:, d * 256:d * 256 + 128], in0=pGr[:, d * 128:(d + 1) * 128], scalar1=gcol, scalar2=0.0, op0=ALU.subtract, op1=ALU.max),
                                 reads=['pGr', 'gam'], writes=[f'Dm{d}'])
                            S.op('dve', lambda e, d=d, gcol=gcol: e.tensor_scalar(out=Dm[:, d * 256 + 128:d * 256 + 256], in0=pGr[:, d * 128:(d + 1) * 128], scalar1=gcol, scalar2=0.0, op0=ALU.subtract, op1=ALU.min),
                                 reads=['pGr', 'gam', f'Dm{d}'], writes=[f'Dm{d}'])
                            S.op('act', lambda e, d=d: e.activation(out=Dm[:, d * 256:d * 256 + 128], in_=Dm[:, d * 256:d * 256 + 128], func=AF.Exp, scale=-1.0), reads=[f'Dm{d}'], writes=[f'Dm{d}'])
                            S.op('act', lambda e, d=d: e.activation(out=Dm[:, d * 256 + 128:d * 256 + 256], in_=Dm[:, d * 256 + 128:d * 256 + 256], func=AF.Exp), reads=[f'Dm{d}'], writes=[f'Dm{d}'])
                            S.op('act', lambda e, d=d: e.activation(out=qe[:, d * 128:(d + 1) * 128], in_=pGr[:, d * 128:(d + 1) * 128], func=AF.Exp), reads=['pGr'], writes=[f'qe{q}{d}'])
                            S.op('dve', lambda e, d=d, ts=ts: e.tensor_tensor(out=qe[:, d * 128:(d + 1) * 128], in0=qe[:, d * 128:(d + 1) * 128], in1=qn[:, ts], op=ALU.mult), reads=[f'qe{q}{d}', 'qn'], writes=[f'qe{q}{d}'])
                            S.op('dve', lambda e, d=d: e.tensor_tensor(out=Dm[:, d * 256:d * 256 + 256], in0=pKQ[:, d * 256:d * 256 + 256], in1=Dm[:, d * 256:d * 256 + 256], op=ALU.mult),
                                 reads=['pKQ', f'Dm{d}'], writes=[f'Dm{d}'])
                            S.op('dve', lambda e, d=d, c=c, x=x: e.scalar_tensor_tensor(out=Ns[:, d * 256:d * 256 + 128], in0=Dm[:, d * 256:d * 256 + 128], scalar=nbeta[:, c, x:x + 1], in1=mS[d][:],
                                                                                       op0=ALU.mult, op1=ALU.mult), reads=[f'Dm{d}', 'nbeta', mSk[d]], writes=[f'Ns{q}{d}'])
                            S.op('pool', lambda e, d=d: e.tensor_tensor(out=QKs[:, d * 128:(d + 1) * 128], in0=Dm[:, d * 256 + 128:d * 256 + 256], in1=mIT[d][:], op=ALU.mult),
                                 reads=[f'Dm{d}', mITk[d]], writes=[f'QKs{q}{d}'])
                            S.op('pe', lambda e, d=d: e.transpose(out=pNT[:, d * 128:(d + 1) * 128], in_=Ns[:, d * 256:d * 256 + 128], identity=ident[:]), reads=[f'Ns{q}{d}', idk], writes=['pNT'])
                            S.op('act', lambda e, d=d: e.copy(out=Ns[:, d * 256 + 128:d * 256 + 256], in_=pNT[:, d * 128:(d + 1) * 128]), reads=['pNT', f'Ns{q}{d}'], writes=[f'Ns{q}{d}'])
                            S.op('pool', lambda e, d=d: e.tensor_tensor(out=Tt[:, d * 128:(d + 1) * 128], in0=Ns[:, d * 256 + 128:d * 256 + 256], in1=ident[:], op=ALU.add),
                                 reads=[f'Ns{q}{d}', idk], writes=[f'Tt{q}{d}'])
                            S.op('act', lambda e, d=d, c=c, x=x: e.activation(out=kta[:, d * 128:(d + 1) * 128], in_=Kt[:, c, :], func=AF.Copy, scale=etail[:, c, x:x + 1]),
                                 reads=['gKt', 'etail'], writes=[f'kta{q}{d}'])
                            S.op('act', lambda e, d=d, c=c, x=x: e.activation(out=bV[:, d * 128:(d + 1) * 128], in_=Vt[:, c, :], func=AF.Copy, scale=beta[:, c, x:x + 1]),
                                 reads=['gVt', 'gbeta'], writes=[f'bV{q}{d}'])

                    def st_neumann(lv, q):
                        Ns, Tt = NsA[q], TtA[q]
                        if lv == 1:
                            prev = [(Ns[:, d * 256:d * 256 + 128], Ns[:, d * 256 + 128:d * 256 + 256], f'Ns{q}{d}') for d in range(2)]
                        else:
                            pb_ = NpA[q][(lv - 1) % 2]
                            prev = [(pb_[:, d * 256:d * 256 + 128], pb_[:, d * 256 + 128:d * 256 + 256], f'NpB{q}{(lv - 1) % 2}') for d in range(2)]
                        nb = NpA[q][lv % 2]; nk = f'NpB{q}{lv % 2}'
                        for d in range(2):
                            Nv, NTv, pk = prev[d]
                            S.op('pe', lambda e, d=d, Nv=Nv, NTv=NTv: e.matmul(pN[:, d * 256:d * 256 + 128], lhsT=NTv, rhs=Nv, start=True, stop=True), reads=[pk], writes=['pN'])
                            if lv < 6:
                                S.op('pe', lambda e, d=d, Nv=Nv, NTv=NTv: e.matmul(pN[:, d * 256 + 128:d * 256 + 256], lhsT=Nv, rhs=NTv, start=True, stop=True), reads=[pk], writes=['pN'])
                        S.op('act', lambda e, nb=nb: e.copy(out=nb[:], in_=pN[:]), reads=['pN'], writes=[nk])
                        for d in range(2):
                            S.op('pe', lambda e, d=d, nb=nb: e.matmul(pT[:, d * 128:(d + 1) * 128], lhsT=nb[:, d * 256:d * 256 + 128], rhs=Tt[:, d * 128:(d + 1) * 128], start=True, stop=True),
                                 reads=[nk, f'Tt{q}{d}'], writes=['pT'])
                        S.op('dve', lambda e: e.tensor_tensor(out=Tt[:], in0=Tt[:], in1=pT[:, 0:256], op=ALU.add), reads=['pT', f'Tt{q}0', f'Tt{q}1'], writes=[f'Tt{q}0', f'Tt{q}1'])

                    def st_chain(k, i, q):
                        QKs, qe, Tt, kta, bV = QKsA[q], qeA[q], TtA[q], ktaA[q], bVA[q]
                        cs_ = [order[0][i], order[1][i]]
                        if k == 0:
                            for d in range(2):
                                c = cs_[d]; ts = slice(c * 128, (c + 1) * 128); x = d * 8 + h
                                S.op('pe', lambda e, d=d, ts=ts: e.matmul(pC[:, d * 128:(d + 1) * 128], lhsT=kn[:, ts], rhs=St[d][:], start=True, stop=True), reads=['kn', f'gS{d}'], writes=['pC'])
                                S.op('dve', lambda e, d=d, c=c, x=x: e.scalar_tensor_tensor(out=Zs[:, d * 128:(d + 1) * 128], in0=pC[:, d * 128:(d + 1) * 128], scalar=nbeg[:, c, x:x + 1],
                                                                                           in1=bV[:, d * 128:(d + 1) * 128], op0=ALU.mult, op1=ALU.add), reads=['pC', 'nbeg', f'bV{q}{d}'], writes=[f'Zs{d}'])
                        elif k == 1:
                            for d in range(2):
                                S.op('pe', lambda e, d=d: e.matmul(pC[:, 256 + d * 128:256 + (d + 1) * 128], lhsT=Tt[:, d * 128:(d + 1) * 128], rhs=Zs[:, d * 128:(d + 1) * 128], start=True, stop=True),
                                     reads=[f'Tt{q}0', f'Tt{q}1', f'Zs{d}'], writes=['pC'])
                            S.op('act', lambda e: e.copy(out=Vn[:], in_=pC[:, 256:512]), reads=['pC'], writes=['Vn'])
                        elif k == 2:
                            for d in range(2):
                                S.op('pe', lambda e, d=d: e.matmul(pD[:, d * 128:(d + 1) * 128], lhsT=qe[:, d * 128:(d + 1) * 128], rhs=St[d][:], start=True, stop=False), reads=[f'qe{q}{d}', f'gS{d}'], writes=['pD'])
                                S.op('pe', lambda e, d=d: e.matmul(pD[:, d * 128:(d + 1) * 128], lhsT=QKs[:, d * 128:(d + 1) * 128], rhs=Vn[:, d * 128:(d + 1) * 128], start=False, stop=True),
                                     reads=[f'QKs{q}{d}', 'Vn'], writes=['pD'])
                            S.op('dve', lambda e, c=cs_[0]: e.tensor_copy(out=oacc[:, c, :], in_=pD[:, 0:128]), reads=['pD'], writes=[('goacc', cs_[0], 0)])
                            S.op('dve', lambda e, c=cs_[1]: e.tensor_copy(out=obw[:, c, :], in_=pD[:, 128:256]), reads=['pD'], writes=[('gobw', cs_[1])])
                        else:
                            for d in range(2):
                                S.op('pe', lambda e, d=d: e.matmul(pD[:, 256 + d * 128:256 + (d + 1) * 128], lhsT=kta[:, d * 128:(d + 1) * 128], rhs=Vn[:, d * 128:(d + 1) * 128], start=True, stop=True),
                                     reads=[f'kta{q}{d}', 'Vn'], writes=['pD'])
                            for d in range(2):
                                c = cs_[d]; x = d * 8 + h
                                S.op('dve', lambda e, d=d, c=c, x=x: e.scalar_tensor_tensor(out=St[d][:], in0=St[d][:], scalar=eC[:, c, x:x + 1], in1=pD[:, 256 + d * 128:256 + (d + 1) * 128],
                                                                                           op0=ALU.mult, op1=ALU.add), reads=[f'gS{d}', 'eC', 'pD'], writes=[f'gS{d}'])

                    st_prep(0, 0)
                    for lv in range(1, 7):
                        st_neumann(lv, 0)
                    for i in range(NCH):
                        q = i % 2
                        C.bg_step()
                        has_next = i + 1 < NCH
                        if has_next:
                            st_prep(i + 1, 1 - q)
                        for k in range(4):
                            if has_next:
                                st_neumann(k + 1, 1 - q)
                            st_chain(k, i, q)
                        if has_next:
                            st_neumann(5, 1 - q)
                            st_neumann(6, 1 - q)
                with C.scope():
                    zt = C.sb([128, NCH, 128], F32, 'gzt'); sq = C.sb([128, NCH, 128], F32, 'gsq'); red = C.sb([128, NCH], F32, 'gred')
                    S.dma('sp', zt[:], yz.rearrange("(c t) f -> t c f", t=128)[:, :, h * 128:(h + 1) * 128], reads=[('tm_out', t) for t in range(NCH)], writes=['gzt'])
                    ok_ = [('goacc', c, 0) for c in range(NCH)]; bk_ = [('gobw', c) for c in range(NCH)]
                    S.op('dve', lambda e: e.tensor_tensor(out=oacc[:], in0=oacc[:], in1=obw[:], op=ALU.add), reads=ok_ + bk_, writes=['goall'])
                    S.op('pool', lambda e: e.tensor_tensor(out=sq[:], in0=oacc[:], in1=oacc[:], op=ALU.mult), reads=['goall'], writes=['gsq'])
                    S.op('dve', lambda e: e.tensor_reduce(out=red[:], in_=sq[:], axis=AX.X, op=ALU.add), reads=['gsq'], writes=['gred'])
                    S.op('act', lambda e: e.activation(out=red[:], in_=red[:], func=AF.Sqrt, scale=1.0 / 128, bias=eps6[:, 0:1]), reads=['gred', 'eps6'], writes=['gred'])
                    S.op('dve', lambda e: e.reciprocal(out=red[:], in_=red[:]), reads=['gred'], writes=['gred'])
                    S.op('dve', lambda e: e.tensor_tensor(out=oacc[:], in0=oacc[:], in1=red[:].unsqueeze(2).broadcast_to([128, NCH, 128]), op=ALU.mult), reads=['goall', 'gred'], writes=['goall'])
                    S.op('dve', lambda e: e.tensor_tensor(out=oacc[:], in0=oacc[:], in1=onorm[:].unsqueeze(1).broadcast_to([128, NCH, 128]), op=ALU.mult), reads=['goall', 'onorm'], writes=['goall'])
                    S.op('act', lambda e: e.activation(out=zt[:], in_=zt[:], func=AF.Silu), reads=['gzt'], writes=['gzt'])
                    S.op('dve', lambda e: e.tensor_tensor(out=oacc[:], in0=oacc[:], in1=zt[:], op=ALU.mult), reads=['goall', 'gzt'], writes=['goall'])
                    S.dma('sp', mixo.rearrange("(c t) f -> t c f", t=128)[:, :, h * 128:(h + 1) * 128], oacc[:], reads=['goall'], writes=[('mixo', 'att', 0), 'mixo'])


def build_gdn_test(NCH, heads):
    C = Ctx(); S = C.S
    NTOK = NCH * 128
    gT = C.dram("gT", [3072, NTOK], kind="ExternalInput"); yz = C.dram("yz", [NTOK, 1056], kind="ExternalInput")
    prm = dict(dt_bias=C.dram("dt_bias", [16], kind="ExternalInput"), A_log=C.dram("A_log", [16], kind="ExternalInput"), out_norm=C.dram("out_norm", [128], kind="ExternalInput"))
    mixo = C.dram("mixo", [NTOK, 1024], kind="ExternalOutput")
    ident, idk = C.ident(); masks = make_masks(C)
    emit_gdn(C, gT, yz, mixo, prm, NCH, heads, ident, idk, masks)
    S.finish()
    return C


def build_full(NCH=34):
    C = Ctx(); S = C.S
    NT = NCH; NTOK = NT * 128; NBLK = 2 * NT + 32
    ein = lambda n, s: C.dram(n, s, kind="ExternalInput")
    xin = ein("xin", [NTOK, D]); cin = ein("cin", [128, 16])
    adaw = [ein(f"adaw{l}", [D, 6 * D]) for l in range(2)]; adab = [ein(f"adab{l}", [128, 96]) for l in range(2)]; nrm = [ein(f"nrm{l}", [128, 16]) for l in range(2)]
    ev_w_in = ein("ev_w_in", [D, 2688]); mucol = ein("mucol", [128, 15])
    aprm = dict(q_norm=ein("q_norm", [64]), k_norm=ein("k_norm", [64]), sink=ein("sink", [8]), rope=ein("rope", [(NCH - 2) * 128, 128]))
    rprm = dict(rw_cols=ein("rw_cols", [128, 28]), gnwb=ein("gnwb", [2, 512]), dec_up=ein("dec_up", [128, 512]), iclr_up=ein("iclr_up", [128, 512]), gate_up=ein("gate_up", [128, 512]))
    wout = [ein("ev_w_out", [D, D]), ein("od_w_out", [D, D])]
    mprm = [dict(w_grp=ein(f"w_grp{l}", [D, 4]), b_grp=ein(f"b_grp{l}", [4]), w_exp=ein(f"w_exp{l}", [D, 32]), b_exp=ein(f"b_exp{l}", [32])) for l in range(2)]
    wgu = [ein(f"wgu{l}", [32, 1024, 1024]) for l in range(2)]; wdn = [ein(f"wdn{l}", [32, 512, 1024]) for l in range(2)]
    od_w_in = ein("od_w_in", [D, 4128]); convcol = ein("convcol", [128, 120])
    gprm = dict(dt_bias=ein("dt_bias", [16]), A_log=ein("A_log", [16]), out_norm=ein("out_norm", [128]))
    out = C.dram("out", [(NCH - 2) * 128, D], kind="ExternalOutput")
    xs = C.dram("xs", [NTOK, D]); mixo = C.dram("mixo_s", [NTOK, D]); frows = C.dram("frows", [NTOK, D])
    xrows = C.dram("xrows", [NBLK * 128, D]); yrows = C.dram("yrows", [NBLK * 128, D])
    yatt = C.dram("yatt", [NTOK, 768]); ybT = C.dram("ybT", [1920, NTOK]); gT = C.dram("gT", [3072, NTOK]); yz = C.dram("yz", [NTOK, 1056])
    mucols3 = C.dram("mucols3", [128, 45])
    ident, idk = C.ident(); masks = make_masks(C)
    wgub = [C.dram(f"wgub{l}", [32 * 128, 8 * 1024], BF16) for l in range(2)]; wdnb = [C.dram(f"wdnb{l}", [32 * 128, 4 * 1024], BF16) for l in range(2)]
    wck = [[], []]

    def convert_weights(l):
        for e_ in range(32):
            C.bgq.append(lambda e_=e_: S.dma('pool', wgub[l][e_ * 128:(e_ + 1) * 128, :].rearrange("p (k n) -> p k n", k=8), wgu[l][e_].rearrange("(k p) n -> p k n", p=128),
                                             writes=[('wconv', l, 'g', e_)])); wck[l].append(('wconv', l, 'g', e_))
            C.bgq.append(lambda e_=e_: S.dma('pool', wdnb[l][e_ * 128:(e_ + 1) * 128, :].rearrange("p (k n) -> p k n", k=4), wdn[l][e_].rearrange("(k p) n -> p k n", p=128),
                                             writes=[('wconv', l, 'd', e_)])); wck[l].append(('wconv', l, 'd', e_))
    convert_weights(0)
    for t in range(NT):
        S.dma(['sp', 'act'][t % 2], xs[t * 128:(t + 1) * 128, :], xin[t * 128:(t + 1) * 128, :], writes=[('xs', t)])
    with C.scope():
        M = emit_mods(C, adaw[0], adab[0], cin, nrm[0])
        with C.scope():
            mu3 = C.sb([128, 45], F32, 'mu3')
            S.dma('sp', mu3[:, 0:15], mucol, writes=['mu3'])
            S.op('dve', lambda e: e.tensor_scalar(out=mu3[:, 15:30], in0=mu3[:, 0:15], scalar1=-1.0, scalar2=1.0, op0=ALU.mult, op1=ALU.add), reads=['mu3'], writes=['mu3'])
            S.op('dve', lambda e: e.tensor_scalar(out=mu3[:, 30:45], in0=mu3[:, 0:15], scalar1=0.5, scalar2=None, op0=ALU.mult), reads=['mu3'], writes=['mu3'])
            S.dma('sp', mucols3, mu3[:], reads=['mu3'], writes=['mucols3'])
        emit_pre(C, M, xs, ev_w_in, 2688, NT, ident, idk, 768, 15, ybT, 0, 768, yatt, 'shift', mucols3)
        emit_attn(C, yatt, mixo, aprm, NCH, ident, idk, masks)
        scr = rwkv_scratch(C, NCH, [0])
        scr1 = {k: {P: v[0] for P in range(4)} for k, v in scr.items()}
        emit_rwkv(C, ybT, mixo, rprm, NCH, [0, 1, 2, 3], ident, idk, masks, scr1)
        C.bg_flush()
        prm = dict(mprm[0]); prm['rowscr'] = rowscr_alloc(C, 'l0')
        with C.scope():
            R = route_alloc(C, NT, NBLK)
            emit_post(C, M, mixo, xs, frows, wout[0], prm, NT, ident, idk, R)
            emit_route(C, R, NT, NBLK, masks)
            emit_moe(C, M, R, frows, xs, xrows, yrows, wgub[0], wdnb[0], prm, NT, NBLK, ident, idk, wkeys=wck[0])
    with C.scope():
        M = emit_mods(C, adaw[1], adab[1], cin, nrm[1])
        convert_weights(1)
        emit_pre(C, M, xs, od_w_in, 4128, NT, ident, idk, 0, 24, gT, 3072, 4128, yz, 'conv', convcol)
        emit_gdn(C, gT, yz, mixo, gprm, NCH, list(range(8)), ident, idk, masks)
        C.bg_flush()
        prm = dict(mprm[1]); prm['rowscr'] = rowscr_alloc(C, 'l1')
        with C.scope():
            R = route_alloc(C, NT, NBLK)
            emit_post(C, M, mixo, xs, frows, wout[1], prm, NT, ident, idk, R)
            emit_route(C, R, NT, NBLK, masks)
            emit_moe(C, M, R, frows, xs, xrows, yrows, wgub[1], wdnb[1], prm, NT, NBLK, ident, idk, out_dram=out, out_tiles=list(range(2, NT)), wkeys=wck[1])
    S.finish()
    return C


def full_inputs(inp, b, NCH=34):
    nl = (NCH - 2) * 128
    f32 = lambda a: np.ascontiguousarray(np.asarray(a, np.float32))
    m = {}
    m['xin'] = f32(np.concatenate([inp['ctx'][b], inp['x'][b][:nl]], axis=0))
    for l in range(2):
        cin, adab, nrm = mods_inputs(inp['c'][b], inp['c_ctx'], inp['ada_b'][l], inp['norm_mix'][l], inp['norm_ffn'][l])
        m['cin'] = cin; m[f'adab{l}'] = adab; m[f'nrm{l}'] = nrm
        m[f'adaw{l}'] = inp['ada_w'][l]
        m[f'w_grp{l}'] = inp['moe_w_grp'][l]; m[f'b_grp{l}'] = inp['moe_b_grp'][l]; m[f'w_exp{l}'] = inp['moe_w_exp'][l]; m[f'b_exp{l}'] = inp['moe_b_exp'][l]
        m[f'wgu{l}'] = inp['moe_w_gate_up'][l]; m[f'wdn{l}'] = inp['moe_w_down'][l]
    m['ev_w_in'] = inp['ev_w_in'][0]; m['mucol'] = col_layout(inp['ev_mu'][0], 15)
    m['q_norm'] = inp['ev_q_norm'][0]; m['k_norm'] = inp['ev_k_norm'][0]; m['sink'] = inp['ev_sink'][0]; m['rope'] = rope_table_host(nl)
    m.update(rwkv_params_host(inp, 0))
    m['ev_w_out'] = inp['ev_w_out'][0]; m['od_w_out'] = inp['od_w_out'][0]; m['od_w_in'] = inp['od_w_in'][0]
    cc = np.zeros((128, 120), np.float32)
    for j in range(5):
        cc[:, j * 24:(j + 1) * 24] = col_layout(inp['od_conv'][0][j], 24)
    m['convcol'] = cc
    m['dt_bias'] = f32(inp['od_dt_bias'][0].reshape(16)); m['A_log'] = f32(inp['od_A_log'][0].reshape(16)); m['out_norm'] = inp['od_out_norm'][0]
    return {k: f32(v) for k, v in m.items()}


_shared_cache = {}


def kernel(**inp):
    inp = {k: np.asarray(v) for k, v in inp.items()}
    C = build_full(34)
    maps = [full_inputs(inp, b) for b in range(4)]
    for b in range(1, 4):
        for k in maps[0]:
            if k not in ('xin', 'cin', 'adab0', 'adab1') and maps[b][k].shape == maps[0][k].shape and k not in ('xin',):
                if np.array_equal(maps[b][k], maps[0][k]):
                    maps[b][k] = maps[0][k]
    in_maps = [maps[c % 4] for c in range(NCORES)]
    res = run_bass_kernel_spmd(C.nc, in_maps, core_ids=list(range(NCORES))).results
    return np.stack([np.asarray(res[b]['out'], np.float32) for b in range(4)], axis=0)
```
